# Optimizing a Trainium2 kernel written in Bass

```python
import math
import jax
import jax.numpy as jnp
from jax import lax
import numpy as np

D_MODEL = 1024
BATCH = 8
SEQ = 2048
DEPTH = 2

GRID_W = 64
CTX_LEN = 256

N_EVEN = (DEPTH + 1) // 2
N_ODD = DEPTH // 2
MIX_W = 1024
NORM_EPS = 1e-6
ROPE_BASE = 10000.0

A_HEADS = 8
A_HEAD = 64
A_DIM = A_HEADS * A_HEAD
LORA_W = 64
LORA_A = 64
LORA_G = 128
W_DECAY_SCALE = 0.606531
RWKV_GN_EPS = 64e-5
POOL_WINDOWS = (2, 4, 8, 16)
POOL_GROUP = 128
B_DIM = POOL_GROUP * 4
C_HEADS = 4
C_HEAD = 64
C_VDIM = 2 * C_HEAD
C_DIM = C_HEADS * C_VDIM
DIFF_EPS = 1e-5
Q_BLOCK = 128
D_HEADS = 4
D_KDIM = 64
D_VDIM = 128
D_DIM = D_HEADS * D_VDIM
RET_CHUNK = 128

A_K_OFF = 0
A_V_OFF = A_DIM
W_LORA_OFF = 2 * A_DIM
A_LORA_OFF = W_LORA_OFF + LORA_W
EVEN_CTX_COLS = A_LORA_OFF + LORA_A
A_R_OFF = EVEN_CTX_COLS
G_LORA_OFF = A_R_OFF + A_DIM
POOL_OFF = G_LORA_OFF + LORA_G
EVEN_IN_COLS = POOL_OFF + B_DIM
C_K_OFF = 0
C_V_OFF = C_HEADS * 2 * C_HEAD
D_K_OFF = C_V_OFF + C_DIM
D_V_OFF = D_K_OFF + D_HEADS * D_KDIM
ODD_CTX_COLS = D_V_OFF + D_DIM
C_Q_OFF = ODD_CTX_COLS
D_Q_OFF = C_Q_OFF + C_HEADS * 2 * C_HEAD
D_G_OFF = D_Q_OFF + D_HEADS * D_KDIM
ODD_IN_COLS = D_G_OFF + D_DIM

MOE_GROUPS = 4
MOE_PER_GROUP = 8
MOE_EXPERTS = MOE_GROUPS * MOE_PER_GROUP
MOE_TOP_K = 2
D_EXPERT = 512
EXPERT_BLOCK = 256

kernel_name = 'hybrid_rwkv7_pool_diffattn_retnet_hmoe_dit'


def _rmsnorm(x, g, eps=NORM_EPS):
    xf = x.astype(jnp.float32)
    y = xf * lax.rsqrt(jnp.mean(xf * xf, axis=-1, keepdims=True) + eps)
    return (y * g.astype(jnp.float32)).astype(x.dtype)


def _group_norm_heads(y, g, b, n_heads, eps):
    shp = y.shape
    yh = y.reshape(shp[:-1] + (n_heads, shp[-1] // n_heads)).astype(jnp.float32)
    mu = jnp.mean(yh, axis=-1, keepdims=True)
    var = jnp.mean(jnp.square(yh - mu), axis=-1, keepdims=True)
    yh = (yh - mu) * lax.rsqrt(var + eps)
    return yh.reshape(shp) * g + b


def _flip(u):
    return None if u is None else jnp.flip(u, axis=1)


def _centred_shift(u, kern):
    up = jnp.pad(u, ((0, 0), (1, 1), (0, 0)))
    return kern[0] * up[:, :-2] + kern[1] * up[:, 1:-1] + kern[2] * up[:, 2:]


def _rope_1d(x, pos):
    half = x.shape[-1] // 2
    inv = ROPE_BASE ** (-jnp.arange(half, dtype=jnp.float32) / half)
    ang = pos.astype(jnp.float32)[:, None] * inv[None, :]
    cos = jnp.cos(ang)[None, :, None, :]
    sin = jnp.sin(ang)[None, :, None, :]
    xf = x.astype(jnp.float32)
    x1, x2 = xf[..., :half], xf[..., half:]
    return jnp.concatenate([x1 * cos - x2 * sin, x1 * sin + x2 * cos], axis=-1).astype(x.dtype)


def _axial_rope(x, row, col):
    n = x.shape[-1] // 2
    return jnp.concatenate([_rope_1d(x[..., :n], row), _rope_1d(x[..., n:], col)], axis=-1)


def _rwkv7_scan(state0, w, k, v, kk, b, r=None):
    seq = (w, k, v, kk, b) if r is None else (w, k, v, kk, b, r)

    def step(S, inp):
        w_t, k_t, v_t, kk_t, b_t = inp[:5]
        sa = jnp.einsum('bhvk,bhk->bhv', S, -kk_t)
        S = S * w_t[:, :, None, :] + sa[..., None] * b_t[:, :, None, :] + v_t[..., None] * k_t[:, :, None, :]
        if r is None:
            return S, None
        return S, jnp.einsum('bhvk,bhk->bhv', S, inp[5])

    S, ys = lax.scan(step, state0, tuple(jnp.swapaxes(a, 0, 1) for a in seq))
    return S, (None if r is None else jnp.swapaxes(ys, 0, 1))


def _rwkv_prep(p, shift_k, w0, w_up, a0, a_up, k_k, k_a, with_r):
    Bn, T = p.shape[:2]
    f32 = jnp.float32

    def heads(u):
        return u.reshape(Bn, T, A_HEADS, A_HEAD).astype(f32)

    k = _centred_shift(p[..., A_K_OFF:A_K_OFF + A_DIM], shift_k[:, 0:A_DIM]).astype(f32)
    v = _centred_shift(p[..., A_V_OFF:A_V_OFF + A_DIM], shift_k[:, A_DIM:2 * A_DIM])
    wd = jnp.tanh(p[..., W_LORA_OFF:W_LORA_OFF + LORA_W])
    ad = p[..., A_LORA_OFF:A_LORA_OFF + LORA_A]
    kk = heads(k * k_k)
    kk = kk * lax.rsqrt(jnp.maximum(jnp.sum(kk * kk, axis=-1, keepdims=True), 1e-12))
    dirs = []
    for d in range(2):
        w = jnp.exp(-W_DECAY_SCALE * jax.nn.sigmoid((w0[d] + wd @ w_up[d]).astype(f32)))
        a = jax.nn.sigmoid((a0[d] + ad @ a_up[d]).astype(f32))
        kt = k * (1.0 + (a - 1.0) * k_a)
        dirs.append((heads(w), heads(kt), heads(a) * kk))
    r = None
    if with_r:
        r = heads(_centred_shift(p[..., A_R_OFF:A_R_OFF + A_DIM], shift_k[:, 2 * A_DIM:3 * A_DIM]))
    return heads(v), kk, dirs, r


def _pool_mixer(u, pool_w, pool_scale):
    Bn, T, _ = u.shape
    uf = u.astype(jnp.float32)
    cs = jnp.concatenate([jnp.zeros((Bn, 1, B_DIM), jnp.float32), jnp.cumsum(uf, axis=1)], axis=1)
    t = jnp.arange(T)
    diffs = []
    for gi, win in enumerate(POOL_WINDOWS):
        lo = jnp.clip(t - win // 2, 0, T)
        hi = jnp.clip(t - win // 2 + win, 0, T)
        sl = slice(gi * POOL_GROUP, (gi + 1) * POOL_GROUP)
        mean = (cs[:, hi, sl] - cs[:, lo, sl]) / (hi - lo).astype(jnp.float32)[None, :, None]
        diffs.append(mean - uf[:, :, sl])
    d = jnp.stack(diffs, axis=2)
    y = jnp.einsum('btgc,gcd->btgd', d, pool_w.astype(jnp.float32)).reshape(Bn, T, B_DIM)
    return (y * pool_scale).astype(u.dtype)


def _even_mixer(h_ctx, h_lat, need_ctx, w_in, w_out, shift_k, w0, w_up, a0, a_up, g_up,
                k_k, k_a, r_k, ln_g, ln_b, pool_w, pool_scale):
    p_lat = h_lat @ w_in
    p_ctx = h_ctx @ (w_in if need_ctx else w_in[:, :EVEN_CTX_COLS])
    v_c, kk_c, (fc, bc), r_c = _rwkv_prep(p_ctx, shift_k, w0, w_up, a0, a_up, k_k, k_a, need_ctx)
    v_l, kk_l, (fl, bl), r_l = _rwkv_prep(p_lat, shift_k, w0, w_up, a0, a_up, k_k, k_a, True)
    zero = jnp.zeros((h_lat.shape[0], A_HEADS, A_HEAD, A_HEAD), jnp.float32)
    s_f, yf_c = _rwkv7_scan(zero, fc[0], fc[1], v_c, kk_c, fc[2], r_c)
    s_b, yb_c = _rwkv7_scan(zero, _flip(bc[0]), _flip(bc[1]), _flip(v_c), _flip(kk_c), _flip(bc[2]), _flip(r_c))
    _, yf_l = _rwkv7_scan(s_f, fl[0], fl[1], v_l, kk_l, fl[2], r_l)
    _, yb_l = _rwkv7_scan(s_b, _flip(bl[0]), _flip(bl[1]), _flip(v_l), _flip(kk_l), _flip(bl[2]), _flip(r_l))
    r_kh = r_k.reshape(A_HEADS, A_HEAD)

    def merge(p, yf, yb, r, v, kt_f, kt_b):
        Bn, T = p.shape[:2]
        y = _group_norm_heads((yf + _flip(yb)).reshape(Bn, T, A_DIM), ln_g, ln_b, A_HEADS, RWKV_GN_EPS)
        bonus = jnp.sum(r * (kt_f + kt_b) * r_kh, axis=-1, keepdims=True) * v
        g = jax.nn.sigmoid(p[..., G_LORA_OFF:G_LORA_OFF + LORA_G]) @ g_up
        y_a = ((y + bonus.reshape(Bn, T, A_DIM)) * g).astype(p.dtype)
        y_b = _pool_mixer(p[..., POOL_OFF:POOL_OFF + B_DIM], pool_w, pool_scale)
        return jnp.concatenate([y_a, y_b], axis=-1) @ w_out

    y_lat = merge(p_lat, yf_l, yb_l, r_l, v_l, fl[1], bl[1])
    y_ctx = merge(p_ctx, yf_c, yb_c, r_c, v_c, fc[1], bc[1]) if need_ctx else None
    return y_ctx, y_lat


def _diff_attend(q1, q2, k1, k2, v, lam):
    sc = C_HEAD ** -0.5
    a1 = jax.nn.softmax(jnp.einsum('bqhd,bkhd->bhqk', q1, k1).astype(jnp.float32) * sc, axis=-1)
    a2 = jax.nn.softmax(jnp.einsum('bqhd,bkhd->bhqk', q2, k2).astype(jnp.float32) * sc, axis=-1)
    return jnp.einsum('bhqk,bkhd->bqhd', (a1 - lam * a2).astype(v.dtype), v)


def _blocked_diff_attend(q1, q2, k1, k2, v, lam):
    Bn, T, H, d = q1.shape
    nb = T // Q_BLOCK

    def blk(q):
        return jnp.swapaxes(q.reshape(Bn, nb, Q_BLOCK, H, d), 0, 1)

    out = lax.map(lambda qs: _diff_attend(qs[0], qs[1], k1, k2, v, lam), (blk(q1), blk(q2)))
    return jnp.swapaxes(out, 0, 1).reshape(Bn, T, H, v.shape[-1])


def _retention_log_gammas():
    lg = jnp.log(1.0 - 2.0 ** (-5.0 - jnp.arange(D_HEADS, dtype=jnp.float32)))
    return lg, lg[::-1]


def _retention_scan(state0, q, k, v, log_gamma):
    Bn, T, H, _ = k.shape
    dv = v.shape[-1]
    nc = T // RET_CHUNK
    pos = jnp.arange(RET_CHUNK, dtype=jnp.float32)
    k_dec = jnp.exp(log_gamma[:, None] * (RET_CHUNK - 1 - pos)[None, :])
    chunk_dec = jnp.exp(log_gamma * RET_CHUNK)
    if q is not None:
        diff = pos[:, None] - pos[None, :]
        inner_dec = jnp.where(diff[None] >= 0, jnp.exp(log_gamma[:, None, None] * jnp.maximum(diff, 0.0)[None]), 0.0)
        q_dec = jnp.swapaxes(jnp.exp(log_gamma[:, None] * (pos + 1.0)[None, :]), 0, 1)[None, :, :, None]

    def chunks(u):
        return jnp.swapaxes(u.reshape(Bn, nc, RET_CHUNK, H, u.shape[-1]), 0, 1)

    def step(R, inp):
        kc, vc = inp[0], inp[1]
        R_new = R * chunk_dec[None, :, None, None] + jnp.einsum('bshk,hs,bshv->bhkv', kc, k_dec, vc)
        if q is None:
            return R_new, None
        qc = inp[2]
        inner = jnp.einsum('bihk,bshk->bhis', qc, kc) * inner_dec[None]
        o = jnp.einsum('bhis,bshv->bihv', inner, vc) + jnp.einsum('bihk,bhkv->bihv', qc, R) * q_dec
        return R_new, o

    xs = (chunks(k), chunks(v)) + (() if q is None else (chunks(q),))
    R, os = lax.scan(step, state0, xs)
    return R, (None if q is None else jnp.swapaxes(os, 0, 1).reshape(Bn, T, H, dv))


def _hsplit(p, off, n_heads, dh):
    return p[..., off:off + n_heads * dh].reshape(p.shape[:2] + (n_heads, dh))


def _odd_mixer(h_ctx, h_lat, need_ctx, lam_init, row, col, w_in, w_out, diff_lambda, diff_subln, ret_norm):
    f32 = jnp.float32
    p_lat = h_lat @ w_in
    p_ctx = h_ctx @ (w_in if need_ctx else w_in[:, :ODD_CTX_COLS])

    def rope(u):
        return _axial_rope(u, row, col)

    ck = _hsplit(p_ctx, C_K_OFF, C_HEADS, 2 * C_HEAD)
    cv = _hsplit(p_ctx, C_V_OFF, C_HEADS, C_VDIM)
    lk = _hsplit(p_lat, C_K_OFF, C_HEADS, 2 * C_HEAD)
    lv = _hsplit(p_lat, C_V_OFF, C_HEADS, C_VDIM)
    lq = _hsplit(p_lat, C_Q_OFF, C_HEADS, 2 * C_HEAD)
    k1 = jnp.concatenate([ck[..., :C_HEAD], rope(lk[..., :C_HEAD])], axis=1)
    k2 = jnp.concatenate([ck[..., C_HEAD:], rope(lk[..., C_HEAD:])], axis=1)
    vv = jnp.concatenate([cv, lv], axis=1)
    lf = diff_lambda.astype(f32)
    lam = jnp.exp(jnp.sum(lf[0] * lf[1])) - jnp.exp(jnp.sum(lf[2] * lf[3])) + lam_init

    def c_post(o):
        o = _rmsnorm(o, diff_subln, DIFF_EPS) * (1.0 - lam_init)
        return o.reshape(o.shape[:2] + (C_DIM,))

    c_lat = c_post(_blocked_diff_attend(rope(lq[..., :C_HEAD]), rope(lq[..., C_HEAD:]), k1, k2, vv, lam))

    sc = D_KDIM ** -0.5
    lg_f, lg_b = _retention_log_gammas()
    ck_d = _hsplit(p_ctx, D_K_OFF, D_HEADS, D_KDIM).astype(f32) * sc
    cv_d = _hsplit(p_ctx, D_V_OFF, D_HEADS, D_VDIM).astype(f32)
    cq_d = _hsplit(p_ctx, D_Q_OFF, D_HEADS, D_KDIM).astype(f32) if need_ctx else None
    lq_d = rope(_hsplit(p_lat, D_Q_OFF, D_HEADS, D_KDIM)).astype(f32)
    lk_d = rope(_hsplit(p_lat, D_K_OFF, D_HEADS, D_KDIM)).astype(f32) * sc
    lv_d = _hsplit(p_lat, D_V_OFF, D_HEADS, D_VDIM).astype(f32)
    zero = jnp.zeros((h_lat.shape[0], D_HEADS, D_KDIM, D_VDIM), f32)
    r_f, of_c = _retention_scan(zero, cq_d, ck_d, cv_d, lg_f)
    r_b, ob_c = _retention_scan(zero, _flip(cq_d), _flip(ck_d), _flip(cv_d), lg_b)
    _, of_l = _retention_scan(r_f, lq_d, lk_d, lv_d, lg_f)
    _, ob_l = _retention_scan(r_b, _flip(lq_d), _flip(lk_d), _flip(lv_d), lg_b)
    ret_g = ret_norm.reshape(D_HEADS, D_VDIM)

    def d_post(o, p):
        o = _rmsnorm(o, ret_g)
        return o.reshape(o.shape[:2] + (D_DIM,)) * jax.nn.silu(p[..., D_G_OFF:D_G_OFF + D_DIM])

    d_lat = d_post(of_l + _flip(ob_l), p_lat)
    y_lat = jnp.concatenate([c_lat, d_lat.astype(c_lat.dtype)], axis=-1) @ w_out
    y_ctx = None
    if need_ctx:
        cq = _hsplit(p_ctx, C_Q_OFF, C_HEADS, 2 * C_HEAD)
        c_ctx_o = c_post(_diff_attend(cq[..., :C_HEAD], cq[..., C_HEAD:], ck[..., :C_HEAD], ck[..., C_HEAD:], cv, lam))
        d_ctx_o = d_post(of_c + _flip(ob_c), p_ctx)
        y_ctx = jnp.concatenate([c_ctx_o, d_ctx_o.astype(c_ctx_o.dtype)], axis=-1) @ w_out
    return y_ctx, y_lat


def _expert_dispatch(h, eidx, gates, w_gate, w_up, w_down):
    N, D = h.shape
    K = eidx.shape[1]
    E = w_gate.shape[0]
    flat_e = eidx.reshape(-1).astype(jnp.int32)
    order = jnp.argsort(flat_e)
    se = flat_e[order]
    tok = (order // K).astype(jnp.int32)
    counts = jnp.bincount(flat_e, length=E)
    padded = (counts + EXPERT_BLOCK - 1) // EXPERT_BLOCK * EXPERT_BLOCK
    pad_end = jnp.cumsum(padded)
    pad_start = pad_end - padded
    start = jnp.cumsum(counts) - counts
    dest = pad_start[se] + jnp.arange(N * K, dtype=jnp.int32) - start[se]
    n_blocks = -(-(N * K) // EXPERT_BLOCK) + E
    rows = jnp.full((n_blocks * EXPERT_BLOCK,), N, jnp.int32).at[dest].set(tok)
    block_e = jnp.minimum(jnp.searchsorted(pad_end, jnp.arange(n_blocks, dtype=jnp.int32) * EXPERT_BLOCK, side='right'), E - 1)
    xb = jnp.concatenate([h, jnp.zeros((1, D), h.dtype)], axis=0)[rows].reshape(n_blocks, EXPERT_BLOCK, D)

    def run(args):
        xe, e = args
        return (jax.nn.silu(xe @ w_gate[e]) * (xe @ w_up[e])) @ w_down[e]

    yb = lax.map(run, (xb, block_e)).reshape(-1, D)
    contrib = yb[dest] * gates.reshape(-1)[order][:, None].astype(h.dtype)
    return jnp.zeros_like(h).at[tok].add(contrib)


def _hier_moe(h, w_rg, w_re, w_gate, w_up, w_down):
    N = h.shape[0]
    lg = (h @ w_rg).astype(jnp.float32)
    grp = jnp.argmax(lg, axis=-1).astype(jnp.int32)
    gw = jnp.take_along_axis(jax.nn.softmax(lg, axis=-1), grp[:, None], axis=-1)
    le = (h @ w_re).astype(jnp.float32).reshape(N, MOE_GROUPS, MOE_PER_GROUP)
    le = jnp.take_along_axis(le, grp[:, None, None], axis=1)[:, 0]
    p_in, e_in = lax.top_k(jax.nn.softmax(le, axis=-1), MOE_TOP_K)
    gates = gw * p_in / jnp.sum(p_in, axis=-1, keepdims=True)
    eidx = grp[:, None] * MOE_PER_GROUP + e_in.astype(jnp.int32)
    return _expert_dispatch(h, eidx, gates, w_gate, w_up, w_down)


def setup_inputs(seed: int = 0) -> dict:
    key = jax.random.key(seed)
    ks = iter(jax.random.split(key, 48))
    D = D_MODEL

    def nrm(shape, s):
        return jax.random.normal(next(ks), shape, jnp.float32) * s

    def gain(shape):
        return 1.0 + nrm(shape, 0.1)

    return {
        'x': nrm((BATCH, SEQ, D), 1.0),
        'c': nrm((BATCH, D), 1.0),
        'ctx': nrm((BATCH, CTX_LEN, D), 1.0),
        'c_ctx': nrm((D,), 1.0),
        'ada_w': nrm((DEPTH, D, 6 * D), 0.5 * D ** -0.5),
        'ada_b': nrm((DEPTH, 6 * D), 0.01),
        'norm_mix': gain((DEPTH, D)),
        'norm_ffn': gain((DEPTH, D)),
        'ev_w_in': nrm((N_EVEN, D, EVEN_IN_COLS), D ** -0.5),
        'ev_w_out': nrm((N_EVEN, MIX_W, D), MIX_W ** -0.5),
        'rwkv_shift': jnp.array([0.25, 0.5, 0.25], jnp.float32)[None, :, None] + nrm((N_EVEN, 3, 3 * A_DIM), 0.05),
        'rwkv_w0': nrm((N_EVEN, 2, A_DIM), 1.0) - 1.5,
        'rwkv_w_up': nrm((N_EVEN, 2, LORA_W, A_DIM), 0.5 * LORA_W ** -0.5),
        'rwkv_a0': nrm((N_EVEN, 2, A_DIM), 0.5),
        'rwkv_a_up': nrm((N_EVEN, 2, LORA_A, A_DIM), 0.5 * LORA_A ** -0.5),
        'rwkv_g_up': nrm((N_EVEN, LORA_G, A_DIM), LORA_G ** -0.5),
        'rwkv_k_k': gain((N_EVEN, A_DIM)),
        'rwkv_k_a': gain((N_EVEN, A_DIM)),
        'rwkv_r_k': nrm((N_EVEN, A_DIM), 0.1),
        'rwkv_ln_g': gain((N_EVEN, A_DIM)),
        'rwkv_ln_b': nrm((N_EVEN, A_DIM), 0.01),
        'pool_w': nrm((N_EVEN, len(POOL_WINDOWS), POOL_GROUP, POOL_GROUP), POOL_GROUP ** -0.5),
        'pool_scale': gain((N_EVEN, B_DIM)),
        'od_w_in': nrm((N_ODD, D, ODD_IN_COLS), D ** -0.5),
        'od_w_out': nrm((N_ODD, MIX_W, D), MIX_W ** -0.5),
        'diff_lambda': nrm((N_ODD, 4, C_HEAD), 0.1),
        'diff_subln': gain((N_ODD, C_VDIM)),
        'ret_norm': gain((N_ODD, D_DIM)),
        'moe_router_group': nrm((DEPTH, D, MOE_GROUPS), D ** -0.5),
        'moe_router_expert': nrm((DEPTH, D, MOE_EXPERTS), D ** -0.5),
        'moe_w_gate': nrm((DEPTH, MOE_EXPERTS, D, D_EXPERT), D ** -0.5),
        'moe_w_up': nrm((DEPTH, MOE_EXPERTS, D, D_EXPERT), D ** -0.5),
        'moe_w_down': nrm((DEPTH, MOE_EXPERTS, D_EXPERT, D), D_EXPERT ** -0.5),
        'final_norm': gain((D,)),
    }


def reference(x, c, ctx, c_ctx, ada_w, ada_b, norm_mix, norm_ffn, ev_w_in, ev_w_out, rwkv_shift,
              rwkv_w0, rwkv_w_up, rwkv_a0, rwkv_a_up, rwkv_g_up, rwkv_k_k, rwkv_k_a, rwkv_r_k,
              rwkv_ln_g, rwkv_ln_b, pool_w, pool_scale, od_w_in, od_w_out, diff_lambda, diff_subln,
              ret_norm, moe_router_group, moe_router_expert, moe_w_gate, moe_w_up, moe_w_down, final_norm):
    Bn, T, D = x.shape
    ROWS = T // GRID_W
    row = jnp.repeat(jnp.arange(ROWS, dtype=jnp.int32), GRID_W)
    col = jnp.tile(jnp.arange(GRID_W, dtype=jnp.int32), ROWS)
    xc = ctx
    for i in range(DEPTH):
        last = i == DEPTH - 1
        mod = jax.nn.silu(c) @ ada_w[i] + ada_b[i]
        mod_c = jax.nn.silu(c_ctx) @ ada_w[i] + ada_b[i]
        sh1, sc1, g1, sh2, sc2, g2 = jnp.split(mod[:, None, :], 6, axis=-1)
        csh1, csc1, cg1, csh2, csc2, cg2 = jnp.split(mod_c, 6)
        h_lat = _rmsnorm(x, norm_mix[i]) * (1.0 + sc1) + sh1
        h_ctx = _rmsnorm(xc, norm_mix[i]) * (1.0 + csc1) + csh1
        j = i // 2
        if i % 2 == 0:
            y_ctx, y_lat = _even_mixer(h_ctx, h_lat, not last, ev_w_in[j], ev_w_out[j], rwkv_shift[j],
                                       rwkv_w0[j], rwkv_w_up[j], rwkv_a0[j], rwkv_a_up[j], rwkv_g_up[j],
                                       rwkv_k_k[j], rwkv_k_a[j], rwkv_r_k[j], rwkv_ln_g[j], rwkv_ln_b[j],
                                       pool_w[j], pool_scale[j])
        else:
            lam_init = 0.8 - 0.6 * math.exp(-0.3 * i)
            y_ctx, y_lat = _odd_mixer(h_ctx, h_lat, not last, lam_init, row, col, od_w_in[j], od_w_out[j],
                                      diff_lambda[j], diff_subln[j], ret_norm[j])
        x = x + g1 * y_lat
        hf_lat = _rmsnorm(x, norm_ffn[i]) * (1.0 + sc2) + sh2
        moe_p = (moe_router_group[i], moe_router_expert[i], moe_w_gate[i], moe_w_up[i], moe_w_down[i])
        if last:
            x = x + g2 * _hier_moe(hf_lat.reshape(-1, D), *moe_p).reshape(x.shape)
        else:
            xc = xc + cg1 * y_ctx
            hf_ctx = _rmsnorm(xc, norm_ffn[i]) * (1.0 + csc2) + csh2
            n_ctx = xc.shape[0] * xc.shape[1]
            f = _hier_moe(jnp.concatenate([hf_ctx.reshape(-1, D), hf_lat.reshape(-1, D)], axis=0), *moe_p)
            xc = xc + cg2 * f[:n_ctx].reshape(xc.shape)
            x = x + g2 * f[n_ctx:].reshape(x.shape)
    return _rmsnorm(x, final_norm)
```

```python
import math
import numpy as np
from contextlib import ExitStack
import concourse.bass as bass
import concourse.mybir as mybir
from concourse.bass_utils import run_bass_kernel_spmd

F32 = mybir.dt.float32
BF16 = mybir.dt.bfloat16
ALU = mybir.AluOpType
AF = mybir.ActivationFunctionType
AX = mybir.AxisListType

ENGS = ['pe', 'act', 'dve', 'pool', 'sp']
DMAQ = ['sp', 'pool', 'act']


def _prod(s):
    r = 1
    for v in s:
        r *= v
    return r


class T:
    __slots__ = ('ap', 'lw', 'rd')

    def __init__(self, ap):
        self.ap = ap
        self.lw = None
        self.rd = {}


class Prog:
    def __init__(self, arena_words=50000, n_dma_sems=8):
        self.nc = bass.Bass("TRN2", target_bir_lowering=False)
        self.es = ExitStack()
        self.ops = {e: [] for e in ENGS}
        self.known = {e: {} for e in ENGS}
        self.pending = {e: [] for e in ENGS}
        self.n_dma_sems = n_dma_sems
        self.dma_rr = {q: 0 for q in DMAQ}
        self.dma_cum = {}
        self.out_tokens = []
        self.nuid = 0
        self.aw = arena_words
        self.arena = self.es.enter_context(self.nc.sbuf_tensor("arena", [128, arena_words], F32))
        self.top = 0
        self.PS = [T(self.es.enter_context(self.nc.psum_tensor(f"psb{i}", [128, 512], F32))[:, :])
                   for i in range(8)]
        self.ps_rr = {'a': 0, 'b': 0, 'c': 0}
        self.ps_groups = {'a': [0, 1, 2, 3], 'b': [4, 5], 'c': [4, 5, 6, 7]}

    def psum(self, g='a'):
        lst = self.ps_groups[g]
        i = self.ps_rr[g]
        self.ps_rr[g] = (i + 1) % len(lst)
        return self.PS[lst[i]]

    def alloc(self, shape, dt=F32):
        shape = list(shape)
        esz = 4 if dt == F32 else 2
        nb = _prod(shape[1:]) * esz
        nw = (nb + 3) // 4
        assert self.top + nw <= self.aw, f"arena overflow {self.top}+{nw}>{self.aw}"
        ap = self.arena[0:shape[0], self.top:self.top + nw]
        self.top += nw
        if dt != F32:
            ap = ap.bitcast(dt)
            ap = ap[:, 0:_prod(shape[1:])]
        if len(shape) > 2:
            names = "abcdefg"[:len(shape) - 1]
            kw = {names[i]: shape[i + 1] for i in range(len(shape) - 2)}
            ap = ap.rearrange("p (" + " ".join(names) + ") -> p " + " ".join(names), **kw)
        return T(ap)

    def mark(self):
        return self.top

    def release(self, m):
        self.barrier()
        self.top = m

    def dram(self, name, shape, dt, kind="Internal"):
        return self.nc.dram_tensor(name, list(shape), dt, kind=kind).ap()

    def barrier(self):
        toks = []
        for f in ENGS:
            if len(self.ops[f]) > 0:
                toks.append(('c', f, len(self.ops[f])))
        for skey, cum in self.dma_cum.items():
            toks.append(('d', skey, cum))
        for e in ENGS:
            self.pending[e] = list(toks)

    def _add_wait(self, e, waits, tok):
        if tok is None:
            return
        kind, key, val = tok
        if kind == 'c' and key == e:
            if e == 'pe':
                return
            if val > len(self.ops[e]):
                return
        kk = (kind, key)
        if self.known[e].get(kk, 0) >= val:
            return
        self.known[e][kk] = val
        waits[kk] = max(waits.get(kk, 0), val)

    def _deps(self, e, reads, writes):
        waits = {}
        if self.pending[e]:
            for tok in self.pending[e]:
                self._add_wait(e, waits, tok)
            self.pending[e] = []
        for t in reads:
            self._add_wait(e, waits, t.lw)
        for t in writes:
            self._add_wait(e, waits, t.lw)
            for tok in t.rd.values():
                self._add_wait(e, waits, tok)
        return waits

    def op(self, e, fn, reads=(), writes=()):
        waits = self._deps(e, reads, writes)
        idx = len(self.ops[e]) + 1
        tok = ('c', e, idx)
        self.ops[e].append(dict(fn=fn, waits=waits, inc=None, flag=False))
        for t in reads:
            t.rd[e] = tok
        for t in writes:
            t.lw = tok
            t.rd = {}
        return tok

    def dma(self, q, out_ap, in_ap, reads=(), writes=(), is_output=False, **kw):
        waits = self._deps(q, reads, writes)
        si = self.dma_rr[q]
        self.dma_rr[q] = (si + 1) % self.n_dma_sems
        skey = (q, si)
        prev = self.dma_cum.get(skey, 0)
        if prev > 0:
            self._add_wait(q, waits, ('d', skey, prev))
        val = prev + 16
        self.dma_cum[skey] = val
        tok = ('d', skey, val)

        def fn(eng, out_ap=out_ap, in_ap=in_ap, kw=kw):
            return eng.dma_start(out=out_ap, in_=in_ap, **kw)
        self.ops[q].append(dict(fn=fn, waits=waits, inc=(skey, 16), flag=True))
        for t in reads:
            t.rd[('dma', skey)] = tok
        for t in writes:
            t.lw = tok
            t.rd = {}
        if is_output:
            self.out_tokens.append(tok)
        return tok

    def build(self):
        nc = self.nc
        waits = {}
        for tok in self.out_tokens:
            self._add_wait('sp', waits, tok)
        self.ops['sp'].append(dict(fn=None, waits=waits, inc=None, flag=False))
        for e in ENGS:
            for o in self.ops[e]:
                for (kind, key), val in o['waits'].items():
                    if kind == 'c':
                        self.ops[key][val - 1]['flag'] = True
        rank = {}
        for e in ENGS:
            r = 0
            rk = []
            for o in self.ops[e]:
                if o['inc'] is None and o['flag']:
                    r += 1
                rk.append(r)
            rank[e] = rk
        csem = {e: self.es.enter_context(nc.semaphore(f"c_{e}")) for e in ENGS}
        dsem = {}
        for q in DMAQ:
            for i in range(self.n_dma_sems):
                if (q, i) in self.dma_cum:
                    dsem[(q, i)] = self.es.enter_context(nc.semaphore(f"d_{q}{i}"))
        block = self.es.enter_context(nc.Block())
        engobj = {'pe': block.tensor, 'act': block.scalar, 'dve': block.vector,
                  'pool': block.gpsimd, 'sp': block.sync}

        def mk(e):
            def body(eng):
                for o in self.ops[e]:
                    for (kind, key), val in o['waits'].items():
                        if kind == 'c':
                            eng.wait_ge(csem[key], rank[key][val - 1])
                        else:
                            eng.wait_ge(dsem[key], val)
                    if o['fn'] is None:
                        continue
                    ins = o['fn'](eng)
                    if o['inc'] is not None:
                        ins.then_inc(dsem[o['inc'][0]], 16)
                    elif o['flag']:
                        ins.then_inc(csem[e], 1)
            return body
        for e in ENGS:
            engobj[e](mk(e))
        self.es.close()
        return nc

    def mm(self, out, lhsT, rhs, start=True, stop=True, reads=(), writes=(), **kw):
        def fn(eng):
            return eng.matmul(out, lhsT, rhs, start=start, stop=stop, **kw)
        return self.op('pe', fn, reads, writes)

    def tr(self, out, in_, ident, reads=(), writes=()):
        def fn(eng):
            return eng.transpose(out, in_, ident)
        return self.op('pe', fn, reads, writes)

    def act(self, out, in_, func, reads=(), writes=(), **kw):
        def fn(e):
            return e.activation(out=out, in_=in_, func=func, **kw)
        return self.op('act', fn, reads, writes)

    def tt(self, e, out, in0, in1, op, reads=(), writes=()):
        def fn(eng):
            return eng.tensor_tensor(out=out, in0=in0, in1=in1, op=op)
        return self.op(e, fn, reads, writes)

    def ts(self, e, out, in0, s1, s2, op0, op1=None, reads=(), writes=()):
        def fn(eng):
            if op1 is None:
                return eng.tensor_scalar(out=out, in0=in0, scalar1=s1, scalar2=None, op0=op0)
            return eng.tensor_scalar(out=out, in0=in0, scalar1=s1, scalar2=s2, op0=op0, op1=op1)
        return self.op(e, fn, reads, writes)

    def stt(self, e, out, in0, scalar, in1, op0, op1, reads=(), writes=()):
        def fn(eng):
            return eng.scalar_tensor_tensor(out=out, in0=in0, scalar=scalar, in1=in1, op0=op0, op1=op1)
        return self.op(e, fn, reads, writes)

    def cp(self, e, out, in_, reads=(), writes=()):
        if e == 'act':
            def fn(eng):
                return eng.copy(out=out, in_=in_)
        else:
            def fn(eng):
                return eng.tensor_copy(out=out, in_=in_)
        return self.op(e, fn, reads, writes)

    def memset(self, e, ap, val, writes=()):
        def fn(eng):
            return eng.memset(ap, val)
        return self.op(e, fn, (), writes)


D = 1024
KD = 8
W_DECAY_SCALE = 0.606531
EV_COLS = 2304
OD_COLS = 3072


def host_consts():
    r = np.arange(128)
    Us = (r[:, None] < r[None, :]).astype(np.float32)
    Ui = (r[:, None] <= r[None, :]).astype(np.float32)
    Ls = (r[:, None] > r[None, :]).astype(np.float32)
    Li = (r[:, None] >= r[None, :]).astype(np.float32)
    blk = np.zeros((128, 128), np.float32)
    blk[:64, :64] = 1
    blk[64:, 64:] = 1
    c = {}
    c['ident'] = np.eye(128, dtype=np.float32)
    c['blk64'] = blk
    rm = np.zeros((2, 128, 896), np.float32)
    for d, (ss, si, tsm) in enumerate([(Us, Ui, Ls), (Ls, Li, Us)]):
        rm[d, :, 0:128] = -ss
        rm[d, :, 128:256] = -si
        rm[d, :, 256:384] = ss
        rm[d, :, 384:512] = si
        rm[d, :, 512:640] = -tsm
        rm[d, :, 640:768] = si
        rm[d, :, 768:896] = ss
    c['rmask'] = rm
    return c


def host_consts_l1(S):
    c = {}
    p = np.arange(128)
    blk32 = p % 32
    partner = np.where(blk32 < 16, p + 16, p - 16)
    perm = np.zeros((128, 128), np.float32)
    perm[partner, p] = 1.0
    c['rope_perm'] = perm
    t = np.arange(S)
    row = (t // 64).astype(np.float32)
    col = (t % 64).astype(np.float32)
    b64 = p % 64
    sub = b64 // 32
    j = (b64 % 16).astype(np.float32)
    inv = (10000.0 ** (-j / 16.0)).astype(np.float32)
    pos = np.where(sub[:, None] == 0, row[None, :], col[None, :]).astype(np.float32)
    ang = (pos * inv[:, None]).astype(np.float32)
    sgn = np.where(blk32 < 16, -1.0, 1.0).astype(np.float32)
    c['ropeC'] = np.cos(ang).astype(np.float32)
    c['ropeS'] = (np.sin(ang) * sgn[:, None]).astype(np.float32)
    lgf = np.log(1.0 - 2.0 ** (-5.0 - np.arange(4, dtype=np.float64)))
    r = np.arange(128, dtype=np.float64)
    retD = np.zeros((8, 128, 128), np.float64)
    retq = np.zeros((8, 128), np.float64)
    retk = np.zeros((128, 8), np.float64)
    for h in range(4):
        for d in range(2):
            lg = lgf[h] if d == 0 else lgf[3 - h]
            hd = h * 2 + d
            s_, i_ = r[:, None], r[None, :]
            if d == 0:
                retD[hd] = np.where(i_ >= s_, np.exp(lg * np.maximum(i_ - s_, 0)), 0.0)
                retq[hd] = np.exp(lg * (r + 1))
                retk[:, hd] = np.exp(lg * (127 - r))
            else:
                retD[hd] = np.where(s_ >= i_, np.exp(lg * np.maximum(s_ - i_, 0)), 0.0)
                retq[hd] = np.exp(lg * (128 - r))
                retk[:, hd] = np.exp(lg * r)
    c['retD'] = retD.astype(np.float32)
    c['retq'] = retq.astype(np.float32)
    c['retk'] = retk.astype(np.float32)
    return c


def build(S, C, dbg=False, RW_DT=(BF16, F32, BF16), STOP_AFTER='all', RW_STOP=99):
    P = Prog()
    NT = C + S
    NCH = NT // 128
    CCH = C // 128
    tiles = []
    for base, ln, isc in ((0, C, 1), (C, S, 0)):
        o = 0
        while o < ln:
            l = min(512, ln - o)
            tiles.append((base + o, l, isc))
            o += l
    IN = lambda n, s: P.dram(n, s, F32, "ExternalInput")
    xin = IN("xin", [NT, D])
    cT = IN("cT", [128, KD, 2])
    ada_w = IN("ada_w", [2, D, 6 * D])
    ada_bT = IN("ada_bT", [128, 2, 48])
    normT = IN("normT", [128, 5, KD])
    ev_w_in = IN("ev_w_in", [D, EV_COLS])
    ev_w_out = IN("ev_w_out", [D, D])
    shiftT = IN("shiftT", [128, 12, 3])
    rvec = IN("rvec", [128, 9, 4])
    lora_up = IN("lora_up", [128, 2, 512])
    g_up = IN("g_up", [128, 512])
    pool_w = IN("pool_w", [4, 128, 128])
    pool_scT = IN("pool_scT", [128, 4])
    pool_inv = IN("pool_inv", [4, NT])
    moe_r = IN("moe_r", [2, D, 36])
    moe_wg = IN("moe_wg", [2, 32, D, 512])
    moe_wu = IN("moe_wu", [2, 32, D, 512])
    moe_wd = IN("moe_wd", [2, 32, 512, D])
    od_w_in = IN("od_w_in", [D, OD_COLS])
    od_w_out = IN("od_w_out", [D, D])
    dlam = IN("dlam", [1, 256])
    sublnT = IN("sublnT", [128, 1])
    retgT = IN("retgT", [128, 4])
    rope_perm = IN("rope_perm", [128, 128])
    ropeC = IN("ropeC", [128, S])
    ropeS = IN("ropeS", [128, S])
    retD = IN("retD", [8, 128, 128])
    retq = IN("retq", [8, 128])
    retk = IN("retk", [128, 8])
    cident = IN("ident", [128, 128])
    cblk = IN("blk64", [128, 128])
    crmask = IN("rmask", [2, 128, 896])
    out = P.dram("out", [S, D], F32, "ExternalOutput")
    xTs = P.dram("xTs", [KD, 128, NT], F32)
    pTs = P.dram("pTs", [24, 128, NT], F32, "ExternalOutput" if dbg else "Internal")
    ymTs = P.dram("ymTs", [KD, 128, NT], BF16, "ExternalOutput" if dbg else "Internal")
    dbgs = {}

    def tap(name, t, shape):
        if dbg:
            d = P.dram("dbg_" + name, shape, F32, "ExternalOutput")
            P.dma('sp', d, t.ap, reads=[t], is_output=True)

    ident = P.alloc([128, 128]); P.dma('sp', ident.ap, cident, writes=[ident])
    identb = P.alloc([128, 128], BF16); P.dma('pool', identb.ap, cident, writes=[identb])
    blk = P.alloc([128, 128]); P.dma('sp', blk.ap, cblk, writes=[blk])
    onesD = P.alloc([128, 128]); P.memset('pool', onesD.ap, 1.0 / D, writes=[onesD])
    normv = P.alloc([128, 5, KD]); P.dma('sp', normv.ap, normT, writes=[normv])
    mod = P.alloc([128, 2, 48, 2])
    m0 = P.mark()
    sc = P.alloc([128, KD, 2]); P.dma('sp', sc.ap, cT, writes=[sc])
    P.act(sc.ap, sc.ap, AF.Silu, reads=[sc], writes=[sc])
    adab = P.alloc([128, 2, 48]); P.dma('sp', adab.ap, ada_bT, writes=[adab])
    wbuf = [P.alloc([128, KD, 1024]) for _ in range(1)]
    for li in range(2):
        for blkc in range(6):
            wb = wbuf[0]
            for k in range(KD):
                P.dma('sp' if k % 2 == 0 else 'act', wb.ap[:, k, :],
                      ada_w[li, k * 128:(k + 1) * 128, blkc * 1024:(blkc + 1) * 1024], writes=[wb])
            for cc in range(8):
                ps = P.psum('a')
                for k in range(KD):
                    P.mm(ps.ap[:, 0:2], wb.ap[:, k, cc * 128:(cc + 1) * 128], sc.ap[:, k, :],
                         start=(k == 0), stop=(k == KD - 1), reads=[wb, sc], writes=[ps])
                j = blkc * 8 + cc
                P.stt('dve', mod.ap[:, li, j, :], ps.ap[:, 0:2], 1.0,
                      adab.ap[:, li, j:j + 1].to_broadcast([128, 2]), ALU.mult, ALU.add,
                      reads=[ps, adab], writes=[mod])
    P.release(m0)
    AB = P.alloc([128, 2, 2, 2, KD, 2])
    for li in range(2):
        for sub in range(2):
            shc = 24 * sub
            scc = 24 * sub + 8
            nidx = li if sub == 0 else 2 + li
            for w in range(2):
                P.ts('dve', AB.ap[:, li, sub, 0, :, w], mod.ap[:, li, scc:scc + 8, w], 1.0, None, ALU.add,
                     reads=[mod], writes=[AB])
                P.tt('dve', AB.ap[:, li, sub, 0, :, w], AB.ap[:, li, sub, 0, :, w], normv.ap[:, nidx, :], ALU.mult,
                     reads=[AB, normv], writes=[AB])
                P.cp('dve', AB.ap[:, li, sub, 1, :, w], mod.ap[:, li, shc:shc + 8, w], reads=[mod], writes=[AB])
    if dbg:
        tap("mod", mod, [128, 2, 48, 2])

    def norm_mod(xT, TT, li, sub, w, hb, sq, rs, final=False):
        P.act(sq.ap[:, :, 0:TT], xT.ap[:, :, 0:TT], AF.Square, reads=[xT], writes=[sq])
        ps = P.psum('a')
        for k in range(KD):
            P.mm(ps.ap[:, 0:TT], onesD.ap, sq.ap[:, k, 0:TT], start=(k == 0), stop=(k == KD - 1),
                 reads=[onesD, sq], writes=[ps])
        P.ts('dve', rs.ap[:, 0:TT], ps.ap[:, 0:TT], 1e-6, None, ALU.add, reads=[ps], writes=[rs])
        P.act(rs.ap[:, 0:TT], rs.ap[:, 0:TT], AF.Ln, reads=[rs], writes=[rs])
        P.act(rs.ap[:, 0:TT], rs.ap[:, 0:TT], AF.Exp, reads=[rs], writes=[rs], scale=-0.5)
        P.tt('dve', sq.ap[:, :, 0:TT], xT.ap[:, :, 0:TT], rs.ap[:, None, 0:TT].to_broadcast([128, KD, TT]), ALU.mult,
             reads=[xT, rs], writes=[sq])
        for k in range(KD):
            if final:
                P.ts('dve' if k % 2 else 'pool', hb.ap[:, k, 0:TT], sq.ap[:, k, 0:TT], normv.ap[:, 4, k:k + 1], None, ALU.mult,
                     reads=[sq, normv], writes=[hb])
            else:
                P.ts('dve' if k % 2 else 'pool', hb.ap[:, k, 0:TT], sq.ap[:, k, 0:TT], AB.ap[:, li, sub, 0, k, w:w + 1],
                     AB.ap[:, li, sub, 1, k, w:w + 1], ALU.mult, ALU.add, reads=[sq, AB], writes=[hb])

    def load_w_bf(dst, src, K):
        for k in range(K):
            P.dma('pool', dst.ap[:, k, :], src[k * 128:(k + 1) * 128, :], writes=[dst])

    def phase_inproj(li, w_in_dram, ncols, first):
        m = P.mark()
        NCC = ncols // 128
        Win = P.alloc([128, KD, ncols], BF16)
        load_w_bf(Win, w_in_dram, KD)
        xtok = [P.alloc([128, D]) for _ in range(2)]
        xT = [P.alloc([128, KD, 512]) for _ in range(2)]
        hb = [P.alloc([128, KD, 512], BF16) for _ in range(2)]
        sq = P.alloc([128, KD, 512]); rs = P.alloc([128, 512])
        pst = [P.alloc([128, 6, 512]) for _ in range(2)]
        for ti, (t0, TT, isc) in enumerate(tiles):
            xt = xT[ti % 2]
            if first:
                for b in range(TT // 128):
                    xk = xtok[b % 2]
                    P.dma('sp', xk.ap, xin[t0 + b * 128:t0 + (b + 1) * 128, :], writes=[xk])
                    for half in range(2):
                        ps = P.psum('a')
                        for j in range(4):
                            k = half * 4 + j
                            P.tr(ps.ap[:, j * 128:(j + 1) * 128], xk.ap[:, k * 128:(k + 1) * 128], ident.ap,
                                 reads=[xk, ident], writes=[ps])
                        P.cp('dve' if half else 'act', xt.ap[:, half * 4:half * 4 + 4, b * 128:(b + 1) * 128],
                             ps.ap.rearrange("p (a b) -> p a b", a=4), reads=[ps], writes=[xt])
                for k in range(KD):
                    P.dma('sp', xTs[k, :, t0:t0 + TT], xt.ap[:, k, 0:TT], reads=[xt])
            else:
                for k in range(KD):
                    P.dma('sp', xt.ap[:, k, 0:TT], xTs[k, :, t0:t0 + TT], writes=[xt])
            h = hb[ti % 2]
            norm_mod(xt, TT, li, 0, isc, h, sq, rs)
            for g in range(NCC // 6):
                st = pst[g % 2]
                for c6 in range(6):
                    cc = g * 6 + c6
                    ps = P.psum('a')
                    for k in range(KD):
                        P.mm(ps.ap[:, 0:TT], Win.ap[:, k, cc * 128:(cc + 1) * 128], h.ap[:, k, 0:TT],
                             start=(k == 0), stop=(k == KD - 1), reads=[Win, h], writes=[ps])
                    P.cp('act' if c6 % 2 else 'dve', st.ap[:, c6, 0:TT], ps.ap[:, 0:TT], reads=[ps], writes=[st])
                P.dma('sp', pTs[g * 6:(g + 1) * 6, :, t0:t0 + TT].rearrange("c p t -> p c t"), st.ap[:, :, 0:TT],
                      reads=[st])
        P.release(m)


    def phase_moe(li, lat_only):
        m = P.mark()
        tl = [t for t in tiles if not (lat_only and t[2])]
        xres = P.alloc([128, KD, NT])
        hfT = P.alloc([128, KD, NT], BF16)
        gT = P.alloc([32, NT])
        wr = P.alloc([128, KD, 36])
        P.dma('sp', wr.ap, moe_r[li].rearrange("(k p) n -> p k n", p=128), writes=[wr])
        m2 = P.mark()
        sq = P.alloc([128, KD, 512]); rs = P.alloc([128, 512])
        lg = P.alloc([128, 36]); oh = P.alloc([128, 4]); st_ = P.alloc([128, 16]); les = P.alloc([128, 8])
        mk1 = P.alloc([128, 8]); mk2 = P.alloc([128, 8]); g8 = P.alloc([128, 8]); g32 = P.alloc([128, 4, 8])
        ex4 = P.alloc([128, 4])
        for (t0, TT, isc) in tl:
            xt = T(xres.ap[:, :, t0:t0 + TT]); hb = T(hfT.ap[:, :, t0:t0 + TT])
            for k in range(KD):
                P.dma('sp' if k % 2 else 'act', xt.ap[:, k, :], xTs[k, :, t0:t0 + TT], writes=[xt, xres])
            norm_mod(xt, TT, li, 1, isc, hb, sq, rs)
            for k in range(KD):
                P.ts('dve', sq.ap[:, k, 0:TT], sq.ap[:, k, 0:TT], AB.ap[:, li, 1, 0, k, isc:isc + 1],
                     AB.ap[:, li, 1, 1, k, isc:isc + 1], ALU.mult, ALU.add, reads=[sq, AB], writes=[sq])
            for b in range(TT // 128):
                bs = slice(b * 128, (b + 1) * 128)
                ps = P.psum('a')
                for k in range(KD):
                    P.mm(ps.ap[:, 0:36], sq.ap[:, k, bs], wr.ap[:, k, :], start=(k == 0), stop=(k == KD - 1),
                         reads=[sq, wr], writes=[ps])
                P.cp('dve', lg.ap, ps.ap[:, 0:36], reads=[ps], writes=[lg])
                def red(out, in_, op):
                    return P.op('dve', lambda e: e.tensor_reduce(out=out, in_=in_, axis=AX.X, op=op), [lg, les, ex4, st_], [st_])
                P.op('dve', lambda e: e.tensor_reduce(out=st_.ap[:, 0:1], in_=lg.ap[:, 0:4], axis=AX.X, op=ALU.max), [lg], [st_])
                P.ts('dve', oh.ap, lg.ap[:, 0:4], st_.ap[:, 0:1], None, ALU.is_equal, reads=[lg, st_], writes=[oh])
                P.ts('dve', st_.ap[:, 1:2], st_.ap[:, 0:1], -1.0, None, ALU.mult, reads=[st_], writes=[st_])
                P.act(ex4.ap, lg.ap[:, 0:4], AF.Exp, reads=[lg, st_], writes=[ex4], bias=st_.ap[:, 1:2])
                P.op('dve', lambda e: e.tensor_reduce(out=st_.ap[:, 2:3], in_=ex4.ap, axis=AX.X, op=ALU.add), [ex4], [st_])
                P.op('dve', lambda e: e.reciprocal(out=st_.ap[:, 3:4], in_=st_.ap[:, 2:3]), [st_], [st_])
                P.ts('dve', les.ap, lg.ap[:, 4:12], oh.ap[:, 0:1], None, ALU.mult, reads=[lg, oh], writes=[les])
                for g in range(1, 4):
                    P.stt('dve', les.ap, lg.ap[:, 4 + 8 * g:12 + 8 * g], oh.ap[:, g:g + 1], les.ap, ALU.mult, ALU.add,
                          reads=[lg, oh, les], writes=[les])
                P.op('dve', lambda e: e.tensor_reduce(out=st_.ap[:, 4:5], in_=les.ap, axis=AX.X, op=ALU.max), [les], [st_])
                P.ts('dve', mk1.ap, les.ap, st_.ap[:, 4:5], None, ALU.is_equal, reads=[les, st_], writes=[mk1])
                P.stt('dve', g8.ap, mk1.ap, -1e30, les.ap, ALU.mult, ALU.add, reads=[mk1, les], writes=[g8])
                P.op('dve', lambda e: e.tensor_reduce(out=st_.ap[:, 5:6], in_=g8.ap, axis=AX.X, op=ALU.max), [g8], [st_])
                P.ts('dve', mk2.ap, g8.ap, st_.ap[:, 5:6], None, ALU.is_equal, reads=[g8, st_], writes=[mk2])
                P.tt('dve', st_.ap[:, 6:7], st_.ap[:, 5:6], st_.ap[:, 4:5], ALU.subtract, reads=[st_], writes=[st_])
                P.act(st_.ap[:, 7:8], st_.ap[:, 6:7], AF.Exp, reads=[st_], writes=[st_])
                P.ts('dve', st_.ap[:, 8:9], st_.ap[:, 7:8], 1.0, None, ALU.add, reads=[st_], writes=[st_])
                P.op('dve', lambda e: e.reciprocal(out=st_.ap[:, 9:10], in_=st_.ap[:, 8:9]), [st_], [st_])
                P.tt('dve', st_.ap[:, 10:11], st_.ap[:, 9:10], st_.ap[:, 3:4], ALU.mult, reads=[st_], writes=[st_])
                P.tt('dve', st_.ap[:, 11:12], st_.ap[:, 10:11], st_.ap[:, 7:8], ALU.mult, reads=[st_], writes=[st_])
                P.ts('dve', g8.ap, mk1.ap, st_.ap[:, 10:11], None, ALU.mult, reads=[mk1, st_], writes=[g8])
                P.stt('dve', g8.ap, mk2.ap, st_.ap[:, 11:12], g8.ap, ALU.mult, ALU.add, reads=[mk2, st_, g8], writes=[g8])
                for g in range(4):
                    P.ts('dve', g32.ap[:, g, :], g8.ap, oh.ap[:, g:g + 1], None, ALU.mult, reads=[g8, oh], writes=[g32])
                pt = P.psum('a')
                P.tr(pt.ap[0:32, 0:128], g32.ap.rearrange("p a b -> p (a b)"), ident.ap, reads=[g32, ident], writes=[pt])
                P.cp('act', gT.ap[:, t0 + b * 128:t0 + (b + 1) * 128], pt.ap[0:32, 0:128], reads=[pt], writes=[gT])
        P.release(m2)
        if dbg:
            tap(f"gT{li}", gT, [32, NT])
        Wg = [P.alloc([128, KD, 512], BF16) for _ in range(2)]
        Wu = [P.alloc([128, KD, 512], BF16) for _ in range(2)]
        Wd = [P.alloc([128, 4, D], BF16) for _ in range(2)]
        selt = [P.alloc([32, 128]) for _ in range(2)]
        gbc = [P.alloc([128, 512]) for _ in range(2)]
        sgl = [P.alloc([128, 512]) for _ in range(2)]
        a1 = [P.alloc([128, 512]) for _ in range(2)]
        actT = [P.alloc([128, 4, 512], BF16) for _ in range(2)]
        it = 0
        for e in range(32):
            wg = Wg[e % 2]; wu = Wu[e % 2]; wd = Wd[e % 2]; se = selt[e % 2]
            for k in range(KD):
                P.dma('pool', wg.ap[:, k, :], moe_wg[li, e, k * 128:(k + 1) * 128, :], writes=[wg])
                P.dma('pool', wu.ap[:, k, :], moe_wu[li, e, k * 128:(k + 1) * 128, :], writes=[wu])
            for k in range(4):
                P.dma('pool', wd.ap[:, k, :], moe_wd[li, e, k * 128:(k + 1) * 128, :], writes=[wd])
            P.cp('pool', se.ap, ident.ap[0:32, e:e + 1].to_broadcast([32, 128]), reads=[ident], writes=[se])
            for (t0, TT, isc) in tl:
                it += 1
                gb = gbc[it % 2]; at = actT[it % 2]
                pg_ = P.psum('b')
                P.mm(pg_.ap[:, 0:TT], se.ap, gT.ap[:, t0:t0 + TT], reads=[se, gT], writes=[pg_])
                P.cp('act', gb.ap[:, 0:TT], pg_.ap[:, 0:TT], reads=[pg_], writes=[gb])
                for fc in range(4):
                    fs = slice(fc * 128, (fc + 1) * 128)
                    pg = P.psum('a'); pu = P.psum('a')
                    for k in range(KD):
                        P.mm(pg.ap[:, 0:TT], wg.ap[:, k, fs], hfT.ap[:, k, t0:t0 + TT], start=(k == 0), stop=(k == KD - 1),
                             reads=[wg, hfT], writes=[pg])
                    for k in range(KD):
                        P.mm(pu.ap[:, 0:TT], wu.ap[:, k, fs], hfT.ap[:, k, t0:t0 + TT], start=(k == 0), stop=(k == KD - 1),
                             reads=[wu, hfT], writes=[pu])
                    sg_ = sgl[fc % 2]; a_ = a1[fc % 2]
                    P.act(sg_.ap[:, 0:TT], pg.ap[:, 0:TT], AF.Silu, reads=[pg], writes=[sg_])
                    P.tt('dve', a_.ap[:, 0:TT], pu.ap[:, 0:TT], sg_.ap[:, 0:TT], ALU.mult, reads=[pu, sg_], writes=[a_])
                    P.tt('pool', at.ap[:, fc, 0:TT], a_.ap[:, 0:TT], gb.ap[:, 0:TT], ALU.mult, reads=[a_, gb], writes=[at])
                for dc in range(KD):
                    po = P.psum('c')
                    for fc in range(4):
                        P.mm(po.ap[:, 0:TT], wd.ap[:, fc, dc * 128:(dc + 1) * 128], at.ap[:, fc, 0:TT],
                             start=(fc == 0), stop=(fc == 3), reads=[wd, at], writes=[po])
                    P.stt('dve', xres.ap[:, dc, t0:t0 + TT], po.ap[:, 0:TT], mod.ap[:, li, 40 + dc, isc:isc + 1],
                          xres.ap[:, dc, t0:t0 + TT], ALU.mult, ALU.add, reads=[po, mod, xres], writes=[xres])
        for (t0, TT, isc) in tl:
            for k in range(KD):
                P.dma('sp', xTs[k, :, t0:t0 + TT], xres.ap[:, k, t0:t0 + TT], reads=[xres])
        P.release(m)


    LAM_INIT = 0.8 - 0.6 * math.exp(-0.3 * 1)
    RET_G128 = []
    for h_ in range(4):
        lgf = [math.log(1.0 - 2.0 ** (-5.0 - j)) for j in range(4)]
        RET_G128.append((math.exp(lgf[h_] * 128), math.exp(lgf[3 - h_] * 128)))

    def phase_l1mix():
        m = P.mark()
        LT = [(t0 - C, TT) for (t0, TT, isc) in tiles if not isc]
        perm = P.alloc([128, 128]); P.dma('sp', perm.ap, rope_perm, writes=[perm])
        rc_ = P.alloc([128, S]); P.dma('sp', rc_.ap, ropeC, writes=[rc_])
        rs_ = P.alloc([128, S]); P.dma('sp', rs_.ap, ropeS, writes=[rs_])
        ones128 = P.alloc([128, 128]); P.memset('pool', ones128.ap, 1.0 / 128, writes=[ones128])
        onesb = P.alloc([128, 128], BF16); P.memset('pool', onesb.ap, 1.0, writes=[onesb])
        subg = P.alloc([128, 1]); P.dma('sp', subg.ap, sublnT, writes=[subg])
        P.ts('dve', subg.ap, subg.ap, 1.0 - LAM_INIT, None, ALU.mult, reads=[subg], writes=[subg])
        rgv = P.alloc([128, 4]); P.dma('sp', rgv.ap, retgT, writes=[rgv])
        rkv = P.alloc([128, 8]); P.dma('sp', rkv.ap, retk, writes=[rkv])
        dl = P.alloc([1, 4, 64]); P.dma('sp', dl.ap, dlam.rearrange("o (a b) -> o a b", a=4), writes=[dl])
        pr2 = P.alloc([1, 2, 64]); s2 = P.alloc([1, 4]); nlam = P.alloc([128, 1]); onesr = P.alloc([1, 128])
        P.memset('dve', onesr.ap, 1.0, writes=[onesr])
        P.tt('dve', pr2.ap[:, 0, :], dl.ap[:, 0, :], dl.ap[:, 1, :], ALU.mult, reads=[dl], writes=[pr2])
        P.tt('dve', pr2.ap[:, 1, :], dl.ap[:, 2, :], dl.ap[:, 3, :], ALU.mult, reads=[dl], writes=[pr2])
        P.op('dve', lambda e: e.tensor_reduce(out=s2.ap[:, 0:2], in_=pr2.ap, axis=AX.X, op=ALU.add), [pr2], [s2])
        P.act(s2.ap[:, 0:2], s2.ap[:, 0:2], AF.Exp, reads=[s2], writes=[s2])
        P.tt('dve', s2.ap[:, 2:3], s2.ap[:, 1:2], s2.ap[:, 0:1], ALU.subtract, reads=[s2], writes=[s2])
        P.ts('dve', s2.ap[:, 3:4], s2.ap[:, 2:3], -LAM_INIT, None, ALU.add, reads=[s2], writes=[s2])
        psl = P.psum('a')
        P.mm(psl.ap[:, 0:1], onesr.ap, s2.ap[:, 3:4], reads=[onesr, s2], writes=[psl])
        P.cp('dve', nlam.ap, psl.ap[:, 0:1], reads=[psl], writes=[nlam])
        raw = [P.alloc([128, NT]) for _ in range(2)]
        vT = P.alloc([128, NT])
        kT = P.alloc([128, NT], BF16); qT = P.alloc([128, S], BF16)
        Vtok = P.alloc([128, NCH, 128], BF16)
        t512 = [P.alloc([128, 512]) for _ in range(6)]
        pTb = [P.alloc([128, 512], BF16) for _ in range(3)]
        ymt = [P.alloc([128, 512], BF16) for _ in range(2)]
        cnt = [0]

        def rr(lst):
            cnt[0] += 1
            return lst[cnt[0] % len(lst)]

        def rope(dst, dcol0, src, scol0, scale=None):
            for (l0, TT) in LT:
                ps = P.psum('a')
                P.mm(ps.ap[:, 0:TT], perm.ap, src.ap[:, scol0 + l0:scol0 + l0 + TT], reads=[perm, src], writes=[ps])
                a = rr(t512); b = rr(t512)
                P.tt('dve', a.ap[:, 0:TT], ps.ap[:, 0:TT], rs_.ap[:, l0:l0 + TT], ALU.mult, reads=[ps, rs_], writes=[a])
                P.tt('pool', b.ap[:, 0:TT], src.ap[:, scol0 + l0:scol0 + l0 + TT], rc_.ap[:, l0:l0 + TT], ALU.mult, reads=[src, rc_], writes=[b])
                if scale is None:
                    P.tt('dve', dst.ap[:, dcol0 + l0:dcol0 + l0 + TT], a.ap[:, 0:TT], b.ap[:, 0:TT], ALU.add, reads=[a, b], writes=[dst])
                else:
                    P.tt('dve', a.ap[:, 0:TT], a.ap[:, 0:TT], b.ap[:, 0:TT], ALU.add, reads=[a, b], writes=[a])
                    P.ts('dve', dst.ap[:, dcol0 + l0:dcol0 + l0 + TT], a.ap[:, 0:TT], scale, None, ALU.mult, reads=[a], writes=[dst])

        def make_vtok(vsrc):
            for c4 in range(0, NCH, 4):
                n = min(4, NCH - c4)
                ps = P.psum('a')
                for j in range(n):
                    P.tr(ps.ap[:, j * 128:(j + 1) * 128], vsrc.ap[:, (c4 + j) * 128:(c4 + j + 1) * 128], ident.ap,
                         reads=[vsrc, ident], writes=[ps])
                P.cp('act', Vtok.ap[:, c4:c4 + n, :], ps.ap[:, 0:n * 128].rearrange("p (a b) -> p a b", a=n), reads=[ps], writes=[Vtok])

        def post_norm(o, TT, eps, gain_ap, extra, dst_chunk, l0):
            sq_ = rr(t512)
            P.act(sq_.ap[:, 0:TT], o.ap[:, 0:TT], AF.Square, reads=[o], writes=[sq_])
            ps = P.psum('a')
            P.mm(ps.ap[:, 0:TT], ones128.ap, sq_.ap[:, 0:TT], reads=[ones128, sq_], writes=[ps])
            P.ts('dve', sq_.ap[:, 0:TT], ps.ap[:, 0:TT], eps, None, ALU.add, reads=[ps], writes=[sq_])
            P.act(sq_.ap[:, 0:TT], sq_.ap[:, 0:TT], AF.Ln, reads=[sq_], writes=[sq_])
            P.act(sq_.ap[:, 0:TT], sq_.ap[:, 0:TT], AF.Exp, reads=[sq_], writes=[sq_], scale=-0.5)
            P.stt('dve', sq_.ap[:, 0:TT], o.ap[:, 0:TT], gain_ap, sq_.ap[:, 0:TT], ALU.mult, ALU.mult, reads=[o, sq_, subg, rgv], writes=[sq_])
            ym = rr(ymt)
            if extra is None:
                P.cp('dve', ym.ap[:, 0:TT], sq_.ap[:, 0:TT], reads=[sq_], writes=[ym])
            else:
                P.tt('dve', ym.ap[:, 0:TT], sq_.ap[:, 0:TT], extra, ALU.mult, reads=[sq_, raw[0], raw[1]], writes=[ym])
            P.dma('sp', ymTs[dst_chunk, :, C + l0:C + l0 + TT], ym.ap[:, 0:TT], reads=[ym])

        A1, S1, A2, S2 = P.PS[0], P.PS[1], P.PS[2], P.PS[3]
        for h in range(4):
            P.dma('sp', raw[0].ap, pTs[h], writes=[raw[0]])
            P.dma('act', raw[1].ap[:, 0:S], pTs[14 + h][:, C:NT], writes=[raw[1]])
            P.dma('sp', vT.ap, pTs[4 + h], writes=[vT])
            P.cp('pool', kT.ap[:, 0:C], raw[0].ap[:, 0:C], reads=[raw[0]], writes=[kT])
            rope(kT, C, raw[0], C)
            rope(qT, 0, raw[1], 0)
            make_vtok(vT)
            for (l0, TT) in LT:
                for kc in range(NCH):
                    for br, (Ab, Sb) in enumerate(((A1, S1), (A2, S2))):
                        hsb = slice(br * 64, br * 64 + 64)
                        sc = P.psum('c')
                        P.mm(sc.ap[:, 0:TT], kT.ap[hsb, kc * 128:(kc + 1) * 128], qT.ap[hsb, l0:l0 + TT], reads=[kT, qT], writes=[sc])
                        pt = rr(pTb)
                        P.act(pt.ap[:, 0:TT], sc.ap[:, 0:TT], AF.Exp, reads=[sc], writes=[pt], scale=0.125)
                        P.mm(Ab.ap[:, 0:TT], Vtok.ap[:, kc, :], pt.ap[:, 0:TT], start=(kc == 0), stop=(kc == NCH - 1), reads=[Vtok, pt], writes=[Ab])
                        P.mm(Sb.ap[:, 0:TT], onesb.ap, pt.ap[:, 0:TT], start=(kc == 0), stop=(kc == NCH - 1), reads=[onesb, pt], writes=[Sb])
                r1 = rr(t512); o1 = rr(t512); r2 = rr(t512); o2 = rr(t512)
                P.op('dve', lambda e, r1=r1, TT=TT: e.reciprocal(out=r1.ap[:, 0:TT], in_=S1.ap[:, 0:TT]), [S1], [r1])
                P.tt('dve', o1.ap[:, 0:TT], A1.ap[:, 0:TT], r1.ap[:, 0:TT], ALU.mult, reads=[A1, r1], writes=[o1])
                P.op('dve', lambda e, r2=r2, TT=TT: e.reciprocal(out=r2.ap[:, 0:TT], in_=S2.ap[:, 0:TT]), [S2], [r2])
                P.tt('dve', o2.ap[:, 0:TT], A2.ap[:, 0:TT], r2.ap[:, 0:TT], ALU.mult, reads=[A2, r2], writes=[o2])
                P.stt('dve', o1.ap[:, 0:TT], o2.ap[:, 0:TT], nlam.ap[:, 0:1], o1.ap[:, 0:TT], ALU.mult, ALU.add, reads=[o2, nlam, o1], writes=[o1])
                post_norm(o1, TT, 1e-5, subg.ap[:, 0:1], None, h, l0)
        kTp = kT
        qTp = qT
        ktok = P.alloc([128, NCH, 128], BF16)
        oT = P.alloc([128, S])
        Rf = P.alloc([128, 128]); Rb = P.alloc([128, 128], BF16)
        dm = P.alloc([128, 128]); qrow = P.alloc([128, 128])
        innm = [P.alloc([128, 128], BF16) for _ in range(2)]
        qd = [P.alloc([128, 128], BF16) for _ in range(2)]
        kd = [P.alloc([128, 64], BF16) for _ in range(2)]
        for h in range(4):
            hq = h % 2
            hsq = slice(hq * 64, hq * 64 + 64)
            if hq == 0:
                P.dma('sp', raw[0].ap, pTs[8 + h // 2], writes=[raw[0]])
                P.dma('act', raw[1].ap[:, 0:S], pTs[18 + h // 2][:, C:NT], writes=[raw[1]])
                P.ts('pool', kTp.ap[:, 0:C], raw[0].ap[:, 0:C], 0.125, None, ALU.mult, reads=[raw[0]], writes=[kTp])
                rope(kTp, C, raw[0], C, scale=0.125)
                rope(qTp, 0, raw[1], 0)
                for c4 in range(0, NCH, 4):
                    n = min(4, NCH - c4)
                    ps = P.psum('a'); psb_ = ps.ap.bitcast(BF16)
                    for j in range(n):
                        P.tr(psb_[:, j * 128:(j + 1) * 128], kTp.ap[:, (c4 + j) * 128:(c4 + j + 1) * 128], identb.ap,
                             reads=[kTp, identb], writes=[ps])
                    P.cp('act', ktok.ap[:, c4:c4 + n, :], psb_[:, 0:n * 128].rearrange("p (a b) -> p a b", a=n), reads=[ps], writes=[ktok])
            P.dma('sp', vT.ap, pTs[10 + h], writes=[vT])
            make_vtok(vT)
            for d in range(2):
                hd = h * 2 + d
                g128 = RET_G128[h][d]
                P.dma('sp', dm.ap, retD[hd], writes=[dm])
                P.dma('sp', qrow.ap, retq[hd:hd + 1, :].partition_broadcast(128), writes=[qrow])
                P.memset('dve', Rf.ap, 0.0, writes=[Rf]); P.memset('pool', Rb.ap, 0.0, writes=[Rb])
                order = list(range(0, NCH)) if d == 0 else list(range(CCH - 1, -1, -1)) + list(range(NCH - 1, CCH - 1, -1))
                for oi, c in enumerate(order):
                    ksl = slice(c * 128, (c + 1) * 128)
                    if c >= CCH:
                        i0 = c * 128 - C
                        im = rr(innm); q_ = rr(qd)
                        ps1 = P.psum('c')
                        P.mm(ps1.ap[:, 0:128], kTp.ap[hsq, ksl], qTp.ap[hsq, i0:i0 + 128], reads=[kTp, qTp], writes=[ps1])
                        P.tt('dve', im.ap, ps1.ap[:, 0:128], dm.ap, ALU.mult, reads=[ps1, dm], writes=[im])
                        P.tt('pool', q_.ap[hsq, :], qTp.ap[hsq, i0:i0 + 128], qrow.ap[hsq, :], ALU.mult, reads=[qTp, qrow], writes=[q_])
                        ps2 = P.psum('c')
                        P.mm(ps2.ap[:, 0:128], Vtok.ap[:, c, :], im.ap, start=True, stop=False, reads=[Vtok, im], writes=[ps2])
                        P.mm(ps2.ap[:, 0:128], Rb.ap[hsq, :], q_.ap[hsq, :], start=False, stop=True, reads=[Rb, q_], writes=[ps2])
                        if d == 0:
                            P.cp('act', oT.ap[:, i0:i0 + 128], ps2.ap[:, 0:128], reads=[ps2], writes=[oT])
                        else:
                            P.tt('dve', oT.ap[:, i0:i0 + 128], ps2.ap[:, 0:128], oT.ap[:, i0:i0 + 128], ALU.add, reads=[ps2, oT], writes=[oT])
                    if oi < len(order) - 1:
                        k_ = rr(kd)
                        P.ts('pool', k_.ap, ktok.ap[:, c, hsq], rkv.ap[:, hd:hd + 1], None, ALU.mult, reads=[ktok, rkv], writes=[k_])
                        ps3 = P.psum('c')
                        P.mm(ps3.ap[hsq, 0:128], k_.ap, Vtok.ap[:, c, :], reads=[k_, Vtok], writes=[ps3])
                        P.stt('dve', Rf.ap[hsq, :], Rf.ap[hsq, :], g128, ps3.ap[hsq, 0:128], ALU.mult, ALU.add, reads=[Rf, ps3], writes=[Rf])
                        P.cp('act', Rb.ap[hsq, :], Rf.ap[hsq, :], reads=[Rf], writes=[Rb])
            P.dma('act', raw[1].ap[:, 0:S], pTs[20 + h][:, C:NT], writes=[raw[1]]) if hq == 1 else \
                P.dma('act', raw[0].ap[:, 0:S], pTs[20 + h][:, C:NT], writes=[raw[0]])
            gsrc = raw[1] if hq == 1 else raw[0]
            P.act(gsrc.ap[:, 0:S], gsrc.ap[:, 0:S], AF.Silu, reads=[gsrc], writes=[gsrc])
            for (l0, TT) in LT:
                ot = T(oT.ap[:, l0:l0 + TT])
                ot.lw = oT.lw
                post_norm(ot, TT, 1e-6, rgv.ap[:, h:h + 1], gsrc.ap[:, l0:l0 + TT], 4 + h, l0)
                oT.rd.update(ot.rd)
        P.release(m)

    if STOP_AFTER == 'mod':
        return P, locals()
    phase_inproj(0, ev_w_in, EV_COLS, True)


    def phase_rwkv():
        m = P.mark()
        DIN, DCH, DST = RW_DT
        idch = ident if DCH == F32 else identb
        idin = ident if DIN == F32 else identb
        seqs = [(0, C), (C, NT)]
        lup = P.alloc([128, 2, 512], BF16); P.dma('pool', lup.ap, lora_up, writes=[lup])
        gup = P.alloc([128, 512], BF16); P.dma('pool', gup.ap, g_up, writes=[gup])
        rv = P.alloc([128, 9, 4]); P.dma('sp', rv.ap, rvec, writes=[rv])
        shv = P.alloc([128, 12, 3]); P.dma('sp', shv.ap, shiftT, writes=[shv])
        rmk = P.alloc([128, 2, 896])
        for d in range(2):
            P.dma('sp', rmk.ap[:, d, :], crmask[d], writes=[rmk])
        omka = P.alloc([128, 4])
        P.ts('dve', omka.ap, rv.ap[:, 5, :], -1.0, 1.0, ALU.mult, ALU.add, reads=[rv], writes=[omka])
        tmpA = P.alloc([128, NT]); tmpB = P.alloc([128, NT])
        wdad = P.alloc([128, NT], BF16); sg = P.alloc([128, NT], BF16)
        P.dma('sp', tmpA.ap, pTs[8], writes=[tmpA])
        P.act(wdad.ap[0:64, :], tmpA.ap[0:64, :], AF.Tanh, reads=[tmpA], writes=[wdad])
        P.cp('dve', wdad.ap[64:128, :], tmpA.ap[64:128, :], reads=[tmpA], writes=[wdad])
        P.dma('sp', tmpB.ap, pTs[13], writes=[tmpB])
        P.act(sg.ap, tmpB.ap, AF.Sigmoid, reads=[tmpB], writes=[sg])
        kc = P.alloc([128, NT]); lw = [P.alloc([128, NT]) for _ in range(2)]
        vc = P.alloc([128, NT], DIN); rc = P.alloc([128, NT], DIN); kk = P.alloc([128, NT], DIN)
        kt = [P.alloc([128, NT], DIN) for _ in range(2)]; bb = [P.alloc([128, NT], DIN) for _ in range(2)]
        MTb = P.alloc([128, 2, NCH, 128], DST); P.memset('pool', MTb.ap, 0.0, writes=[MTb])
        Sbk = P.alloc([128, 2, NCH, 128], DST); P.memset('pool', Sbk.ap, 0.0, writes=[Sbk])
        Gst = P.alloc([128, 2, NCH, 64]); Qs = P.alloc([128, 2, NCH, 128], DST); Y0 = P.alloc([128, NCH, 128])
        Vpad = [P.alloc([128, 2, 128], DCH) for _ in range(2)]
        P2p = [[P.alloc([128, 128], DCH) for _ in range(2)] for _ in range(2)]
        for t_ in Vpad + P2p[0] + P2p[1]:
            P.memset('pool', t_.ap, 0.0, writes=[t_])
        Vtk = [P.alloc([128, 128], DCH) for _ in range(2)]
        lwtok = [P.alloc([128, 128]) for _ in range(2)]
        E1 = [P.alloc([128, 128]) for _ in range(2)]; E0 = [P.alloc([128, 128]) for _ in range(2)]
        Ei = [P.alloc([128, 128]) for _ in range(2)]; nWC = [P.alloc([128, 1]) for _ in range(2)]
        QR = [P.alloc([128, 2, 128], DCH) for _ in range(2)]
        Bt = [P.alloc([128, 128], DCH) for _ in range(2)]; Kt = [P.alloc([128, 128], DCH) for _ in range(2)]
        nBh = [P.alloc([128, 128], DCH) for _ in range(2)]; K2 = [P.alloc([128, 128], DCH) for _ in range(2)]
        TK = [P.alloc([128, 3, 128], DCH) for _ in range(2)]
        evA = [P.alloc([128, 256], DCH) for _ in range(2)]; evB = [P.alloc([128, 256], DCH) for _ in range(2)]
        Xr = [P.alloc([128, 128], DCH) for _ in range(3)]; XTr = [P.alloc([128, 128], DCH) for _ in range(3)]
        TTr = [P.alloc([128, 128], DCH) for _ in range(3)]
        n2v = [P.alloc([128, 64], DCH) for _ in range(2)]; Pcat = [P.alloc([128, 128], DCH) for _ in range(2)]
        Sst = [P.alloc([128, 64], DST) for _ in range(2)]
        t512 = [P.alloc([128, 512]) for _ in range(4)]
        ymt = [P.alloc([128, 512], BF16) for _ in range(2)]
        cnt = [0]

        def rr(lst):
            cnt[0] += 1
            return lst[cnt[0] % len(lst)]

        def conv(dst, src, idx):
            for (a, b) in seqs:
                P.ts('dve', dst.ap[:, a:b], src.ap[:, a:b], shv.ap[:, idx, 1:2], None, ALU.mult, reads=[src, shv], writes=[dst])
                P.stt('dve', dst.ap[:, a + 1:b], src.ap[:, a:b - 1], shv.ap[:, idx, 0:1], dst.ap[:, a + 1:b], ALU.mult, ALU.add,
                      reads=[src, shv, dst], writes=[dst])
                P.stt('dve', dst.ap[:, a:b - 1], src.ap[:, a + 1:b], shv.ap[:, idx, 2:3], dst.ap[:, a:b - 1], ALU.mult, ALU.add,
                      reads=[src, shv, dst], writes=[dst])

        for pr in range(4):
            if RW_STOP == 0:
                break
            cs_ = slice(pr * 128, (pr + 1) * 128)
            P.dma('sp', tmpA.ap, pTs[pr], writes=[tmpA]); conv(kc, tmpA, pr)
            P.dma('sp', tmpB.ap, pTs[4 + pr], writes=[tmpB]); conv(vc, tmpB, 4 + pr)
            P.dma('sp', tmpA.ap, pTs[9 + pr], writes=[tmpA]); conv(rc, tmpA, 8 + pr)
            P.ts('dve', tmpA.ap, kc.ap, rv.ap[:, 4, pr:pr + 1], None, ALU.mult, reads=[kc, rv], writes=[tmpA])
            P.act(tmpB.ap, tmpA.ap, AF.Square, reads=[tmpA], writes=[tmpB])
            for (t0, TT, isc) in tiles:
                ps = P.psum('a')
                P.mm(ps.ap[:, 0:TT], blk.ap, tmpB.ap[:, t0:t0 + TT], reads=[blk, tmpB], writes=[ps])
                tq = rr(t512)
                P.ts('dve', tq.ap[:, 0:TT], ps.ap[:, 0:TT], 1e-12, None, ALU.max, reads=[ps], writes=[tq])
                P.act(tq.ap[:, 0:TT], tq.ap[:, 0:TT], AF.Ln, reads=[tq], writes=[tq])
                P.act(tq.ap[:, 0:TT], tq.ap[:, 0:TT], AF.Exp, reads=[tq], writes=[tq], scale=-0.5)
                P.tt('dve', kk.ap[:, t0:t0 + TT], tmpA.ap[:, t0:t0 + TT], tq.ap[:, 0:TT], ALU.mult, reads=[tmpA, tq], writes=[kk])
            for d in range(2):
                for (t0, TT, isc) in tiles:
                    ps = P.psum('a')
                    P.mm(ps.ap[:, 0:TT], lup.ap[0:64, d, cs_], wdad.ap[0:64, t0:t0 + TT], reads=[lup, wdad], writes=[ps])
                    P.act(lw[d].ap[:, t0:t0 + TT], ps.ap[:, 0:TT], AF.Sigmoid, reads=[ps, rv], writes=[lw[d]], bias=rv.ap[:, d, pr:pr + 1])
                    ps2 = P.psum('a')
                    P.mm(ps2.ap[:, 0:TT], lup.ap[64:128, d, cs_], wdad.ap[64:128, t0:t0 + TT], reads=[lup, wdad], writes=[ps2])
                    ta = rr(t512)
                    P.act(ta.ap[:, 0:TT], ps2.ap[:, 0:TT], AF.Sigmoid, reads=[ps2, rv], writes=[ta], bias=rv.ap[:, 2 + d, pr:pr + 1])
                    P.tt('dve', bb[d].ap[:, t0:t0 + TT], ta.ap[:, 0:TT], kk.ap[:, t0:t0 + TT], ALU.mult, reads=[ta, kk], writes=[bb[d]])
                    P.ts('dve', ta.ap[:, 0:TT], ta.ap[:, 0:TT], rv.ap[:, 5, pr:pr + 1], omka.ap[:, pr:pr + 1], ALU.mult, ALU.add,
                         reads=[ta, rv, omka], writes=[ta])
                    P.tt('dve', kt[d].ap[:, t0:t0 + TT], ta.ap[:, 0:TT], kc.ap[:, t0:t0 + TT], ALU.mult, reads=[ta, kc], writes=[kt[d]])
                P.ts('pool', lw[d].ap, lw[d].ap, -W_DECAY_SCALE, None, ALU.mult, reads=[lw[d]], writes=[lw[d]])
            if RW_STOP == 1:
                break
            PS6 = P.PS[6]
            for c in range(NCH):
                cs = slice(c * 128, (c + 1) * 128)
                vp = Vpad[c % 2]; vt = Vtk[c % 2]
                psb = P.psum('b')
                pv_ = psb.ap if DIN == F32 else psb.ap.bitcast(BF16)
                P.tr(pv_[:, 0:128], vc.ap[:, cs], idin.ap, reads=[vc, idin], writes=[psb])
                P.cp('act', vt.ap, pv_[:, 0:128], reads=[psb], writes=[vt])
                for hp in range(2):
                    P.cp('pool', vp.ap[:, hp, hp * 64:hp * 64 + 64], vt.ap[:, hp * 64:hp * 64 + 64], reads=[vt], writes=[vp])
                nmm = 0
                for d in range(2):
                    i2 = (c * 2 + d) % 2
                    lt = lwtok[i2]; e1 = E1[i2]; e0 = E0[i2]; ei = Ei[i2]; nw = nWC[i2]
                    qr = QR[i2]; bt = Bt[i2]; ktt = Kt[i2]; nb = nBh[i2]; k2 = K2[i2]; tk = TK[i2]
                    ps = P.psum('b')
                    P.tr(ps.ap[:, 0:128], lw[d].ap[:, cs], ident.ap, reads=[lw[d], ident], writes=[ps])
                    P.cp('dve', lt.ap, ps.ap[:, 0:128], reads=[ps], writes=[lt])
                    psc = P.psum('b')
                    P.mm(psc.ap[:, 0:256], lt.ap, rmk.ap[:, d, 640:896], reads=[lt, rmk], writes=[psc])
                    P.act(e1.ap, psc.ap[:, 0:128], AF.Exp, reads=[psc], writes=[e1])
                    P.act(e0.ap, psc.ap[:, 128:256], AF.Exp, reads=[psc], writes=[e0])
                    P.act(ei.ap, psc.ap[:, 0:128], AF.Exp, reads=[psc], writes=[ei], scale=-1.0)
                    wc = e1.ap[:, 127:128] if d == 0 else e1.ap[:, 0:1]
                    P.ts('dve', nw.ap, wc, -1.0, None, ALU.mult, reads=[e1], writes=[nw])
                    P.tt('dve', qr.ap[:, 0, :], kk.ap[:, cs], e0.ap, ALU.mult, reads=[kk, e0], writes=[qr])
                    P.tt('dve', qr.ap[:, 1, :], rc.ap[:, cs], e1.ap, ALU.mult, reads=[rc, e1], writes=[qr])
                    P.tt('pool', bt.ap, bb[d].ap[:, cs], ei.ap, ALU.mult, reads=[bb[d], ei], writes=[bt])
                    P.tt('pool', ktt.ap, kt[d].ap[:, cs], ei.ap, ALU.mult, reads=[kt[d], ei], writes=[ktt])
                    P.ts('dve', nb.ap, bt.ap, nw.ap[:, 0:1], None, ALU.mult, reads=[bt, nw], writes=[nb])
                    P.ts('dve', k2.ap, ktt.ap, wc, None, ALU.mult, reads=[ktt, e1], writes=[k2])
                    pst = P.psum('b'); pstb = pst.ap if DCH == F32 else pst.ap.bitcast(BF16)
                    P.tr(pstb[:, 0:128], qr.ap[:, 0, :], idch.ap, reads=[qr, idch], writes=[pst])
                    P.tr(pstb[:, 128:256], nb.ap, idch.ap, reads=[nb, idch], writes=[pst])
                    P.tr(pstb[:, 256:384], k2.ap, idch.ap, reads=[k2, idch], writes=[pst])
                    P.cp('act', tk.ap, pstb[:, 0:384].rearrange("p (a b) -> p a b", a=3), reads=[pst], writes=[tk])
                    if RW_STOP == 2:
                        continue
                    for hp in range(2):
                        hs = slice(hp * 64, hp * 64 + 64)
                        i3 = (cnt[0]) % 2
                        cnt[0] += 1
                        ea = evA[i3]; eb = evB[i3]
                        qr2 = qr.ap[hs, :, :].rearrange("p a b -> p (a b)")
                        p1 = P.psum('a')
                        P.mm(p1.ap[:, 0:256], bt.ap[hs, :], qr2, reads=[bt, qr], writes=[p1])
                        P.tt('dve', ea.ap, p1.ap[:, 0:256], rmk.ap[:, d, 0:256], ALU.mult, reads=[p1, rmk], writes=[ea])
                        p2 = P.psum('a')
                        P.mm(p2.ap[:, 0:256], ktt.ap[hs, :], qr2, reads=[ktt, qr], writes=[p2])
                        P.tt('dve', eb.ap, p2.ap[:, 0:256], rmk.ap[:, d, 256:512], ALU.mult, reads=[p2, rmk], writes=[eb])
                        p3 = P.psum('a')
                        P.mm(p3.ap[:, 0:128], qr.ap[hs, 0, :], bt.ap[hs, :], reads=[qr, bt], writes=[p3])
                        X = rr(Xr)
                        P.tt('dve', X.ap, p3.ap[:, 0:128], rmk.ap[:, d, 512:640], ALU.mult, reads=[p3, rmk], writes=[X])
                        if RW_STOP == 25:
                            continue
                        XT = rr(XTr)
                        P.cp('pool', XT.ap, ea.ap[:, 0:128], reads=[ea], writes=[XT])
                        TTc = rr(TTr)
                        P.tt('pool', TTc.ap, ea.ap[:, 0:128], ident.ap, ALU.add, reads=[ea, ident], writes=[TTc])
                        for j in range(1, 7):
                            pX = P.psum('a')
                            P.mm(pX.ap[:, 0:128], XT.ap, X.ap, reads=[XT, X], writes=[pX])
                            Xn = Xr[(Xr.index(X) + 1) % 3]
                            P.cp('act', Xn.ap, pX.ap[:, 0:128], reads=[pX], writes=[Xn])
                            if j < 6:
                                pXT = P.psum('a')
                                P.mm(pXT.ap[:, 0:128], X.ap, XT.ap, reads=[XT, X], writes=[pXT])
                                XTn = XTr[(XTr.index(XT) + 1) % 3]
                                P.cp('dve', XTn.ap, pXT.ap[:, 0:128], reads=[pXT], writes=[XTn])
                            pT = P.psum('a')
                            P.mm(pT.ap[:, 0:128], Xn.ap, TTc.ap, reads=[Xn, TTc], writes=[pT])
                            TTn = TTr[(TTr.index(TTc) + 1) % 3]
                            P.tt('dve', TTn.ap, pT.ap[:, 0:128], TTc.ap, ALU.add, reads=[pT, TTc], writes=[TTn])
                            X = Xn
                            if j < 6:
                                XT = XTn
                            TTc = TTn
                        if RW_STOP == 26:
                            continue
                        nv = n2v[i3]; pc = Pcat[i3]; p2p = P2p[hp][d]
                        p4 = P.psum('a')
                        P.mm(p4.ap[:, 0:64], eb.ap[:, 0:128], vt.ap[:, hs], reads=[eb, vt], writes=[p4])
                        P.cp('act', nv.ap, p4.ap[:, 0:64], reads=[p4], writes=[nv])
                        if RW_STOP == 261:
                            continue
                        p5 = P.psum('a')
                        P.mm(p5.ap[:, 0:64], TTc.ap, tk.ap[:, 0, hs], reads=[TTc, tk], writes=[p5])
                        P.mm(p5.ap[:, 64:128], TTc.ap, nv.ap, reads=[TTc, nv], writes=[p5])
                        P.cp('dve', pc.ap, p5.ap[:, 0:128], reads=[p5], writes=[pc])
                        if RW_STOP == 262:
                            continue
                        P.cp('pool', p2p.ap[:, hs], pc.ap[:, 64:128], reads=[pc], writes=[p2p])
                        if RW_STOP == 27:
                            continue
                        p6 = P.psum('a')
                        P.mm(p6.ap[hs, 0:64], pc.ap[:, 0:64], tk.ap[:, 1, hs], reads=[pc, tk], writes=[p6])
                        P.stt('dve', MTb.ap[hs, d, c, hs], ident.ap[hs, hs], wc[hs, :], p6.ap[hs, 0:64], ALU.mult, ALU.add,
                              reads=[ident, e1, p6], writes=[MTb])
                        p7 = P.psum('a')
                        P.mm(p7.ap[hs, 0:64], tk.ap[:, 2, hs], vt.ap[:, hs], start=True, stop=False, reads=[tk, vt], writes=[p7])
                        P.mm(p7.ap[hs, 0:64], tk.ap[:, 1, hs], pc.ap[:, 64:128], start=False, stop=True, reads=[tk, pc], writes=[p7])
                        P.cp('act', Gst.ap[hs, d, c, :], p7.ap[hs, 0:64], reads=[p7], writes=[Gst])
                        p8 = P.psum('a')
                        P.mm(p8.ap[hs, 0:128], pc.ap[:, 0:64], ea.ap[:, 128:256], reads=[pc, ea], writes=[p8])
                        P.tt('dve', Qs.ap[hs, d, c, :], p8.ap[hs, 0:128], qr.ap[hs, 1, :], ALU.add, reads=[p8, qr], writes=[Qs])
                        if RW_STOP == 28:
                            continue
                        P.mm(PS6.ap[:, 0:128], vp.ap[:, hp, :], eb.ap[:, 128:256], start=(nmm == 0), stop=False,
                             reads=[vp, eb], writes=[PS6])
                        P.mm(PS6.ap[:, 0:128], p2p.ap, ea.ap[:, 128:256], start=False, stop=(nmm == 3),
                             reads=[p2p, ea], writes=[PS6])
                        nmm += 1
                if RW_STOP > 2 and RW_STOP not in (25, 26, 27, 28, 261, 262):
                    P.cp('dve', Y0.ap[:, c, :], PS6.ap[:, 0:128], reads=[PS6], writes=[Y0])
            if RW_STOP <= 3 or RW_STOP in (25, 26, 27, 28, 261, 262):
                break
            for d in range(2):
                order = list(range(0, CCH)) + list(range(CCH, NCH)) if d == 0 else \
                    list(range(CCH - 1, -1, -1)) + list(range(NCH - 1, CCH - 1, -1))
                s_cur = Sst[0]
                P.memset('dve', s_cur.ap, 0.0, writes=[s_cur])
                P.memset('dve', Sbk.ap[:, d, order[0], :], 0.0, writes=[Sbk])
                for i, c in enumerate(order[:-1]):
                    ps = P.psum('b')
                    P.mm(ps.ap[:, 0:64], MTb.ap[:, d, c, :], s_cur.ap, reads=[MTb, s_cur], writes=[ps])
                    s_nx = Sst[(i + 1) % 2]
                    P.tt('dve', s_nx.ap, ps.ap[:, 0:64], Gst.ap[:, d, c, :], ALU.add, reads=[ps, Gst], writes=[s_nx])
                    c2 = order[i + 1]
                    for hp in range(2):
                        hs = slice(hp * 64, hp * 64 + 64)
                        P.cp('pool', Sbk.ap[hs, d, c2, hs], s_nx.ap[hs, :], reads=[s_nx], writes=[Sbk])
                    s_cur = s_nx
            if RW_STOP == 4:
                break
            for c in range(NCH):
                ps = P.psum('b')
                P.mm(ps.ap[:, 0:128], Sbk.ap[:, 0, c, :], Qs.ap[:, 0, c, :], start=True, stop=False, reads=[Sbk, Qs], writes=[ps])
                P.mm(ps.ap[:, 0:128], Sbk.ap[:, 1, c, :], Qs.ap[:, 1, c, :], start=False, stop=True, reads=[Sbk, Qs], writes=[ps])
                P.tt('dve', tmpA.ap[:, c * 128:(c + 1) * 128], ps.ap[:, 0:128], Y0.ap[:, c, :], ALU.add, reads=[ps, Y0], writes=[tmpA])
            if dbg and pr == 0:
                tap("yr0", tmpA, [128, NT])
            for ti, (t0, TT, isc) in enumerate(tiles):
                tsl = slice(t0, t0 + TT)
                ps = P.psum('a')
                P.mm(ps.ap[:, 0:TT], blk.ap, tmpA.ap[:, tsl], reads=[blk, tmpA], writes=[ps])
                dc = rr(t512)
                P.stt('dve', dc.ap[:, 0:TT], ps.ap[:, 0:TT], -1.0 / 64, tmpA.ap[:, tsl], ALU.mult, ALU.add, reads=[ps, tmpA], writes=[dc])
                sq_ = rr(t512)
                P.act(sq_.ap[:, 0:TT], dc.ap[:, 0:TT], AF.Square, reads=[dc], writes=[sq_])
                ps2 = P.psum('a')
                P.mm(ps2.ap[:, 0:TT], blk.ap, sq_.ap[:, 0:TT], reads=[blk, sq_], writes=[ps2])
                P.ts('dve', sq_.ap[:, 0:TT], ps2.ap[:, 0:TT], 1.0 / 64, 64e-5, ALU.mult, ALU.add, reads=[ps2], writes=[sq_])
                P.act(sq_.ap[:, 0:TT], sq_.ap[:, 0:TT], AF.Ln, reads=[sq_], writes=[sq_])
                P.act(sq_.ap[:, 0:TT], sq_.ap[:, 0:TT], AF.Exp, reads=[sq_], writes=[sq_], scale=-0.5)
                P.tt('dve', dc.ap[:, 0:TT], dc.ap[:, 0:TT], sq_.ap[:, 0:TT], ALU.mult, reads=[dc, sq_], writes=[dc])
                P.ts('dve', dc.ap[:, 0:TT], dc.ap[:, 0:TT], rv.ap[:, 7, pr:pr + 1], rv.ap[:, 8, pr:pr + 1], ALU.mult, ALU.add,
                     reads=[dc, rv], writes=[dc])
                bo = rr(t512)
                P.tt('pool', bo.ap[:, 0:TT], kt[0].ap[:, tsl], kt[1].ap[:, tsl], ALU.add, reads=[kt[0], kt[1]], writes=[bo])
                P.tt('pool', bo.ap[:, 0:TT], bo.ap[:, 0:TT], rc.ap[:, tsl], ALU.mult, reads=[bo, rc], writes=[bo])
                P.ts('pool', bo.ap[:, 0:TT], bo.ap[:, 0:TT], rv.ap[:, 6, pr:pr + 1], None, ALU.mult, reads=[bo, rv], writes=[bo])
                ps3 = P.psum('a')
                P.mm(ps3.ap[:, 0:TT], blk.ap, bo.ap[:, 0:TT], reads=[blk, bo], writes=[ps3])
                P.tt('dve', bo.ap[:, 0:TT], ps3.ap[:, 0:TT], vc.ap[:, tsl], ALU.mult, reads=[ps3, vc], writes=[bo])
                P.tt('dve', dc.ap[:, 0:TT], dc.ap[:, 0:TT], bo.ap[:, 0:TT], ALU.add, reads=[dc, bo], writes=[dc])
                ps4 = P.psum('a')
                P.mm(ps4.ap[:, 0:TT], gup.ap[:, cs_], sg.ap[:, tsl], reads=[gup, sg], writes=[ps4])
                ym = ymt[ti % 2]
                P.tt('dve', ym.ap[:, 0:TT], ps4.ap[:, 0:TT], dc.ap[:, 0:TT], ALU.mult, reads=[ps4, dc], writes=[ym])
                P.dma('sp', ymTs[pr, :, tsl], ym.ap[:, 0:TT], reads=[ym])
        P.release(m)

    def phase_pool():
        m = P.mark()
        seqs = [(0, C), (C, NT)]
        pw = P.alloc([128, 4, 128], BF16)
        for gi in range(4):
            P.dma('pool', pw.ap[:, gi, :], pool_w[gi], writes=[pw])
        psc = P.alloc([128, 4]); P.dma('sp', psc.ap, pool_scT, writes=[psc])
        u = P.alloc([128, NT]); acc = P.alloc([128, NT]); inv = P.alloc([128, NT]); df = P.alloc([128, NT], BF16)
        ymt = [P.alloc([128, 512], BF16) for _ in range(2)]
        for gi, win in enumerate((2, 4, 8, 16)):
            P.dma('sp', u.ap, pTs[14 + gi], writes=[u])
            P.dma('sp', inv.ap, pool_inv[gi:gi + 1, :].partition_broadcast(128), writes=[inv])
            P.cp('pool', acc.ap, u.ap, reads=[u], writes=[acc])
            for o in range(-(win // 2), win // 2):
                if o == 0:
                    continue
                for (a, b) in seqs:
                    if o < 0:
                        P.tt('dve', acc.ap[:, a - o:b], acc.ap[:, a - o:b], u.ap[:, a:b + o], ALU.add, reads=[acc, u], writes=[acc])
                    else:
                        P.tt('dve', acc.ap[:, a:b - o], acc.ap[:, a:b - o], u.ap[:, a + o:b], ALU.add, reads=[acc, u], writes=[acc])
            P.tt('dve', acc.ap, acc.ap, inv.ap, ALU.mult, reads=[acc, inv], writes=[acc])
            P.tt('dve', df.ap, acc.ap, u.ap, ALU.subtract, reads=[acc, u], writes=[df])
            for ti, (t0, TT, isc) in enumerate(tiles):
                ps = P.psum('a')
                P.mm(ps.ap[:, 0:TT], pw.ap[:, gi, :], df.ap[:, t0:t0 + TT], reads=[pw, df], writes=[ps])
                ym = ymt[ti % 2]
                P.ts('dve', ym.ap[:, 0:TT], ps.ap[:, 0:TT], psc.ap[:, gi:gi + 1], None, ALU.mult, reads=[ps, psc], writes=[ym])
                P.dma('sp', ymTs[4 + gi, :, t0:t0 + TT], ym.ap[:, 0:TT], reads=[ym])
        P.release(m)

    RUN_RWKV = STOP_AFTER not in ('inproj',)
    RUN_POOL = STOP_AFTER not in ('inproj', 'rwkv')

    def phase_outproj(li, w_out_dram, lat_only):
        m = P.mark()
        Wout = P.alloc([128, KD, D], BF16)
        load_w_bf(Wout, w_out_dram, KD)
        ymb = [P.alloc([128, KD, 512], BF16) for _ in range(2)]
        xT = [P.alloc([128, KD, 512]) for _ in range(2)]
        for ti, (t0, TT, isc) in enumerate(tiles):
            if lat_only and isc:
                continue
            ym = ymb[ti % 2]; xt = xT[ti % 2]
            for k in range(KD):
                P.dma('sp', ym.ap[:, k, 0:TT], ymTs[k, :, t0:t0 + TT], writes=[ym])
                P.dma('act', xt.ap[:, k, 0:TT], xTs[k, :, t0:t0 + TT], writes=[xt])
            for dc in range(KD):
                ps = P.psum('a')
                for k in range(KD):
                    P.mm(ps.ap[:, 0:TT], Wout.ap[:, k, dc * 128:(dc + 1) * 128], ym.ap[:, k, 0:TT],
                         start=(k == 0), stop=(k == KD - 1), reads=[Wout, ym], writes=[ps])
                P.stt('dve', xt.ap[:, dc, 0:TT], ps.ap[:, 0:TT], mod.ap[:, li, 16 + dc, isc:isc + 1], xt.ap[:, dc, 0:TT],
                      ALU.mult, ALU.add, reads=[ps, mod, xt], writes=[xt])
            for k in range(KD):
                P.dma('sp', xTs[k, :, t0:t0 + TT], xt.ap[:, k, 0:TT], reads=[xt])
        P.release(m)

    def phase_final():
        m = P.mark()
        xT = [P.alloc([128, KD, 512]) for _ in range(2)]
        sq = P.alloc([128, KD, 512]); rs = P.alloc([128, 512])
        ob = [P.alloc([128, KD, 512]) for _ in range(2)]
        otok = [P.alloc([128, D]) for _ in range(2)]
        for ti, (t0, TT, isc) in enumerate(tiles):
            if isc:
                continue
            xt = xT[ti % 2]; o = ob[ti % 2]
            for k in range(KD):
                P.dma('sp', xt.ap[:, k, 0:TT], xTs[k, :, t0:t0 + TT], writes=[xt])
            norm_mod(xt, TT, 0, 0, 0, o, sq, rs, final=True)
            for b in range(TT // 128):
                ot = otok[b % 2]
                for half in range(2):
                    ps = P.psum('a')
                    for j in range(4):
                        k = half * 4 + j
                        P.tr(ps.ap[:, j * 128:(j + 1) * 128], o.ap[:, k, b * 128:(b + 1) * 128], ident.ap,
                             reads=[o, ident], writes=[ps])
                    P.cp('dve' if half else 'act', ot.ap[:, half * 512:(half + 1) * 512], ps.ap, reads=[ps], writes=[ot])
                r0 = t0 - C + b * 128
                P.dma('sp', out[r0:r0 + 128, :], ot.ap, reads=[ot], is_output=True)
        P.release(m)

    if RUN_RWKV:
        phase_rwkv()
    if RUN_POOL:
        phase_pool()
    if STOP_AFTER in ('inproj', 'rwkv', 'pool'):
        phase_final()
        return P, locals()
    phase_outproj(0, ev_w_out, False)
    if STOP_AFTER == 'l0mix':
        phase_final()
        return P, locals()
    phase_moe(0, False)
    if STOP_AFTER == 'l0':
        phase_final()
        return P, locals()
    phase_inproj(1, od_w_in, OD_COLS, False)
    phase_l1mix()
    phase_outproj(1, od_w_out, True)
    if STOP_AFTER == 'l1mix':
        phase_final()
        return P, locals()
    phase_moe(1, True)
    phase_final()
    return P, locals()


def fm(v, nch=None):
    v = np.asarray(v, np.float32)
    return np.ascontiguousarray(v.reshape(-1, 128).T)


def make_inputs(b, S, C, inp):
    NT = C + S
    m = {}
    m['xin'] = np.ascontiguousarray(np.concatenate([inp['ctx'][b], inp['x'][b]], 0))
    m['cT'] = np.ascontiguousarray(np.stack([fm(inp['c'][b]), fm(inp['c_ctx'])], -1))
    m['ada_w'] = inp['ada_w']
    m['ada_bT'] = np.ascontiguousarray(np.stack([fm(inp['ada_b'][0]), fm(inp['ada_b'][1])], 1))
    m['normT'] = np.ascontiguousarray(np.stack([fm(inp['norm_mix'][0]), fm(inp['norm_mix'][1]), fm(inp['norm_ffn'][0]),
                                               fm(inp['norm_ffn'][1]), fm(inp['final_norm'])], 1))
    m['ev_w_in'] = inp['ev_w_in'][0]
    m['ev_w_out'] = inp['ev_w_out'][0]
    sh = inp['rwkv_shift'][0]
    m['shiftT'] = np.ascontiguousarray(np.stack([fm(sh[0]), fm(sh[1]), fm(sh[2])], -1))
    rv = [inp['rwkv_w0'][0][0], inp['rwkv_w0'][0][1], inp['rwkv_a0'][0][0], inp['rwkv_a0'][0][1], inp['rwkv_k_k'][0],
          inp['rwkv_k_a'][0], inp['rwkv_r_k'][0], inp['rwkv_ln_g'][0], inp['rwkv_ln_b'][0]]
    m['rvec'] = np.ascontiguousarray(np.stack([fm(v) for v in rv], 1))
    lu = np.zeros((128, 2, 512), np.float32)
    for d in range(2):
        lu[0:64, d] = inp['rwkv_w_up'][0][d]
        lu[64:128, d] = inp['rwkv_a_up'][0][d]
    m['lora_up'] = lu
    m['g_up'] = inp['rwkv_g_up'][0]
    m['pool_w'] = inp['pool_w'][0]
    m['pool_scT'] = fm(inp['pool_scale'][0])
    pi = np.zeros((4, NT), np.float32)
    for gi, win in enumerate((2, 4, 8, 16)):
        for (a, Tn) in ((0, C), (C, S)):
            t = np.arange(Tn)
            lo = np.clip(t - win // 2, 0, Tn)
            hi = np.clip(t - win // 2 + win, 0, Tn)
            pi[gi, a:a + Tn] = 1.0 / (hi - lo)
    m['pool_inv'] = pi
    m['moe_r'] = np.ascontiguousarray(np.concatenate([inp['moe_router_group'], inp['moe_router_expert']], -1))
    m['moe_wg'] = inp['moe_w_gate']
    m['moe_wu'] = inp['moe_w_up']
    m['moe_wd'] = inp['moe_w_down']
    m['od_w_in'] = inp['od_w_in'][0]
    m['od_w_out'] = inp['od_w_out'][0]
    m['dlam'] = np.ascontiguousarray(inp['diff_lambda'][0].reshape(1, 256))
    m['sublnT'] = np.ascontiguousarray(inp['diff_subln'][0].reshape(128, 1))
    m['retgT'] = fm(inp['ret_norm'][0])
    m.update(host_consts())
    m.update(host_consts_l1(S))
    return m


def kernel(**inp):
    inp = {k: np.asarray(v) for k, v in inp.items()}
    S, C = inp['x'].shape[1], inp['ctx'].shape[1]
    B = inp['x'].shape[0]
    P, _ = build(S, C)
    nc = P.build()
    in_maps = [make_inputs(b, S, C, inp) for b in range(B)]
    names = set()
    res = run_bass_kernel_spmd(nc, in_maps, core_ids=list(range(B)))
    return np.stack([np.asarray(r["out"], np.float32) for r in res.results], 0)
```

```python
import math
import numpy as np
from contextlib import ExitStack
import concourse.bass as bass
import concourse.mybir as mybir
from concourse.bass_utils import run_bass_kernel_spmd

F32 = mybir.dt.float32
BF16 = mybir.dt.bfloat16
ALU = mybir.AluOpType
AF = mybir.ActivationFunctionType
AX = mybir.AxisListType

ENGS = ['pe', 'act', 'dve', 'pool', 'sp']
DMAQ = ['sp', 'pool', 'act']


def _prod(s):
    r = 1
    for v in s:
        r *= v
    return r


class T:
    __slots__ = ('ap', 'lw', 'rd')

    def __init__(self, ap):
        self.ap = ap
        self.lw = None
        self.rd = {}


class Prog:
    def __init__(self, arena_words=50000, n_dma_sems=8):
        self.nc = bass.Bass("TRN2", target_bir_lowering=False)
        self.es = ExitStack()
        self.ops = {e: [] for e in ENGS}
        self.known = {e: {} for e in ENGS}
        self.pending = {e: [] for e in ENGS}
        self.n_dma_sems = n_dma_sems
        self.dma_rr = {q: 0 for q in DMAQ}
        self.dma_cum = {}
        self.out_tokens = []
        self.nuid = 0
        self.aw = arena_words
        self.arena = self.es.enter_context(self.nc.sbuf_tensor("arena", [128, arena_words], F32))
        self.top = 0
        self.PS = [T(self.es.enter_context(self.nc.psum_tensor(f"psb{i}", [128, 512], F32))[:, :])
                   for i in range(8)]
        self.ps_rr = {'a': 0, 'b': 0, 'c': 0, 'r': 0}
        self.ps_groups = {'a': [0, 1, 2, 3], 'b': [4, 5], 'c': [4, 5, 6, 7], 'r': [0, 1, 2, 3, 7]}

    def psum(self, g='a'):
        lst = self.ps_groups[g]
        i = self.ps_rr[g]
        self.ps_rr[g] = (i + 1) % len(lst)
        return self.PS[lst[i]]

    def alloc(self, shape, dt=F32):
        shape = list(shape)
        esz = 4 if dt == F32 else 2
        nb = _prod(shape[1:]) * esz
        nw = (nb + 3) // 4
        assert self.top + nw <= self.aw, f"arena overflow {self.top}+{nw}>{self.aw}"
        ap = self.arena[0:shape[0], self.top:self.top + nw]
        self.top += nw
        if dt != F32:
            ap = ap.bitcast(dt)
            ap = ap[:, 0:_prod(shape[1:])]
        if len(shape) > 2:
            names = "abcdefg"[:len(shape) - 1]
            kw = {names[i]: shape[i + 1] for i in range(len(shape) - 2)}
            ap = ap.rearrange("p (" + " ".join(names) + ") -> p " + " ".join(names), **kw)
        return T(ap)

    def mark(self):
        return self.top

    def release(self, m):
        self.barrier()
        self.top = m

    def dram(self, name, shape, dt, kind="Internal"):
        return self.nc.dram_tensor(name, list(shape), dt, kind=kind).ap()

    def barrier(self):
        toks = []
        for f in ENGS:
            if len(self.ops[f]) > 0:
                toks.append(('c', f, len(self.ops[f])))
        for skey, cum in self.dma_cum.items():
            toks.append(('d', skey, cum))
        for e in ENGS:
            self.pending[e] = list(toks)

    def _add_wait(self, e, waits, tok):
        if tok is None:
            return
        kind, key, val = tok
        if kind == 'c' and key == e:
            if e == 'pe':
                return
            if val > len(self.ops[e]):
                return
        kk = (kind, key)
        if self.known[e].get(kk, 0) >= val:
            return
        self.known[e][kk] = val
        waits[kk] = max(waits.get(kk, 0), val)

    def _deps(self, e, reads, writes):
        waits = {}
        if self.pending[e]:
            for tok in self.pending[e]:
                self._add_wait(e, waits, tok)
            self.pending[e] = []
        for t in reads:
            self._add_wait(e, waits, t.lw)
        for t in writes:
            self._add_wait(e, waits, t.lw)
            for tok in t.rd.values():
                self._add_wait(e, waits, tok)
        return waits

    def op(self, e, fn, reads=(), writes=()):
        waits = self._deps(e, reads, writes)
        idx = len(self.ops[e]) + 1
        tok = ('c', e, idx)
        self.ops[e].append(dict(fn=fn, waits=waits, inc=None, flag=False))
        for t in reads:
            t.rd[e] = tok
        for t in writes:
            t.lw = tok
            t.rd = {}
        return tok

    def dma(self, q, out_ap, in_ap, reads=(), writes=(), is_output=False, **kw):
        waits = self._deps(q, reads, writes)
        si = self.dma_rr[q]
        self.dma_rr[q] = (si + 1) % self.n_dma_sems
        skey = (q, si)
        prev = self.dma_cum.get(skey, 0)
        if prev > 0:
            self._add_wait(q, waits, ('d', skey, prev))
        val = prev + 16
        self.dma_cum[skey] = val
        tok = ('d', skey, val)

        def fn(eng, out_ap=out_ap, in_ap=in_ap, kw=kw):
            return eng.dma_start(out=out_ap, in_=in_ap, **kw)
        self.ops[q].append(dict(fn=fn, waits=waits, inc=(skey, 16), flag=True))
        for t in reads:
            t.rd[('dma', skey)] = tok
        for t in writes:
            t.lw = tok
            t.rd = {}
        if is_output:
            self.out_tokens.append(tok)
        return tok

    def build(self):
        nc = self.nc
        waits = {}
        for tok in self.out_tokens:
            self._add_wait('sp', waits, tok)
        self.ops['sp'].append(dict(fn=None, waits=waits, inc=None, flag=False))
        for e in ENGS:
            for o in self.ops[e]:
                for (kind, key), val in o['waits'].items():
                    if kind == 'c':
                        self.ops[key][val - 1]['flag'] = True
        rank = {}
        for e in ENGS:
            r = 0
            rk = []
            for o in self.ops[e]:
                if o['inc'] is None and o['flag']:
                    r += 1
                rk.append(r)
            rank[e] = rk
        csem = {e: self.es.enter_context(nc.semaphore(f"c_{e}")) for e in ENGS}
        dsem = {}
        for q in DMAQ:
            for i in range(self.n_dma_sems):
                if (q, i) in self.dma_cum:
                    dsem[(q, i)] = self.es.enter_context(nc.semaphore(f"d_{q}{i}"))
        block = self.es.enter_context(nc.Block())
        engobj = {'pe': block.tensor, 'act': block.scalar, 'dve': block.vector,
                  'pool': block.gpsimd, 'sp': block.sync}

        def mk(e):
            def body(eng):
                for o in self.ops[e]:
                    for (kind, key), val in o['waits'].items():
                        if kind == 'c':
                            eng.wait_ge(csem[key], rank[key][val - 1])
                        else:
                            eng.wait_ge(dsem[key], val)
                    if o['fn'] is None:
                        continue
                    ins = o['fn'](eng)
                    if o['inc'] is not None:
                        ins.then_inc(dsem[o['inc'][0]], 16)
                    elif o['flag']:
                        ins.then_inc(csem[e], 1)
            return body
        for e in ENGS:
            engobj[e](mk(e))
        self.es.close()
        return nc

    def mm(self, out, lhsT, rhs, start=True, stop=True, reads=(), writes=(), **kw):
        def fn(eng):
            return eng.matmul(out, lhsT, rhs, start=start, stop=stop, **kw)
        return self.op('pe', fn, reads, writes)

    def tr(self, out, in_, ident, reads=(), writes=()):
        def fn(eng):
            return eng.transpose(out, in_, ident)
        return self.op('pe', fn, reads, writes)

    def act(self, out, in_, func, reads=(), writes=(), **kw):
        def fn(e):
            return e.activation(out=out, in_=in_, func=func, **kw)
        return self.op('act', fn, reads, writes)

    def tt(self, e, out, in0, in1, op, reads=(), writes=()):
        def fn(eng):
            return eng.tensor_tensor(out=out, in0=in0, in1=in1, op=op)
        return self.op(e, fn, reads, writes)

    def ts(self, e, out, in0, s1, s2, op0, op1=None, reads=(), writes=()):
        def fn(eng):
            if op1 is None:
                return eng.tensor_scalar(out=out, in0=in0, scalar1=s1, scalar2=None, op0=op0)
            return eng.tensor_scalar(out=out, in0=in0, scalar1=s1, scalar2=s2, op0=op0, op1=op1)
        return self.op(e, fn, reads, writes)

    def stt(self, e, out, in0, scalar, in1, op0, op1, reads=(), writes=()):
        def fn(eng):
            return eng.scalar_tensor_tensor(out=out, in0=in0, scalar=scalar, in1=in1, op0=op0, op1=op1)
        return self.op(e, fn, reads, writes)

    def cp(self, e, out, in_, reads=(), writes=()):
        if e == 'act':
            def fn(eng):
                return eng.copy(out=out, in_=in_)
        else:
            def fn(eng):
                return eng.tensor_copy(out=out, in_=in_)
        return self.op(e, fn, reads, writes)

    def memset(self, e, ap, val, writes=()):
        def fn(eng):
            return eng.memset(ap, val)
        return self.op(e, fn, (), writes)


D = 1024
KD = 8
W_DECAY_SCALE = 0.606531
EV_COLS = 2304
OD_COLS = 3072


def host_consts():
    r = np.arange(128)
    Us = (r[:, None] < r[None, :]).astype(np.float32)
    Ui = (r[:, None] <= r[None, :]).astype(np.float32)
    Ls = (r[:, None] > r[None, :]).astype(np.float32)
    Li = (r[:, None] >= r[None, :]).astype(np.float32)
    blk = np.zeros((128, 128), np.float32)
    blk[:64, :64] = 1
    blk[64:, 64:] = 1
    c = {}
    c['ident'] = np.eye(128, dtype=np.float32)
    c['blk64'] = blk
    rm = np.zeros((2, 128, 896), np.float32)
    for d, (ss, si, tsm) in enumerate([(Us, Ui, Ls), (Ls, Li, Us)]):
        rm[d, :, 0:128] = -ss
        rm[d, :, 128:256] = -si
        rm[d, :, 256:384] = ss
        rm[d, :, 384:512] = si
        rm[d, :, 512:640] = -tsm
        rm[d, :, 640:768] = si
        rm[d, :, 768:896] = ss
    c['rmask'] = rm
    return c


def host_consts_l1(S):
    c = {}
    p = np.arange(128)
    blk32 = p % 32
    partner = np.where(blk32 < 16, p + 16, p - 16)
    perm = np.zeros((128, 128), np.float32)
    perm[partner, p] = 1.0
    c['rope_perm'] = perm
    t = np.arange(S)
    row = (t // 64).astype(np.float32)
    col = (t % 64).astype(np.float32)
    b64 = p % 64
    sub = b64 // 32
    j = (b64 % 16).astype(np.float32)
    inv = (10000.0 ** (-j / 16.0)).astype(np.float32)
    pos = np.where(sub[:, None] == 0, row[None, :], col[None, :]).astype(np.float32)
    ang = (pos * inv[:, None]).astype(np.float32)
    sgn = np.where(blk32 < 16, -1.0, 1.0).astype(np.float32)
    c['ropeC'] = np.cos(ang).astype(np.float32)
    c['ropeS'] = (np.sin(ang) * sgn[:, None]).astype(np.float32)
    lgf = np.log(1.0 - 2.0 ** (-5.0 - np.arange(4, dtype=np.float64)))
    r = np.arange(128, dtype=np.float64)
    retD = np.zeros((8, 128, 128), np.float64)
    retq = np.zeros((8, 128), np.float64)
    retk = np.zeros((128, 8), np.float64)
    for h in range(4):
        for d in range(2):
            lg = lgf[h] if d == 0 else lgf[3 - h]
            hd = h * 2 + d
            s_, i_ = r[:, None], r[None, :]
            if d == 0:
                retD[hd] = np.where(i_ >= s_, np.exp(lg * np.maximum(i_ - s_, 0)), 0.0)
                retq[hd] = np.exp(lg * (r + 1))
                retk[:, hd] = np.exp(lg * (127 - r))
            else:
                retD[hd] = np.where(s_ >= i_, np.exp(lg * np.maximum(s_ - i_, 0)), 0.0)
                retq[hd] = np.exp(lg * (128 - r))
                retk[:, hd] = np.exp(lg * r)
    c['retD'] = retD.astype(np.float32)
    c['retq'] = retq.astype(np.float32)
    c['retk'] = retk.astype(np.float32)
    return c


def build(S, C, dbg=False, RW_DT=(BF16, F32, BF16), STOP_AFTER='all', RW_STOP=99):
    P = Prog()
    NT = C + S
    NCH = NT // 128
    CCH = C // 128
    tiles = []
    for base, ln, isc in ((0, C, 1), (C, S, 0)):
        o = 0
        while o < ln:
            l = min(512, ln - o)
            tiles.append((base + o, l, isc))
            o += l
    IN = lambda n, s: P.dram(n, s, F32, "ExternalInput")
    xin = IN("xin", [NT, D])
    cT = IN("cT", [128, KD, 2])
    ada_w = IN("ada_w", [2, D, 6 * D])
    ada_bT = IN("ada_bT", [128, 2, 48])
    normT = IN("normT", [128, 5, KD])
    ev_w_in = IN("ev_w_in", [D, EV_COLS])
    ev_w_out = IN("ev_w_out", [D, D])
    shiftT = IN("shiftT", [128, 12, 3])
    rvec = IN("rvec", [128, 9, 4])
    lora_up = IN("lora_up", [128, 2, 512])
    g_up = IN("g_up", [128, 512])
    pool_w = IN("pool_w", [4, 128, 128])
    pool_scT = IN("pool_scT", [128, 4])
    pool_inv = IN("pool_inv", [4, NT])
    moe_r = IN("moe_r", [2, D, 36])
    moe_wg = IN("moe_wg", [2, 32, D, 512])
    moe_wu = IN("moe_wu", [2, 32, D, 512])
    moe_wd = IN("moe_wd", [2, 32, 512, D])
    od_w_in = IN("od_w_in", [D, OD_COLS])
    od_w_out = IN("od_w_out", [D, D])
    dlam = IN("dlam", [1, 256])
    sublnT = IN("sublnT", [128, 1])
    retgT = IN("retgT", [128, 4])
    rope_perm = IN("rope_perm", [128, 128])
    ropeC = IN("ropeC", [128, S])
    ropeS = IN("ropeS", [128, S])
    retD = IN("retD", [8, 128, 128])
    retq = IN("retq", [8, 128])
    retk = IN("retk", [128, 8])
    cident = IN("ident", [128, 128])
    cblk = IN("blk64", [128, 128])
    crmask = IN("rmask", [2, 128, 896])
    out = P.dram("out", [S, D], F32, "ExternalOutput")
    xTs = P.dram("xTs", [KD, 128, NT], F32)
    pTs = P.dram("pTs", [24, 128, NT], F32, "ExternalOutput" if dbg else "Internal")
    ymTs = P.dram("ymTs", [KD, 128, NT], BF16, "ExternalOutput" if dbg else "Internal")
    dbgs = {}

    def tap(name, t, shape):
        if dbg:
            d = P.dram("dbg_" + name, shape, F32, "ExternalOutput")
            P.dma('sp', d, t.ap, reads=[t], is_output=True)

    ident = P.alloc([128, 128]); P.dma('sp', ident.ap, cident, writes=[ident])
    identb = P.alloc([128, 128], BF16); P.dma('pool', identb.ap, cident, writes=[identb])
    blk = P.alloc([128, 128]); P.dma('sp', blk.ap, cblk, writes=[blk])
    onesD = P.alloc([128, 128]); P.memset('pool', onesD.ap, 1.0 / D, writes=[onesD])
    normv = P.alloc([128, 5, KD]); P.dma('sp', normv.ap, normT, writes=[normv])
    mod = P.alloc([128, 2, 48, 2])
    m0 = P.mark()
    sc = P.alloc([128, KD, 2]); P.dma('sp', sc.ap, cT, writes=[sc])
    P.act(sc.ap, sc.ap, AF.Silu, reads=[sc], writes=[sc])
    adab = P.alloc([128, 2, 48]); P.dma('sp', adab.ap, ada_bT, writes=[adab])
    wbuf = [P.alloc([128, KD, 1024]) for _ in range(1)]
    for li in range(2):
        for blkc in range(6):
            wb = wbuf[0]
            for k in range(KD):
                P.dma('sp' if k % 2 == 0 else 'act', wb.ap[:, k, :],
                      ada_w[li, k * 128:(k + 1) * 128, blkc * 1024:(blkc + 1) * 1024], writes=[wb])
            for cc in range(8):
                ps = P.psum('a')
                for k in range(KD):
                    P.mm(ps.ap[:, 0:2], wb.ap[:, k, cc * 128:(cc + 1) * 128], sc.ap[:, k, :],
                         start=(k == 0), stop=(k == KD - 1), reads=[wb, sc], writes=[ps])
                j = blkc * 8 + cc
                P.stt('dve', mod.ap[:, li, j, :], ps.ap[:, 0:2], 1.0,
                      adab.ap[:, li, j:j + 1].to_broadcast([128, 2]), ALU.mult, ALU.add,
                      reads=[ps, adab], writes=[mod])
    P.release(m0)
    AB = P.alloc([128, 2, 2, 2, KD, 2])
    for li in range(2):
        for sub in range(2):
            shc = 24 * sub
            scc = 24 * sub + 8
            nidx = li if sub == 0 else 2 + li
            for w in range(2):
                P.ts('dve', AB.ap[:, li, sub, 0, :, w], mod.ap[:, li, scc:scc + 8, w], 1.0, None, ALU.add,
                     reads=[mod], writes=[AB])
                P.tt('dve', AB.ap[:, li, sub, 0, :, w], AB.ap[:, li, sub, 0, :, w], normv.ap[:, nidx, :], ALU.mult,
                     reads=[AB, normv], writes=[AB])
                P.cp('dve', AB.ap[:, li, sub, 1, :, w], mod.ap[:, li, shc:shc + 8, w], reads=[mod], writes=[AB])
    if dbg:
        tap("mod", mod, [128, 2, 48, 2])

    def norm_mod(xT, TT, li, sub, w, hb, sq, rs, final=False):
        P.act(sq.ap[:, :, 0:TT], xT.ap[:, :, 0:TT], AF.Square, reads=[xT], writes=[sq])
        ps = P.psum('a')
        for k in range(KD):
            P.mm(ps.ap[:, 0:TT], onesD.ap, sq.ap[:, k, 0:TT], start=(k == 0), stop=(k == KD - 1),
                 reads=[onesD, sq], writes=[ps])
        P.ts('dve', rs.ap[:, 0:TT], ps.ap[:, 0:TT], 1e-6, None, ALU.add, reads=[ps], writes=[rs])
        P.act(rs.ap[:, 0:TT], rs.ap[:, 0:TT], AF.Ln, reads=[rs], writes=[rs])
        P.act(rs.ap[:, 0:TT], rs.ap[:, 0:TT], AF.Exp, reads=[rs], writes=[rs], scale=-0.5)
        P.tt('dve', sq.ap[:, :, 0:TT], xT.ap[:, :, 0:TT], rs.ap[:, None, 0:TT].to_broadcast([128, KD, TT]), ALU.mult,
             reads=[xT, rs], writes=[sq])
        for k in range(KD):
            if final:
                P.ts('dve' if k % 2 else 'pool', hb.ap[:, k, 0:TT], sq.ap[:, k, 0:TT], normv.ap[:, 4, k:k + 1], None, ALU.mult,
                     reads=[sq, normv], writes=[hb])
            else:
                P.ts('dve' if k % 2 else 'pool', hb.ap[:, k, 0:TT], sq.ap[:, k, 0:TT], AB.ap[:, li, sub, 0, k, w:w + 1],
                     AB.ap[:, li, sub, 1, k, w:w + 1], ALU.mult, ALU.add, reads=[sq, AB], writes=[hb])

    def load_w_bf(dst, src, K):
        for k in range(K):
            P.dma('pool', dst.ap[:, k, :], src[k * 128:(k + 1) * 128, :], writes=[dst])

    def phase_inproj(li, w_in_dram, ncols, first):
        m = P.mark()
        NCC = ncols // 128
        Win = P.alloc([128, KD, ncols], BF16)
        load_w_bf(Win, w_in_dram, KD)
        xtok = [P.alloc([128, D]) for _ in range(2)]
        xT = [P.alloc([128, KD, 512]) for _ in range(2)]
        hb = [P.alloc([128, KD, 512], BF16) for _ in range(2)]
        sq = P.alloc([128, KD, 512]); rs = P.alloc([128, 512])
        pst = [P.alloc([128, 6, 512]) for _ in range(2)]
        for ti, (t0, TT, isc) in enumerate(tiles):
            xt = xT[ti % 2]
            if first:
                for b in range(TT // 128):
                    xk = xtok[b % 2]
                    P.dma('sp', xk.ap, xin[t0 + b * 128:t0 + (b + 1) * 128, :], writes=[xk])
                    for half in range(2):
                        ps = P.psum('a')
                        for j in range(4):
                            k = half * 4 + j
                            P.tr(ps.ap[:, j * 128:(j + 1) * 128], xk.ap[:, k * 128:(k + 1) * 128], ident.ap,
                                 reads=[xk, ident], writes=[ps])
                        P.cp('dve' if half else 'act', xt.ap[:, half * 4:half * 4 + 4, b * 128:(b + 1) * 128],
                             ps.ap.rearrange("p (a b) -> p a b", a=4), reads=[ps], writes=[xt])
                for k in range(KD):
                    P.dma('sp', xTs[k, :, t0:t0 + TT], xt.ap[:, k, 0:TT], reads=[xt])
            else:
                for k in range(KD):
                    P.dma('sp', xt.ap[:, k, 0:TT], xTs[k, :, t0:t0 + TT], writes=[xt])
            h = hb[ti % 2]
            norm_mod(xt, TT, li, 0, isc, h, sq, rs)
            for g in range(NCC // 6):
                st = pst[g % 2]
                for c6 in range(6):
                    cc = g * 6 + c6
                    ps = P.psum('a')
                    for k in range(KD):
                        P.mm(ps.ap[:, 0:TT], Win.ap[:, k, cc * 128:(cc + 1) * 128], h.ap[:, k, 0:TT],
                             start=(k == 0), stop=(k == KD - 1), reads=[Win, h], writes=[ps])
                    P.cp('act' if c6 % 2 else 'dve', st.ap[:, c6, 0:TT], ps.ap[:, 0:TT], reads=[ps], writes=[st])
                P.dma('sp', pTs[g * 6:(g + 1) * 6, :, t0:t0 + TT].rearrange("c p t -> p c t"), st.ap[:, :, 0:TT],
                      reads=[st])
        P.release(m)


    def phase_moe(li, lat_only):
        m = P.mark()
        tl = [t for t in tiles if not (lat_only and t[2])]
        xres = P.alloc([128, KD, NT])
        hfT = P.alloc([128, KD, NT], BF16)
        gT = P.alloc([32, NT])
        wr = P.alloc([128, KD, 36])
        P.dma('sp', wr.ap, moe_r[li].rearrange("(k p) n -> p k n", p=128), writes=[wr])
        m2 = P.mark()
        sq = P.alloc([128, KD, 512]); rs = P.alloc([128, 512])
        lg = P.alloc([128, 36]); oh = P.alloc([128, 4]); st_ = P.alloc([128, 16]); les = P.alloc([128, 8])
        mk1 = P.alloc([128, 8]); mk2 = P.alloc([128, 8]); g8 = P.alloc([128, 8]); g32 = P.alloc([128, 4, 8])
        ex4 = P.alloc([128, 4])
        for (t0, TT, isc) in tl:
            xt = T(xres.ap[:, :, t0:t0 + TT]); hb = T(hfT.ap[:, :, t0:t0 + TT])
            for k in range(KD):
                P.dma('sp' if k % 2 else 'act', xt.ap[:, k, :], xTs[k, :, t0:t0 + TT], writes=[xt, xres])
            norm_mod(xt, TT, li, 1, isc, hb, sq, rs)
            for k in range(KD):
                P.ts('dve', sq.ap[:, k, 0:TT], sq.ap[:, k, 0:TT], AB.ap[:, li, 1, 0, k, isc:isc + 1],
                     AB.ap[:, li, 1, 1, k, isc:isc + 1], ALU.mult, ALU.add, reads=[sq, AB], writes=[sq])
            for b in range(TT // 128):
                bs = slice(b * 128, (b + 1) * 128)
                ps = P.psum('a')
                for k in range(KD):
                    P.mm(ps.ap[:, 0:36], sq.ap[:, k, bs], wr.ap[:, k, :], start=(k == 0), stop=(k == KD - 1),
                         reads=[sq, wr], writes=[ps])
                P.cp('dve', lg.ap, ps.ap[:, 0:36], reads=[ps], writes=[lg])
                def red(out, in_, op):
                    return P.op('dve', lambda e: e.tensor_reduce(out=out, in_=in_, axis=AX.X, op=op), [lg, les, ex4, st_], [st_])
                P.op('dve', lambda e: e.tensor_reduce(out=st_.ap[:, 0:1], in_=lg.ap[:, 0:4], axis=AX.X, op=ALU.max), [lg], [st_])
                P.ts('dve', oh.ap, lg.ap[:, 0:4], st_.ap[:, 0:1], None, ALU.is_equal, reads=[lg, st_], writes=[oh])
                P.ts('dve', st_.ap[:, 1:2], st_.ap[:, 0:1], -1.0, None, ALU.mult, reads=[st_], writes=[st_])
                P.act(ex4.ap, lg.ap[:, 0:4], AF.Exp, reads=[lg, st_], writes=[ex4], bias=st_.ap[:, 1:2])
                P.op('dve', lambda e: e.tensor_reduce(out=st_.ap[:, 2:3], in_=ex4.ap, axis=AX.X, op=ALU.add), [ex4], [st_])
                P.op('dve', lambda e: e.reciprocal(out=st_.ap[:, 3:4], in_=st_.ap[:, 2:3]), [st_], [st_])
                P.ts('dve', les.ap, lg.ap[:, 4:12], oh.ap[:, 0:1], None, ALU.mult, reads=[lg, oh], writes=[les])
                for g in range(1, 4):
                    P.stt('dve', les.ap, lg.ap[:, 4 + 8 * g:12 + 8 * g], oh.ap[:, g:g + 1], les.ap, ALU.mult, ALU.add,
                          reads=[lg, oh, les], writes=[les])
                P.op('dve', lambda e: e.tensor_reduce(out=st_.ap[:, 4:5], in_=les.ap, axis=AX.X, op=ALU.max), [les], [st_])
                P.ts('dve', mk1.ap, les.ap, st_.ap[:, 4:5], None, ALU.is_equal, reads=[les, st_], writes=[mk1])
                P.stt('dve', g8.ap, mk1.ap, -1e30, les.ap, ALU.mult, ALU.add, reads=[mk1, les], writes=[g8])
                P.op('dve', lambda e: e.tensor_reduce(out=st_.ap[:, 5:6], in_=g8.ap, axis=AX.X, op=ALU.max), [g8], [st_])
                P.ts('dve', mk2.ap, g8.ap, st_.ap[:, 5:6], None, ALU.is_equal, reads=[g8, st_], writes=[mk2])
                P.tt('dve', st_.ap[:, 6:7], st_.ap[:, 5:6], st_.ap[:, 4:5], ALU.subtract, reads=[st_], writes=[st_])
                P.act(st_.ap[:, 7:8], st_.ap[:, 6:7], AF.Exp, reads=[st_], writes=[st_])
                P.ts('dve', st_.ap[:, 8:9], st_.ap[:, 7:8], 1.0, None, ALU.add, reads=[st_], writes=[st_])
                P.op('dve', lambda e: e.reciprocal(out=st_.ap[:, 9:10], in_=st_.ap[:, 8:9]), [st_], [st_])
                P.tt('dve', st_.ap[:, 10:11], st_.ap[:, 9:10], st_.ap[:, 3:4], ALU.mult, reads=[st_], writes=[st_])
                P.tt('dve', st_.ap[:, 11:12], st_.ap[:, 10:11], st_.ap[:, 7:8], ALU.mult, reads=[st_], writes=[st_])
                P.ts('dve', g8.ap, mk1.ap, st_.ap[:, 10:11], None, ALU.mult, reads=[mk1, st_], writes=[g8])
                P.stt('dve', g8.ap, mk2.ap, st_.ap[:, 11:12], g8.ap, ALU.mult, ALU.add, reads=[mk2, st_, g8], writes=[g8])
                for g in range(4):
                    P.ts('dve', g32.ap[:, g, :], g8.ap, oh.ap[:, g:g + 1], None, ALU.mult, reads=[g8, oh], writes=[g32])
                pt = P.psum('a')
                P.tr(pt.ap[0:32, 0:128], g32.ap.rearrange("p a b -> p (a b)"), ident.ap, reads=[g32, ident], writes=[pt])
                P.cp('act', gT.ap[:, t0 + b * 128:t0 + (b + 1) * 128], pt.ap[0:32, 0:128], reads=[pt], writes=[gT])
        P.release(m2)
        if dbg:
            tap(f"gT{li}", gT, [32, NT])
        Wg = [P.alloc([128, KD, 512], BF16) for _ in range(2)]
        Wu = [P.alloc([128, KD, 512], BF16) for _ in range(2)]
        Wd = [P.alloc([128, 4, D], BF16) for _ in range(2)]
        selt = [P.alloc([32, 128]) for _ in range(2)]
        gbc = [P.alloc([128, 512]) for _ in range(2)]
        sgl = [P.alloc([128, 512]) for _ in range(2)]
        a1 = [P.alloc([128, 512]) for _ in range(2)]
        actT = [P.alloc([128, 4, 512], BF16) for _ in range(2)]
        it = 0
        pend = [None]
        for e in range(32):
            wg = Wg[e % 2]; wu = Wu[e % 2]; wd = Wd[e % 2]; se = selt[e % 2]
            for k in range(KD):
                P.dma('pool', wg.ap[:, k, :], moe_wg[li, e, k * 128:(k + 1) * 128, :], writes=[wg])
                P.dma('pool', wu.ap[:, k, :], moe_wu[li, e, k * 128:(k + 1) * 128, :], writes=[wu])
            for k in range(4):
                P.dma('pool', wd.ap[:, k, :], moe_wd[li, e, k * 128:(k + 1) * 128, :], writes=[wd])
            P.cp('pool', se.ap, ident.ap[0:32, e:e + 1].to_broadcast([32, 128]), reads=[ident], writes=[se])
            for (t0, TT, isc) in tl:
                it += 1
                gb = gbc[it % 2]; at = actT[it % 2]
                pg_ = P.psum('b')
                P.mm(pg_.ap[:, 0:TT], se.ap, gT.ap[:, t0:t0 + TT], reads=[se, gT], writes=[pg_])
                P.cp('act', gb.ap[:, 0:TT], pg_.ap[:, 0:TT], reads=[pg_], writes=[gb])
                for fc in range(4):
                    fs = slice(fc * 128, (fc + 1) * 128)
                    pg = P.psum('a'); pu = P.psum('a')
                    for k in range(KD):
                        P.mm(pg.ap[:, 0:TT], wg.ap[:, k, fs], hfT.ap[:, k, t0:t0 + TT], start=(k == 0), stop=(k == KD - 1),
                             reads=[wg, hfT], writes=[pg])
                    for k in range(KD):
                        P.mm(pu.ap[:, 0:TT], wu.ap[:, k, fs], hfT.ap[:, k, t0:t0 + TT], start=(k == 0), stop=(k == KD - 1),
                             reads=[wu, hfT], writes=[pu])
                    sg_ = sgl[fc % 2]; a_ = a1[fc % 2]
                    P.act(sg_.ap[:, 0:TT], pg.ap[:, 0:TT], AF.Silu, reads=[pg], writes=[sg_])
                    P.tt('dve', a_.ap[:, 0:TT], pu.ap[:, 0:TT], sg_.ap[:, 0:TT], ALU.mult, reads=[pu, sg_], writes=[a_])
                    P.tt('pool', at.ap[:, fc, 0:TT], a_.ap[:, 0:TT], gb.ap[:, 0:TT], ALU.mult, reads=[a_, gb], writes=[at])
                def down(wd=wd, at=at, t0=t0, TT=TT, isc=isc):
                    for dc in range(KD):
                        po = P.psum('c')
                        for fc in range(4):
                            P.mm(po.ap[:, 0:TT], wd.ap[:, fc, dc * 128:(dc + 1) * 128], at.ap[:, fc, 0:TT],
                                 start=(fc == 0), stop=(fc == 3), reads=[wd, at], writes=[po])
                        P.stt('dve', xres.ap[:, dc, t0:t0 + TT], po.ap[:, 0:TT], mod.ap[:, li, 40 + dc, isc:isc + 1],
                              xres.ap[:, dc, t0:t0 + TT], ALU.mult, ALU.add, reads=[po, mod, xres], writes=[xres])
                if pend[0] is not None:
                    pend[0]()
                pend[0] = down
        pend[0]()
        for (t0, TT, isc) in tl:
            for k in range(KD):
                P.dma('sp', xTs[k, :, t0:t0 + TT], xres.ap[:, k, t0:t0 + TT], reads=[xres])
        P.release(m)


    LAM_INIT = 0.8 - 0.6 * math.exp(-0.3 * 1)
    RET_G128 = []
    for h_ in range(4):
        lgf = [math.log(1.0 - 2.0 ** (-5.0 - j)) for j in range(4)]
        RET_G128.append((math.exp(lgf[h_] * 128), math.exp(lgf[3 - h_] * 128)))

    def phase_l1mix():
        m = P.mark()
        LT = [(t0 - C, TT) for (t0, TT, isc) in tiles if not isc]
        perm = P.alloc([128, 128]); P.dma('sp', perm.ap, rope_perm, writes=[perm])
        rc_ = P.alloc([128, S]); P.dma('sp', rc_.ap, ropeC, writes=[rc_])
        rs_ = P.alloc([128, S]); P.dma('sp', rs_.ap, ropeS, writes=[rs_])
        ones128 = P.alloc([128, 128]); P.memset('pool', ones128.ap, 1.0 / 128, writes=[ones128])
        onesb = P.alloc([128, 128], BF16); P.memset('pool', onesb.ap, 1.0, writes=[onesb])
        subg = P.alloc([128, 1]); P.dma('sp', subg.ap, sublnT, writes=[subg])
        P.ts('dve', subg.ap, subg.ap, 1.0 - LAM_INIT, None, ALU.mult, reads=[subg], writes=[subg])
        rgv = P.alloc([128, 4]); P.dma('sp', rgv.ap, retgT, writes=[rgv])
        rkv = P.alloc([128, 8]); P.dma('sp', rkv.ap, retk, writes=[rkv])
        dl = P.alloc([1, 4, 64]); P.dma('sp', dl.ap, dlam.rearrange("o (a b) -> o a b", a=4), writes=[dl])
        pr2 = P.alloc([1, 2, 64]); s2 = P.alloc([1, 4]); nlam = P.alloc([128, 1]); onesr = P.alloc([1, 128])
        P.memset('dve', onesr.ap, 1.0, writes=[onesr])
        P.tt('dve', pr2.ap[:, 0, :], dl.ap[:, 0, :], dl.ap[:, 1, :], ALU.mult, reads=[dl], writes=[pr2])
        P.tt('dve', pr2.ap[:, 1, :], dl.ap[:, 2, :], dl.ap[:, 3, :], ALU.mult, reads=[dl], writes=[pr2])
        P.op('dve', lambda e: e.tensor_reduce(out=s2.ap[:, 0:2], in_=pr2.ap, axis=AX.X, op=ALU.add), [pr2], [s2])
        P.act(s2.ap[:, 0:2], s2.ap[:, 0:2], AF.Exp, reads=[s2], writes=[s2])
        P.tt('dve', s2.ap[:, 2:3], s2.ap[:, 1:2], s2.ap[:, 0:1], ALU.subtract, reads=[s2], writes=[s2])
        P.ts('dve', s2.ap[:, 3:4], s2.ap[:, 2:3], -LAM_INIT, None, ALU.add, reads=[s2], writes=[s2])
        psl = P.psum('a')
        P.mm(psl.ap[:, 0:1], onesr.ap, s2.ap[:, 3:4], reads=[onesr, s2], writes=[psl])
        P.cp('dve', nlam.ap, psl.ap[:, 0:1], reads=[psl], writes=[nlam])
        raw = [P.alloc([128, NT]) for _ in range(2)]
        vT = P.alloc([128, NT])
        kT = P.alloc([128, NT], BF16); qT = P.alloc([128, S], BF16)
        Vtok = P.alloc([128, NCH, 128], BF16)
        t512 = [P.alloc([128, 512]) for _ in range(6)]
        pTb = [P.alloc([128, 512], BF16) for _ in range(3)]
        ymt = [P.alloc([128, 512], BF16) for _ in range(2)]
        cnt = [0]

        def rr(lst):
            cnt[0] += 1
            return lst[cnt[0] % len(lst)]

        def rope(dst, dcol0, src, scol0, scale=None):
            for (l0, TT) in LT:
                ps = P.psum('a')
                P.mm(ps.ap[:, 0:TT], perm.ap, src.ap[:, scol0 + l0:scol0 + l0 + TT], reads=[perm, src], writes=[ps])
                a = rr(t512); b = rr(t512)
                P.tt('dve', a.ap[:, 0:TT], ps.ap[:, 0:TT], rs_.ap[:, l0:l0 + TT], ALU.mult, reads=[ps, rs_], writes=[a])
                P.tt('pool', b.ap[:, 0:TT], src.ap[:, scol0 + l0:scol0 + l0 + TT], rc_.ap[:, l0:l0 + TT], ALU.mult, reads=[src, rc_], writes=[b])
                if scale is None:
                    P.tt('dve', dst.ap[:, dcol0 + l0:dcol0 + l0 + TT], a.ap[:, 0:TT], b.ap[:, 0:TT], ALU.add, reads=[a, b], writes=[dst])
                else:
                    P.tt('dve', a.ap[:, 0:TT], a.ap[:, 0:TT], b.ap[:, 0:TT], ALU.add, reads=[a, b], writes=[a])
                    P.ts('dve', dst.ap[:, dcol0 + l0:dcol0 + l0 + TT], a.ap[:, 0:TT], scale, None, ALU.mult, reads=[a], writes=[dst])

        def make_vtok(vsrc):
            for c4 in range(0, NCH, 4):
                n = min(4, NCH - c4)
                ps = P.psum('a')
                for j in range(n):
                    P.tr(ps.ap[:, j * 128:(j + 1) * 128], vsrc.ap[:, (c4 + j) * 128:(c4 + j + 1) * 128], ident.ap,
                         reads=[vsrc, ident], writes=[ps])
                P.cp('act', Vtok.ap[:, c4:c4 + n, :], ps.ap[:, 0:n * 128].rearrange("p (a b) -> p a b", a=n), reads=[ps], writes=[Vtok])

        def post_norm(o, TT, eps, gain_ap, extra, dst_chunk, l0):
            sq_ = rr(t512)
            P.act(sq_.ap[:, 0:TT], o.ap[:, 0:TT], AF.Square, reads=[o], writes=[sq_])
            ps = P.psum('a')
            P.mm(ps.ap[:, 0:TT], ones128.ap, sq_.ap[:, 0:TT], reads=[ones128, sq_], writes=[ps])
            P.ts('dve', sq_.ap[:, 0:TT], ps.ap[:, 0:TT], eps, None, ALU.add, reads=[ps], writes=[sq_])
            P.act(sq_.ap[:, 0:TT], sq_.ap[:, 0:TT], AF.Ln, reads=[sq_], writes=[sq_])
            P.act(sq_.ap[:, 0:TT], sq_.ap[:, 0:TT], AF.Exp, reads=[sq_], writes=[sq_], scale=-0.5)
            P.stt('dve', sq_.ap[:, 0:TT], o.ap[:, 0:TT], gain_ap, sq_.ap[:, 0:TT], ALU.mult, ALU.mult, reads=[o, sq_, subg, rgv], writes=[sq_])
            ym = rr(ymt)
            if extra is None:
                P.cp('dve', ym.ap[:, 0:TT], sq_.ap[:, 0:TT], reads=[sq_], writes=[ym])
            else:
                P.tt('dve', ym.ap[:, 0:TT], sq_.ap[:, 0:TT], extra, ALU.mult, reads=[sq_, raw[0], raw[1]], writes=[ym])
            P.dma('sp', ymTs[dst_chunk, :, C + l0:C + l0 + TT], ym.ap[:, 0:TT], reads=[ym])

        A1, S1, A2, S2 = P.PS[0], P.PS[1], P.PS[2], P.PS[3]
        for h in range(4):
            P.dma('sp', raw[0].ap, pTs[h], writes=[raw[0]])
            P.dma('act', raw[1].ap[:, 0:S], pTs[14 + h][:, C:NT], writes=[raw[1]])
            P.dma('sp', vT.ap, pTs[4 + h], writes=[vT])
            P.cp('pool', kT.ap[:, 0:C], raw[0].ap[:, 0:C], reads=[raw[0]], writes=[kT])
            rope(kT, C, raw[0], C)
            rope(qT, 0, raw[1], 0)
            make_vtok(vT)
            for (l0, TT) in LT:
                for kc in range(NCH):
                    for br, (Ab, Sb) in enumerate(((A1, S1), (A2, S2))):
                        hsb = slice(br * 64, br * 64 + 64)
                        sc = P.psum('c')
                        P.mm(sc.ap[:, 0:TT], kT.ap[hsb, kc * 128:(kc + 1) * 128], qT.ap[hsb, l0:l0 + TT], reads=[kT, qT], writes=[sc])
                        pt = rr(pTb)
                        P.act(pt.ap[:, 0:TT], sc.ap[:, 0:TT], AF.Exp, reads=[sc], writes=[pt], scale=0.125)
                        P.mm(Ab.ap[:, 0:TT], Vtok.ap[:, kc, :], pt.ap[:, 0:TT], start=(kc == 0), stop=(kc == NCH - 1), reads=[Vtok, pt], writes=[Ab])
                        P.mm(Sb.ap[:, 0:TT], onesb.ap, pt.ap[:, 0:TT], start=(kc == 0), stop=(kc == NCH - 1), reads=[onesb, pt], writes=[Sb])
                r1 = rr(t512); o1 = rr(t512); r2 = rr(t512); o2 = rr(t512)
                P.op('dve', lambda e, r1=r1, TT=TT: e.reciprocal(out=r1.ap[:, 0:TT], in_=S1.ap[:, 0:TT]), [S1], [r1])
                P.tt('dve', o1.ap[:, 0:TT], A1.ap[:, 0:TT], r1.ap[:, 0:TT], ALU.mult, reads=[A1, r1], writes=[o1])
                P.op('dve', lambda e, r2=r2, TT=TT: e.reciprocal(out=r2.ap[:, 0:TT], in_=S2.ap[:, 0:TT]), [S2], [r2])
                P.tt('dve', o2.ap[:, 0:TT], A2.ap[:, 0:TT], r2.ap[:, 0:TT], ALU.mult, reads=[A2, r2], writes=[o2])
                P.stt('dve', o1.ap[:, 0:TT], o2.ap[:, 0:TT], nlam.ap[:, 0:1], o1.ap[:, 0:TT], ALU.mult, ALU.add, reads=[o2, nlam, o1], writes=[o1])
                post_norm(o1, TT, 1e-5, subg.ap[:, 0:1], None, h, l0)
        kTp = kT
        qTp = qT
        ktok = P.alloc([128, NCH, 128], BF16)
        oT = P.alloc([128, S])
        Rf = P.alloc([128, 128]); Rb = P.alloc([128, 128], BF16)
        dm = P.alloc([128, 128]); qrow = P.alloc([128, 128])
        innm = [P.alloc([128, 128], BF16) for _ in range(2)]
        qd = [P.alloc([128, 128], BF16) for _ in range(2)]
        kd = [P.alloc([128, 64], BF16) for _ in range(2)]
        for h in range(4):
            hq = h % 2
            hsq = slice(hq * 64, hq * 64 + 64)
            if hq == 0:
                P.dma('sp', raw[0].ap, pTs[8 + h // 2], writes=[raw[0]])
                P.dma('act', raw[1].ap[:, 0:S], pTs[18 + h // 2][:, C:NT], writes=[raw[1]])
                P.ts('pool', kTp.ap[:, 0:C], raw[0].ap[:, 0:C], 0.125, None, ALU.mult, reads=[raw[0]], writes=[kTp])
                rope(kTp, C, raw[0], C, scale=0.125)
                rope(qTp, 0, raw[1], 0)
                for c4 in range(0, NCH, 4):
                    n = min(4, NCH - c4)
                    ps = P.psum('a'); psb_ = ps.ap.bitcast(BF16)
                    for j in range(n):
                        P.tr(psb_[:, j * 128:(j + 1) * 128], kTp.ap[:, (c4 + j) * 128:(c4 + j + 1) * 128], identb.ap,
                             reads=[kTp, identb], writes=[ps])
                    P.cp('act', ktok.ap[:, c4:c4 + n, :], psb_[:, 0:n * 128].rearrange("p (a b) -> p a b", a=n), reads=[ps], writes=[ktok])
            P.dma('sp', vT.ap, pTs[10 + h], writes=[vT])
            make_vtok(vT)
            for d in range(2):
                hd = h * 2 + d
                g128 = RET_G128[h][d]
                P.dma('sp', dm.ap, retD[hd], writes=[dm])
                P.dma('sp', qrow.ap, retq[hd:hd + 1, :].partition_broadcast(128), writes=[qrow])
                P.memset('dve', Rf.ap, 0.0, writes=[Rf]); P.memset('pool', Rb.ap, 0.0, writes=[Rb])
                order = list(range(0, NCH)) if d == 0 else list(range(CCH - 1, -1, -1)) + list(range(NCH - 1, CCH - 1, -1))
                for oi, c in enumerate(order):
                    ksl = slice(c * 128, (c + 1) * 128)
                    if c >= CCH:
                        i0 = c * 128 - C
                        im = rr(innm); q_ = rr(qd)
                        ps1 = P.psum('c')
                        P.mm(ps1.ap[:, 0:128], kTp.ap[hsq, ksl], qTp.ap[hsq, i0:i0 + 128], reads=[kTp, qTp], writes=[ps1])
                        P.tt('dve', im.ap, ps1.ap[:, 0:128], dm.ap, ALU.mult, reads=[ps1, dm], writes=[im])
                        P.tt('pool', q_.ap[hsq, :], qTp.ap[hsq, i0:i0 + 128], qrow.ap[hsq, :], ALU.mult, reads=[qTp, qrow], writes=[q_])
                        ps2 = P.psum('c')
                        P.mm(ps2.ap[:, 0:128], Vtok.ap[:, c, :], im.ap, start=True, stop=False, reads=[Vtok, im], writes=[ps2])
                        P.mm(ps2.ap[:, 0:128], Rb.ap[hsq, :], q_.ap[hsq, :], start=False, stop=True, reads=[Rb, q_], writes=[ps2])
                        if d == 0:
                            P.cp('act', oT.ap[:, i0:i0 + 128], ps2.ap[:, 0:128], reads=[ps2], writes=[oT])
                        else:
                            P.tt('dve', oT.ap[:, i0:i0 + 128], ps2.ap[:, 0:128], oT.ap[:, i0:i0 + 128], ALU.add, reads=[ps2, oT], writes=[oT])
                    if oi < len(order) - 1:
                        k_ = rr(kd)
                        P.ts('pool', k_.ap, ktok.ap[:, c, hsq], rkv.ap[:, hd:hd + 1], None, ALU.mult, reads=[ktok, rkv], writes=[k_])
                        ps3 = P.psum('c')
                        P.mm(ps3.ap[hsq, 0:128], k_.ap, Vtok.ap[:, c, :], reads=[k_, Vtok], writes=[ps3])
                        P.stt('dve', Rf.ap[hsq, :], Rf.ap[hsq, :], g128, ps3.ap[hsq, 0:128], ALU.mult, ALU.add, reads=[Rf, ps3], writes=[Rf])
                        P.cp('act', Rb.ap[hsq, :], Rf.ap[hsq, :], reads=[Rf], writes=[Rb])
            P.dma('act', raw[1].ap[:, 0:S], pTs[20 + h][:, C:NT], writes=[raw[1]]) if hq == 1 else \
                P.dma('act', raw[0].ap[:, 0:S], pTs[20 + h][:, C:NT], writes=[raw[0]])
            gsrc = raw[1] if hq == 1 else raw[0]
            P.act(gsrc.ap[:, 0:S], gsrc.ap[:, 0:S], AF.Silu, reads=[gsrc], writes=[gsrc])
            for (l0, TT) in LT:
                ot = T(oT.ap[:, l0:l0 + TT])
                ot.lw = oT.lw
                post_norm(ot, TT, 1e-6, rgv.ap[:, h:h + 1], gsrc.ap[:, l0:l0 + TT], 4 + h, l0)
                oT.rd.update(ot.rd)
        P.release(m)

    if STOP_AFTER == 'mod':
        return P, locals()
    phase_inproj(0, ev_w_in, EV_COLS, True)


    def phase_rwkv():
        m = P.mark()
        DIN, DCH, DST = RW_DT[:3]
        DCN = RW_DT[3] if len(RW_DT) > 3 else DCH
        idcn = ident if DCN == F32 else identb
        idch = ident if DCH == F32 else identb
        idin = ident if DIN == F32 else identb
        seqs = [(0, C), (C, NT)]
        lup = P.alloc([128, 2, 512], BF16); P.dma('pool', lup.ap, lora_up, writes=[lup])
        gup = P.alloc([128, 512], BF16); P.dma('pool', gup.ap, g_up, writes=[gup])
        rv = P.alloc([128, 9, 4]); P.dma('sp', rv.ap, rvec, writes=[rv])
        shv = P.alloc([128, 12, 3]); P.dma('sp', shv.ap, shiftT, writes=[shv])
        rmk = P.alloc([128, 2, 896])
        for d in range(2):
            P.dma('sp', rmk.ap[:, d, :], crmask[d], writes=[rmk])
        omka = P.alloc([128, 4])
        P.ts('dve', omka.ap, rv.ap[:, 5, :], -1.0, 1.0, ALU.mult, ALU.add, reads=[rv], writes=[omka])
        tmpA = P.alloc([128, NT]); tmpB = P.alloc([128, NT])
        wdad = P.alloc([128, NT], BF16); sg = P.alloc([128, NT], BF16)
        P.dma('sp', tmpA.ap, pTs[8], writes=[tmpA])
        P.act(wdad.ap[0:64, :], tmpA.ap[0:64, :], AF.Tanh, reads=[tmpA], writes=[wdad])
        P.cp('dve', wdad.ap[64:128, :], tmpA.ap[64:128, :], reads=[tmpA], writes=[wdad])
        P.dma('sp', tmpB.ap, pTs[13], writes=[tmpB])
        P.act(sg.ap, tmpB.ap, AF.Sigmoid, reads=[tmpB], writes=[sg])
        kc = P.alloc([128, NT]); lw = [P.alloc([128, NT]) for _ in range(2)]
        vc = P.alloc([128, NT], DIN); rc = P.alloc([128, NT], DIN); kk = P.alloc([128, NT], DIN)
        kt = [P.alloc([128, NT], DIN) for _ in range(2)]; bb = [P.alloc([128, NT], DIN) for _ in range(2)]
        MTb = P.alloc([128, 2, NCH, 128], DST); P.memset('pool', MTb.ap, 0.0, writes=[MTb])
        Sbk = P.alloc([128, 2, NCH, 128], DST); P.memset('pool', Sbk.ap, 0.0, writes=[Sbk])
        Gst = P.alloc([128, 2, NCH, 64]); Qs = P.alloc([128, 2, NCH, 128], DST); Y0 = P.alloc([128, NCH, 128])
        Vpad = [P.alloc([128, 2, 128], DCH) for _ in range(2)]
        P2p = [[P.alloc([128, 128], DCH) for _ in range(2)] for _ in range(2)]
        for t_ in Vpad + P2p[0] + P2p[1]:
            P.memset('pool', t_.ap, 0.0, writes=[t_])
        Vtk = [P.alloc([128, 128], DCH) for _ in range(2)]
        lwtok = [P.alloc([128, 128]) for _ in range(2)]
        E1 = [P.alloc([128, 128]) for _ in range(2)]; E0 = [P.alloc([128, 128]) for _ in range(2)]
        Ei = [P.alloc([128, 128]) for _ in range(2)]; nWC = [P.alloc([128, 1]) for _ in range(2)]
        QR = [P.alloc([128, 2, 128], DCH) for _ in range(2)]
        Bt = [P.alloc([128, 128], DCH) for _ in range(2)]; Kt = [P.alloc([128, 128], DCH) for _ in range(2)]
        nBh = [P.alloc([128, 128], DCH) for _ in range(2)]; K2 = [P.alloc([128, 128], DCH) for _ in range(2)]
        TK = [P.alloc([128, 3, 128], DCH) for _ in range(2)]
        evA = [P.alloc([128, 256], DCH) for _ in range(2)]; evB = [P.alloc([128, 256], DCH) for _ in range(2)]
        Xr = [[P.alloc([128, 128], DCN) for _ in range(3)] for _ in range(2)]; XTr = [[P.alloc([128, 128], DCN) for _ in range(3)] for _ in range(2)]
        TTr = [[P.alloc([128, 128], DCN) for _ in range(3)] for _ in range(2)]
        TTf = [P.alloc([128, 128], DCH) for _ in range(2)]
        n2v = [P.alloc([128, 64], DCH) for _ in range(2)]; Pcat = [P.alloc([128, 128], DCH) for _ in range(2)]
        Sst = [P.alloc([128, 64], DST) for _ in range(2)]
        t512 = [P.alloc([128, 512]) for _ in range(4)]
        ymt = [P.alloc([128, 512], BF16) for _ in range(2)]
        cnt = [0]

        def rr(lst):
            cnt[0] += 1
            return lst[cnt[0] % len(lst)]

        def conv(dst, src, idx):
            for (a, b) in seqs:
                P.ts('dve', dst.ap[:, a:b], src.ap[:, a:b], shv.ap[:, idx, 1:2], None, ALU.mult, reads=[src, shv], writes=[dst])
                P.stt('dve', dst.ap[:, a + 1:b], src.ap[:, a:b - 1], shv.ap[:, idx, 0:1], dst.ap[:, a + 1:b], ALU.mult, ALU.add,
                      reads=[src, shv, dst], writes=[dst])
                P.stt('dve', dst.ap[:, a:b - 1], src.ap[:, a + 1:b], shv.ap[:, idx, 2:3], dst.ap[:, a:b - 1], ALU.mult, ALU.add,
                      reads=[src, shv, dst], writes=[dst])

        for pr in range(4):
            if RW_STOP == 0:
                break
            cs_ = slice(pr * 128, (pr + 1) * 128)
            P.dma('sp', tmpA.ap, pTs[pr], writes=[tmpA]); conv(kc, tmpA, pr)
            P.dma('sp', tmpB.ap, pTs[4 + pr], writes=[tmpB]); conv(vc, tmpB, 4 + pr)
            P.dma('sp', tmpA.ap, pTs[9 + pr], writes=[tmpA]); conv(rc, tmpA, 8 + pr)
            P.ts('dve', tmpA.ap, kc.ap, rv.ap[:, 4, pr:pr + 1], None, ALU.mult, reads=[kc, rv], writes=[tmpA])
            P.act(tmpB.ap, tmpA.ap, AF.Square, reads=[tmpA], writes=[tmpB])
            for (t0, TT, isc) in tiles:
                ps = P.psum('a')
                P.mm(ps.ap[:, 0:TT], blk.ap, tmpB.ap[:, t0:t0 + TT], reads=[blk, tmpB], writes=[ps])
                tq = rr(t512)
                P.ts('dve', tq.ap[:, 0:TT], ps.ap[:, 0:TT], 1e-12, None, ALU.max, reads=[ps], writes=[tq])
                P.act(tq.ap[:, 0:TT], tq.ap[:, 0:TT], AF.Ln, reads=[tq], writes=[tq])
                P.act(tq.ap[:, 0:TT], tq.ap[:, 0:TT], AF.Exp, reads=[tq], writes=[tq], scale=-0.5)
                P.tt('dve', kk.ap[:, t0:t0 + TT], tmpA.ap[:, t0:t0 + TT], tq.ap[:, 0:TT], ALU.mult, reads=[tmpA, tq], writes=[kk])
            for d in range(2):
                for (t0, TT, isc) in tiles:
                    ps = P.psum('a')
                    P.mm(ps.ap[:, 0:TT], lup.ap[0:64, d, cs_], wdad.ap[0:64, t0:t0 + TT], reads=[lup, wdad], writes=[ps])
                    P.act(lw[d].ap[:, t0:t0 + TT], ps.ap[:, 0:TT], AF.Sigmoid, reads=[ps, rv], writes=[lw[d]], bias=rv.ap[:, d, pr:pr + 1])
                    ps2 = P.psum('a')
                    P.mm(ps2.ap[:, 0:TT], lup.ap[64:128, d, cs_], wdad.ap[64:128, t0:t0 + TT], reads=[lup, wdad], writes=[ps2])
                    ta = rr(t512)
                    P.act(ta.ap[:, 0:TT], ps2.ap[:, 0:TT], AF.Sigmoid, reads=[ps2, rv], writes=[ta], bias=rv.ap[:, 2 + d, pr:pr + 1])
                    P.tt('dve', bb[d].ap[:, t0:t0 + TT], ta.ap[:, 0:TT], kk.ap[:, t0:t0 + TT], ALU.mult, reads=[ta, kk], writes=[bb[d]])
                    P.ts('dve', ta.ap[:, 0:TT], ta.ap[:, 0:TT], rv.ap[:, 5, pr:pr + 1], omka.ap[:, pr:pr + 1], ALU.mult, ALU.add,
                         reads=[ta, rv, omka], writes=[ta])
                    P.tt('dve', kt[d].ap[:, t0:t0 + TT], ta.ap[:, 0:TT], kc.ap[:, t0:t0 + TT], ALU.mult, reads=[ta, kc], writes=[kt[d]])
                P.ts('pool', lw[d].ap, lw[d].ap, -W_DECAY_SCALE, None, ALU.mult, reads=[lw[d]], writes=[lw[d]])
            if RW_STOP == 1:
                break
            PS6 = P.PS[6]
            for c in range(NCH):
                cs = slice(c * 128, (c + 1) * 128)
                vp = Vpad[c % 2]; vt = Vtk[c % 2]
                psb = P.psum('b')
                pv_ = psb.ap if DIN == F32 else psb.ap.bitcast(BF16)
                P.tr(pv_[:, 0:128], vc.ap[:, cs], idin.ap, reads=[vc, idin], writes=[psb])
                P.cp('act', vt.ap, pv_[:, 0:128], reads=[psb], writes=[vt])
                for hp in range(2):
                    P.cp('pool', vp.ap[:, hp, hp * 64:hp * 64 + 64], vt.ap[:, hp * 64:hp * 64 + 64], reads=[vt], writes=[vp])
                nmm = 0
                for d in range(2):
                    i2 = (c * 2 + d) % 2
                    lt = lwtok[i2]; e1 = E1[i2]; e0 = E0[i2]; ei = Ei[i2]; nw = nWC[i2]
                    qr = QR[i2]; bt = Bt[i2]; ktt = Kt[i2]; nb = nBh[i2]; k2 = K2[i2]; tk = TK[i2]
                    ps = P.psum('b')
                    P.tr(ps.ap[:, 0:128], lw[d].ap[:, cs], ident.ap, reads=[lw[d], ident], writes=[ps])
                    P.cp('dve', lt.ap, ps.ap[:, 0:128], reads=[ps], writes=[lt])
                    psc = P.psum('b')
                    P.mm(psc.ap[:, 0:256], lt.ap, rmk.ap[:, d, 640:896], reads=[lt, rmk], writes=[psc])
                    P.act(e1.ap, psc.ap[:, 0:128], AF.Exp, reads=[psc], writes=[e1])
                    P.act(e0.ap, psc.ap[:, 128:256], AF.Exp, reads=[psc], writes=[e0])
                    P.act(ei.ap, psc.ap[:, 0:128], AF.Exp, reads=[psc], writes=[ei], scale=-1.0)
                    wc = e1.ap[:, 127:128] if d == 0 else e1.ap[:, 0:1]
                    P.ts('dve', nw.ap, wc, -1.0, None, ALU.mult, reads=[e1], writes=[nw])
                    P.tt('dve', qr.ap[:, 0, :], kk.ap[:, cs], e0.ap, ALU.mult, reads=[kk, e0], writes=[qr])
                    P.tt('dve', qr.ap[:, 1, :], rc.ap[:, cs], e1.ap, ALU.mult, reads=[rc, e1], writes=[qr])
                    P.tt('pool', bt.ap, bb[d].ap[:, cs], ei.ap, ALU.mult, reads=[bb[d], ei], writes=[bt])
                    P.tt('pool', ktt.ap, kt[d].ap[:, cs], ei.ap, ALU.mult, reads=[kt[d], ei], writes=[ktt])
                    P.ts('dve', nb.ap, bt.ap, nw.ap[:, 0:1], None, ALU.mult, reads=[bt, nw], writes=[nb])
                    P.ts('dve', k2.ap, ktt.ap, wc, None, ALU.mult, reads=[ktt, e1], writes=[k2])
                    pst = P.psum('b'); pstb = pst.ap if DCH == F32 else pst.ap.bitcast(BF16)
                    P.tr(pstb[:, 0:128], qr.ap[:, 0, :], idch.ap, reads=[qr, idch], writes=[pst])
                    P.tr(pstb[:, 128:256], nb.ap, idch.ap, reads=[nb, idch], writes=[pst])
                    P.tr(pstb[:, 256:384], k2.ap, idch.ap, reads=[k2, idch], writes=[pst])
                    P.cp('act', tk.ap, pstb[:, 0:384].rearrange("p (a b) -> p a b", a=3), reads=[pst], writes=[tk])
                    if RW_STOP == 2:
                        continue
                    H = [dict(), dict()]
                    qr2s = [qr.ap[slice(hp * 64, hp * 64 + 64), :, :].rearrange("p a b -> p (a b)") for hp in range(2)]
                    for hp in range(2):
                        hs = slice(hp * 64, hp * 64 + 64)
                        ea = evA[hp]; eb = evB[hp]
                        qr2 = qr2s[hp]
                        p1 = P.psum('r')
                        P.mm(p1.ap[:, 0:256], bt.ap[hs, :], qr2, reads=[bt, qr], writes=[p1])
                        P.tt('dve', ea.ap, p1.ap[:, 0:256], rmk.ap[:, d, 0:256], ALU.mult, reads=[p1, rmk], writes=[ea])
                        p2 = P.psum('r')
                        P.mm(p2.ap[:, 0:256], ktt.ap[hs, :], qr2, reads=[ktt, qr], writes=[p2])
                        P.tt('dve', eb.ap, p2.ap[:, 0:256], rmk.ap[:, d, 256:512], ALU.mult, reads=[p2, rmk], writes=[eb])
                        p3 = P.psum('r')
                        P.mm(p3.ap[:, 0:128], qr.ap[hs, 0, :], bt.ap[hs, :], reads=[qr, bt], writes=[p3])
                        X = Xr[hp][0]
                        P.tt('dve', X.ap, p3.ap[:, 0:128], rmk.ap[:, d, 512:640], ALU.mult, reads=[p3, rmk], writes=[X])
                        XT = XTr[hp][0]
                        P.cp('pool', XT.ap, ea.ap[:, 0:128], reads=[ea], writes=[XT])
                        TTc = TTr[hp][0]
                        P.tt('pool', TTc.ap, ea.ap[:, 0:128], ident.ap, ALU.add, reads=[ea, ident], writes=[TTc])
                        H[hp] = dict(X=X, XT=XT, TT=TTc, xi=0, xti=0, ti=0)
                    for j in range(1, 7):
                        for hp in range(2):
                            st = H[hp]
                            X = st['X']; XT = st['XT']; TTc = st['TT']
                            pX = P.psum('r')
                            P.mm(pX.ap[:, 0:128], XT.ap, X.ap, reads=[XT, X], writes=[pX])
                            st['xi'] = (st['xi'] + 1) % 3
                            Xn = Xr[hp][st['xi']]
                            P.cp('act', Xn.ap, pX.ap[:, 0:128], reads=[pX], writes=[Xn])
                            if j < 6:
                                pXT = P.psum('r')
                                P.mm(pXT.ap[:, 0:128], X.ap, XT.ap, reads=[XT, X], writes=[pXT])
                                st['xti'] = (st['xti'] + 1) % 3
                                XTn = XTr[hp][st['xti']]
                                P.cp('dve', XTn.ap, pXT.ap[:, 0:128], reads=[pXT], writes=[XTn])
                                st['XT'] = XTn
                            pT = P.psum('r')
                            P.mm(pT.ap[:, 0:128], Xn.ap, TTc.ap, reads=[Xn, TTc], writes=[pT])
                            st['ti'] = (st['ti'] + 1) % 3
                            TTn = TTr[hp][st['ti']]
                            P.tt('dve', TTn.ap, pT.ap[:, 0:128], TTc.ap, ALU.add, reads=[pT, TTc], writes=[TTn])
                            st['X'] = Xn
                            st['TT'] = TTn
                    for hp in range(2):
                        hs = slice(hp * 64, hp * 64 + 64)
                        ea = evA[hp]; eb = evB[hp]; TTc = H[hp]['TT']
                        nv = n2v[hp]; pc = Pcat[hp]; p2p = P2p[hp][d]
                        p4 = P.psum('r')
                        P.mm(p4.ap[:, 0:64], eb.ap[:, 0:128], vt.ap[:, hs], reads=[eb, vt], writes=[p4])
                        P.cp('act', nv.ap, p4.ap[:, 0:64], reads=[p4], writes=[nv])
                        p5 = P.psum('r')
                        P.mm(p5.ap[:, 0:64], TTc.ap, tk.ap[:, 0, hs], reads=[TTc, tk], writes=[p5])
                        P.mm(p5.ap[:, 64:128], TTc.ap, nv.ap, reads=[TTc, nv], writes=[p5])
                        P.cp('dve', pc.ap, p5.ap[:, 0:128], reads=[p5], writes=[pc])
                        P.cp('pool', p2p.ap[:, hs], pc.ap[:, 64:128], reads=[pc], writes=[p2p])
                        p6 = P.psum('r')
                        P.mm(p6.ap[hs, 0:64], pc.ap[:, 0:64], tk.ap[:, 1, hs], reads=[pc, tk], writes=[p6])
                        P.stt('dve', MTb.ap[hs, d, c, hs], ident.ap[hs, hs], wc[hs, :], p6.ap[hs, 0:64], ALU.mult, ALU.add,
                              reads=[ident, e1, p6], writes=[MTb])
                        p7 = P.psum('r')
                        P.mm(p7.ap[hs, 0:64], tk.ap[:, 2, hs], vt.ap[:, hs], start=True, stop=False, reads=[tk, vt], writes=[p7])
                        P.mm(p7.ap[hs, 0:64], tk.ap[:, 1, hs], pc.ap[:, 64:128], start=False, stop=True, reads=[tk, pc], writes=[p7])
                        P.cp('act', Gst.ap[hs, d, c, :], p7.ap[hs, 0:64], reads=[p7], writes=[Gst])
                        p8 = P.psum('r')
                        P.mm(p8.ap[hs, 0:128], pc.ap[:, 0:64], ea.ap[:, 128:256], reads=[pc, ea], writes=[p8])
                        P.tt('dve', Qs.ap[hs, d, c, :], p8.ap[hs, 0:128], qr.ap[hs, 1, :], ALU.add, reads=[p8, qr], writes=[Qs])
                        P.mm(PS6.ap[:, 0:128], vp.ap[:, hp, :], eb.ap[:, 128:256], start=(nmm == 0), stop=False,
                             reads=[vp, eb], writes=[PS6])
                        P.mm(PS6.ap[:, 0:128], p2p.ap, ea.ap[:, 128:256], start=False, stop=(nmm == 3),
                             reads=[p2p, ea], writes=[PS6])
                        nmm += 1
                if RW_STOP > 2 and RW_STOP not in (25, 26, 27, 28, 261, 262):
                    P.cp('dve', Y0.ap[:, c, :], PS6.ap[:, 0:128], reads=[PS6], writes=[Y0])
            if RW_STOP <= 3 or RW_STOP in (25, 26, 27, 28, 261, 262):
                break
            for d in range(2):
                order = list(range(0, CCH)) + list(range(CCH, NCH)) if d == 0 else \
                    list(range(CCH - 1, -1, -1)) + list(range(NCH - 1, CCH - 1, -1))
                s_cur = Sst[0]
                P.memset('dve', s_cur.ap, 0.0, writes=[s_cur])
                P.memset('dve', Sbk.ap[:, d, order[0], :], 0.0, writes=[Sbk])
                for i, c in enumerate(order[:-1]):
                    ps = P.psum('b')
                    P.mm(ps.ap[:, 0:64], MTb.ap[:, d, c, :], s_cur.ap, reads=[MTb, s_cur], writes=[ps])
                    s_nx = Sst[(i + 1) % 2]
                    P.tt('dve', s_nx.ap, ps.ap[:, 0:64], Gst.ap[:, d, c, :], ALU.add, reads=[ps, Gst], writes=[s_nx])
                    c2 = order[i + 1]
                    for hp in range(2):
                        hs = slice(hp * 64, hp * 64 + 64)
                        P.cp('pool', Sbk.ap[hs, d, c2, hs], s_nx.ap[hs, :], reads=[s_nx], writes=[Sbk])
                    s_cur = s_nx
            if RW_STOP == 4:
                break
            for c in range(NCH):
                ps = P.psum('b')
                P.mm(ps.ap[:, 0:128], Sbk.ap[:, 0, c, :], Qs.ap[:, 0, c, :], start=True, stop=False, reads=[Sbk, Qs], writes=[ps])
                P.mm(ps.ap[:, 0:128], Sbk.ap[:, 1, c, :], Qs.ap[:, 1, c, :], start=False, stop=True, reads=[Sbk, Qs], writes=[ps])
                P.tt('dve', tmpA.ap[:, c * 128:(c + 1) * 128], ps.ap[:, 0:128], Y0.ap[:, c, :], ALU.add, reads=[ps, Y0], writes=[tmpA])
            if dbg and pr == 0:
                tap("yr0", tmpA, [128, NT])
            for ti, (t0, TT, isc) in enumerate(tiles):
                tsl = slice(t0, t0 + TT)
                ps = P.psum('a')
                P.mm(ps.ap[:, 0:TT], blk.ap, tmpA.ap[:, tsl], reads=[blk, tmpA], writes=[ps])
                dc = rr(t512)
                P.stt('dve', dc.ap[:, 0:TT], ps.ap[:, 0:TT], -1.0 / 64, tmpA.ap[:, tsl], ALU.mult, ALU.add, reads=[ps, tmpA], writes=[dc])
                sq_ = rr(t512)
                P.act(sq_.ap[:, 0:TT], dc.ap[:, 0:TT], AF.Square, reads=[dc], writes=[sq_])
                ps2 = P.psum('a')
                P.mm(ps2.ap[:, 0:TT], blk.ap, sq_.ap[:, 0:TT], reads=[blk, sq_], writes=[ps2])
                P.ts('dve', sq_.ap[:, 0:TT], ps2.ap[:, 0:TT], 1.0 / 64, 64e-5, ALU.mult, ALU.add, reads=[ps2], writes=[sq_])
                P.act(sq_.ap[:, 0:TT], sq_.ap[:, 0:TT], AF.Ln, reads=[sq_], writes=[sq_])
                P.act(sq_.ap[:, 0:TT], sq_.ap[:, 0:TT], AF.Exp, reads=[sq_], writes=[sq_], scale=-0.5)
                P.tt('dve', dc.ap[:, 0:TT], dc.ap[:, 0:TT], sq_.ap[:, 0:TT], ALU.mult, reads=[dc, sq_], writes=[dc])
                P.ts('dve', dc.ap[:, 0:TT], dc.ap[:, 0:TT], rv.ap[:, 7, pr:pr + 1], rv.ap[:, 8, pr:pr + 1], ALU.mult, ALU.add,
                     reads=[dc, rv], writes=[dc])
                bo = rr(t512)
                P.tt('pool', bo.ap[:, 0:TT], kt[0].ap[:, tsl], kt[1].ap[:, tsl], ALU.add, reads=[kt[0], kt[1]], writes=[bo])
                P.tt('pool', bo.ap[:, 0:TT], bo.ap[:, 0:TT], rc.ap[:, tsl], ALU.mult, reads=[bo, rc], writes=[bo])
                P.ts('pool', bo.ap[:, 0:TT], bo.ap[:, 0:TT], rv.ap[:, 6, pr:pr + 1], None, ALU.mult, reads=[bo, rv], writes=[bo])
                ps3 = P.psum('a')
                P.mm(ps3.ap[:, 0:TT], blk.ap, bo.ap[:, 0:TT], reads=[blk, bo], writes=[ps3])
                P.tt('dve', bo.ap[:, 0:TT], ps3.ap[:, 0:TT], vc.ap[:, tsl], ALU.mult, reads=[ps3, vc], writes=[bo])
                P.tt('dve', dc.ap[:, 0:TT], dc.ap[:, 0:TT], bo.ap[:, 0:TT], ALU.add, reads=[dc, bo], writes=[dc])
                ps4 = P.psum('a')
                P.mm(ps4.ap[:, 0:TT], gup.ap[:, cs_], sg.ap[:, tsl], reads=[gup, sg], writes=[ps4])
                ym = ymt[ti % 2]
                P.tt('dve', ym.ap[:, 0:TT], ps4.ap[:, 0:TT], dc.ap[:, 0:TT], ALU.mult, reads=[ps4, dc], writes=[ym])
                P.dma('sp', ymTs[pr, :, tsl], ym.ap[:, 0:TT], reads=[ym])
        P.release(m)

    def phase_pool():
        m = P.mark()
        seqs = [(0, C), (C, NT)]
        pw = P.alloc([128, 4, 128], BF16)
        for gi in range(4):
            P.dma('pool', pw.ap[:, gi, :], pool_w[gi], writes=[pw])
        psc = P.alloc([128, 4]); P.dma('sp', psc.ap, pool_scT, writes=[psc])
        u = P.alloc([128, NT]); acc = P.alloc([128, NT]); inv = P.alloc([128, NT]); df = P.alloc([128, NT], BF16)
        ymt = [P.alloc([128, 512], BF16) for _ in range(2)]
        for gi, win in enumerate((2, 4, 8, 16)):
            P.dma('sp', u.ap, pTs[14 + gi], writes=[u])
            P.dma('sp', inv.ap, pool_inv[gi:gi + 1, :].partition_broadcast(128), writes=[inv])
            P.cp('pool', acc.ap, u.ap, reads=[u], writes=[acc])
            for o in range(-(win // 2), win // 2):
                if o == 0:
                    continue
                for (a, b) in seqs:
                    if o < 0:
                        P.tt('dve', acc.ap[:, a - o:b], acc.ap[:, a - o:b], u.ap[:, a:b + o], ALU.add, reads=[acc, u], writes=[acc])
                    else:
                        P.tt('dve', acc.ap[:, a:b - o], acc.ap[:, a:b - o], u.ap[:, a + o:b], ALU.add, reads=[acc, u], writes=[acc])
            P.tt('dve', acc.ap, acc.ap, inv.ap, ALU.mult, reads=[acc, inv], writes=[acc])
            P.tt('dve', df.ap, acc.ap, u.ap, ALU.subtract, reads=[acc, u], writes=[df])
            for ti, (t0, TT, isc) in enumerate(tiles):
                ps = P.psum('a')
                P.mm(ps.ap[:, 0:TT], pw.ap[:, gi, :], df.ap[:, t0:t0 + TT], reads=[pw, df], writes=[ps])
                ym = ymt[ti % 2]
                P.ts('dve', ym.ap[:, 0:TT], ps.ap[:, 0:TT], psc.ap[:, gi:gi + 1], None, ALU.mult, reads=[ps, psc], writes=[ym])
                P.dma('sp', ymTs[4 + gi, :, t0:t0 + TT], ym.ap[:, 0:TT], reads=[ym])
        P.release(m)

    RUN_RWKV = STOP_AFTER not in ('inproj',)
    RUN_POOL = STOP_AFTER not in ('inproj', 'rwkv')

    def phase_outproj(li, w_out_dram, lat_only):
        m = P.mark()
        Wout = P.alloc([128, KD, D], BF16)
        load_w_bf(Wout, w_out_dram, KD)
        ymb = [P.alloc([128, KD, 512], BF16) for _ in range(2)]
        xT = [P.alloc([128, KD, 512]) for _ in range(2)]
        for ti, (t0, TT, isc) in enumerate(tiles):
            if lat_only and isc:
                continue
            ym = ymb[ti % 2]; xt = xT[ti % 2]
            for k in range(KD):
                P.dma('sp', ym.ap[:, k, 0:TT], ymTs[k, :, t0:t0 + TT], writes=[ym])
                P.dma('act', xt.ap[:, k, 0:TT], xTs[k, :, t0:t0 + TT], writes=[xt])
            for dc in range(KD):
                ps = P.psum('a')
                for k in range(KD):
                    P.mm(ps.ap[:, 0:TT], Wout.ap[:, k, dc * 128:(dc + 1) * 128], ym.ap[:, k, 0:TT],
                         start=(k == 0), stop=(k == KD - 1), reads=[Wout, ym], writes=[ps])
                P.stt('dve', xt.ap[:, dc, 0:TT], ps.ap[:, 0:TT], mod.ap[:, li, 16 + dc, isc:isc + 1], xt.ap[:, dc, 0:TT],
                      ALU.mult, ALU.add, reads=[ps, mod, xt], writes=[xt])
            for k in range(KD):
                P.dma('sp', xTs[k, :, t0:t0 + TT], xt.ap[:, k, 0:TT], reads=[xt])
        P.release(m)

    def phase_final():
        m = P.mark()
        xT = [P.alloc([128, KD, 512]) for _ in range(2)]
        sq = P.alloc([128, KD, 512]); rs = P.alloc([128, 512])
        ob = [P.alloc([128, KD, 512]) for _ in range(2)]
        otok = [P.alloc([128, D]) for _ in range(2)]
        for ti, (t0, TT, isc) in enumerate(tiles):
            if isc:
                continue
            xt = xT[ti % 2]; o = ob[ti % 2]
            for k in range(KD):
                P.dma('sp', xt.ap[:, k, 0:TT], xTs[k, :, t0:t0 + TT], writes=[xt])
            norm_mod(xt, TT, 0, 0, 0, o, sq, rs, final=True)
            for b in range(TT // 128):
                ot = otok[b % 2]
                for half in range(2):
                    ps = P.psum('a')
                    for j in range(4):
                        k = half * 4 + j
                        P.tr(ps.ap[:, j * 128:(j + 1) * 128], o.ap[:, k, b * 128:(b + 1) * 128], ident.ap,
                             reads=[o, ident], writes=[ps])
                    P.cp('dve' if half else 'act', ot.ap[:, half * 512:(half + 1) * 512], ps.ap, reads=[ps], writes=[ot])
                r0 = t0 - C + b * 128
                P.dma('sp', out[r0:r0 + 128, :], ot.ap, reads=[ot], is_output=True)
        P.release(m)

    if RUN_RWKV:
        phase_rwkv()
    if RUN_POOL:
        phase_pool()
    if STOP_AFTER in ('inproj', 'rwkv', 'pool'):
        phase_final()
        return P, locals()
    phase_outproj(0, ev_w_out, False)
    if STOP_AFTER == 'l0mix':
        phase_final()
        return P, locals()
    phase_moe(0, False)
    if STOP_AFTER == 'l0':
        phase_final()
        return P, locals()
    phase_inproj(1, od_w_in, OD_COLS, False)
    phase_l1mix()
    phase_outproj(1, od_w_out, True)
    if STOP_AFTER == 'l1mix':
        phase_final()
        return P, locals()
    phase_moe(1, True)
    phase_final()
    return P, locals()


def fm(v, nch=None):
    v = np.asarray(v, np.float32)
    return np.ascontiguousarray(v.reshape(-1, 128).T)


def make_inputs(b, S, C, inp):
    NT = C + S
    m = {}
    m['xin'] = np.ascontiguousarray(np.concatenate([inp['ctx'][b], inp['x'][b]], 0))
    m['cT'] = np.ascontiguousarray(np.stack([fm(inp['c'][b]), fm(inp['c_ctx'])], -1))
    m['ada_w'] = inp['ada_w']
    m['ada_bT'] = np.ascontiguousarray(np.stack([fm(inp['ada_b'][0]), fm(inp['ada_b'][1])], 1))
    m['normT'] = np.ascontiguousarray(np.stack([fm(inp['norm_mix'][0]), fm(inp['norm_mix'][1]), fm(inp['norm_ffn'][0]),
                                               fm(inp['norm_ffn'][1]), fm(inp['final_norm'])], 1))
    m['ev_w_in'] = inp['ev_w_in'][0]
    m['ev_w_out'] = inp['ev_w_out'][0]
    sh = inp['rwkv_shift'][0]
    m['shiftT'] = np.ascontiguousarray(np.stack([fm(sh[0]), fm(sh[1]), fm(sh[2])], -1))
    rv = [inp['rwkv_w0'][0][0], inp['rwkv_w0'][0][1], inp['rwkv_a0'][0][0], inp['rwkv_a0'][0][1], inp['rwkv_k_k'][0],
          inp['rwkv_k_a'][0], inp['rwkv_r_k'][0], inp['rwkv_ln_g'][0], inp['rwkv_ln_b'][0]]
    m['rvec'] = np.ascontiguousarray(np.stack([fm(v) for v in rv], 1))
    lu = np.zeros((128, 2, 512), np.float32)
    for d in range(2):
        lu[0:64, d] = inp['rwkv_w_up'][0][d]
        lu[64:128, d] = inp['rwkv_a_up'][0][d]
    m['lora_up'] = lu
    m['g_up'] = inp['rwkv_g_up'][0]
    m['pool_w'] = inp['pool_w'][0]
    m['pool_scT'] = fm(inp['pool_scale'][0])
    pi = np.zeros((4, NT), np.float32)
    for gi, win in enumerate((2, 4, 8, 16)):
        for (a, Tn) in ((0, C), (C, S)):
            t = np.arange(Tn)
            lo = np.clip(t - win // 2, 0, Tn)
            hi = np.clip(t - win // 2 + win, 0, Tn)
            pi[gi, a:a + Tn] = 1.0 / (hi - lo)
    m['pool_inv'] = pi
    m['moe_r'] = np.ascontiguousarray(np.concatenate([inp['moe_router_group'], inp['moe_router_expert']], -1))
    m['moe_wg'] = inp['moe_w_gate']
    m['moe_wu'] = inp['moe_w_up']
    m['moe_wd'] = inp['moe_w_down']
    m['od_w_in'] = inp['od_w_in'][0]
    m['od_w_out'] = inp['od_w_out'][0]
    m['dlam'] = np.ascontiguousarray(inp['diff_lambda'][0].reshape(1, 256))
    m['sublnT'] = np.ascontiguousarray(inp['diff_subln'][0].reshape(128, 1))
    m['retgT'] = fm(inp['ret_norm'][0])
    m.update(host_consts())
    m.update(host_consts_l1(S))
    return m


def kernel(**inp):
    inp = {k: np.asarray(v) for k, v in inp.items()}
    S, C = inp['x'].shape[1], inp['ctx'].shape[1]
    B = inp['x'].shape[0]
    P, _ = build(S, C)
    nc = P.build()
    in_maps = [make_inputs(b, S, C, inp) for b in range(B)]
    names = set()
    res = run_bass_kernel_spmd(nc, in_maps, core_ids=list(range(B)))
    return np.stack([np.asarray(r["out"], np.float32) for r in res.results], 0)
```

```python
import math
import numpy as np
from contextlib import ExitStack
import concourse.bass as bass
import concourse.mybir as mybir
from concourse.bass_utils import run_bass_kernel_spmd

F32 = mybir.dt.float32
BF16 = mybir.dt.bfloat16
ALU = mybir.AluOpType
AF = mybir.ActivationFunctionType
AX = mybir.AxisListType

ENGS = ['pe', 'act', 'dve', 'pool', 'sp']
DMAQ = ['sp', 'pool', 'act']


def _prod(s):
    r = 1
    for v in s:
        r *= v
    return r


class T:
    __slots__ = ('ap', 'lw', 'rd')

    def __init__(self, ap):
        self.ap = ap
        self.lw = None
        self.rd = {}


class Prog:
    def __init__(self, arena_words=50000, n_dma_sems=8):
        self.nc = bass.Bass("TRN2", target_bir_lowering=False)
        self.es = ExitStack()
        self.ops = {e: [] for e in ENGS}
        self.known = {e: {} for e in ENGS}
        self.pending = {e: [] for e in ENGS}
        self.n_dma_sems = n_dma_sems
        self.dma_rr = {q: 0 for q in DMAQ}
        self.dma_cum = {}
        self.out_tokens = []
        self.nuid = 0
        self.aw = arena_words
        self.arena = self.es.enter_context(self.nc.sbuf_tensor("arena", [128, arena_words], F32))
        self.top = 0
        self.PS = [T(self.es.enter_context(self.nc.psum_tensor(f"psb{i}", [128, 512], F32))[:, :])
                   for i in range(8)]
        self.ps_rr = {'a': 0, 'b': 0, 'c': 0, 'r': 0}
        self.ps_groups = {'a': [0, 1, 2, 3], 'b': [4, 5], 'c': [4, 5, 6, 7], 'r': [0, 1, 2, 3, 7]}

    def psum(self, g='a'):
        lst = self.ps_groups[g]
        i = self.ps_rr[g]
        self.ps_rr[g] = (i + 1) % len(lst)
        return self.PS[lst[i]]

    def alloc(self, shape, dt=F32):
        shape = list(shape)
        esz = 4 if dt == F32 else 2
        nb = _prod(shape[1:]) * esz
        nw = (nb + 3) // 4
        assert self.top + nw <= self.aw, f"arena overflow {self.top}+{nw}>{self.aw}"
        ap = self.arena[0:shape[0], self.top:self.top + nw]
        self.top += nw
        if dt != F32:
            ap = ap.bitcast(dt)
            ap = ap[:, 0:_prod(shape[1:])]
        if len(shape) > 2:
            names = "abcdefg"[:len(shape) - 1]
            kw = {names[i]: shape[i + 1] for i in range(len(shape) - 2)}
            ap = ap.rearrange("p (" + " ".join(names) + ") -> p " + " ".join(names), **kw)
        return T(ap)

    def mark(self):
        return self.top

    def release(self, m):
        self.barrier()
        self.top = m

    def dram(self, name, shape, dt, kind="Internal"):
        return self.nc.dram_tensor(name, list(shape), dt, kind=kind).ap()

    def barrier(self):
        toks = []
        for f in ENGS:
            if len(self.ops[f]) > 0:
                toks.append(('c', f, len(self.ops[f])))
        for skey, cum in self.dma_cum.items():
            toks.append(('d', skey, cum))
        for e in ENGS:
            self.pending[e] = list(toks)

    def _add_wait(self, e, waits, tok):
        if tok is None:
            return
        kind, key, val = tok
        if kind == 'c' and key == e:
            if e == 'pe':
                return
            if val > len(self.ops[e]):
                return
        kk = (kind, key)
        if self.known[e].get(kk, 0) >= val:
            return
        self.known[e][kk] = val
        waits[kk] = max(waits.get(kk, 0), val)

    def _deps(self, e, reads, writes):
        waits = {}
        if self.pending[e]:
            for tok in self.pending[e]:
                self._add_wait(e, waits, tok)
            self.pending[e] = []
        for t in reads:
            self._add_wait(e, waits, t.lw)
        for t in writes:
            self._add_wait(e, waits, t.lw)
            for tok in t.rd.values():
                self._add_wait(e, waits, tok)
        return waits

    def op(self, e, fn, reads=(), writes=()):
        waits = self._deps(e, reads, writes)
        idx = len(self.ops[e]) + 1
        tok = ('c', e, idx)
        self.ops[e].append(dict(fn=fn, waits=waits, inc=None, flag=False))
        for t in reads:
            t.rd[e] = tok
        for t in writes:
            t.lw = tok
            t.rd = {}
        return tok

    def dma(self, q, out_ap, in_ap, reads=(), writes=(), is_output=False, **kw):
        waits = self._deps(q, reads, writes)
        si = self.dma_rr[q]
        self.dma_rr[q] = (si + 1) % self.n_dma_sems
        skey = (q, si)
        prev = self.dma_cum.get(skey, 0)
        if prev > 0:
            self._add_wait(q, waits, ('d', skey, prev))
        val = prev + 16
        self.dma_cum[skey] = val
        tok = ('d', skey, val)

        def fn(eng, out_ap=out_ap, in_ap=in_ap, kw=kw):
            return eng.dma_start(out=out_ap, in_=in_ap, **kw)
        self.ops[q].append(dict(fn=fn, waits=waits, inc=(skey, 16), flag=True))
        for t in reads:
            t.rd[('dma', skey)] = tok
        for t in writes:
            t.lw = tok
            t.rd = {}
        if is_output:
            self.out_tokens.append(tok)
        return tok

    def build(self):
        nc = self.nc
        waits = {}
        for tok in self.out_tokens:
            self._add_wait('sp', waits, tok)
        self.ops['sp'].append(dict(fn=None, waits=waits, inc=None, flag=False))
        for e in ENGS:
            for o in self.ops[e]:
                for (kind, key), val in o['waits'].items():
                    if kind == 'c':
                        self.ops[key][val - 1]['flag'] = True
        rank = {}
        for e in ENGS:
            r = 0
            rk = []
            for o in self.ops[e]:
                if o['inc'] is None and o['flag']:
                    r += 1
                rk.append(r)
            rank[e] = rk
        csem = {e: self.es.enter_context(nc.semaphore(f"c_{e}")) for e in ENGS}
        dsem = {}
        for q in DMAQ:
            for i in range(self.n_dma_sems):
                if (q, i) in self.dma_cum:
                    dsem[(q, i)] = self.es.enter_context(nc.semaphore(f"d_{q}{i}"))
        block = self.es.enter_context(nc.Block())
        engobj = {'pe': block.tensor, 'act': block.scalar, 'dve': block.vector,
                  'pool': block.gpsimd, 'sp': block.sync}

        def mk(e):
            def body(eng):
                for o in self.ops[e]:
                    for (kind, key), val in o['waits'].items():
                        if kind == 'c':
                            eng.wait_ge(csem[key], rank[key][val - 1])
                        else:
                            eng.wait_ge(dsem[key], val)
                    if o['fn'] is None:
                        continue
                    ins = o['fn'](eng)
                    if o['inc'] is not None:
                        ins.then_inc(dsem[o['inc'][0]], 16)
                    elif o['flag']:
                        ins.then_inc(csem[e], 1)
            return body
        for e in ENGS:
            engobj[e](mk(e))
        self.es.close()
        return nc

    def mm(self, out, lhsT, rhs, start=True, stop=True, reads=(), writes=(), **kw):
        def fn(eng):
            return eng.matmul(out, lhsT, rhs, start=start, stop=stop, **kw)
        return self.op('pe', fn, reads, writes)

    def tr(self, out, in_, ident, reads=(), writes=()):
        def fn(eng):
            return eng.transpose(out, in_, ident)
        return self.op('pe', fn, reads, writes)

    def act(self, out, in_, func, reads=(), writes=(), **kw):
        def fn(e):
            return e.activation(out=out, in_=in_, func=func, **kw)
        return self.op('act', fn, reads, writes)

    def tt(self, e, out, in0, in1, op, reads=(), writes=()):
        def fn(eng):
            return eng.tensor_tensor(out=out, in0=in0, in1=in1, op=op)
        return self.op(e, fn, reads, writes)

    def ts(self, e, out, in0, s1, s2, op0, op1=None, reads=(), writes=()):
        def fn(eng):
            if op1 is None:
                return eng.tensor_scalar(out=out, in0=in0, scalar1=s1, scalar2=None, op0=op0)
            return eng.tensor_scalar(out=out, in0=in0, scalar1=s1, scalar2=s2, op0=op0, op1=op1)
        return self.op(e, fn, reads, writes)

    def stt(self, e, out, in0, scalar, in1, op0, op1, reads=(), writes=()):
        def fn(eng):
            return eng.scalar_tensor_tensor(out=out, in0=in0, scalar=scalar, in1=in1, op0=op0, op1=op1)
        return self.op(e, fn, reads, writes)

    def cp(self, e, out, in_, reads=(), writes=()):
        if e == 'act':
            def fn(eng):
                return eng.copy(out=out, in_=in_)
        else:
            def fn(eng):
                return eng.tensor_copy(out=out, in_=in_)
        return self.op(e, fn, reads, writes)

    def memset(self, e, ap, val, writes=()):
        def fn(eng):
            return eng.memset(ap, val)
        return self.op(e, fn, (), writes)


D = 1024
KD = 8
W_DECAY_SCALE = 0.606531
EV_COLS = 2304
OD_COLS = 3072


def host_consts():
    r = np.arange(128)
    Us = (r[:, None] < r[None, :]).astype(np.float32)
    Ui = (r[:, None] <= r[None, :]).astype(np.float32)
    Ls = (r[:, None] > r[None, :]).astype(np.float32)
    Li = (r[:, None] >= r[None, :]).astype(np.float32)
    blk = np.zeros((128, 128), np.float32)
    blk[:64, :64] = 1
    blk[64:, 64:] = 1
    c = {}
    c['ident'] = np.eye(128, dtype=np.float32)
    c['blk64'] = blk
    rm = np.zeros((2, 128, 896), np.float32)
    for d, (ss, si, tsm) in enumerate([(Us, Ui, Ls), (Ls, Li, Us)]):
        rm[d, :, 0:128] = -ss
        rm[d, :, 128:256] = -si
        rm[d, :, 256:384] = ss
        rm[d, :, 384:512] = si
        rm[d, :, 512:640] = -tsm
        rm[d, :, 640:768] = si
        rm[d, :, 768:896] = ss
    c['rmask'] = rm
    return c


def host_consts_l1(S):
    c = {}
    p = np.arange(128)
    blk32 = p % 32
    partner = np.where(blk32 < 16, p + 16, p - 16)
    perm = np.zeros((128, 128), np.float32)
    perm[partner, p] = 1.0
    c['rope_perm'] = perm
    t = np.arange(S)
    row = (t // 64).astype(np.float32)
    col = (t % 64).astype(np.float32)
    b64 = p % 64
    sub = b64 // 32
    j = (b64 % 16).astype(np.float32)
    inv = (10000.0 ** (-j / 16.0)).astype(np.float32)
    pos = np.where(sub[:, None] == 0, row[None, :], col[None, :]).astype(np.float32)
    ang = (pos * inv[:, None]).astype(np.float32)
    sgn = np.where(blk32 < 16, -1.0, 1.0).astype(np.float32)
    c['ropeC'] = np.cos(ang).astype(np.float32)
    c['ropeS'] = (np.sin(ang) * sgn[:, None]).astype(np.float32)
    lgf = np.log(1.0 - 2.0 ** (-5.0 - np.arange(4, dtype=np.float64)))
    r = np.arange(128, dtype=np.float64)
    retD = np.zeros((8, 128, 128), np.float64)
    retq = np.zeros((8, 128), np.float64)
    retk = np.zeros((128, 8), np.float64)
    for h in range(4):
        for d in range(2):
            lg = lgf[h] if d == 0 else lgf[3 - h]
            hd = h * 2 + d
            s_, i_ = r[:, None], r[None, :]
            if d == 0:
                retD[hd] = np.where(i_ >= s_, np.exp(lg * np.maximum(i_ - s_, 0)), 0.0)
                retq[hd] = np.exp(lg * (r + 1))
                retk[:, hd] = np.exp(lg * (127 - r))
            else:
                retD[hd] = np.where(s_ >= i_, np.exp(lg * np.maximum(s_ - i_, 0)), 0.0)
                retq[hd] = np.exp(lg * (128 - r))
                retk[:, hd] = np.exp(lg * r)
    c['retD'] = retD.astype(np.float32)
    c['retq'] = retq.astype(np.float32)
    c['retk'] = retk.astype(np.float32)
    return c


def build(S, C, dbg=False, RW_DT=(BF16, F32, BF16), STOP_AFTER='all', RW_STOP=99):
    P = Prog()
    NT = C + S
    NCH = NT // 128
    CCH = C // 128
    tiles = []
    for base, ln, isc in ((0, C, 1), (C, S, 0)):
        o = 0
        while o < ln:
            l = min(512, ln - o)
            tiles.append((base + o, l, isc))
            o += l
    IN = lambda n, s: P.dram(n, s, F32, "ExternalInput")
    xin = IN("xin", [NT, D])
    cT = IN("cT", [128, KD, 2])
    ada_w = IN("ada_w", [2, D, 6 * D])
    ada_bT = IN("ada_bT", [128, 2, 48])
    normT = IN("normT", [128, 5, KD])
    ev_w_in = IN("ev_w_in", [D, EV_COLS])
    ev_w_out = IN("ev_w_out", [D, D])
    shiftT = IN("shiftT", [128, 12, 3])
    rvec = IN("rvec", [128, 9, 4])
    lora_up = IN("lora_up", [128, 2, 512])
    g_up = IN("g_up", [128, 512])
    pool_w = IN("pool_w", [4, 128, 128])
    pool_scT = IN("pool_scT", [128, 4])
    pool_inv = IN("pool_inv", [4, NT])
    moe_r = IN("moe_r", [2, D, 36])
    moe_wg = IN("moe_wg", [2, 32, D, 512])
    moe_wu = IN("moe_wu", [2, 32, D, 512])
    moe_wd = IN("moe_wd", [2, 32, 512, D])
    od_w_in = IN("od_w_in", [D, OD_COLS])
    od_w_out = IN("od_w_out", [D, D])
    dlam = IN("dlam", [1, 256])
    sublnT = IN("sublnT", [128, 1])
    retgT = IN("retgT", [128, 4])
    rope_perm = IN("rope_perm", [128, 128])
    ropeC = IN("ropeC", [128, S])
    ropeS = IN("ropeS", [128, S])
    retD = IN("retD", [8, 128, 128])
    retq = IN("retq", [8, 128])
    retk = IN("retk", [128, 8])
    cident = IN("ident", [128, 128])
    cblk = IN("blk64", [128, 128])
    crmask = IN("rmask", [2, 128, 896])
    out = P.dram("out", [S, D], F32, "ExternalOutput")
    xTs = P.dram("xTs", [KD, 128, NT], F32)
    pTs = P.dram("pTs", [24, 128, NT], F32, "ExternalOutput" if dbg else "Internal")
    ymTs = P.dram("ymTs", [KD, 128, NT], BF16, "ExternalOutput" if dbg else "Internal")
    dbgs = {}

    def tap(name, t, shape):
        if dbg:
            d = P.dram("dbg_" + name, shape, F32, "ExternalOutput")
            P.dma('sp', d, t.ap, reads=[t], is_output=True)

    ident = P.alloc([128, 128]); P.dma('sp', ident.ap, cident, writes=[ident])
    identb = P.alloc([128, 128], BF16); P.dma('pool', identb.ap, cident, writes=[identb])
    blk = P.alloc([128, 128]); P.dma('sp', blk.ap, cblk, writes=[blk])
    onesD = P.alloc([128, 128]); P.memset('pool', onesD.ap, 1.0 / D, writes=[onesD])
    normv = P.alloc([128, 5, KD]); P.dma('sp', normv.ap, normT, writes=[normv])
    mod = P.alloc([128, 2, 48, 2])
    m0 = P.mark()
    sc = P.alloc([128, KD, 2]); P.dma('sp', sc.ap, cT, writes=[sc])
    P.act(sc.ap, sc.ap, AF.Silu, reads=[sc], writes=[sc])
    adab = P.alloc([128, 2, 48]); P.dma('sp', adab.ap, ada_bT, writes=[adab])
    wbuf = [P.alloc([128, KD, 1024]) for _ in range(1)]
    for li in range(2):
        for blkc in range(6):
            wb = wbuf[0]
            for k in range(KD):
                P.dma('sp' if k % 2 == 0 else 'act', wb.ap[:, k, :],
                      ada_w[li, k * 128:(k + 1) * 128, blkc * 1024:(blkc + 1) * 1024], writes=[wb])
            for cc in range(8):
                ps = P.psum('a')
                for k in range(KD):
                    P.mm(ps.ap[:, 0:2], wb.ap[:, k, cc * 128:(cc + 1) * 128], sc.ap[:, k, :],
                         start=(k == 0), stop=(k == KD - 1), reads=[wb, sc], writes=[ps])
                j = blkc * 8 + cc
                P.stt('dve', mod.ap[:, li, j, :], ps.ap[:, 0:2], 1.0,
                      adab.ap[:, li, j:j + 1].to_broadcast([128, 2]), ALU.mult, ALU.add,
                      reads=[ps, adab], writes=[mod])
    P.release(m0)
    AB = P.alloc([128, 2, 2, 2, KD, 2])
    for li in range(2):
        for sub in range(2):
            shc = 24 * sub
            scc = 24 * sub + 8
            nidx = li if sub == 0 else 2 + li
            for w in range(2):
                P.ts('dve', AB.ap[:, li, sub, 0, :, w], mod.ap[:, li, scc:scc + 8, w], 1.0, None, ALU.add,
                     reads=[mod], writes=[AB])
                P.tt('dve', AB.ap[:, li, sub, 0, :, w], AB.ap[:, li, sub, 0, :, w], normv.ap[:, nidx, :], ALU.mult,
                     reads=[AB, normv], writes=[AB])
                P.cp('dve', AB.ap[:, li, sub, 1, :, w], mod.ap[:, li, shc:shc + 8, w], reads=[mod], writes=[AB])
    if dbg:
        tap("mod", mod, [128, 2, 48, 2])

    def norm_mod(xT, TT, li, sub, w, hb, sq, rs, final=False):
        P.act(sq.ap[:, :, 0:TT], xT.ap[:, :, 0:TT], AF.Square, reads=[xT], writes=[sq])
        ps = P.psum('a')
        for k in range(KD):
            P.mm(ps.ap[:, 0:TT], onesD.ap, sq.ap[:, k, 0:TT], start=(k == 0), stop=(k == KD - 1),
                 reads=[onesD, sq], writes=[ps])
        P.ts('dve', rs.ap[:, 0:TT], ps.ap[:, 0:TT], 1e-6, None, ALU.add, reads=[ps], writes=[rs])
        P.act(rs.ap[:, 0:TT], rs.ap[:, 0:TT], AF.Ln, reads=[rs], writes=[rs])
        P.act(rs.ap[:, 0:TT], rs.ap[:, 0:TT], AF.Exp, reads=[rs], writes=[rs], scale=-0.5)
        P.tt('dve', sq.ap[:, :, 0:TT], xT.ap[:, :, 0:TT], rs.ap[:, None, 0:TT].to_broadcast([128, KD, TT]), ALU.mult,
             reads=[xT, rs], writes=[sq])
        for k in range(KD):
            if final:
                P.ts('dve' if k % 2 else 'pool', hb.ap[:, k, 0:TT], sq.ap[:, k, 0:TT], normv.ap[:, 4, k:k + 1], None, ALU.mult,
                     reads=[sq, normv], writes=[hb])
            else:
                P.ts('dve' if k % 2 else 'pool', hb.ap[:, k, 0:TT], sq.ap[:, k, 0:TT], AB.ap[:, li, sub, 0, k, w:w + 1],
                     AB.ap[:, li, sub, 1, k, w:w + 1], ALU.mult, ALU.add, reads=[sq, AB], writes=[hb])

    def load_w_bf(dst, src, K):
        for k in range(K):
            P.dma('pool', dst.ap[:, k, :], src[k * 128:(k + 1) * 128, :], writes=[dst])

    def phase_inproj(li, w_in_dram, ncols, first):
        m = P.mark()
        NCC = ncols // 128
        Win = P.alloc([128, KD, ncols], BF16)
        load_w_bf(Win, w_in_dram, KD)
        xtok = [P.alloc([128, D]) for _ in range(2)]
        xT = [P.alloc([128, KD, 512]) for _ in range(2)]
        hb = [P.alloc([128, KD, 512], BF16) for _ in range(2)]
        sq = P.alloc([128, KD, 512]); rs = P.alloc([128, 512])
        pst = [P.alloc([128, 6, 512]) for _ in range(2)]
        for ti, (t0, TT, isc) in enumerate(tiles):
            xt = xT[ti % 2]
            if first:
                for b in range(TT // 128):
                    xk = xtok[b % 2]
                    P.dma('sp', xk.ap, xin[t0 + b * 128:t0 + (b + 1) * 128, :], writes=[xk])
                    for half in range(2):
                        ps = P.psum('a')
                        for j in range(4):
                            k = half * 4 + j
                            P.tr(ps.ap[:, j * 128:(j + 1) * 128], xk.ap[:, k * 128:(k + 1) * 128], ident.ap,
                                 reads=[xk, ident], writes=[ps])
                        P.cp('dve' if half else 'act', xt.ap[:, half * 4:half * 4 + 4, b * 128:(b + 1) * 128],
                             ps.ap.rearrange("p (a b) -> p a b", a=4), reads=[ps], writes=[xt])
                for k in range(KD):
                    P.dma('sp', xTs[k, :, t0:t0 + TT], xt.ap[:, k, 0:TT], reads=[xt])
            else:
                for k in range(KD):
                    P.dma('sp', xt.ap[:, k, 0:TT], xTs[k, :, t0:t0 + TT], writes=[xt])
            h = hb[ti % 2]
            norm_mod(xt, TT, li, 0, isc, h, sq, rs)
            for g in range(NCC // 6):
                st = pst[g % 2]
                for c6 in range(6):
                    cc = g * 6 + c6
                    ps = P.psum('a')
                    for k in range(KD):
                        P.mm(ps.ap[:, 0:TT], Win.ap[:, k, cc * 128:(cc + 1) * 128], h.ap[:, k, 0:TT],
                             start=(k == 0), stop=(k == KD - 1), reads=[Win, h], writes=[ps])
                    P.cp('act' if c6 % 2 else 'dve', st.ap[:, c6, 0:TT], ps.ap[:, 0:TT], reads=[ps], writes=[st])
                P.dma('sp', pTs[g * 6:(g + 1) * 6, :, t0:t0 + TT].rearrange("c p t -> p c t"), st.ap[:, :, 0:TT],
                      reads=[st])
        P.release(m)


    def phase_moe(li, lat_only):
        m = P.mark()
        tl = [t for t in tiles if not (lat_only and t[2])]
        xres = P.alloc([128, KD, NT])
        hfT = P.alloc([128, KD, NT], BF16)
        gT = P.alloc([32, NT])
        wr = P.alloc([128, KD, 36])
        P.dma('sp', wr.ap, moe_r[li].rearrange("(k p) n -> p k n", p=128), writes=[wr])
        m2 = P.mark()
        sq = P.alloc([128, KD, 512]); rs = P.alloc([128, 512])
        lg = P.alloc([128, 36]); oh = P.alloc([128, 4]); st_ = P.alloc([128, 16]); les = P.alloc([128, 8])
        mk1 = P.alloc([128, 8]); mk2 = P.alloc([128, 8]); g8 = P.alloc([128, 8]); g32 = P.alloc([128, 4, 8])
        ex4 = P.alloc([128, 4])
        for (t0, TT, isc) in tl:
            xt = T(xres.ap[:, :, t0:t0 + TT]); hb = T(hfT.ap[:, :, t0:t0 + TT])
            for k in range(KD):
                P.dma('sp' if k % 2 else 'act', xt.ap[:, k, :], xTs[k, :, t0:t0 + TT], writes=[xt, xres])
            norm_mod(xt, TT, li, 1, isc, hb, sq, rs)
            for k in range(KD):
                P.ts('dve', sq.ap[:, k, 0:TT], sq.ap[:, k, 0:TT], AB.ap[:, li, 1, 0, k, isc:isc + 1],
                     AB.ap[:, li, 1, 1, k, isc:isc + 1], ALU.mult, ALU.add, reads=[sq, AB], writes=[sq])
            for b in range(TT // 128):
                bs = slice(b * 128, (b + 1) * 128)
                ps = P.psum('a')
                for k in range(KD):
                    P.mm(ps.ap[:, 0:36], sq.ap[:, k, bs], wr.ap[:, k, :], start=(k == 0), stop=(k == KD - 1),
                         reads=[sq, wr], writes=[ps])
                P.cp('dve', lg.ap, ps.ap[:, 0:36], reads=[ps], writes=[lg])
                def red(out, in_, op):
                    return P.op('dve', lambda e: e.tensor_reduce(out=out, in_=in_, axis=AX.X, op=op), [lg, les, ex4, st_], [st_])
                P.op('dve', lambda e: e.tensor_reduce(out=st_.ap[:, 0:1], in_=lg.ap[:, 0:4], axis=AX.X, op=ALU.max), [lg], [st_])
                P.ts('dve', oh.ap, lg.ap[:, 0:4], st_.ap[:, 0:1], None, ALU.is_equal, reads=[lg, st_], writes=[oh])
                P.ts('dve', st_.ap[:, 1:2], st_.ap[:, 0:1], -1.0, None, ALU.mult, reads=[st_], writes=[st_])
                P.act(ex4.ap, lg.ap[:, 0:4], AF.Exp, reads=[lg, st_], writes=[ex4], bias=st_.ap[:, 1:2])
                P.op('dve', lambda e: e.tensor_reduce(out=st_.ap[:, 2:3], in_=ex4.ap, axis=AX.X, op=ALU.add), [ex4], [st_])
                P.op('dve', lambda e: e.reciprocal(out=st_.ap[:, 3:4], in_=st_.ap[:, 2:3]), [st_], [st_])
                P.ts('dve', les.ap, lg.ap[:, 4:12], oh.ap[:, 0:1], None, ALU.mult, reads=[lg, oh], writes=[les])
                for g in range(1, 4):
                    P.stt('dve', les.ap, lg.ap[:, 4 + 8 * g:12 + 8 * g], oh.ap[:, g:g + 1], les.ap, ALU.mult, ALU.add,
                          reads=[lg, oh, les], writes=[les])
                P.op('dve', lambda e: e.tensor_reduce(out=st_.ap[:, 4:5], in_=les.ap, axis=AX.X, op=ALU.max), [les], [st_])
                P.ts('dve', mk1.ap, les.ap, st_.ap[:, 4:5], None, ALU.is_equal, reads=[les, st_], writes=[mk1])
                P.stt('dve', g8.ap, mk1.ap, -1e30, les.ap, ALU.mult, ALU.add, reads=[mk1, les], writes=[g8])
                P.op('dve', lambda e: e.tensor_reduce(out=st_.ap[:, 5:6], in_=g8.ap, axis=AX.X, op=ALU.max), [g8], [st_])
                P.ts('dve', mk2.ap, g8.ap, st_.ap[:, 5:6], None, ALU.is_equal, reads=[g8, st_], writes=[mk2])
                P.tt('dve', st_.ap[:, 6:7], st_.ap[:, 5:6], st_.ap[:, 4:5], ALU.subtract, reads=[st_], writes=[st_])
                P.act(st_.ap[:, 7:8], st_.ap[:, 6:7], AF.Exp, reads=[st_], writes=[st_])
                P.ts('dve', st_.ap[:, 8:9], st_.ap[:, 7:8], 1.0, None, ALU.add, reads=[st_], writes=[st_])
                P.op('dve', lambda e: e.reciprocal(out=st_.ap[:, 9:10], in_=st_.ap[:, 8:9]), [st_], [st_])
                P.tt('dve', st_.ap[:, 10:11], st_.ap[:, 9:10], st_.ap[:, 3:4], ALU.mult, reads=[st_], writes=[st_])
                P.tt('dve', st_.ap[:, 11:12], st_.ap[:, 10:11], st_.ap[:, 7:8], ALU.mult, reads=[st_], writes=[st_])
                P.ts('dve', g8.ap, mk1.ap, st_.ap[:, 10:11], None, ALU.mult, reads=[mk1, st_], writes=[g8])
                P.stt('dve', g8.ap, mk2.ap, st_.ap[:, 11:12], g8.ap, ALU.mult, ALU.add, reads=[mk2, st_, g8], writes=[g8])
                for g in range(4):
                    P.ts('dve', g32.ap[:, g, :], g8.ap, oh.ap[:, g:g + 1], None, ALU.mult, reads=[g8, oh], writes=[g32])
                pt = P.psum('a')
                P.tr(pt.ap[0:32, 0:128], g32.ap.rearrange("p a b -> p (a b)"), ident.ap, reads=[g32, ident], writes=[pt])
                P.cp('act', gT.ap[:, t0 + b * 128:t0 + (b + 1) * 128], pt.ap[0:32, 0:128], reads=[pt], writes=[gT])
        P.release(m2)
        if dbg:
            tap(f"gT{li}", gT, [32, NT])
        Wg = [P.alloc([128, KD, 512], BF16) for _ in range(2)]
        Wu = [P.alloc([128, KD, 512], BF16) for _ in range(2)]
        Wd = [P.alloc([128, 4, D], BF16) for _ in range(2)]
        selt = [P.alloc([32, 128]) for _ in range(2)]
        gbc = [P.alloc([128, 512]) for _ in range(2)]
        sgl = [P.alloc([128, 512]) for _ in range(2)]
        a1 = [P.alloc([128, 512]) for _ in range(2)]
        actT = [P.alloc([128, 4, 512], BF16) for _ in range(2)]
        it = 0
        pend = [None]
        def load_gu(e):
            wg = Wg[e % 2]; wu = Wu[e % 2]
            for k in range(KD):
                P.dma('pool', wg.ap[:, k, :], moe_wg[li, e, k * 128:(k + 1) * 128, :], writes=[wg])
                P.dma('pool', wu.ap[:, k, :], moe_wu[li, e, k * 128:(k + 1) * 128, :], writes=[wu])

        def load_d(e):
            wd = Wd[e % 2]
            for k in range(4):
                P.dma('pool', wd.ap[:, k, :], moe_wd[li, e, k * 128:(k + 1) * 128, :], writes=[wd])

        load_gu(0)
        load_d(0)
        for e in range(32):
            wg = Wg[e % 2]; wu = Wu[e % 2]; wd = Wd[e % 2]; se = selt[e % 2]
            P.cp('pool', se.ap, ident.ap[0:32, e:e + 1].to_broadcast([32, 128]), reads=[ident], writes=[se])
            if e + 1 < 32:
                load_gu(e + 1)
            for tix, (t0, TT, isc) in enumerate(tl):
                it += 1
                gb = gbc[it % 2]; at = actT[it % 2]
                pg_ = P.psum('b')
                P.mm(pg_.ap[:, 0:TT], se.ap, gT.ap[:, t0:t0 + TT], reads=[se, gT], writes=[pg_])
                P.cp('act', gb.ap[:, 0:TT], pg_.ap[:, 0:TT], reads=[pg_], writes=[gb])
                for fc in range(4):
                    fs = slice(fc * 128, (fc + 1) * 128)
                    pg = P.psum('a'); pu = P.psum('a')
                    for k in range(KD):
                        P.mm(pg.ap[:, 0:TT], wg.ap[:, k, fs], hfT.ap[:, k, t0:t0 + TT], start=(k == 0), stop=(k == KD - 1),
                             reads=[wg, hfT], writes=[pg])
                    for k in range(KD):
                        P.mm(pu.ap[:, 0:TT], wu.ap[:, k, fs], hfT.ap[:, k, t0:t0 + TT], start=(k == 0), stop=(k == KD - 1),
                             reads=[wu, hfT], writes=[pu])
                    sg_ = sgl[fc % 2]; a_ = a1[fc % 2]
                    P.act(sg_.ap[:, 0:TT], pg.ap[:, 0:TT], AF.Silu, reads=[pg], writes=[sg_])
                    P.tt('dve', a_.ap[:, 0:TT], pu.ap[:, 0:TT], sg_.ap[:, 0:TT], ALU.mult, reads=[pu, sg_], writes=[a_])
                    P.tt('pool', at.ap[:, fc, 0:TT], a_.ap[:, 0:TT], gb.ap[:, 0:TT], ALU.mult, reads=[a_, gb], writes=[at])
                def down(wd=wd, at=at, t0=t0, TT=TT, isc=isc):
                    for dc in range(KD):
                        po = P.psum('c')
                        for fc in range(4):
                            P.mm(po.ap[:, 0:TT], wd.ap[:, fc, dc * 128:(dc + 1) * 128], at.ap[:, fc, 0:TT],
                                 start=(fc == 0), stop=(fc == 3), reads=[wd, at], writes=[po])
                        P.stt('dve', xres.ap[:, dc, t0:t0 + TT], po.ap[:, 0:TT], mod.ap[:, li, 40 + dc, isc:isc + 1],
                              xres.ap[:, dc, t0:t0 + TT], ALU.mult, ALU.add, reads=[po, mod, xres], writes=[xres])
                if pend[0] is not None:
                    pend[0]()
                pend[0] = down
                if tix == 0 and e + 1 < 32:
                    load_d(e + 1)
        pend[0]()
        for (t0, TT, isc) in tl:
            for k in range(KD):
                P.dma('sp', xTs[k, :, t0:t0 + TT], xres.ap[:, k, t0:t0 + TT], reads=[xres])
        P.release(m)


    LAM_INIT = 0.8 - 0.6 * math.exp(-0.3 * 1)
    RET_G128 = []
    for h_ in range(4):
        lgf = [math.log(1.0 - 2.0 ** (-5.0 - j)) for j in range(4)]
        RET_G128.append((math.exp(lgf[h_] * 128), math.exp(lgf[3 - h_] * 128)))

    def phase_l1mix():
        m = P.mark()
        LT = [(t0 - C, TT) for (t0, TT, isc) in tiles if not isc]
        perm = P.alloc([128, 128]); P.dma('sp', perm.ap, rope_perm, writes=[perm])
        rc_ = P.alloc([128, S]); P.dma('sp', rc_.ap, ropeC, writes=[rc_])
        rs_ = P.alloc([128, S]); P.dma('sp', rs_.ap, ropeS, writes=[rs_])
        ones128 = P.alloc([128, 128]); P.memset('pool', ones128.ap, 1.0 / 128, writes=[ones128])
        onesb = P.alloc([128, 128], BF16); P.memset('pool', onesb.ap, 1.0, writes=[onesb])
        subg = P.alloc([128, 1]); P.dma('sp', subg.ap, sublnT, writes=[subg])
        P.ts('dve', subg.ap, subg.ap, 1.0 - LAM_INIT, None, ALU.mult, reads=[subg], writes=[subg])
        rgv = P.alloc([128, 4]); P.dma('sp', rgv.ap, retgT, writes=[rgv])
        rkv = P.alloc([128, 8]); P.dma('sp', rkv.ap, retk, writes=[rkv])
        dl = P.alloc([1, 4, 64]); P.dma('sp', dl.ap, dlam.rearrange("o (a b) -> o a b", a=4), writes=[dl])
        pr2 = P.alloc([1, 2, 64]); s2 = P.alloc([1, 4]); nlam = P.alloc([128, 1]); onesr = P.alloc([1, 128])
        P.memset('dve', onesr.ap, 1.0, writes=[onesr])
        P.tt('dve', pr2.ap[:, 0, :], dl.ap[:, 0, :], dl.ap[:, 1, :], ALU.mult, reads=[dl], writes=[pr2])
        P.tt('dve', pr2.ap[:, 1, :], dl.ap[:, 2, :], dl.ap[:, 3, :], ALU.mult, reads=[dl], writes=[pr2])
        P.op('dve', lambda e: e.tensor_reduce(out=s2.ap[:, 0:2], in_=pr2.ap, axis=AX.X, op=ALU.add), [pr2], [s2])
        P.act(s2.ap[:, 0:2], s2.ap[:, 0:2], AF.Exp, reads=[s2], writes=[s2])
        P.tt('dve', s2.ap[:, 2:3], s2.ap[:, 1:2], s2.ap[:, 0:1], ALU.subtract, reads=[s2], writes=[s2])
        P.ts('dve', s2.ap[:, 3:4], s2.ap[:, 2:3], -LAM_INIT, None, ALU.add, reads=[s2], writes=[s2])
        psl = P.psum('a')
        P.mm(psl.ap[:, 0:1], onesr.ap, s2.ap[:, 3:4], reads=[onesr, s2], writes=[psl])
        P.cp('dve', nlam.ap, psl.ap[:, 0:1], reads=[psl], writes=[nlam])
        raw = [P.alloc([128, NT]) for _ in range(2)]
        vT = P.alloc([128, NT])
        kT = P.alloc([128, NT], BF16); qT = P.alloc([128, S], BF16)
        Vtok = P.alloc([128, NCH, 128], BF16)
        t512 = [P.alloc([128, 512]) for _ in range(6)]
        pTb = [P.alloc([128, 512], BF16) for _ in range(3)]
        ymt = [P.alloc([128, 512], BF16) for _ in range(2)]
        cnt = [0]

        def rr(lst):
            cnt[0] += 1
            return lst[cnt[0] % len(lst)]

        def rope(dst, dcol0, src, scol0, scale=None):
            for (l0, TT) in LT:
                ps = P.psum('a')
                P.mm(ps.ap[:, 0:TT], perm.ap, src.ap[:, scol0 + l0:scol0 + l0 + TT], reads=[perm, src], writes=[ps])
                a = rr(t512); b = rr(t512)
                P.tt('dve', a.ap[:, 0:TT], ps.ap[:, 0:TT], rs_.ap[:, l0:l0 + TT], ALU.mult, reads=[ps, rs_], writes=[a])
                P.tt('pool', b.ap[:, 0:TT], src.ap[:, scol0 + l0:scol0 + l0 + TT], rc_.ap[:, l0:l0 + TT], ALU.mult, reads=[src, rc_], writes=[b])
                if scale is None:
                    P.tt('dve', dst.ap[:, dcol0 + l0:dcol0 + l0 + TT], a.ap[:, 0:TT], b.ap[:, 0:TT], ALU.add, reads=[a, b], writes=[dst])
                else:
                    P.tt('dve', a.ap[:, 0:TT], a.ap[:, 0:TT], b.ap[:, 0:TT], ALU.add, reads=[a, b], writes=[a])
                    P.ts('dve', dst.ap[:, dcol0 + l0:dcol0 + l0 + TT], a.ap[:, 0:TT], scale, None, ALU.mult, reads=[a], writes=[dst])

        def make_vtok(vsrc):
            for c4 in range(0, NCH, 4):
                n = min(4, NCH - c4)
                ps = P.psum('a')
                for j in range(n):
                    P.tr(ps.ap[:, j * 128:(j + 1) * 128], vsrc.ap[:, (c4 + j) * 128:(c4 + j + 1) * 128], ident.ap,
                         reads=[vsrc, ident], writes=[ps])
                P.cp('act', Vtok.ap[:, c4:c4 + n, :], ps.ap[:, 0:n * 128].rearrange("p (a b) -> p a b", a=n), reads=[ps], writes=[Vtok])

        def post_norm(o, TT, eps, gain_ap, extra, dst_chunk, l0):
            sq_ = rr(t512)
            P.act(sq_.ap[:, 0:TT], o.ap[:, 0:TT], AF.Square, reads=[o], writes=[sq_])
            ps = P.psum('a')
            P.mm(ps.ap[:, 0:TT], ones128.ap, sq_.ap[:, 0:TT], reads=[ones128, sq_], writes=[ps])
            P.ts('dve', sq_.ap[:, 0:TT], ps.ap[:, 0:TT], eps, None, ALU.add, reads=[ps], writes=[sq_])
            P.act(sq_.ap[:, 0:TT], sq_.ap[:, 0:TT], AF.Ln, reads=[sq_], writes=[sq_])
            P.act(sq_.ap[:, 0:TT], sq_.ap[:, 0:TT], AF.Exp, reads=[sq_], writes=[sq_], scale=-0.5)
            P.stt('dve', sq_.ap[:, 0:TT], o.ap[:, 0:TT], gain_ap, sq_.ap[:, 0:TT], ALU.mult, ALU.mult, reads=[o, sq_, subg, rgv], writes=[sq_])
            ym = rr(ymt)
            if extra is None:
                P.cp('dve', ym.ap[:, 0:TT], sq_.ap[:, 0:TT], reads=[sq_], writes=[ym])
            else:
                P.tt('dve', ym.ap[:, 0:TT], sq_.ap[:, 0:TT], extra, ALU.mult, reads=[sq_, raw[0], raw[1]], writes=[ym])
            P.dma('sp', ymTs[dst_chunk, :, C + l0:C + l0 + TT], ym.ap[:, 0:TT], reads=[ym])

        A1, S1, A2, S2 = P.PS[0], P.PS[1], P.PS[2], P.PS[3]
        for h in range(4):
            P.dma('sp', raw[0].ap, pTs[h], writes=[raw[0]])
            P.dma('act', raw[1].ap[:, 0:S], pTs[14 + h][:, C:NT], writes=[raw[1]])
            P.dma('sp', vT.ap, pTs[4 + h], writes=[vT])
            P.cp('pool', kT.ap[:, 0:C], raw[0].ap[:, 0:C], reads=[raw[0]], writes=[kT])
            rope(kT, C, raw[0], C)
            rope(qT, 0, raw[1], 0)
            make_vtok(vT)
            for (l0, TT) in LT:
                for kc in range(NCH):
                    for br, (Ab, Sb) in enumerate(((A1, S1), (A2, S2))):
                        hsb = slice(br * 64, br * 64 + 64)
                        sc = P.psum('c')
                        P.mm(sc.ap[:, 0:TT], kT.ap[hsb, kc * 128:(kc + 1) * 128], qT.ap[hsb, l0:l0 + TT], reads=[kT, qT], writes=[sc])
                        pt = rr(pTb)
                        P.act(pt.ap[:, 0:TT], sc.ap[:, 0:TT], AF.Exp, reads=[sc], writes=[pt], scale=0.125)
                        P.mm(Ab.ap[:, 0:TT], Vtok.ap[:, kc, :], pt.ap[:, 0:TT], start=(kc == 0), stop=(kc == NCH - 1), reads=[Vtok, pt], writes=[Ab])
                        P.mm(Sb.ap[:, 0:TT], onesb.ap, pt.ap[:, 0:TT], start=(kc == 0), stop=(kc == NCH - 1), reads=[onesb, pt], writes=[Sb])
                r1 = rr(t512); o1 = rr(t512); r2 = rr(t512); o2 = rr(t512)
                P.op('dve', lambda e, r1=r1, TT=TT: e.reciprocal(out=r1.ap[:, 0:TT], in_=S1.ap[:, 0:TT]), [S1], [r1])
                P.tt('dve', o1.ap[:, 0:TT], A1.ap[:, 0:TT], r1.ap[:, 0:TT], ALU.mult, reads=[A1, r1], writes=[o1])
                P.op('dve', lambda e, r2=r2, TT=TT: e.reciprocal(out=r2.ap[:, 0:TT], in_=S2.ap[:, 0:TT]), [S2], [r2])
                P.tt('dve', o2.ap[:, 0:TT], A2.ap[:, 0:TT], r2.ap[:, 0:TT], ALU.mult, reads=[A2, r2], writes=[o2])
                P.stt('dve', o1.ap[:, 0:TT], o2.ap[:, 0:TT], nlam.ap[:, 0:1], o1.ap[:, 0:TT], ALU.mult, ALU.add, reads=[o2, nlam, o1], writes=[o1])
                post_norm(o1, TT, 1e-5, subg.ap[:, 0:1], None, h, l0)
        kTp = kT
        qTp = qT
        ktok = P.alloc([128, NCH, 128], BF16)
        oT = P.alloc([128, S])
        Rf = P.alloc([128, 128]); Rb = P.alloc([128, 128], BF16)
        dm = P.alloc([128, 128]); qrow = P.alloc([128, 128])
        innm = [P.alloc([128, 128], BF16) for _ in range(2)]
        qd = [P.alloc([128, 128], BF16) for _ in range(2)]
        kd = [P.alloc([128, 64], BF16) for _ in range(2)]
        for h in range(4):
            hq = h % 2
            hsq = slice(hq * 64, hq * 64 + 64)
            if hq == 0:
                P.dma('sp', raw[0].ap, pTs[8 + h // 2], writes=[raw[0]])
                P.dma('act', raw[1].ap[:, 0:S], pTs[18 + h // 2][:, C:NT], writes=[raw[1]])
                P.ts('pool', kTp.ap[:, 0:C], raw[0].ap[:, 0:C], 0.125, None, ALU.mult, reads=[raw[0]], writes=[kTp])
                rope(kTp, C, raw[0], C, scale=0.125)
                rope(qTp, 0, raw[1], 0)
                for c4 in range(0, NCH, 4):
                    n = min(4, NCH - c4)
                    ps = P.psum('a'); psb_ = ps.ap.bitcast(BF16)
                    for j in range(n):
                        P.tr(psb_[:, j * 128:(j + 1) * 128], kTp.ap[:, (c4 + j) * 128:(c4 + j + 1) * 128], identb.ap,
                             reads=[kTp, identb], writes=[ps])
                    P.cp('act', ktok.ap[:, c4:c4 + n, :], psb_[:, 0:n * 128].rearrange("p (a b) -> p a b", a=n), reads=[ps], writes=[ktok])
            P.dma('sp', vT.ap, pTs[10 + h], writes=[vT])
            make_vtok(vT)
            for d in range(2):
                hd = h * 2 + d
                g128 = RET_G128[h][d]
                P.dma('sp', dm.ap, retD[hd], writes=[dm])
                P.dma('sp', qrow.ap, retq[hd:hd + 1, :].partition_broadcast(128), writes=[qrow])
                P.memset('dve', Rf.ap, 0.0, writes=[Rf]); P.memset('pool', Rb.ap, 0.0, writes=[Rb])
                order = list(range(0, NCH)) if d == 0 else list(range(CCH - 1, -1, -1)) + list(range(NCH - 1, CCH - 1, -1))
                for oi, c in enumerate(order):
                    ksl = slice(c * 128, (c + 1) * 128)
                    if c >= CCH:
                        i0 = c * 128 - C
                        im = rr(innm); q_ = rr(qd)
                        ps1 = P.psum('c')
                        P.mm(ps1.ap[:, 0:128], kTp.ap[hsq, ksl], qTp.ap[hsq, i0:i0 + 128], reads=[kTp, qTp], writes=[ps1])
                        P.tt('dve', im.ap, ps1.ap[:, 0:128], dm.ap, ALU.mult, reads=[ps1, dm], writes=[im])
                        P.tt('pool', q_.ap[hsq, :], qTp.ap[hsq, i0:i0 + 128], qrow.ap[hsq, :], ALU.mult, reads=[qTp, qrow], writes=[q_])
                        ps2 = P.psum('c')
                        P.mm(ps2.ap[:, 0:128], Vtok.ap[:, c, :], im.ap, start=True, stop=False, reads=[Vtok, im], writes=[ps2])
                        P.mm(ps2.ap[:, 0:128], Rb.ap[hsq, :], q_.ap[hsq, :], start=False, stop=True, reads=[Rb, q_], writes=[ps2])
                        if d == 0:
                            P.cp('act', oT.ap[:, i0:i0 + 128], ps2.ap[:, 0:128], reads=[ps2], writes=[oT])
                        else:
                            P.tt('dve', oT.ap[:, i0:i0 + 128], ps2.ap[:, 0:128], oT.ap[:, i0:i0 + 128], ALU.add, reads=[ps2, oT], writes=[oT])
                    if oi < len(order) - 1:
                        k_ = rr(kd)
                        P.ts('pool', k_.ap, ktok.ap[:, c, hsq], rkv.ap[:, hd:hd + 1], None, ALU.mult, reads=[ktok, rkv], writes=[k_])
                        ps3 = P.psum('c')
                        P.mm(ps3.ap[hsq, 0:128], k_.ap, Vtok.ap[:, c, :], reads=[k_, Vtok], writes=[ps3])
                        P.stt('dve', Rf.ap[hsq, :], Rf.ap[hsq, :], g128, ps3.ap[hsq, 0:128], ALU.mult, ALU.add, reads=[Rf, ps3], writes=[Rf])
                        P.cp('act', Rb.ap[hsq, :], Rf.ap[hsq, :], reads=[Rf], writes=[Rb])
            P.dma('act', raw[1].ap[:, 0:S], pTs[20 + h][:, C:NT], writes=[raw[1]]) if hq == 1 else \
                P.dma('act', raw[0].ap[:, 0:S], pTs[20 + h][:, C:NT], writes=[raw[0]])
            gsrc = raw[1] if hq == 1 else raw[0]
            P.act(gsrc.ap[:, 0:S], gsrc.ap[:, 0:S], AF.Silu, reads=[gsrc], writes=[gsrc])
            for (l0, TT) in LT:
                ot = T(oT.ap[:, l0:l0 + TT])
                ot.lw = oT.lw
                post_norm(ot, TT, 1e-6, rgv.ap[:, h:h + 1], gsrc.ap[:, l0:l0 + TT], 4 + h, l0)
                oT.rd.update(ot.rd)
        P.release(m)

    if STOP_AFTER == 'mod':
        return P, locals()
    phase_inproj(0, ev_w_in, EV_COLS, True)


    def phase_rwkv():
        m = P.mark()
        DIN, DCH, DST = RW_DT[:3]
        DCN = RW_DT[3] if len(RW_DT) > 3 else DCH
        idcn = ident if DCN == F32 else identb
        idch = ident if DCH == F32 else identb
        idin = ident if DIN == F32 else identb
        seqs = [(0, C), (C, NT)]
        lup = P.alloc([128, 2, 512], BF16); P.dma('pool', lup.ap, lora_up, writes=[lup])
        gup = P.alloc([128, 512], BF16); P.dma('pool', gup.ap, g_up, writes=[gup])
        rv = P.alloc([128, 9, 4]); P.dma('sp', rv.ap, rvec, writes=[rv])
        shv = P.alloc([128, 12, 3]); P.dma('sp', shv.ap, shiftT, writes=[shv])
        rmk = P.alloc([128, 2, 896])
        for d in range(2):
            P.dma('sp', rmk.ap[:, d, :], crmask[d], writes=[rmk])
        omka = P.alloc([128, 4])
        P.ts('dve', omka.ap, rv.ap[:, 5, :], -1.0, 1.0, ALU.mult, ALU.add, reads=[rv], writes=[omka])
        tmpA = P.alloc([128, NT]); tmpB = P.alloc([128, NT])
        wdad = P.alloc([128, NT], BF16); sg = P.alloc([128, NT], BF16)
        P.dma('sp', tmpA.ap, pTs[8], writes=[tmpA])
        P.act(wdad.ap[0:64, :], tmpA.ap[0:64, :], AF.Tanh, reads=[tmpA], writes=[wdad])
        P.cp('dve', wdad.ap[64:128, :], tmpA.ap[64:128, :], reads=[tmpA], writes=[wdad])
        P.dma('sp', tmpB.ap, pTs[13], writes=[tmpB])
        P.act(sg.ap, tmpB.ap, AF.Sigmoid, reads=[tmpB], writes=[sg])
        kc = P.alloc([128, NT]); lw = [P.alloc([128, NT]) for _ in range(2)]
        vc = P.alloc([128, NT], DIN); rc = P.alloc([128, NT], DIN); kk = P.alloc([128, NT], DIN)
        kt = [P.alloc([128, NT], DIN) for _ in range(2)]; bb = [P.alloc([128, NT], DIN) for _ in range(2)]
        MTb = P.alloc([128, 2, NCH, 128], DST); P.memset('pool', MTb.ap, 0.0, writes=[MTb])
        Sbk = P.alloc([128, 2, NCH, 128], DST); P.memset('pool', Sbk.ap, 0.0, writes=[Sbk])
        Gst = P.alloc([128, 2, NCH, 64]); Qs = P.alloc([128, 2, NCH, 128], DST); Y0 = P.alloc([128, NCH, 128])
        Vpad = [P.alloc([128, 2, 128], DCH) for _ in range(2)]
        P2p = [[P.alloc([128, 128], DCH) for _ in range(2)] for _ in range(2)]
        for t_ in Vpad + P2p[0] + P2p[1]:
            P.memset('pool', t_.ap, 0.0, writes=[t_])
        Vtk = [P.alloc([128, 128], DCH) for _ in range(2)]
        lwtok = [P.alloc([128, 128]) for _ in range(2)]
        E1 = [P.alloc([128, 128]) for _ in range(2)]; E0 = [P.alloc([128, 128]) for _ in range(2)]
        Ei = [P.alloc([128, 128]) for _ in range(2)]; nWC = [P.alloc([128, 1]) for _ in range(2)]
        QR = [P.alloc([128, 2, 128], DCH) for _ in range(2)]
        Bt = [P.alloc([128, 128], DCH) for _ in range(2)]; Kt = [P.alloc([128, 128], DCH) for _ in range(2)]
        nBh = [P.alloc([128, 128], DCH) for _ in range(2)]; K2 = [P.alloc([128, 128], DCH) for _ in range(2)]
        TK = [P.alloc([128, 3, 128], DCH) for _ in range(2)]
        evA = [P.alloc([128, 256], DCH) for _ in range(2)]; evB = [P.alloc([128, 256], DCH) for _ in range(2)]
        Xr = [[P.alloc([128, 128], DCN) for _ in range(3)] for _ in range(2)]; XTr = [[P.alloc([128, 128], DCN) for _ in range(3)] for _ in range(2)]
        TTr = [[P.alloc([128, 128], DCN) for _ in range(3)] for _ in range(2)]
        TTf = [P.alloc([128, 128], DCH) for _ in range(2)]
        n2v = [P.alloc([128, 64], DCH) for _ in range(2)]; Pcat = [P.alloc([128, 128], DCH) for _ in range(2)]
        Sst = [P.alloc([128, 64], DST) for _ in range(2)]
        t512 = [P.alloc([128, 512]) for _ in range(4)]
        ymt = [P.alloc([128, 512], BF16) for _ in range(2)]
        cnt = [0]

        def rr(lst):
            cnt[0] += 1
            return lst[cnt[0] % len(lst)]

        def conv(dst, src, idx):
            for (a, b) in seqs:
                P.ts('dve', dst.ap[:, a:b], src.ap[:, a:b], shv.ap[:, idx, 1:2], None, ALU.mult, reads=[src, shv], writes=[dst])
                P.stt('dve', dst.ap[:, a + 1:b], src.ap[:, a:b - 1], shv.ap[:, idx, 0:1], dst.ap[:, a + 1:b], ALU.mult, ALU.add,
                      reads=[src, shv, dst], writes=[dst])
                P.stt('dve', dst.ap[:, a:b - 1], src.ap[:, a + 1:b], shv.ap[:, idx, 2:3], dst.ap[:, a:b - 1], ALU.mult, ALU.add,
                      reads=[src, shv, dst], writes=[dst])

        for pr in range(4):
            if RW_STOP == 0:
                break
            cs_ = slice(pr * 128, (pr + 1) * 128)
            P.dma('sp', tmpA.ap, pTs[pr], writes=[tmpA]); conv(kc, tmpA, pr)
            P.dma('sp', tmpB.ap, pTs[4 + pr], writes=[tmpB]); conv(vc, tmpB, 4 + pr)
            P.dma('sp', tmpA.ap, pTs[9 + pr], writes=[tmpA]); conv(rc, tmpA, 8 + pr)
            P.ts('dve', tmpA.ap, kc.ap, rv.ap[:, 4, pr:pr + 1], None, ALU.mult, reads=[kc, rv], writes=[tmpA])
            P.act(tmpB.ap, tmpA.ap, AF.Square, reads=[tmpA], writes=[tmpB])
            for (t0, TT, isc) in tiles:
                ps = P.psum('a')
                P.mm(ps.ap[:, 0:TT], blk.ap, tmpB.ap[:, t0:t0 + TT], reads=[blk, tmpB], writes=[ps])
                tq = rr(t512)
                P.ts('dve', tq.ap[:, 0:TT], ps.ap[:, 0:TT], 1e-12, None, ALU.max, reads=[ps], writes=[tq])
                P.act(tq.ap[:, 0:TT], tq.ap[:, 0:TT], AF.Ln, reads=[tq], writes=[tq])
                P.act(tq.ap[:, 0:TT], tq.ap[:, 0:TT], AF.Exp, reads=[tq], writes=[tq], scale=-0.5)
                P.tt('dve', kk.ap[:, t0:t0 + TT], tmpA.ap[:, t0:t0 + TT], tq.ap[:, 0:TT], ALU.mult, reads=[tmpA, tq], writes=[kk])
            for d in range(2):
                for (t0, TT, isc) in tiles:
                    ps = P.psum('a')
                    P.mm(ps.ap[:, 0:TT], lup.ap[0:64, d, cs_], wdad.ap[0:64, t0:t0 + TT], reads=[lup, wdad], writes=[ps])
                    P.act(lw[d].ap[:, t0:t0 + TT], ps.ap[:, 0:TT], AF.Sigmoid, reads=[ps, rv], writes=[lw[d]], bias=rv.ap[:, d, pr:pr + 1])
                    ps2 = P.psum('a')
                    P.mm(ps2.ap[:, 0:TT], lup.ap[64:128, d, cs_], wdad.ap[64:128, t0:t0 + TT], reads=[lup, wdad], writes=[ps2])
                    ta = rr(t512)
                    P.act(ta.ap[:, 0:TT], ps2.ap[:, 0:TT], AF.Sigmoid, reads=[ps2, rv], writes=[ta], bias=rv.ap[:, 2 + d, pr:pr + 1])
                    P.tt('dve', bb[d].ap[:, t0:t0 + TT], ta.ap[:, 0:TT], kk.ap[:, t0:t0 + TT], ALU.mult, reads=[ta, kk], writes=[bb[d]])
                    P.ts('dve', ta.ap[:, 0:TT], ta.ap[:, 0:TT], rv.ap[:, 5, pr:pr + 1], omka.ap[:, pr:pr + 1], ALU.mult, ALU.add,
                         reads=[ta, rv, omka], writes=[ta])
                    P.tt('dve', kt[d].ap[:, t0:t0 + TT], ta.ap[:, 0:TT], kc.ap[:, t0:t0 + TT], ALU.mult, reads=[ta, kc], writes=[kt[d]])
                P.ts('pool', lw[d].ap, lw[d].ap, -W_DECAY_SCALE, None, ALU.mult, reads=[lw[d]], writes=[lw[d]])
            if RW_STOP == 1:
                break
            PS6 = P.PS[6]
            for c in range(NCH):
                cs = slice(c * 128, (c + 1) * 128)
                vp = Vpad[c % 2]; vt = Vtk[c % 2]
                psb = P.psum('b')
                pv_ = psb.ap if DIN == F32 else psb.ap.bitcast(BF16)
                P.tr(pv_[:, 0:128], vc.ap[:, cs], idin.ap, reads=[vc, idin], writes=[psb])
                P.cp('act', vt.ap, pv_[:, 0:128], reads=[psb], writes=[vt])
                for hp in range(2):
                    P.cp('pool', vp.ap[:, hp, hp * 64:hp * 64 + 64], vt.ap[:, hp * 64:hp * 64 + 64], reads=[vt], writes=[vp])
                nmm = 0
                for d in range(2):
                    i2 = (c * 2 + d) % 2
                    lt = lwtok[i2]; e1 = E1[i2]; e0 = E0[i2]; ei = Ei[i2]; nw = nWC[i2]
                    qr = QR[i2]; bt = Bt[i2]; ktt = Kt[i2]; nb = nBh[i2]; k2 = K2[i2]; tk = TK[i2]
                    ps = P.psum('b')
                    P.tr(ps.ap[:, 0:128], lw[d].ap[:, cs], ident.ap, reads=[lw[d], ident], writes=[ps])
                    P.cp('dve', lt.ap, ps.ap[:, 0:128], reads=[ps], writes=[lt])
                    psc = P.psum('b')
                    P.mm(psc.ap[:, 0:256], lt.ap, rmk.ap[:, d, 640:896], reads=[lt, rmk], writes=[psc])
                    P.act(e1.ap, psc.ap[:, 0:128], AF.Exp, reads=[psc], writes=[e1])
                    P.act(e0.ap, psc.ap[:, 128:256], AF.Exp, reads=[psc], writes=[e0])
                    P.act(ei.ap, psc.ap[:, 0:128], AF.Exp, reads=[psc], writes=[ei], scale=-1.0)
                    wc = e1.ap[:, 127:128] if d == 0 else e1.ap[:, 0:1]
                    P.ts('dve', nw.ap, wc, -1.0, None, ALU.mult, reads=[e1], writes=[nw])
                    P.tt('dve', qr.ap[:, 0, :], kk.ap[:, cs], e0.ap, ALU.mult, reads=[kk, e0], writes=[qr])
                    P.tt('dve', qr.ap[:, 1, :], rc.ap[:, cs], e1.ap, ALU.mult, reads=[rc, e1], writes=[qr])
                    P.tt('pool', bt.ap, bb[d].ap[:, cs], ei.ap, ALU.mult, reads=[bb[d], ei], writes=[bt])
                    P.tt('pool', ktt.ap, kt[d].ap[:, cs], ei.ap, ALU.mult, reads=[kt[d], ei], writes=[ktt])
                    P.ts('dve', nb.ap, bt.ap, nw.ap[:, 0:1], None, ALU.mult, reads=[bt, nw], writes=[nb])
                    P.ts('dve', k2.ap, ktt.ap, wc, None, ALU.mult, reads=[ktt, e1], writes=[k2])
                    pst = P.psum('b'); pstb = pst.ap if DCH == F32 else pst.ap.bitcast(BF16)
                    P.tr(pstb[:, 0:128], qr.ap[:, 0, :], idch.ap, reads=[qr, idch], writes=[pst])
                    P.tr(pstb[:, 128:256], nb.ap, idch.ap, reads=[nb, idch], writes=[pst])
                    P.tr(pstb[:, 256:384], k2.ap, idch.ap, reads=[k2, idch], writes=[pst])
                    P.cp('act', tk.ap, pstb[:, 0:384].rearrange("p (a b) -> p a b", a=3), reads=[pst], writes=[tk])
                    if RW_STOP == 2:
                        continue
                    H = [dict(), dict()]
                    qr2s = [qr.ap[slice(hp * 64, hp * 64 + 64), :, :].rearrange("p a b -> p (a b)") for hp in range(2)]
                    for hp in range(2):
                        hs = slice(hp * 64, hp * 64 + 64)
                        ea = evA[hp]; eb = evB[hp]
                        qr2 = qr2s[hp]
                        p1 = P.psum('r')
                        P.mm(p1.ap[:, 0:256], bt.ap[hs, :], qr2, reads=[bt, qr], writes=[p1])
                        P.tt('dve', ea.ap, p1.ap[:, 0:256], rmk.ap[:, d, 0:256], ALU.mult, reads=[p1, rmk], writes=[ea])
                        p2 = P.psum('r')
                        P.mm(p2.ap[:, 0:256], ktt.ap[hs, :], qr2, reads=[ktt, qr], writes=[p2])
                        P.tt('dve', eb.ap, p2.ap[:, 0:256], rmk.ap[:, d, 256:512], ALU.mult, reads=[p2, rmk], writes=[eb])
                        p3 = P.psum('r')
                        P.mm(p3.ap[:, 0:128], qr.ap[hs, 0, :], bt.ap[hs, :], reads=[qr, bt], writes=[p3])
                        X = Xr[hp][0]
                        P.tt('dve', X.ap, p3.ap[:, 0:128], rmk.ap[:, d, 512:640], ALU.mult, reads=[p3, rmk], writes=[X])
                        XT = XTr[hp][0]
                        P.cp('pool', XT.ap, ea.ap[:, 0:128], reads=[ea], writes=[XT])
                        TTc = TTr[hp][0]
                        P.tt('pool', TTc.ap, ea.ap[:, 0:128], ident.ap, ALU.add, reads=[ea, ident], writes=[TTc])
                        H[hp] = dict(X=X, XT=XT, TT=TTc, xi=0, xti=0, ti=0)
                    for j in range(1, 7):
                        for hp in range(2):
                            st = H[hp]
                            X = st['X']; XT = st['XT']; TTc = st['TT']
                            pX = P.psum('r')
                            P.mm(pX.ap[:, 0:128], XT.ap, X.ap, reads=[XT, X], writes=[pX])
                            st['xi'] = (st['xi'] + 1) % 3
                            Xn = Xr[hp][st['xi']]
                            P.cp('act', Xn.ap, pX.ap[:, 0:128], reads=[pX], writes=[Xn])
                            if j < 6:
                                pXT = P.psum('r')
                                P.mm(pXT.ap[:, 0:128], X.ap, XT.ap, reads=[XT, X], writes=[pXT])
                                st['xti'] = (st['xti'] + 1) % 3
                                XTn = XTr[hp][st['xti']]
                                P.cp('dve', XTn.ap, pXT.ap[:, 0:128], reads=[pXT], writes=[XTn])
                                st['XT'] = XTn
                            pT = P.psum('r')
                            P.mm(pT.ap[:, 0:128], Xn.ap, TTc.ap, reads=[Xn, TTc], writes=[pT])
                            st['ti'] = (st['ti'] + 1) % 3
                            TTn = TTr[hp][st['ti']]
                            P.tt('dve', TTn.ap, pT.ap[:, 0:128], TTc.ap, ALU.add, reads=[pT, TTc], writes=[TTn])
                            st['X'] = Xn
                            st['TT'] = TTn
                    for hp in range(2):
                        hs = slice(hp * 64, hp * 64 + 64)
                        ea = evA[hp]; eb = evB[hp]; TTc = H[hp]['TT']
                        nv = n2v[hp]; pc = Pcat[hp]; p2p = P2p[hp][d]
                        p4 = P.psum('r')
                        P.mm(p4.ap[:, 0:64], eb.ap[:, 0:128], vt.ap[:, hs], reads=[eb, vt], writes=[p4])
                        P.cp('act', nv.ap, p4.ap[:, 0:64], reads=[p4], writes=[nv])
                        p5 = P.psum('r')
                        P.mm(p5.ap[:, 0:64], TTc.ap, tk.ap[:, 0, hs], reads=[TTc, tk], writes=[p5])
                        P.mm(p5.ap[:, 64:128], TTc.ap, nv.ap, reads=[TTc, nv], writes=[p5])
                        P.cp('dve', pc.ap, p5.ap[:, 0:128], reads=[p5], writes=[pc])
                        P.cp('pool', p2p.ap[:, hs], pc.ap[:, 64:128], reads=[pc], writes=[p2p])
                        p6 = P.psum('r')
                        P.mm(p6.ap[hs, 0:64], pc.ap[:, 0:64], tk.ap[:, 1, hs], reads=[pc, tk], writes=[p6])
                        P.stt('dve', MTb.ap[hs, d, c, hs], ident.ap[hs, hs], wc[hs, :], p6.ap[hs, 0:64], ALU.mult, ALU.add,
                              reads=[ident, e1, p6], writes=[MTb])
                        p7 = P.psum('r')
                        P.mm(p7.ap[hs, 0:64], tk.ap[:, 2, hs], vt.ap[:, hs], start=True, stop=False, reads=[tk, vt], writes=[p7])
                        P.mm(p7.ap[hs, 0:64], tk.ap[:, 1, hs], pc.ap[:, 64:128], start=False, stop=True, reads=[tk, pc], writes=[p7])
                        P.cp('act', Gst.ap[hs, d, c, :], p7.ap[hs, 0:64], reads=[p7], writes=[Gst])
                        p8 = P.psum('r')
                        P.mm(p8.ap[hs, 0:128], pc.ap[:, 0:64], ea.ap[:, 128:256], reads=[pc, ea], writes=[p8])
                        P.tt('dve', Qs.ap[hs, d, c, :], p8.ap[hs, 0:128], qr.ap[hs, 1, :], ALU.add, reads=[p8, qr], writes=[Qs])
                        P.mm(PS6.ap[:, 0:128], vp.ap[:, hp, :], eb.ap[:, 128:256], start=(nmm == 0), stop=False,
                             reads=[vp, eb], writes=[PS6])
                        P.mm(PS6.ap[:, 0:128], p2p.ap, ea.ap[:, 128:256], start=False, stop=(nmm == 3),
                             reads=[p2p, ea], writes=[PS6])
                        nmm += 1
                if RW_STOP > 2 and RW_STOP not in (25, 26, 27, 28, 261, 262):
                    P.cp('dve', Y0.ap[:, c, :], PS6.ap[:, 0:128], reads=[PS6], writes=[Y0])
            if RW_STOP <= 3 or RW_STOP in (25, 26, 27, 28, 261, 262):
                break
            for d in range(2):
                order = list(range(0, CCH)) + list(range(CCH, NCH)) if d == 0 else \
                    list(range(CCH - 1, -1, -1)) + list(range(NCH - 1, CCH - 1, -1))
                s_cur = Sst[0]
                P.memset('dve', s_cur.ap, 0.0, writes=[s_cur])
                P.memset('dve', Sbk.ap[:, d, order[0], :], 0.0, writes=[Sbk])
                for i, c in enumerate(order[:-1]):
                    ps = P.psum('b')
                    P.mm(ps.ap[:, 0:64], MTb.ap[:, d, c, :], s_cur.ap, reads=[MTb, s_cur], writes=[ps])
                    s_nx = Sst[(i + 1) % 2]
                    P.tt('dve', s_nx.ap, ps.ap[:, 0:64], Gst.ap[:, d, c, :], ALU.add, reads=[ps, Gst], writes=[s_nx])
                    c2 = order[i + 1]
                    for hp in range(2):
                        hs = slice(hp * 64, hp * 64 + 64)
                        P.cp('pool', Sbk.ap[hs, d, c2, hs], s_nx.ap[hs, :], reads=[s_nx], writes=[Sbk])
                    s_cur = s_nx
            if RW_STOP == 4:
                break
            for c in range(NCH):
                ps = P.psum('b')
                P.mm(ps.ap[:, 0:128], Sbk.ap[:, 0, c, :], Qs.ap[:, 0, c, :], start=True, stop=False, reads=[Sbk, Qs], writes=[ps])
                P.mm(ps.ap[:, 0:128], Sbk.ap[:, 1, c, :], Qs.ap[:, 1, c, :], start=False, stop=True, reads=[Sbk, Qs], writes=[ps])
                P.tt('dve', tmpA.ap[:, c * 128:(c + 1) * 128], ps.ap[:, 0:128], Y0.ap[:, c, :], ALU.add, reads=[ps, Y0], writes=[tmpA])
            if dbg and pr == 0:
                tap("yr0", tmpA, [128, NT])
            for ti, (t0, TT, isc) in enumerate(tiles):
                tsl = slice(t0, t0 + TT)
                ps = P.psum('a')
                P.mm(ps.ap[:, 0:TT], blk.ap, tmpA.ap[:, tsl], reads=[blk, tmpA], writes=[ps])
                dc = rr(t512)
                P.stt('dve', dc.ap[:, 0:TT], ps.ap[:, 0:TT], -1.0 / 64, tmpA.ap[:, tsl], ALU.mult, ALU.add, reads=[ps, tmpA], writes=[dc])
                sq_ = rr(t512)
                P.act(sq_.ap[:, 0:TT], dc.ap[:, 0:TT], AF.Square, reads=[dc], writes=[sq_])
                ps2 = P.psum('a')
                P.mm(ps2.ap[:, 0:TT], blk.ap, sq_.ap[:, 0:TT], reads=[blk, sq_], writes=[ps2])
                P.ts('dve', sq_.ap[:, 0:TT], ps2.ap[:, 0:TT], 1.0 / 64, 64e-5, ALU.mult, ALU.add, reads=[ps2], writes=[sq_])
                P.act(sq_.ap[:, 0:TT], sq_.ap[:, 0:TT], AF.Ln, reads=[sq_], writes=[sq_])
                P.act(sq_.ap[:, 0:TT], sq_.ap[:, 0:TT], AF.Exp, reads=[sq_], writes=[sq_], scale=-0.5)
                P.tt('dve', dc.ap[:, 0:TT], dc.ap[:, 0:TT], sq_.ap[:, 0:TT], ALU.mult, reads=[dc, sq_], writes=[dc])
                P.ts('dve', dc.ap[:, 0:TT], dc.ap[:, 0:TT], rv.ap[:, 7, pr:pr + 1], rv.ap[:, 8, pr:pr + 1], ALU.mult, ALU.add,
                     reads=[dc, rv], writes=[dc])
                bo = rr(t512)
                P.tt('pool', bo.ap[:, 0:TT], kt[0].ap[:, tsl], kt[1].ap[:, tsl], ALU.add, reads=[kt[0], kt[1]], writes=[bo])
                P.tt('pool', bo.ap[:, 0:TT], bo.ap[:, 0:TT], rc.ap[:, tsl], ALU.mult, reads=[bo, rc], writes=[bo])
                P.ts('pool', bo.ap[:, 0:TT], bo.ap[:, 0:TT], rv.ap[:, 6, pr:pr + 1], None, ALU.mult, reads=[bo, rv], writes=[bo])
                ps3 = P.psum('a')
                P.mm(ps3.ap[:, 0:TT], blk.ap, bo.ap[:, 0:TT], reads=[blk, bo], writes=[ps3])
                P.tt('dve', bo.ap[:, 0:TT], ps3.ap[:, 0:TT], vc.ap[:, tsl], ALU.mult, reads=[ps3, vc], writes=[bo])
                P.tt('dve', dc.ap[:, 0:TT], dc.ap[:, 0:TT], bo.ap[:, 0:TT], ALU.add, reads=[dc, bo], writes=[dc])
                ps4 = P.psum('a')
                P.mm(ps4.ap[:, 0:TT], gup.ap[:, cs_], sg.ap[:, tsl], reads=[gup, sg], writes=[ps4])
                ym = ymt[ti % 2]
                P.tt('dve', ym.ap[:, 0:TT], ps4.ap[:, 0:TT], dc.ap[:, 0:TT], ALU.mult, reads=[ps4, dc], writes=[ym])
                P.dma('sp', ymTs[pr, :, tsl], ym.ap[:, 0:TT], reads=[ym])
        P.release(m)

    def phase_pool():
        m = P.mark()
        seqs = [(0, C), (C, NT)]
        pw = P.alloc([128, 4, 128], BF16)
        for gi in range(4):
            P.dma('pool', pw.ap[:, gi, :], pool_w[gi], writes=[pw])
        psc = P.alloc([128, 4]); P.dma('sp', psc.ap, pool_scT, writes=[psc])
        u = P.alloc([128, NT]); acc = P.alloc([128, NT]); inv = P.alloc([128, NT]); df = P.alloc([128, NT], BF16)
        ymt = [P.alloc([128, 512], BF16) for _ in range(2)]
        for gi, win in enumerate((2, 4, 8, 16)):
            P.dma('sp', u.ap, pTs[14 + gi], writes=[u])
            P.dma('sp', inv.ap, pool_inv[gi:gi + 1, :].partition_broadcast(128), writes=[inv])
            P.cp('pool', acc.ap, u.ap, reads=[u], writes=[acc])
            for o in range(-(win // 2), win // 2):
                if o == 0:
                    continue
                for (a, b) in seqs:
                    if o < 0:
                        P.tt('dve', acc.ap[:, a - o:b], acc.ap[:, a - o:b], u.ap[:, a:b + o], ALU.add, reads=[acc, u], writes=[acc])
                    else:
                        P.tt('dve', acc.ap[:, a:b - o], acc.ap[:, a:b - o], u.ap[:, a + o:b], ALU.add, reads=[acc, u], writes=[acc])
            P.tt('dve', acc.ap, acc.ap, inv.ap, ALU.mult, reads=[acc, inv], writes=[acc])
            P.tt('dve', df.ap, acc.ap, u.ap, ALU.subtract, reads=[acc, u], writes=[df])
            for ti, (t0, TT, isc) in enumerate(tiles):
                ps = P.psum('a')
                P.mm(ps.ap[:, 0:TT], pw.ap[:, gi, :], df.ap[:, t0:t0 + TT], reads=[pw, df], writes=[ps])
                ym = ymt[ti % 2]
                P.ts('dve', ym.ap[:, 0:TT], ps.ap[:, 0:TT], psc.ap[:, gi:gi + 1], None, ALU.mult, reads=[ps, psc], writes=[ym])
                P.dma('sp', ymTs[4 + gi, :, t0:t0 + TT], ym.ap[:, 0:TT], reads=[ym])
        P.release(m)

    RUN_RWKV = STOP_AFTER not in ('inproj',)
    RUN_POOL = STOP_AFTER not in ('inproj', 'rwkv')

    def phase_outproj(li, w_out_dram, lat_only):
        m = P.mark()
        Wout = P.alloc([128, KD, D], BF16)
        load_w_bf(Wout, w_out_dram, KD)
        ymb = [P.alloc([128, KD, 512], BF16) for _ in range(2)]
        xT = [P.alloc([128, KD, 512]) for _ in range(2)]
        for ti, (t0, TT, isc) in enumerate(tiles):
            if lat_only and isc:
                continue
            ym = ymb[ti % 2]; xt = xT[ti % 2]
            for k in range(KD):
                P.dma('sp', ym.ap[:, k, 0:TT], ymTs[k, :, t0:t0 + TT], writes=[ym])
                P.dma('act', xt.ap[:, k, 0:TT], xTs[k, :, t0:t0 + TT], writes=[xt])
            for dc in range(KD):
                ps = P.psum('a')
                for k in range(KD):
                    P.mm(ps.ap[:, 0:TT], Wout.ap[:, k, dc * 128:(dc + 1) * 128], ym.ap[:, k, 0:TT],
                         start=(k == 0), stop=(k == KD - 1), reads=[Wout, ym], writes=[ps])
                P.stt('dve', xt.ap[:, dc, 0:TT], ps.ap[:, 0:TT], mod.ap[:, li, 16 + dc, isc:isc + 1], xt.ap[:, dc, 0:TT],
                      ALU.mult, ALU.add, reads=[ps, mod, xt], writes=[xt])
            for k in range(KD):
                P.dma('sp', xTs[k, :, t0:t0 + TT], xt.ap[:, k, 0:TT], reads=[xt])
        P.release(m)

    def phase_final():
        m = P.mark()
        xT = [P.alloc([128, KD, 512]) for _ in range(2)]
        sq = P.alloc([128, KD, 512]); rs = P.alloc([128, 512])
        ob = [P.alloc([128, KD, 512]) for _ in range(2)]
        otok = [P.alloc([128, D]) for _ in range(2)]
        for ti, (t0, TT, isc) in enumerate(tiles):
            if isc:
                continue
            xt = xT[ti % 2]; o = ob[ti % 2]
            for k in range(KD):
                P.dma('sp', xt.ap[:, k, 0:TT], xTs[k, :, t0:t0 + TT], writes=[xt])
            norm_mod(xt, TT, 0, 0, 0, o, sq, rs, final=True)
            for b in range(TT // 128):
                ot = otok[b % 2]
                for half in range(2):
                    ps = P.psum('a')
                    for j in range(4):
                        k = half * 4 + j
                        P.tr(ps.ap[:, j * 128:(j + 1) * 128], o.ap[:, k, b * 128:(b + 1) * 128], ident.ap,
                             reads=[o, ident], writes=[ps])
                    P.cp('dve' if half else 'act', ot.ap[:, half * 512:(half + 1) * 512], ps.ap, reads=[ps], writes=[ot])
                r0 = t0 - C + b * 128
                P.dma('sp', out[r0:r0 + 128, :], ot.ap, reads=[ot], is_output=True)
        P.release(m)

    if RUN_RWKV:
        phase_rwkv()
    if RUN_POOL:
        phase_pool()
    if STOP_AFTER in ('inproj', 'rwkv', 'pool'):
        phase_final()
        return P, locals()
    phase_outproj(0, ev_w_out, False)
    if STOP_AFTER == 'l0mix':
        phase_final()
        return P, locals()
    phase_moe(0, False)
    if STOP_AFTER == 'l0':
        phase_final()
        return P, locals()
    phase_inproj(1, od_w_in, OD_COLS, False)
    phase_l1mix()
    phase_outproj(1, od_w_out, True)
    if STOP_AFTER == 'l1mix':
        phase_final()
        return P, locals()
    phase_moe(1, True)
    phase_final()
    return P, locals()


def fm(v, nch=None):
    v = np.asarray(v, np.float32)
    return np.ascontiguousarray(v.reshape(-1, 128).T)


def make_inputs(b, S, C, inp):
    NT = C + S
    m = {}
    m['xin'] = np.ascontiguousarray(np.concatenate([inp['ctx'][b], inp['x'][b]], 0))
    m['cT'] = np.ascontiguousarray(np.stack([fm(inp['c'][b]), fm(inp['c_ctx'])], -1))
    m['ada_w'] = inp['ada_w']
    m['ada_bT'] = np.ascontiguousarray(np.stack([fm(inp['ada_b'][0]), fm(inp['ada_b'][1])], 1))
    m['normT'] = np.ascontiguousarray(np.stack([fm(inp['norm_mix'][0]), fm(inp['norm_mix'][1]), fm(inp['norm_ffn'][0]),
                                               fm(inp['norm_ffn'][1]), fm(inp['final_norm'])], 1))
    m['ev_w_in'] = inp['ev_w_in'][0]
    m['ev_w_out'] = inp['ev_w_out'][0]
    sh = inp['rwkv_shift'][0]
    m['shiftT'] = np.ascontiguousarray(np.stack([fm(sh[0]), fm(sh[1]), fm(sh[2])], -1))
    rv = [inp['rwkv_w0'][0][0], inp['rwkv_w0'][0][1], inp['rwkv_a0'][0][0], inp['rwkv_a0'][0][1], inp['rwkv_k_k'][0],
          inp['rwkv_k_a'][0], inp['rwkv_r_k'][0], inp['rwkv_ln_g'][0], inp['rwkv_ln_b'][0]]
    m['rvec'] = np.ascontiguousarray(np.stack([fm(v) for v in rv], 1))
    lu = np.zeros((128, 2, 512), np.float32)
    for d in range(2):
        lu[0:64, d] = inp['rwkv_w_up'][0][d]
        lu[64:128, d] = inp['rwkv_a_up'][0][d]
    m['lora_up'] = lu
    m['g_up'] = inp['rwkv_g_up'][0]
    m['pool_w'] = inp['pool_w'][0]
    m['pool_scT'] = fm(inp['pool_scale'][0])
    pi = np.zeros((4, NT), np.float32)
    for gi, win in enumerate((2, 4, 8, 16)):
        for (a, Tn) in ((0, C), (C, S)):
            t = np.arange(Tn)
            lo = np.clip(t - win // 2, 0, Tn)
            hi = np.clip(t - win // 2 + win, 0, Tn)
            pi[gi, a:a + Tn] = 1.0 / (hi - lo)
    m['pool_inv'] = pi
    m['moe_r'] = np.ascontiguousarray(np.concatenate([inp['moe_router_group'], inp['moe_router_expert']], -1))
    m['moe_wg'] = inp['moe_w_gate']
    m['moe_wu'] = inp['moe_w_up']
    m['moe_wd'] = inp['moe_w_down']
    m['od_w_in'] = inp['od_w_in'][0]
    m['od_w_out'] = inp['od_w_out'][0]
    m['dlam'] = np.ascontiguousarray(inp['diff_lambda'][0].reshape(1, 256))
    m['sublnT'] = np.ascontiguousarray(inp['diff_subln'][0].reshape(128, 1))
    m['retgT'] = fm(inp['ret_norm'][0])
    m.update(host_consts())
    m.update(host_consts_l1(S))
    return m


def kernel(**inp):
    inp = {k: np.asarray(v) for k, v in inp.items()}
    S, C = inp['x'].shape[1], inp['ctx'].shape[1]
    B = inp['x'].shape[0]
    P, _ = build(S, C)
    nc = P.build()
    in_maps = [make_inputs(b, S, C, inp) for b in range(B)]
    names = set()
    res = run_bass_kernel_spmd(nc, in_maps, core_ids=list(range(B)))
    return np.stack([np.asarray(r["out"], np.float32) for r in res.results], 0)
```

```python
import math
import numpy as np
from contextlib import ExitStack
import concourse.bass as bass
import concourse.mybir as mybir
from concourse.bass_utils import run_bass_kernel_spmd

F32 = mybir.dt.float32
BF16 = mybir.dt.bfloat16
ALU = mybir.AluOpType
AF = mybir.ActivationFunctionType
AX = mybir.AxisListType

ENGS = ['pe', 'act', 'dve', 'pool', 'sp']
DMAQ = ['sp', 'pool', 'act']


def _prod(s):
    r = 1
    for v in s:
        r *= v
    return r


class T:
    __slots__ = ('ap', 'lw', 'rd')

    def __init__(self, ap):
        self.ap = ap
        self.lw = None
        self.rd = {}


class Prog:
    def __init__(self, arena_words=50000, n_dma_sems=8):
        self.nc = bass.Bass("TRN2", target_bir_lowering=False)
        self.es = ExitStack()
        self.ops = {e: [] for e in ENGS}
        self.known = {e: {} for e in ENGS}
        self.pending = {e: [] for e in ENGS}
        self.n_dma_sems = n_dma_sems
        self.dma_rr = {q: 0 for q in DMAQ}
        self.dma_cum = {}
        self.out_tokens = []
        self.nuid = 0
        self.aw = arena_words
        self.arena = self.es.enter_context(self.nc.sbuf_tensor("arena", [128, arena_words], F32))
        self.top = 0
        self.PS = [T(self.es.enter_context(self.nc.psum_tensor(f"psb{i}", [128, 512], F32))[:, :])
                   for i in range(8)]
        self.ps_rr = {'a': 0, 'b': 0, 'c': 0, 'r': 0}
        self.ps_groups = {'a': [0, 1, 2, 3], 'b': [4, 5], 'c': [4, 5, 6, 7], 'r': [0, 1, 2, 3, 7]}

    def psum(self, g='a'):
        lst = self.ps_groups[g]
        i = self.ps_rr[g]
        self.ps_rr[g] = (i + 1) % len(lst)
        return self.PS[lst[i]]

    def alloc(self, shape, dt=F32):
        shape = list(shape)
        esz = 4 if dt == F32 else 2
        nb = _prod(shape[1:]) * esz
        nw = (nb + 3) // 4
        assert self.top + nw <= self.aw, f"arena overflow {self.top}+{nw}>{self.aw}"
        ap = self.arena[0:shape[0], self.top:self.top + nw]
        self.top += nw
        if dt != F32:
            ap = ap.bitcast(dt)
            ap = ap[:, 0:_prod(shape[1:])]
        if len(shape) > 2:
            names = "abcdefg"[:len(shape) - 1]
            kw = {names[i]: shape[i + 1] for i in range(len(shape) - 2)}
            ap = ap.rearrange("p (" + " ".join(names) + ") -> p " + " ".join(names), **kw)
        return T(ap)

    def mark(self):
        return self.top

    def release(self, m):
        self.barrier()
        self.top = m

    def dram(self, name, shape, dt, kind="Internal"):
        return self.nc.dram_tensor(name, list(shape), dt, kind=kind).ap()

    def barrier(self):
        toks = []
        for f in ENGS:
            if len(self.ops[f]) > 0:
                toks.append(('c', f, len(self.ops[f])))
        for skey, cum in self.dma_cum.items():
            toks.append(('d', skey, cum))
        for e in ENGS:
            self.pending[e] = list(toks)

    def _add_wait(self, e, waits, tok):
        if tok is None:
            return
        kind, key, val = tok
        if kind == 'c' and key == e:
            if e == 'pe':
                return
            if val > len(self.ops[e]):
                return
        kk = (kind, key)
        if self.known[e].get(kk, 0) >= val:
            return
        self.known[e][kk] = val
        waits[kk] = max(waits.get(kk, 0), val)

    def _deps(self, e, reads, writes):
        waits = {}
        if self.pending[e]:
            for tok in self.pending[e]:
                self._add_wait(e, waits, tok)
            self.pending[e] = []
        for t in reads:
            self._add_wait(e, waits, t.lw)
        for t in writes:
            self._add_wait(e, waits, t.lw)
            for tok in t.rd.values():
                self._add_wait(e, waits, tok)
        return waits

    def op(self, e, fn, reads=(), writes=()):
        waits = self._deps(e, reads, writes)
        idx = len(self.ops[e]) + 1
        tok = ('c', e, idx)
        self.ops[e].append(dict(fn=fn, waits=waits, inc=None, flag=False))
        for t in reads:
            t.rd[e] = tok
        for t in writes:
            t.lw = tok
            t.rd = {}
        return tok

    def dma(self, q, out_ap, in_ap, reads=(), writes=(), is_output=False, **kw):
        waits = self._deps(q, reads, writes)
        si = self.dma_rr[q]
        self.dma_rr[q] = (si + 1) % self.n_dma_sems
        skey = (q, si)
        prev = self.dma_cum.get(skey, 0)
        if prev > 0:
            self._add_wait(q, waits, ('d', skey, prev))
        val = prev + 16
        self.dma_cum[skey] = val
        tok = ('d', skey, val)

        def fn(eng, out_ap=out_ap, in_ap=in_ap, kw=kw):
            return eng.dma_start(out=out_ap, in_=in_ap, **kw)
        self.ops[q].append(dict(fn=fn, waits=waits, inc=(skey, 16), flag=True))
        for t in reads:
            t.rd[('dma', skey)] = tok
        for t in writes:
            t.lw = tok
            t.rd = {}
        if is_output:
            self.out_tokens.append(tok)
        return tok

    def dma_group(self, q, pairs, reads=(), writes=(), is_output=False):
        waits = self._deps(q, reads, writes)
        si = self.dma_rr[q]
        self.dma_rr[q] = (si + 1) % self.n_dma_sems
        skey = (q, si)
        prev = self.dma_cum.get(skey, 0)
        if prev > 0:
            self._add_wait(q, waits, ('d', skey, prev))
        val = prev
        for i, (out_ap, in_ap) in enumerate(pairs):
            val += 16

            def fn(eng, out_ap=out_ap, in_ap=in_ap):
                return eng.dma_start(out=out_ap, in_=in_ap)
            self.ops[q].append(dict(fn=fn, waits=waits if i == 0 else {}, inc=(skey, 16), flag=True))
        self.dma_cum[skey] = val
        tok = ('d', skey, val)
        for t in reads:
            t.rd[('dma', skey)] = tok
        for t in writes:
            t.lw = tok
            t.rd = {}
        if is_output:
            self.out_tokens.append(tok)
        return tok

    def build(self):
        nc = self.nc
        waits = {}
        for tok in self.out_tokens:
            self._add_wait('sp', waits, tok)
        self.ops['sp'].append(dict(fn=None, waits=waits, inc=None, flag=False))
        for e in ENGS:
            for o in self.ops[e]:
                for (kind, key), val in o['waits'].items():
                    if kind == 'c':
                        self.ops[key][val - 1]['flag'] = True
        rank = {}
        for e in ENGS:
            r = 0
            rk = []
            for o in self.ops[e]:
                if o['inc'] is None and o['flag']:
                    r += 1
                rk.append(r)
            rank[e] = rk
        csem = {e: self.es.enter_context(nc.semaphore(f"c_{e}")) for e in ENGS}
        dsem = {}
        for q in DMAQ:
            for i in range(self.n_dma_sems):
                if (q, i) in self.dma_cum:
                    dsem[(q, i)] = self.es.enter_context(nc.semaphore(f"d_{q}{i}"))
        block = self.es.enter_context(nc.Block())
        engobj = {'pe': block.tensor, 'act': block.scalar, 'dve': block.vector,
                  'pool': block.gpsimd, 'sp': block.sync}

        def mk(e):
            def body(eng):
                for o in self.ops[e]:
                    for (kind, key), val in o['waits'].items():
                        if kind == 'c':
                            eng.wait_ge(csem[key], rank[key][val - 1])
                        else:
                            eng.wait_ge(dsem[key], val)
                    if o['fn'] is None:
                        continue
                    ins = o['fn'](eng)
                    if o['inc'] is not None:
                        ins.then_inc(dsem[o['inc'][0]], 16)
                    elif o['flag']:
                        ins.then_inc(csem[e], 1)
            return body
        for e in ENGS:
            engobj[e](mk(e))
        self.es.close()
        return nc

    def mm(self, out, lhsT, rhs, start=True, stop=True, reads=(), writes=(), **kw):
        def fn(eng):
            return eng.matmul(out, lhsT, rhs, start=start, stop=stop, **kw)
        return self.op('pe', fn, reads, writes)

    def tr(self, out, in_, ident, reads=(), writes=()):
        def fn(eng):
            return eng.transpose(out, in_, ident)
        return self.op('pe', fn, reads, writes)

    def act(self, out, in_, func, reads=(), writes=(), **kw):
        def fn(e):
            return e.activation(out=out, in_=in_, func=func, **kw)
        return self.op('act', fn, reads, writes)

    def tt(self, e, out, in0, in1, op, reads=(), writes=()):
        def fn(eng):
            return eng.tensor_tensor(out=out, in0=in0, in1=in1, op=op)
        return self.op(e, fn, reads, writes)

    def ts(self, e, out, in0, s1, s2, op0, op1=None, reads=(), writes=()):
        def fn(eng):
            if op1 is None:
                return eng.tensor_scalar(out=out, in0=in0, scalar1=s1, scalar2=None, op0=op0)
            return eng.tensor_scalar(out=out, in0=in0, scalar1=s1, scalar2=s2, op0=op0, op1=op1)
        return self.op(e, fn, reads, writes)

    def stt(self, e, out, in0, scalar, in1, op0, op1, reads=(), writes=()):
        def fn(eng):
            return eng.scalar_tensor_tensor(out=out, in0=in0, scalar=scalar, in1=in1, op0=op0, op1=op1)
        return self.op(e, fn, reads, writes)

    def cp(self, e, out, in_, reads=(), writes=()):
        if e == 'act':
            def fn(eng):
                return eng.copy(out=out, in_=in_)
        else:
            def fn(eng):
                return eng.tensor_copy(out=out, in_=in_)
        return self.op(e, fn, reads, writes)

    def memset(self, e, ap, val, writes=()):
        def fn(eng):
            return eng.memset(ap, val)
        return self.op(e, fn, (), writes)


D = 1024
KD = 8
W_DECAY_SCALE = 0.606531
EV_COLS = 2304
OD_COLS = 3072


def host_consts():
    r = np.arange(128)
    Us = (r[:, None] < r[None, :]).astype(np.float32)
    Ui = (r[:, None] <= r[None, :]).astype(np.float32)
    Ls = (r[:, None] > r[None, :]).astype(np.float32)
    Li = (r[:, None] >= r[None, :]).astype(np.float32)
    blk = np.zeros((128, 128), np.float32)
    blk[:64, :64] = 1
    blk[64:, 64:] = 1
    c = {}
    c['ident'] = np.eye(128, dtype=np.float32)
    c['blk64'] = blk
    rm = np.zeros((2, 128, 896), np.float32)
    for d, (ss, si, tsm) in enumerate([(Us, Ui, Ls), (Ls, Li, Us)]):
        rm[d, :, 0:128] = -ss
        rm[d, :, 128:256] = -si
        rm[d, :, 256:384] = ss
        rm[d, :, 384:512] = si
        rm[d, :, 512:640] = -tsm
        rm[d, :, 640:768] = si
        rm[d, :, 768:896] = ss
    c['rmask'] = rm
    return c


def host_consts_l1(S):
    c = {}
    p = np.arange(128)
    blk32 = p % 32
    partner = np.where(blk32 < 16, p + 16, p - 16)
    perm = np.zeros((128, 128), np.float32)
    perm[partner, p] = 1.0
    c['rope_perm'] = perm
    t = np.arange(S)
    row = (t // 64).astype(np.float32)
    col = (t % 64).astype(np.float32)
    b64 = p % 64
    sub = b64 // 32
    j = (b64 % 16).astype(np.float32)
    inv = (10000.0 ** (-j / 16.0)).astype(np.float32)
    pos = np.where(sub[:, None] == 0, row[None, :], col[None, :]).astype(np.float32)
    ang = (pos * inv[:, None]).astype(np.float32)
    sgn = np.where(blk32 < 16, -1.0, 1.0).astype(np.float32)
    c['ropeC'] = np.cos(ang).astype(np.float32)
    c['ropeS'] = (np.sin(ang) * sgn[:, None]).astype(np.float32)
    lgf = np.log(1.0 - 2.0 ** (-5.0 - np.arange(4, dtype=np.float64)))
    r = np.arange(128, dtype=np.float64)
    retD = np.zeros((8, 128, 128), np.float64)
    retq = np.zeros((8, 128), np.float64)
    retk = np.zeros((128, 8), np.float64)
    for h in range(4):
        for d in range(2):
            lg = lgf[h] if d == 0 else lgf[3 - h]
            hd = h * 2 + d
            s_, i_ = r[:, None], r[None, :]
            if d == 0:
                retD[hd] = np.where(i_ >= s_, np.exp(lg * np.maximum(i_ - s_, 0)), 0.0)
                retq[hd] = np.exp(lg * (r + 1))
                retk[:, hd] = np.exp(lg * (127 - r))
            else:
                retD[hd] = np.where(s_ >= i_, np.exp(lg * np.maximum(s_ - i_, 0)), 0.0)
                retq[hd] = np.exp(lg * (128 - r))
                retk[:, hd] = np.exp(lg * r)
    c['retD'] = retD.astype(np.float32)
    c['retq'] = retq.astype(np.float32)
    c['retk'] = retk.astype(np.float32)
    return c


def build(S, C, dbg=False, RW_DT=(BF16, F32, BF16), STOP_AFTER='all', RW_STOP=99):
    P = Prog()
    NT = C + S
    NCH = NT // 128
    CCH = C // 128
    tiles = []
    for base, ln, isc in ((0, C, 1), (C, S, 0)):
        o = 0
        while o < ln:
            l = min(512, ln - o)
            tiles.append((base + o, l, isc))
            o += l
    IN = lambda n, s: P.dram(n, s, F32, "ExternalInput")
    xin = IN("xin", [NT, D])
    cT = IN("cT", [128, KD, 2])
    ada_w = IN("ada_w", [2, D, 6 * D])
    ada_bT = IN("ada_bT", [128, 2, 48])
    normT = IN("normT", [128, 5, KD])
    ev_w_in = IN("ev_w_in", [D, EV_COLS])
    ev_w_out = IN("ev_w_out", [D, D])
    shiftT = IN("shiftT", [128, 12, 3])
    rvec = IN("rvec", [128, 9, 4])
    lora_up = IN("lora_up", [128, 2, 512])
    g_up = IN("g_up", [128, 512])
    pool_w = IN("pool_w", [4, 128, 128])
    pool_scT = IN("pool_scT", [128, 4])
    pool_inv = IN("pool_inv", [4, NT])
    moe_r = IN("moe_r", [2, D, 36])
    moe_wg = IN("moe_wg", [2, 32, D, 512])
    moe_wu = IN("moe_wu", [2, 32, D, 512])
    moe_wd = IN("moe_wd", [2, 32, 512, D])
    od_w_in = IN("od_w_in", [D, OD_COLS])
    od_w_out = IN("od_w_out", [D, D])
    dlam = IN("dlam", [1, 256])
    sublnT = IN("sublnT", [128, 1])
    retgT = IN("retgT", [128, 4])
    rope_perm = IN("rope_perm", [128, 128])
    ropeC = IN("ropeC", [128, S])
    ropeS = IN("ropeS", [128, S])
    retD = IN("retD", [8, 128, 128])
    retq = IN("retq", [8, 128])
    retk = IN("retk", [128, 8])
    cident = IN("ident", [128, 128])
    cblk = IN("blk64", [128, 128])
    crmask = IN("rmask", [2, 128, 896])
    out = P.dram("out", [S, D], F32, "ExternalOutput")
    xTs = P.dram("xTs", [KD, 128, NT], F32)
    pTs = P.dram("pTs", [24, 128, NT], F32, "ExternalOutput" if dbg else "Internal")
    ymTs = P.dram("ymTs", [KD, 128, NT], BF16, "ExternalOutput" if dbg else "Internal")
    dbgs = {}

    def tap(name, t, shape):
        if dbg:
            d = P.dram("dbg_" + name, shape, F32, "ExternalOutput")
            P.dma('sp', d, t.ap, reads=[t], is_output=True)

    ident = P.alloc([128, 128]); P.dma('sp', ident.ap, cident, writes=[ident])
    identb = P.alloc([128, 128], BF16); P.dma('pool', identb.ap, cident, writes=[identb])
    blk = P.alloc([128, 128]); P.dma('sp', blk.ap, cblk, writes=[blk])
    onesD = P.alloc([128, 128]); P.memset('pool', onesD.ap, 1.0 / D, writes=[onesD])
    normv = P.alloc([128, 5, KD]); P.dma('sp', normv.ap, normT, writes=[normv])
    mod = P.alloc([128, 2, 48, 2])
    m0 = P.mark()
    sc = P.alloc([128, KD, 2]); P.dma('sp', sc.ap, cT, writes=[sc])
    P.act(sc.ap, sc.ap, AF.Silu, reads=[sc], writes=[sc])
    adab = P.alloc([128, 2, 48]); P.dma('sp', adab.ap, ada_bT, writes=[adab])
    wbuf = [P.alloc([128, KD, 1024]) for _ in range(2)]
    for li in range(2):
        for blkc in range(6):
            wb = wbuf[(li * 6 + blkc) % 2]
            P.dma_group('sp' if blkc % 2 == 0 else 'act',
                        [(wb.ap[:, k, :], ada_w[li, k * 128:(k + 1) * 128, blkc * 1024:(blkc + 1) * 1024]) for k in range(KD)],
                        writes=[wb])
            for cc in range(8):
                ps = P.psum('a')
                for k in range(KD):
                    P.mm(ps.ap[:, 0:2], wb.ap[:, k, cc * 128:(cc + 1) * 128], sc.ap[:, k, :],
                         start=(k == 0), stop=(k == KD - 1), reads=[wb, sc], writes=[ps])
                j = blkc * 8 + cc
                P.stt('dve', mod.ap[:, li, j, :], ps.ap[:, 0:2], 1.0,
                      adab.ap[:, li, j:j + 1].to_broadcast([128, 2]), ALU.mult, ALU.add,
                      reads=[ps, adab], writes=[mod])
    P.release(m0)
    AB = P.alloc([128, 2, 2, 2, KD, 2])
    for li in range(2):
        for sub in range(2):
            shc = 24 * sub
            scc = 24 * sub + 8
            nidx = li if sub == 0 else 2 + li
            for w in range(2):
                P.ts('dve', AB.ap[:, li, sub, 0, :, w], mod.ap[:, li, scc:scc + 8, w], 1.0, None, ALU.add,
                     reads=[mod], writes=[AB])
                P.tt('dve', AB.ap[:, li, sub, 0, :, w], AB.ap[:, li, sub, 0, :, w], normv.ap[:, nidx, :], ALU.mult,
                     reads=[AB, normv], writes=[AB])
                P.cp('dve', AB.ap[:, li, sub, 1, :, w], mod.ap[:, li, shc:shc + 8, w], reads=[mod], writes=[AB])
    if dbg:
        tap("mod", mod, [128, 2, 48, 2])

    def norm_mod(xT, TT, li, sub, w, hb, sq, rs, final=False):
        P.act(sq.ap[:, :, 0:TT], xT.ap[:, :, 0:TT], AF.Square, reads=[xT], writes=[sq])
        ps = P.psum('a')
        for k in range(KD):
            P.mm(ps.ap[:, 0:TT], onesD.ap, sq.ap[:, k, 0:TT], start=(k == 0), stop=(k == KD - 1),
                 reads=[onesD, sq], writes=[ps])
        P.ts('dve', rs.ap[:, 0:TT], ps.ap[:, 0:TT], 1e-6, None, ALU.add, reads=[ps], writes=[rs])
        P.act(rs.ap[:, 0:TT], rs.ap[:, 0:TT], AF.Ln, reads=[rs], writes=[rs])
        P.act(rs.ap[:, 0:TT], rs.ap[:, 0:TT], AF.Exp, reads=[rs], writes=[rs], scale=-0.5)
        P.tt('dve', sq.ap[:, :, 0:TT], xT.ap[:, :, 0:TT], rs.ap[:, None, 0:TT].to_broadcast([128, KD, TT]), ALU.mult,
             reads=[xT, rs], writes=[sq])
        for k in range(KD):
            if final:
                P.ts('dve' if k % 2 else 'pool', hb.ap[:, k, 0:TT], sq.ap[:, k, 0:TT], normv.ap[:, 4, k:k + 1], None, ALU.mult,
                     reads=[sq, normv], writes=[hb])
            else:
                P.ts('dve' if k % 2 else 'pool', hb.ap[:, k, 0:TT], sq.ap[:, k, 0:TT], AB.ap[:, li, sub, 0, k, w:w + 1],
                     AB.ap[:, li, sub, 1, k, w:w + 1], ALU.mult, ALU.add, reads=[sq, AB], writes=[hb])

    def load_w_bf(dst, src, K):
        P.dma_group('pool', [(dst.ap[:, k, :], src[k * 128:(k + 1) * 128, :]) for k in range(K)], writes=[dst])

    def phase_inproj(li, w_in_dram, ncols, first):
        m = P.mark()
        NCC = ncols // 128
        Win = P.alloc([128, KD, ncols], BF16)
        load_w_bf(Win, w_in_dram, KD)
        xtok = [P.alloc([128, D]) for _ in range(2)]
        xT = [P.alloc([128, KD, 512]) for _ in range(2)]
        hb = [P.alloc([128, KD, 512], BF16) for _ in range(2)]
        sq = P.alloc([128, KD, 512]); rs = P.alloc([128, 512])
        pst = [P.alloc([128, 6, 512]) for _ in range(2)]
        for ti, (t0, TT, isc) in enumerate(tiles):
            xt = xT[ti % 2]
            if first:
                for b in range(TT // 128):
                    xk = xtok[b % 2]
                    P.dma('sp', xk.ap, xin[t0 + b * 128:t0 + (b + 1) * 128, :], writes=[xk])
                    for half in range(2):
                        ps = P.psum('a')
                        for j in range(4):
                            k = half * 4 + j
                            P.tr(ps.ap[:, j * 128:(j + 1) * 128], xk.ap[:, k * 128:(k + 1) * 128], ident.ap,
                                 reads=[xk, ident], writes=[ps])
                        P.cp('dve' if half else 'act', xt.ap[:, half * 4:half * 4 + 4, b * 128:(b + 1) * 128],
                             ps.ap.rearrange("p (a b) -> p a b", a=4), reads=[ps], writes=[xt])
                P.dma_group('act', [(xTs[k, :, t0:t0 + TT], xt.ap[:, k, 0:TT]) for k in range(KD)], reads=[xt])
            else:
                P.dma_group('sp', [(xt.ap[:, k, 0:TT], xTs[k, :, t0:t0 + TT]) for k in range(KD)], writes=[xt])
            h = hb[ti % 2]
            norm_mod(xt, TT, li, 0, isc, h, sq, rs)
            for g in range(NCC // 6):
                st = pst[g % 2]
                for c6 in range(6):
                    cc = g * 6 + c6
                    ps = P.psum('a')
                    for k in range(KD):
                        P.mm(ps.ap[:, 0:TT], Win.ap[:, k, cc * 128:(cc + 1) * 128], h.ap[:, k, 0:TT],
                             start=(k == 0), stop=(k == KD - 1), reads=[Win, h], writes=[ps])
                    P.cp('act' if c6 % 2 else 'dve', st.ap[:, c6, 0:TT], ps.ap[:, 0:TT], reads=[ps], writes=[st])
                P.dma('sp', pTs[g * 6:(g + 1) * 6, :, t0:t0 + TT].rearrange("c p t -> p c t"), st.ap[:, :, 0:TT],
                      reads=[st])
        P.release(m)


    def phase_moe(li, lat_only):
        m = P.mark()
        tl = [t for t in tiles if not (lat_only and t[2])]
        xres = P.alloc([128, KD, NT])
        hfT = P.alloc([128, KD, NT], BF16)
        gT = P.alloc([32, NT])
        wr = P.alloc([128, KD, 36])
        P.dma('sp', wr.ap, moe_r[li].rearrange("(k p) n -> p k n", p=128), writes=[wr])
        m2 = P.mark()
        sq = P.alloc([128, KD, 512]); rs = P.alloc([128, 512])
        lg = P.alloc([128, 36]); oh = P.alloc([128, 4]); st_ = P.alloc([128, 16]); les = P.alloc([128, 8])
        mk1 = P.alloc([128, 8]); mk2 = P.alloc([128, 8]); g8 = P.alloc([128, 8]); g32 = P.alloc([128, 4, 8])
        ex4 = P.alloc([128, 4])
        for (t0, TT, isc) in tl:
            xt = T(xres.ap[:, :, t0:t0 + TT]); hb = T(hfT.ap[:, :, t0:t0 + TT])
            P.dma_group('sp', [(xt.ap[:, k, :], xTs[k, :, t0:t0 + TT]) for k in range(KD)], writes=[xt, xres])
            norm_mod(xt, TT, li, 1, isc, hb, sq, rs)
            for k in range(KD):
                P.ts('dve', sq.ap[:, k, 0:TT], sq.ap[:, k, 0:TT], AB.ap[:, li, 1, 0, k, isc:isc + 1],
                     AB.ap[:, li, 1, 1, k, isc:isc + 1], ALU.mult, ALU.add, reads=[sq, AB], writes=[sq])
            for b in range(TT // 128):
                bs = slice(b * 128, (b + 1) * 128)
                ps = P.psum('a')
                for k in range(KD):
                    P.mm(ps.ap[:, 0:36], sq.ap[:, k, bs], wr.ap[:, k, :], start=(k == 0), stop=(k == KD - 1),
                         reads=[sq, wr], writes=[ps])
                P.cp('dve', lg.ap, ps.ap[:, 0:36], reads=[ps], writes=[lg])
                def red(out, in_, op):
                    return P.op('dve', lambda e: e.tensor_reduce(out=out, in_=in_, axis=AX.X, op=op), [lg, les, ex4, st_], [st_])
                P.op('dve', lambda e: e.tensor_reduce(out=st_.ap[:, 0:1], in_=lg.ap[:, 0:4], axis=AX.X, op=ALU.max), [lg], [st_])
                P.ts('dve', oh.ap, lg.ap[:, 0:4], st_.ap[:, 0:1], None, ALU.is_equal, reads=[lg, st_], writes=[oh])
                P.ts('dve', st_.ap[:, 1:2], st_.ap[:, 0:1], -1.0, None, ALU.mult, reads=[st_], writes=[st_])
                P.act(ex4.ap, lg.ap[:, 0:4], AF.Exp, reads=[lg, st_], writes=[ex4], bias=st_.ap[:, 1:2])
                P.op('dve', lambda e: e.tensor_reduce(out=st_.ap[:, 2:3], in_=ex4.ap, axis=AX.X, op=ALU.add), [ex4], [st_])
                P.op('dve', lambda e: e.reciprocal(out=st_.ap[:, 3:4], in_=st_.ap[:, 2:3]), [st_], [st_])
                P.ts('dve', les.ap, lg.ap[:, 4:12], oh.ap[:, 0:1], None, ALU.mult, reads=[lg, oh], writes=[les])
                for g in range(1, 4):
                    P.stt('dve', les.ap, lg.ap[:, 4 + 8 * g:12 + 8 * g], oh.ap[:, g:g + 1], les.ap, ALU.mult, ALU.add,
                          reads=[lg, oh, les], writes=[les])
                P.op('dve', lambda e: e.tensor_reduce(out=st_.ap[:, 4:5], in_=les.ap, axis=AX.X, op=ALU.max), [les], [st_])
                P.ts('dve', mk1.ap, les.ap, st_.ap[:, 4:5], None, ALU.is_equal, reads=[les, st_], writes=[mk1])
                P.stt('dve', g8.ap, mk1.ap, -1e30, les.ap, ALU.mult, ALU.add, reads=[mk1, les], writes=[g8])
                P.op('dve', lambda e: e.tensor_reduce(out=st_.ap[:, 5:6], in_=g8.ap, axis=AX.X, op=ALU.max), [g8], [st_])
                P.ts('dve', mk2.ap, g8.ap, st_.ap[:, 5:6], None, ALU.is_equal, reads=[g8, st_], writes=[mk2])
                P.tt('dve', st_.ap[:, 6:7], st_.ap[:, 5:6], st_.ap[:, 4:5], ALU.subtract, reads=[st_], writes=[st_])
                P.act(st_.ap[:, 7:8], st_.ap[:, 6:7], AF.Exp, reads=[st_], writes=[st_])
                P.ts('dve', st_.ap[:, 8:9], st_.ap[:, 7:8], 1.0, None, ALU.add, reads=[st_], writes=[st_])
                P.op('dve', lambda e: e.reciprocal(out=st_.ap[:, 9:10], in_=st_.ap[:, 8:9]), [st_], [st_])
                P.tt('dve', st_.ap[:, 10:11], st_.ap[:, 9:10], st_.ap[:, 3:4], ALU.mult, reads=[st_], writes=[st_])
                P.tt('dve', st_.ap[:, 11:12], st_.ap[:, 10:11], st_.ap[:, 7:8], ALU.mult, reads=[st_], writes=[st_])
                P.ts('dve', g8.ap, mk1.ap, st_.ap[:, 10:11], None, ALU.mult, reads=[mk1, st_], writes=[g8])
                P.stt('dve', g8.ap, mk2.ap, st_.ap[:, 11:12], g8.ap, ALU.mult, ALU.add, reads=[mk2, st_, g8], writes=[g8])
                for g in range(4):
                    P.ts('dve', g32.ap[:, g, :], g8.ap, oh.ap[:, g:g + 1], None, ALU.mult, reads=[g8, oh], writes=[g32])
                pt = P.psum('a')
                P.tr(pt.ap[0:32, 0:128], g32.ap.rearrange("p a b -> p (a b)"), ident.ap, reads=[g32, ident], writes=[pt])
                P.cp('act', gT.ap[:, t0 + b * 128:t0 + (b + 1) * 128], pt.ap[0:32, 0:128], reads=[pt], writes=[gT])
        P.release(m2)
        if dbg:
            tap(f"gT{li}", gT, [32, NT])
        Wg = [P.alloc([128, KD, 512], BF16) for _ in range(2)]
        Wu = [P.alloc([128, KD, 512], BF16) for _ in range(2)]
        Wd = [P.alloc([128, 4, D], BF16) for _ in range(2)]
        selt = [P.alloc([32, 128]) for _ in range(2)]
        gbc = [P.alloc([128, 512]) for _ in range(2)]
        sgl = [P.alloc([128, 512]) for _ in range(2)]
        a1 = [P.alloc([128, 512]) for _ in range(2)]
        actT = [P.alloc([128, 4, 512], BF16) for _ in range(2)]
        it = 0
        pend = [None]
        def load_gu(e):
            wg = Wg[e % 2]; wu = Wu[e % 2]
            P.dma_group('pool', [(wg.ap[:, k, :], moe_wg[li, e, k * 128:(k + 1) * 128, :]) for k in range(KD)], writes=[wg])
            P.dma_group('pool', [(wu.ap[:, k, :], moe_wu[li, e, k * 128:(k + 1) * 128, :]) for k in range(KD)], writes=[wu])

        def load_d(e):
            wd = Wd[e % 2]
            P.dma_group('pool', [(wd.ap[:, k, :], moe_wd[li, e, k * 128:(k + 1) * 128, :]) for k in range(4)], writes=[wd])

        load_gu(0)
        load_d(0)
        for e in range(32):
            wg = Wg[e % 2]; wu = Wu[e % 2]; wd = Wd[e % 2]; se = selt[e % 2]
            P.cp('pool', se.ap, ident.ap[0:32, e:e + 1].to_broadcast([32, 128]), reads=[ident], writes=[se])
            if e + 1 < 32:
                load_gu(e + 1)
            for tix, (t0, TT, isc) in enumerate(tl):
                it += 1
                gb = gbc[it % 2]; at = actT[it % 2]
                pg_ = P.psum('b')
                P.mm(pg_.ap[:, 0:TT], se.ap, gT.ap[:, t0:t0 + TT], reads=[se, gT], writes=[pg_])
                P.cp('act', gb.ap[:, 0:TT], pg_.ap[:, 0:TT], reads=[pg_], writes=[gb])
                for fc in range(4):
                    fs = slice(fc * 128, (fc + 1) * 128)
                    pg = P.psum('a'); pu = P.psum('a')
                    for k in range(KD):
                        P.mm(pg.ap[:, 0:TT], wg.ap[:, k, fs], hfT.ap[:, k, t0:t0 + TT], start=(k == 0), stop=(k == KD - 1),
                             reads=[wg, hfT], writes=[pg])
                    for k in range(KD):
                        P.mm(pu.ap[:, 0:TT], wu.ap[:, k, fs], hfT.ap[:, k, t0:t0 + TT], start=(k == 0), stop=(k == KD - 1),
                             reads=[wu, hfT], writes=[pu])
                    sg_ = sgl[fc % 2]; a_ = a1[fc % 2]
                    P.act(sg_.ap[:, 0:TT], pg.ap[:, 0:TT], AF.Silu, reads=[pg], writes=[sg_])
                    P.tt('dve', a_.ap[:, 0:TT], pu.ap[:, 0:TT], sg_.ap[:, 0:TT], ALU.mult, reads=[pu, sg_], writes=[a_])
                    P.tt('pool', at.ap[:, fc, 0:TT], a_.ap[:, 0:TT], gb.ap[:, 0:TT], ALU.mult, reads=[a_, gb], writes=[at])
                def down(wd=wd, at=at, t0=t0, TT=TT, isc=isc):
                    for dc in range(KD):
                        po = P.psum('c')
                        for fc in range(4):
                            P.mm(po.ap[:, 0:TT], wd.ap[:, fc, dc * 128:(dc + 1) * 128], at.ap[:, fc, 0:TT],
                                 start=(fc == 0), stop=(fc == 3), reads=[wd, at], writes=[po])
                        P.stt('dve', xres.ap[:, dc, t0:t0 + TT], po.ap[:, 0:TT], mod.ap[:, li, 40 + dc, isc:isc + 1],
                              xres.ap[:, dc, t0:t0 + TT], ALU.mult, ALU.add, reads=[po, mod, xres], writes=[xres])
                if pend[0] is not None:
                    pend[0]()
                pend[0] = down
                if tix == 0 and e + 1 < 32:
                    load_d(e + 1)
        pend[0]()
        for (t0, TT, isc) in tl:
            P.dma_group('sp', [(xTs[k, :, t0:t0 + TT], xres.ap[:, k, t0:t0 + TT]) for k in range(KD)], reads=[xres])
        P.release(m)


    LAM_INIT = 0.8 - 0.6 * math.exp(-0.3 * 1)
    RET_G128 = []
    for h_ in range(4):
        lgf = [math.log(1.0 - 2.0 ** (-5.0 - j)) for j in range(4)]
        RET_G128.append((math.exp(lgf[h_] * 128), math.exp(lgf[3 - h_] * 128)))

    def phase_l1mix():
        m = P.mark()
        LT = [(t0 - C, TT) for (t0, TT, isc) in tiles if not isc]
        perm = P.alloc([128, 128]); P.dma('sp', perm.ap, rope_perm, writes=[perm])
        rc_ = P.alloc([128, S]); P.dma('sp', rc_.ap, ropeC, writes=[rc_])
        rs_ = P.alloc([128, S]); P.dma('sp', rs_.ap, ropeS, writes=[rs_])
        ones128 = P.alloc([128, 128]); P.memset('pool', ones128.ap, 1.0 / 128, writes=[ones128])
        onesb = P.alloc([128, 128], BF16); P.memset('pool', onesb.ap, 1.0, writes=[onesb])
        subg = P.alloc([128, 1]); P.dma('sp', subg.ap, sublnT, writes=[subg])
        P.ts('dve', subg.ap, subg.ap, 1.0 - LAM_INIT, None, ALU.mult, reads=[subg], writes=[subg])
        rgv = P.alloc([128, 4]); P.dma('sp', rgv.ap, retgT, writes=[rgv])
        rkv = P.alloc([128, 8]); P.dma('sp', rkv.ap, retk, writes=[rkv])
        dl = P.alloc([1, 4, 64]); P.dma('sp', dl.ap, dlam.rearrange("o (a b) -> o a b", a=4), writes=[dl])
        pr2 = P.alloc([1, 2, 64]); s2 = P.alloc([1, 4]); nlam = P.alloc([128, 1]); onesr = P.alloc([1, 128])
        P.memset('dve', onesr.ap, 1.0, writes=[onesr])
        P.tt('dve', pr2.ap[:, 0, :], dl.ap[:, 0, :], dl.ap[:, 1, :], ALU.mult, reads=[dl], writes=[pr2])
        P.tt('dve', pr2.ap[:, 1, :], dl.ap[:, 2, :], dl.ap[:, 3, :], ALU.mult, reads=[dl], writes=[pr2])
        P.op('dve', lambda e: e.tensor_reduce(out=s2.ap[:, 0:2], in_=pr2.ap, axis=AX.X, op=ALU.add), [pr2], [s2])
        P.act(s2.ap[:, 0:2], s2.ap[:, 0:2], AF.Exp, reads=[s2], writes=[s2])
        P.tt('dve', s2.ap[:, 2:3], s2.ap[:, 1:2], s2.ap[:, 0:1], ALU.subtract, reads=[s2], writes=[s2])
        P.ts('dve', s2.ap[:, 3:4], s2.ap[:, 2:3], -LAM_INIT, None, ALU.add, reads=[s2], writes=[s2])
        psl = P.psum('a')
        P.mm(psl.ap[:, 0:1], onesr.ap, s2.ap[:, 3:4], reads=[onesr, s2], writes=[psl])
        P.cp('dve', nlam.ap, psl.ap[:, 0:1], reads=[psl], writes=[nlam])
        raw = [P.alloc([128, NT]) for _ in range(2)]
        vT = P.alloc([128, NT])
        kT = P.alloc([128, NT], BF16); qT = P.alloc([128, S], BF16)
        Vtok = P.alloc([128, NCH, 128], BF16)
        t512 = [P.alloc([128, 512]) for _ in range(6)]
        pTb = [P.alloc([128, 512], BF16) for _ in range(5)]
        ymt = [P.alloc([128, 512], BF16) for _ in range(2)]
        cnt = [0]

        def rr(lst):
            cnt[0] += 1
            return lst[cnt[0] % len(lst)]

        def rope(dst, dcol0, src, scol0, scale=None):
            for (l0, TT) in LT:
                ps = P.psum('a')
                P.mm(ps.ap[:, 0:TT], perm.ap, src.ap[:, scol0 + l0:scol0 + l0 + TT], reads=[perm, src], writes=[ps])
                a = rr(t512); b = rr(t512)
                P.tt('dve', a.ap[:, 0:TT], ps.ap[:, 0:TT], rs_.ap[:, l0:l0 + TT], ALU.mult, reads=[ps, rs_], writes=[a])
                P.tt('pool', b.ap[:, 0:TT], src.ap[:, scol0 + l0:scol0 + l0 + TT], rc_.ap[:, l0:l0 + TT], ALU.mult, reads=[src, rc_], writes=[b])
                if scale is None:
                    P.tt('dve', dst.ap[:, dcol0 + l0:dcol0 + l0 + TT], a.ap[:, 0:TT], b.ap[:, 0:TT], ALU.add, reads=[a, b], writes=[dst])
                else:
                    P.tt('dve', a.ap[:, 0:TT], a.ap[:, 0:TT], b.ap[:, 0:TT], ALU.add, reads=[a, b], writes=[a])
                    P.ts('dve', dst.ap[:, dcol0 + l0:dcol0 + l0 + TT], a.ap[:, 0:TT], scale, None, ALU.mult, reads=[a], writes=[dst])

        def make_vtok(vsrc):
            for c4 in range(0, NCH, 4):
                n = min(4, NCH - c4)
                ps = P.psum('a')
                for j in range(n):
                    P.tr(ps.ap[:, j * 128:(j + 1) * 128], vsrc.ap[:, (c4 + j) * 128:(c4 + j + 1) * 128], ident.ap,
                         reads=[vsrc, ident], writes=[ps])
                P.cp('act', Vtok.ap[:, c4:c4 + n, :], ps.ap[:, 0:n * 128].rearrange("p (a b) -> p a b", a=n), reads=[ps], writes=[Vtok])

        def post_norm(o, TT, eps, gain_ap, extra, dst_chunk, l0):
            sq_ = rr(t512)
            P.act(sq_.ap[:, 0:TT], o.ap[:, 0:TT], AF.Square, reads=[o], writes=[sq_])
            ps = P.psum('a')
            P.mm(ps.ap[:, 0:TT], ones128.ap, sq_.ap[:, 0:TT], reads=[ones128, sq_], writes=[ps])
            P.ts('dve', sq_.ap[:, 0:TT], ps.ap[:, 0:TT], eps, None, ALU.add, reads=[ps], writes=[sq_])
            P.act(sq_.ap[:, 0:TT], sq_.ap[:, 0:TT], AF.Ln, reads=[sq_], writes=[sq_])
            P.act(sq_.ap[:, 0:TT], sq_.ap[:, 0:TT], AF.Exp, reads=[sq_], writes=[sq_], scale=-0.5)
            P.stt('dve', sq_.ap[:, 0:TT], o.ap[:, 0:TT], gain_ap, sq_.ap[:, 0:TT], ALU.mult, ALU.mult, reads=[o, sq_, subg, rgv], writes=[sq_])
            ym = rr(ymt)
            if extra is None:
                P.cp('dve', ym.ap[:, 0:TT], sq_.ap[:, 0:TT], reads=[sq_], writes=[ym])
            else:
                P.tt('dve', ym.ap[:, 0:TT], sq_.ap[:, 0:TT], extra, ALU.mult, reads=[sq_, raw[0], raw[1]], writes=[ym])
            P.dma('sp', ymTs[dst_chunk, :, C + l0:C + l0 + TT], ym.ap[:, 0:TT], reads=[ym])

        A1, S1, A2, S2 = P.PS[0], P.PS[1], P.PS[2], P.PS[3]
        for h in range(4):
            P.dma('sp', raw[0].ap, pTs[h], writes=[raw[0]])
            P.dma('act', raw[1].ap[:, 0:S], pTs[14 + h][:, C:NT], writes=[raw[1]])
            P.dma('sp', vT.ap, pTs[4 + h], writes=[vT])
            P.cp('pool', kT.ap[:, 0:C], raw[0].ap[:, 0:C], reads=[raw[0]], writes=[kT])
            rope(kT, C, raw[0], C)
            rope(qT, 0, raw[1], 0)
            make_vtok(vT)
            for (l0, TT) in LT:
                pendq = []

                def emit_av(kc, br, pt, TT=TT):
                    Ab, Sb = ((A1, S1), (A2, S2))[br]
                    P.mm(Ab.ap[:, 0:TT], Vtok.ap[:, kc, :], pt.ap[:, 0:TT], start=(kc == 0), stop=(kc == NCH - 1), reads=[Vtok, pt], writes=[Ab])
                    P.mm(Sb.ap[:, 0:TT], onesb.ap, pt.ap[:, 0:TT], start=(kc == 0), stop=(kc == NCH - 1), reads=[onesb, pt], writes=[Sb])
                for kc in range(NCH):
                    for br in range(2):
                        hsb = slice(br * 64, br * 64 + 64)
                        sc = P.psum('c')
                        P.mm(sc.ap[:, 0:TT], kT.ap[hsb, kc * 128:(kc + 1) * 128], qT.ap[hsb, l0:l0 + TT], reads=[kT, qT], writes=[sc])
                        pt = rr(pTb)
                        P.act(pt.ap[:, 0:TT], sc.ap[:, 0:TT], AF.Exp, reads=[sc], writes=[pt], scale=0.125)
                        pendq.append((kc, br, pt))
                        if len(pendq) > 2:
                            emit_av(*pendq.pop(0))
                while pendq:
                    emit_av(*pendq.pop(0))
                r1 = rr(t512); o1 = rr(t512); r2 = rr(t512); o2 = rr(t512)
                P.op('dve', lambda e, r1=r1, TT=TT: e.reciprocal(out=r1.ap[:, 0:TT], in_=S1.ap[:, 0:TT]), [S1], [r1])
                P.tt('dve', o1.ap[:, 0:TT], A1.ap[:, 0:TT], r1.ap[:, 0:TT], ALU.mult, reads=[A1, r1], writes=[o1])
                P.op('dve', lambda e, r2=r2, TT=TT: e.reciprocal(out=r2.ap[:, 0:TT], in_=S2.ap[:, 0:TT]), [S2], [r2])
                P.tt('dve', o2.ap[:, 0:TT], A2.ap[:, 0:TT], r2.ap[:, 0:TT], ALU.mult, reads=[A2, r2], writes=[o2])
                P.stt('dve', o1.ap[:, 0:TT], o2.ap[:, 0:TT], nlam.ap[:, 0:1], o1.ap[:, 0:TT], ALU.mult, ALU.add, reads=[o2, nlam, o1], writes=[o1])
                post_norm(o1, TT, 1e-5, subg.ap[:, 0:1], None, h, l0)
        kTp = kT
        qTp = qT
        ktok = P.alloc([128, NCH, 128], BF16)
        oT = P.alloc([128, S])
        Rf = P.alloc([128, 128]); Rb = P.alloc([128, 128], BF16)
        dm = P.alloc([128, 128]); qrow = P.alloc([128, 128])
        innm = [P.alloc([128, 128], BF16) for _ in range(2)]
        qd = [P.alloc([128, 128], BF16) for _ in range(2)]
        kd = [P.alloc([128, 64], BF16) for _ in range(2)]
        for h in range(4):
            hq = h % 2
            hsq = slice(hq * 64, hq * 64 + 64)
            if hq == 0:
                P.dma('sp', raw[0].ap, pTs[8 + h // 2], writes=[raw[0]])
                P.dma('act', raw[1].ap[:, 0:S], pTs[18 + h // 2][:, C:NT], writes=[raw[1]])
                P.ts('pool', kTp.ap[:, 0:C], raw[0].ap[:, 0:C], 0.125, None, ALU.mult, reads=[raw[0]], writes=[kTp])
                rope(kTp, C, raw[0], C, scale=0.125)
                rope(qTp, 0, raw[1], 0)
                for c4 in range(0, NCH, 4):
                    n = min(4, NCH - c4)
                    ps = P.psum('a'); psb_ = ps.ap.bitcast(BF16)
                    for j in range(n):
                        P.tr(psb_[:, j * 128:(j + 1) * 128], kTp.ap[:, (c4 + j) * 128:(c4 + j + 1) * 128], identb.ap,
                             reads=[kTp, identb], writes=[ps])
                    P.cp('act', ktok.ap[:, c4:c4 + n, :], psb_[:, 0:n * 128].rearrange("p (a b) -> p a b", a=n), reads=[ps], writes=[ktok])
            P.dma('sp', vT.ap, pTs[10 + h], writes=[vT])
            make_vtok(vT)
            for d in range(2):
                hd = h * 2 + d
                g128 = RET_G128[h][d]
                P.dma('sp', dm.ap, retD[hd], writes=[dm])
                P.dma('sp', qrow.ap, retq[hd:hd + 1, :].partition_broadcast(128), writes=[qrow])
                P.memset('dve', Rf.ap, 0.0, writes=[Rf]); P.memset('pool', Rb.ap, 0.0, writes=[Rb])
                order = list(range(0, NCH)) if d == 0 else list(range(CCH - 1, -1, -1)) + list(range(NCH - 1, CCH - 1, -1))
                for oi, c in enumerate(order):
                    ksl = slice(c * 128, (c + 1) * 128)
                    if c >= CCH:
                        i0 = c * 128 - C
                        im = rr(innm); q_ = rr(qd)
                        ps1 = P.psum('c')
                        P.mm(ps1.ap[:, 0:128], kTp.ap[hsq, ksl], qTp.ap[hsq, i0:i0 + 128], reads=[kTp, qTp], writes=[ps1])
                        P.tt('dve', im.ap, ps1.ap[:, 0:128], dm.ap, ALU.mult, reads=[ps1, dm], writes=[im])
                        P.tt('pool', q_.ap[hsq, :], qTp.ap[hsq, i0:i0 + 128], qrow.ap[hsq, :], ALU.mult, reads=[qTp, qrow], writes=[q_])
                        ps2 = P.psum('c')
                        P.mm(ps2.ap[:, 0:128], Vtok.ap[:, c, :], im.ap, start=True, stop=False, reads=[Vtok, im], writes=[ps2])
                        P.mm(ps2.ap[:, 0:128], Rb.ap[hsq, :], q_.ap[hsq, :], start=False, stop=True, reads=[Rb, q_], writes=[ps2])
                        if d == 0:
                            P.cp('act', oT.ap[:, i0:i0 + 128], ps2.ap[:, 0:128], reads=[ps2], writes=[oT])
                        else:
                            P.tt('dve', oT.ap[:, i0:i0 + 128], ps2.ap[:, 0:128], oT.ap[:, i0:i0 + 128], ALU.add, reads=[ps2, oT], writes=[oT])
                    if oi < len(order) - 1:
                        k_ = rr(kd)
                        P.ts('pool', k_.ap, ktok.ap[:, c, hsq], rkv.ap[:, hd:hd + 1], None, ALU.mult, reads=[ktok, rkv], writes=[k_])
                        ps3 = P.psum('c')
                        P.mm(ps3.ap[hsq, 0:128], k_.ap, Vtok.ap[:, c, :], reads=[k_, Vtok], writes=[ps3])
                        P.stt('dve', Rf.ap[hsq, :], Rf.ap[hsq, :], g128, ps3.ap[hsq, 0:128], ALU.mult, ALU.add, reads=[Rf, ps3], writes=[Rf])
                        P.cp('act', Rb.ap[hsq, :], Rf.ap[hsq, :], reads=[Rf], writes=[Rb])
            P.dma('act', raw[1].ap[:, 0:S], pTs[20 + h][:, C:NT], writes=[raw[1]]) if hq == 1 else \
                P.dma('act', raw[0].ap[:, 0:S], pTs[20 + h][:, C:NT], writes=[raw[0]])
            gsrc = raw[1] if hq == 1 else raw[0]
            P.act(gsrc.ap[:, 0:S], gsrc.ap[:, 0:S], AF.Silu, reads=[gsrc], writes=[gsrc])
            for (l0, TT) in LT:
                ot = T(oT.ap[:, l0:l0 + TT])
                ot.lw = oT.lw
                post_norm(ot, TT, 1e-6, rgv.ap[:, h:h + 1], gsrc.ap[:, l0:l0 + TT], 4 + h, l0)
                oT.rd.update(ot.rd)
        P.release(m)

    if STOP_AFTER == 'mod':
        return P, locals()
    phase_inproj(0, ev_w_in, EV_COLS, True)


    def phase_rwkv():
        m = P.mark()
        DIN, DCH, DST = RW_DT[:3]
        DCN = RW_DT[3] if len(RW_DT) > 3 else DCH
        idcn = ident if DCN == F32 else identb
        idch = ident if DCH == F32 else identb
        idin = ident if DIN == F32 else identb
        seqs = [(0, C), (C, NT)]
        lup = P.alloc([128, 2, 512], BF16); P.dma('pool', lup.ap, lora_up, writes=[lup])
        gup = P.alloc([128, 512], BF16); P.dma('pool', gup.ap, g_up, writes=[gup])
        rv = P.alloc([128, 9, 4]); P.dma('sp', rv.ap, rvec, writes=[rv])
        shv = P.alloc([128, 12, 3]); P.dma('sp', shv.ap, shiftT, writes=[shv])
        rmk = P.alloc([128, 2, 896])
        for d in range(2):
            P.dma('sp', rmk.ap[:, d, :], crmask[d], writes=[rmk])
        omka = P.alloc([128, 4])
        P.ts('dve', omka.ap, rv.ap[:, 5, :], -1.0, 1.0, ALU.mult, ALU.add, reads=[rv], writes=[omka])
        tmpA = P.alloc([128, NT]); tmpB = P.alloc([128, NT])
        wdad = P.alloc([128, NT], BF16); sg = P.alloc([128, NT], BF16)
        P.dma('sp', tmpA.ap, pTs[8], writes=[tmpA])
        P.act(wdad.ap[0:64, :], tmpA.ap[0:64, :], AF.Tanh, reads=[tmpA], writes=[wdad])
        P.cp('dve', wdad.ap[64:128, :], tmpA.ap[64:128, :], reads=[tmpA], writes=[wdad])
        P.dma('sp', tmpB.ap, pTs[13], writes=[tmpB])
        P.act(sg.ap, tmpB.ap, AF.Sigmoid, reads=[tmpB], writes=[sg])
        kc = P.alloc([128, NT]); lw = [P.alloc([128, NT]) for _ in range(2)]
        vc = P.alloc([128, NT], DIN); rc = P.alloc([128, NT], DIN); kk = P.alloc([128, NT], DIN)
        kt = [P.alloc([128, NT], DIN) for _ in range(2)]; bb = [P.alloc([128, NT], DIN) for _ in range(2)]
        MTb = P.alloc([128, 2, NCH, 128], DST); P.memset('pool', MTb.ap, 0.0, writes=[MTb])
        Sbk = P.alloc([128, 2, NCH, 128], DST); P.memset('pool', Sbk.ap, 0.0, writes=[Sbk])
        Gst = P.alloc([128, 2, NCH, 64]); Qs = P.alloc([128, 2, NCH, 128], DST); Y0 = P.alloc([128, NCH, 128])
        Vpad = [P.alloc([128, 2, 128], DCH) for _ in range(2)]
        P2p = [[P.alloc([128, 128], DCH) for _ in range(2)] for _ in range(2)]
        for t_ in Vpad + P2p[0] + P2p[1]:
            P.memset('pool', t_.ap, 0.0, writes=[t_])
        Vtk = [P.alloc([128, 128], DCH) for _ in range(2)]
        lwtok = [P.alloc([128, 128]) for _ in range(2)]
        E1 = [P.alloc([128, 128]) for _ in range(2)]; E0 = [P.alloc([128, 128]) for _ in range(2)]
        Ei = [P.alloc([128, 128]) for _ in range(2)]; nWC = [P.alloc([128, 1]) for _ in range(2)]
        QR = [P.alloc([128, 2, 128], DCH) for _ in range(2)]
        Bt = [P.alloc([128, 128], DCH) for _ in range(2)]; Kt = [P.alloc([128, 128], DCH) for _ in range(2)]
        nBh = [P.alloc([128, 128], DCH) for _ in range(2)]; K2 = [P.alloc([128, 128], DCH) for _ in range(2)]
        TK = [P.alloc([128, 3, 128], DCH) for _ in range(2)]
        evA = [P.alloc([128, 256], DCH) for _ in range(2)]; evB = [P.alloc([128, 256], DCH) for _ in range(2)]
        Xr = [[P.alloc([128, 128], DCN) for _ in range(3)] for _ in range(2)]; XTr = [[P.alloc([128, 128], DCN) for _ in range(3)] for _ in range(2)]
        TTr = [[P.alloc([128, 128], DCN) for _ in range(3)] for _ in range(2)]
        TTf = [P.alloc([128, 128], DCH) for _ in range(2)]
        n2v = [P.alloc([128, 64], DCH) for _ in range(2)]; Pcat = [P.alloc([128, 128], DCH) for _ in range(2)]
        Sst = [P.alloc([128, 64], DST) for _ in range(2)]
        t512 = [P.alloc([128, 512]) for _ in range(4)]
        ymt = [P.alloc([128, 512], BF16) for _ in range(2)]
        cnt = [0]

        def rr(lst):
            cnt[0] += 1
            return lst[cnt[0] % len(lst)]

        def conv(dst, src, idx):
            for (a, b) in seqs:
                P.ts('dve', dst.ap[:, a:b], src.ap[:, a:b], shv.ap[:, idx, 1:2], None, ALU.mult, reads=[src, shv], writes=[dst])
                P.stt('dve', dst.ap[:, a + 1:b], src.ap[:, a:b - 1], shv.ap[:, idx, 0:1], dst.ap[:, a + 1:b], ALU.mult, ALU.add,
                      reads=[src, shv, dst], writes=[dst])
                P.stt('dve', dst.ap[:, a:b - 1], src.ap[:, a + 1:b], shv.ap[:, idx, 2:3], dst.ap[:, a:b - 1], ALU.mult, ALU.add,
                      reads=[src, shv, dst], writes=[dst])

        for pr in range(4):
            if RW_STOP == 0:
                break
            cs_ = slice(pr * 128, (pr + 1) * 128)
            P.dma('sp', tmpA.ap, pTs[pr], writes=[tmpA]); conv(kc, tmpA, pr)
            P.dma('sp', tmpB.ap, pTs[4 + pr], writes=[tmpB]); conv(vc, tmpB, 4 + pr)
            P.dma('sp', tmpA.ap, pTs[9 + pr], writes=[tmpA]); conv(rc, tmpA, 8 + pr)
            P.ts('dve', tmpA.ap, kc.ap, rv.ap[:, 4, pr:pr + 1], None, ALU.mult, reads=[kc, rv], writes=[tmpA])
            P.act(tmpB.ap, tmpA.ap, AF.Square, reads=[tmpA], writes=[tmpB])
            for (t0, TT, isc) in tiles:
                ps = P.psum('a')
                P.mm(ps.ap[:, 0:TT], blk.ap, tmpB.ap[:, t0:t0 + TT], reads=[blk, tmpB], writes=[ps])
                tq = rr(t512)
                P.ts('dve', tq.ap[:, 0:TT], ps.ap[:, 0:TT], 1e-12, None, ALU.max, reads=[ps], writes=[tq])
                P.act(tq.ap[:, 0:TT], tq.ap[:, 0:TT], AF.Ln, reads=[tq], writes=[tq])
                P.act(tq.ap[:, 0:TT], tq.ap[:, 0:TT], AF.Exp, reads=[tq], writes=[tq], scale=-0.5)
                P.tt('dve', kk.ap[:, t0:t0 + TT], tmpA.ap[:, t0:t0 + TT], tq.ap[:, 0:TT], ALU.mult, reads=[tmpA, tq], writes=[kk])
            for d in range(2):
                for (t0, TT, isc) in tiles:
                    ps = P.psum('a')
                    P.mm(ps.ap[:, 0:TT], lup.ap[0:64, d, cs_], wdad.ap[0:64, t0:t0 + TT], reads=[lup, wdad], writes=[ps])
                    P.act(lw[d].ap[:, t0:t0 + TT], ps.ap[:, 0:TT], AF.Sigmoid, reads=[ps, rv], writes=[lw[d]], bias=rv.ap[:, d, pr:pr + 1])
                    ps2 = P.psum('a')
                    P.mm(ps2.ap[:, 0:TT], lup.ap[64:128, d, cs_], wdad.ap[64:128, t0:t0 + TT], reads=[lup, wdad], writes=[ps2])
                    ta = rr(t512)
                    P.act(ta.ap[:, 0:TT], ps2.ap[:, 0:TT], AF.Sigmoid, reads=[ps2, rv], writes=[ta], bias=rv.ap[:, 2 + d, pr:pr + 1])
                    P.tt('dve', bb[d].ap[:, t0:t0 + TT], ta.ap[:, 0:TT], kk.ap[:, t0:t0 + TT], ALU.mult, reads=[ta, kk], writes=[bb[d]])
                    P.ts('dve', ta.ap[:, 0:TT], ta.ap[:, 0:TT], rv.ap[:, 5, pr:pr + 1], omka.ap[:, pr:pr + 1], ALU.mult, ALU.add,
                         reads=[ta, rv, omka], writes=[ta])
                    P.tt('dve', kt[d].ap[:, t0:t0 + TT], ta.ap[:, 0:TT], kc.ap[:, t0:t0 + TT], ALU.mult, reads=[ta, kc], writes=[kt[d]])
                P.ts('pool', lw[d].ap, lw[d].ap, -W_DECAY_SCALE, None, ALU.mult, reads=[lw[d]], writes=[lw[d]])
            if RW_STOP == 1:
                break
            PS6 = P.PS[6]
            for c in range(NCH):
                cs = slice(c * 128, (c + 1) * 128)
                vp = Vpad[c % 2]; vt = Vtk[c % 2]
                psb = P.psum('b')
                pv_ = psb.ap if DIN == F32 else psb.ap.bitcast(BF16)
                P.tr(pv_[:, 0:128], vc.ap[:, cs], idin.ap, reads=[vc, idin], writes=[psb])
                P.cp('act', vt.ap, pv_[:, 0:128], reads=[psb], writes=[vt])
                for hp in range(2):
                    P.cp('pool', vp.ap[:, hp, hp * 64:hp * 64 + 64], vt.ap[:, hp * 64:hp * 64 + 64], reads=[vt], writes=[vp])
                nmm = 0
                for d in range(2):
                    i2 = (c * 2 + d) % 2
                    lt = lwtok[i2]; e1 = E1[i2]; e0 = E0[i2]; ei = Ei[i2]; nw = nWC[i2]
                    qr = QR[i2]; bt = Bt[i2]; ktt = Kt[i2]; nb = nBh[i2]; k2 = K2[i2]; tk = TK[i2]
                    ps = P.psum('b')
                    P.tr(ps.ap[:, 0:128], lw[d].ap[:, cs], ident.ap, reads=[lw[d], ident], writes=[ps])
                    P.cp('dve', lt.ap, ps.ap[:, 0:128], reads=[ps], writes=[lt])
                    psc = P.psum('b')
                    P.mm(psc.ap[:, 0:256], lt.ap, rmk.ap[:, d, 640:896], reads=[lt, rmk], writes=[psc])
                    P.act(e1.ap, psc.ap[:, 0:128], AF.Exp, reads=[psc], writes=[e1])
                    P.act(e0.ap, psc.ap[:, 128:256], AF.Exp, reads=[psc], writes=[e0])
                    P.act(ei.ap, psc.ap[:, 0:128], AF.Exp, reads=[psc], writes=[ei], scale=-1.0)
                    wc = e1.ap[:, 127:128] if d == 0 else e1.ap[:, 0:1]
                    P.ts('dve', nw.ap, wc, -1.0, None, ALU.mult, reads=[e1], writes=[nw])
                    P.tt('dve', qr.ap[:, 0, :], kk.ap[:, cs], e0.ap, ALU.mult, reads=[kk, e0], writes=[qr])
                    P.tt('dve', qr.ap[:, 1, :], rc.ap[:, cs], e1.ap, ALU.mult, reads=[rc, e1], writes=[qr])
                    P.tt('pool', bt.ap, bb[d].ap[:, cs], ei.ap, ALU.mult, reads=[bb[d], ei], writes=[bt])
                    P.tt('pool', ktt.ap, kt[d].ap[:, cs], ei.ap, ALU.mult, reads=[kt[d], ei], writes=[ktt])
                    P.ts('dve', nb.ap, bt.ap, nw.ap[:, 0:1], None, ALU.mult, reads=[bt, nw], writes=[nb])
                    P.ts('dve', k2.ap, ktt.ap, wc, None, ALU.mult, reads=[ktt, e1], writes=[k2])
                    pst = P.psum('b'); pstb = pst.ap if DCH == F32 else pst.ap.bitcast(BF16)
                    P.tr(pstb[:, 0:128], qr.ap[:, 0, :], idch.ap, reads=[qr, idch], writes=[pst])
                    P.tr(pstb[:, 128:256], nb.ap, idch.ap, reads=[nb, idch], writes=[pst])
                    P.tr(pstb[:, 256:384], k2.ap, idch.ap, reads=[k2, idch], writes=[pst])
                    P.cp('act', tk.ap, pstb[:, 0:384].rearrange("p (a b) -> p a b", a=3), reads=[pst], writes=[tk])
                    if RW_STOP == 2:
                        continue
                    H = [dict(), dict()]
                    qr2s = [qr.ap[slice(hp * 64, hp * 64 + 64), :, :].rearrange("p a b -> p (a b)") for hp in range(2)]
                    for hp in range(2):
                        hs = slice(hp * 64, hp * 64 + 64)
                        ea = evA[hp]; eb = evB[hp]
                        qr2 = qr2s[hp]
                        p1 = P.psum('r')
                        P.mm(p1.ap[:, 0:256], bt.ap[hs, :], qr2, reads=[bt, qr], writes=[p1])
                        P.tt('dve', ea.ap, p1.ap[:, 0:256], rmk.ap[:, d, 0:256], ALU.mult, reads=[p1, rmk], writes=[ea])
                        p2 = P.psum('r')
                        P.mm(p2.ap[:, 0:256], ktt.ap[hs, :], qr2, reads=[ktt, qr], writes=[p2])
                        P.tt('dve', eb.ap, p2.ap[:, 0:256], rmk.ap[:, d, 256:512], ALU.mult, reads=[p2, rmk], writes=[eb])
                        p3 = P.psum('r')
                        P.mm(p3.ap[:, 0:128], qr.ap[hs, 0, :], bt.ap[hs, :], reads=[qr, bt], writes=[p3])
                        X = Xr[hp][0]
                        P.tt('dve', X.ap, p3.ap[:, 0:128], rmk.ap[:, d, 512:640], ALU.mult, reads=[p3, rmk], writes=[X])
                        XT = XTr[hp][0]
                        P.cp('pool', XT.ap, ea.ap[:, 0:128], reads=[ea], writes=[XT])
                        TTc = TTr[hp][0]
                        P.tt('pool', TTc.ap, ea.ap[:, 0:128], ident.ap, ALU.add, reads=[ea, ident], writes=[TTc])
                        H[hp] = dict(X=X, XT=XT, TT=TTc, xi=0, xti=0, ti=0)
                    for j in range(1, 7):
                        for hp in range(2):
                            st = H[hp]
                            X = st['X']; XT = st['XT']; TTc = st['TT']
                            pX = P.psum('r')
                            P.mm(pX.ap[:, 0:128], XT.ap, X.ap, reads=[XT, X], writes=[pX])
                            st['xi'] = (st['xi'] + 1) % 3
                            Xn = Xr[hp][st['xi']]
                            P.cp('act', Xn.ap, pX.ap[:, 0:128], reads=[pX], writes=[Xn])
                            if j < 6:
                                pXT = P.psum('r')
                                P.mm(pXT.ap[:, 0:128], X.ap, XT.ap, reads=[XT, X], writes=[pXT])
                                st['xti'] = (st['xti'] + 1) % 3
                                XTn = XTr[hp][st['xti']]
                                P.cp('dve', XTn.ap, pXT.ap[:, 0:128], reads=[pXT], writes=[XTn])
                                st['XT'] = XTn
                            pT = P.psum('r')
                            P.mm(pT.ap[:, 0:128], Xn.ap, TTc.ap, reads=[Xn, TTc], writes=[pT])
                            st['ti'] = (st['ti'] + 1) % 3
                            TTn = TTr[hp][st['ti']]
                            P.tt('dve', TTn.ap, pT.ap[:, 0:128], TTc.ap, ALU.add, reads=[pT, TTc], writes=[TTn])
                            st['X'] = Xn
                            st['TT'] = TTn
                    for hp in range(2):
                        hs = slice(hp * 64, hp * 64 + 64)
                        ea = evA[hp]; eb = evB[hp]; TTc = H[hp]['TT']
                        nv = n2v[hp]; pc = Pcat[hp]; p2p = P2p[hp][d]
                        p4 = P.psum('r')
                        P.mm(p4.ap[:, 0:64], eb.ap[:, 0:128], vt.ap[:, hs], reads=[eb, vt], writes=[p4])
                        P.cp('act', nv.ap, p4.ap[:, 0:64], reads=[p4], writes=[nv])
                        p5 = P.psum('r')
                        P.mm(p5.ap[:, 0:64], TTc.ap, tk.ap[:, 0, hs], reads=[TTc, tk], writes=[p5])
                        P.mm(p5.ap[:, 64:128], TTc.ap, nv.ap, reads=[TTc, nv], writes=[p5])
                        P.cp('dve', pc.ap, p5.ap[:, 0:128], reads=[p5], writes=[pc])
                        P.cp('pool', p2p.ap[:, hs], pc.ap[:, 64:128], reads=[pc], writes=[p2p])
                        p6 = P.psum('r')
                        P.mm(p6.ap[hs, 0:64], pc.ap[:, 0:64], tk.ap[:, 1, hs], reads=[pc, tk], writes=[p6])
                        P.stt('dve', MTb.ap[hs, d, c, hs], ident.ap[hs, hs], wc[hs, :], p6.ap[hs, 0:64], ALU.mult, ALU.add,
                              reads=[ident, e1, p6], writes=[MTb])
                        p7 = P.psum('r')
                        P.mm(p7.ap[hs, 0:64], tk.ap[:, 2, hs], vt.ap[:, hs], start=True, stop=False, reads=[tk, vt], writes=[p7])
                        P.mm(p7.ap[hs, 0:64], tk.ap[:, 1, hs], pc.ap[:, 64:128], start=False, stop=True, reads=[tk, pc], writes=[p7])
                        P.cp('act', Gst.ap[hs, d, c, :], p7.ap[hs, 0:64], reads=[p7], writes=[Gst])
                        p8 = P.psum('r')
                        P.mm(p8.ap[hs, 0:128], pc.ap[:, 0:64], ea.ap[:, 128:256], reads=[pc, ea], writes=[p8])
                        P.tt('dve', Qs.ap[hs, d, c, :], p8.ap[hs, 0:128], qr.ap[hs, 1, :], ALU.add, reads=[p8, qr], writes=[Qs])
                        P.mm(PS6.ap[:, 0:128], vp.ap[:, hp, :], eb.ap[:, 128:256], start=(nmm == 0), stop=False,
                             reads=[vp, eb], writes=[PS6])
                        P.mm(PS6.ap[:, 0:128], p2p.ap, ea.ap[:, 128:256], start=False, stop=(nmm == 3),
                             reads=[p2p, ea], writes=[PS6])
                        nmm += 1
                if RW_STOP > 2 and RW_STOP not in (25, 26, 27, 28, 261, 262):
                    P.cp('dve', Y0.ap[:, c, :], PS6.ap[:, 0:128], reads=[PS6], writes=[Y0])
            if RW_STOP <= 3 or RW_STOP in (25, 26, 27, 28, 261, 262):
                break
            for d in range(2):
                order = list(range(0, CCH)) + list(range(CCH, NCH)) if d == 0 else \
                    list(range(CCH - 1, -1, -1)) + list(range(NCH - 1, CCH - 1, -1))
                s_cur = Sst[0]
                P.memset('dve', s_cur.ap, 0.0, writes=[s_cur])
                P.memset('dve', Sbk.ap[:, d, order[0], :], 0.0, writes=[Sbk])
                for i, c in enumerate(order[:-1]):
                    ps = P.psum('b')
                    P.mm(ps.ap[:, 0:64], MTb.ap[:, d, c, :], s_cur.ap, reads=[MTb, s_cur], writes=[ps])
                    s_nx = Sst[(i + 1) % 2]
                    P.tt('dve', s_nx.ap, ps.ap[:, 0:64], Gst.ap[:, d, c, :], ALU.add, reads=[ps, Gst], writes=[s_nx])
                    c2 = order[i + 1]
                    for hp in range(2):
                        hs = slice(hp * 64, hp * 64 + 64)
                        P.cp('pool', Sbk.ap[hs, d, c2, hs], s_nx.ap[hs, :], reads=[s_nx], writes=[Sbk])
                    s_cur = s_nx
            if RW_STOP == 4:
                break
            for c in range(NCH):
                ps = P.psum('b')
                P.mm(ps.ap[:, 0:128], Sbk.ap[:, 0, c, :], Qs.ap[:, 0, c, :], start=True, stop=False, reads=[Sbk, Qs], writes=[ps])
                P.mm(ps.ap[:, 0:128], Sbk.ap[:, 1, c, :], Qs.ap[:, 1, c, :], start=False, stop=True, reads=[Sbk, Qs], writes=[ps])
                P.tt('dve', tmpA.ap[:, c * 128:(c + 1) * 128], ps.ap[:, 0:128], Y0.ap[:, c, :], ALU.add, reads=[ps, Y0], writes=[tmpA])
            if dbg and pr == 0:
                tap("yr0", tmpA, [128, NT])
            for ti, (t0, TT, isc) in enumerate(tiles):
                tsl = slice(t0, t0 + TT)
                ps = P.psum('a')
                P.mm(ps.ap[:, 0:TT], blk.ap, tmpA.ap[:, tsl], reads=[blk, tmpA], writes=[ps])
                dc = rr(t512)
                P.stt('dve', dc.ap[:, 0:TT], ps.ap[:, 0:TT], -1.0 / 64, tmpA.ap[:, tsl], ALU.mult, ALU.add, reads=[ps, tmpA], writes=[dc])
                sq_ = rr(t512)
                P.act(sq_.ap[:, 0:TT], dc.ap[:, 0:TT], AF.Square, reads=[dc], writes=[sq_])
                ps2 = P.psum('a')
                P.mm(ps2.ap[:, 0:TT], blk.ap, sq_.ap[:, 0:TT], reads=[blk, sq_], writes=[ps2])
                P.ts('dve', sq_.ap[:, 0:TT], ps2.ap[:, 0:TT], 1.0 / 64, 64e-5, ALU.mult, ALU.add, reads=[ps2], writes=[sq_])
                P.act(sq_.ap[:, 0:TT], sq_.ap[:, 0:TT], AF.Ln, reads=[sq_], writes=[sq_])
                P.act(sq_.ap[:, 0:TT], sq_.ap[:, 0:TT], AF.Exp, reads=[sq_], writes=[sq_], scale=-0.5)
                P.tt('dve', dc.ap[:, 0:TT], dc.ap[:, 0:TT], sq_.ap[:, 0:TT], ALU.mult, reads=[dc, sq_], writes=[dc])
                P.ts('dve', dc.ap[:, 0:TT], dc.ap[:, 0:TT], rv.ap[:, 7, pr:pr + 1], rv.ap[:, 8, pr:pr + 1], ALU.mult, ALU.add,
                     reads=[dc, rv], writes=[dc])
                bo = rr(t512)
                P.tt('pool', bo.ap[:, 0:TT], kt[0].ap[:, tsl], kt[1].ap[:, tsl], ALU.add, reads=[kt[0], kt[1]], writes=[bo])
                P.tt('pool', bo.ap[:, 0:TT], bo.ap[:, 0:TT], rc.ap[:, tsl], ALU.mult, reads=[bo, rc], writes=[bo])
                P.ts('pool', bo.ap[:, 0:TT], bo.ap[:, 0:TT], rv.ap[:, 6, pr:pr + 1], None, ALU.mult, reads=[bo, rv], writes=[bo])
                ps3 = P.psum('a')
                P.mm(ps3.ap[:, 0:TT], blk.ap, bo.ap[:, 0:TT], reads=[blk, bo], writes=[ps3])
                P.tt('dve', bo.ap[:, 0:TT], ps3.ap[:, 0:TT], vc.ap[:, tsl], ALU.mult, reads=[ps3, vc], writes=[bo])
                P.tt('dve', dc.ap[:, 0:TT], dc.ap[:, 0:TT], bo.ap[:, 0:TT], ALU.add, reads=[dc, bo], writes=[dc])
                ps4 = P.psum('a')
                P.mm(ps4.ap[:, 0:TT], gup.ap[:, cs_], sg.ap[:, tsl], reads=[gup, sg], writes=[ps4])
                ym = ymt[ti % 2]
                P.tt('dve', ym.ap[:, 0:TT], ps4.ap[:, 0:TT], dc.ap[:, 0:TT], ALU.mult, reads=[ps4, dc], writes=[ym])
                P.dma('sp', ymTs[pr, :, tsl], ym.ap[:, 0:TT], reads=[ym])
        P.release(m)

    def phase_pool():
        m = P.mark()
        seqs = [(0, C), (C, NT)]
        pw = P.alloc([128, 4, 128], BF16)
        for gi in range(4):
            P.dma('pool', pw.ap[:, gi, :], pool_w[gi], writes=[pw])
        psc = P.alloc([128, 4]); P.dma('sp', psc.ap, pool_scT, writes=[psc])
        u = P.alloc([128, NT]); acc = P.alloc([128, NT]); inv = P.alloc([128, NT]); df = P.alloc([128, NT], BF16)
        ymt = [P.alloc([128, 512], BF16) for _ in range(2)]
        for gi, win in enumerate((2, 4, 8, 16)):
            P.dma('sp', u.ap, pTs[14 + gi], writes=[u])
            P.dma('sp', inv.ap, pool_inv[gi:gi + 1, :].partition_broadcast(128), writes=[inv])
            P.cp('pool', acc.ap, u.ap, reads=[u], writes=[acc])
            for o in range(-(win // 2), win // 2):
                if o == 0:
                    continue
                for (a, b) in seqs:
                    if o < 0:
                        P.tt('dve', acc.ap[:, a - o:b], acc.ap[:, a - o:b], u.ap[:, a:b + o], ALU.add, reads=[acc, u], writes=[acc])
                    else:
                        P.tt('dve', acc.ap[:, a:b - o], acc.ap[:, a:b - o], u.ap[:, a + o:b], ALU.add, reads=[acc, u], writes=[acc])
            P.tt('dve', acc.ap, acc.ap, inv.ap, ALU.mult, reads=[acc, inv], writes=[acc])
            P.tt('dve', df.ap, acc.ap, u.ap, ALU.subtract, reads=[acc, u], writes=[df])
            for ti, (t0, TT, isc) in enumerate(tiles):
                ps = P.psum('a')
                P.mm(ps.ap[:, 0:TT], pw.ap[:, gi, :], df.ap[:, t0:t0 + TT], reads=[pw, df], writes=[ps])
                ym = ymt[ti % 2]
                P.ts('dve', ym.ap[:, 0:TT], ps.ap[:, 0:TT], psc.ap[:, gi:gi + 1], None, ALU.mult, reads=[ps, psc], writes=[ym])
                P.dma('sp', ymTs[4 + gi, :, t0:t0 + TT], ym.ap[:, 0:TT], reads=[ym])
        P.release(m)

    RUN_RWKV = STOP_AFTER not in ('inproj',)
    RUN_POOL = STOP_AFTER not in ('inproj', 'rwkv')

    def phase_outproj(li, w_out_dram, lat_only):
        m = P.mark()
        Wout = P.alloc([128, KD, D], BF16)
        load_w_bf(Wout, w_out_dram, KD)
        ymb = [P.alloc([128, KD, 512], BF16) for _ in range(2)]
        xT = [P.alloc([128, KD, 512]) for _ in range(2)]
        for ti, (t0, TT, isc) in enumerate(tiles):
            if lat_only and isc:
                continue
            ym = ymb[ti % 2]; xt = xT[ti % 2]
            P.dma_group('sp', [(ym.ap[:, k, 0:TT], ymTs[k, :, t0:t0 + TT]) for k in range(KD)], writes=[ym])
            P.dma_group('act', [(xt.ap[:, k, 0:TT], xTs[k, :, t0:t0 + TT]) for k in range(KD)], writes=[xt])
            for dc in range(KD):
                ps = P.psum('a')
                for k in range(KD):
                    P.mm(ps.ap[:, 0:TT], Wout.ap[:, k, dc * 128:(dc + 1) * 128], ym.ap[:, k, 0:TT],
                         start=(k == 0), stop=(k == KD - 1), reads=[Wout, ym], writes=[ps])
                P.stt('dve', xt.ap[:, dc, 0:TT], ps.ap[:, 0:TT], mod.ap[:, li, 16 + dc, isc:isc + 1], xt.ap[:, dc, 0:TT],
                      ALU.mult, ALU.add, reads=[ps, mod, xt], writes=[xt])
            P.dma_group('sp', [(xTs[k, :, t0:t0 + TT], xt.ap[:, k, 0:TT]) for k in range(KD)], reads=[xt])
        P.release(m)

    def phase_final():
        m = P.mark()
        xT = [P.alloc([128, KD, 512]) for _ in range(2)]
        sq = P.alloc([128, KD, 512]); rs = P.alloc([128, 512])
        ob = [P.alloc([128, KD, 512]) for _ in range(2)]
        otok = [P.alloc([128, D]) for _ in range(2)]
        for ti, (t0, TT, isc) in enumerate(tiles):
            if isc:
                continue
            xt = xT[ti % 2]; o = ob[ti % 2]
            P.dma_group('sp', [(xt.ap[:, k, 0:TT], xTs[k, :, t0:t0 + TT]) for k in range(KD)], writes=[xt])
            norm_mod(xt, TT, 0, 0, 0, o, sq, rs, final=True)
            for b in range(TT // 128):
                ot = otok[b % 2]
                for half in range(2):
                    ps = P.psum('a')
                    for j in range(4):
                        k = half * 4 + j
                        P.tr(ps.ap[:, j * 128:(j + 1) * 128], o.ap[:, k, b * 128:(b + 1) * 128], ident.ap,
                             reads=[o, ident], writes=[ps])
                    P.cp('dve' if half else 'act', ot.ap[:, half * 512:(half + 1) * 512], ps.ap, reads=[ps], writes=[ot])
                r0 = t0 - C + b * 128
                P.dma('sp', out[r0:r0 + 128, :], ot.ap, reads=[ot], is_output=True)
        P.release(m)

    if RUN_RWKV:
        phase_rwkv()
    if RUN_POOL:
        phase_pool()
    if STOP_AFTER in ('inproj', 'rwkv', 'pool'):
        phase_final()
        return P, locals()
    phase_outproj(0, ev_w_out, False)
    if STOP_AFTER == 'l0mix':
        phase_final()
        return P, locals()
    phase_moe(0, False)
    if STOP_AFTER == 'l0':
        phase_final()
        return P, locals()
    phase_inproj(1, od_w_in, OD_COLS, False)
    phase_l1mix()
    phase_outproj(1, od_w_out, True)
    if STOP_AFTER == 'l1mix':
        phase_final()
        return P, locals()
    phase_moe(1, True)
    phase_final()
    return P, locals()


def fm(v, nch=None):
    v = np.asarray(v, np.float32)
    return np.ascontiguousarray(v.reshape(-1, 128).T)


def make_inputs(b, S, C, inp):
    NT = C + S
    m = {}
    m['xin'] = np.ascontiguousarray(np.concatenate([inp['ctx'][b], inp['x'][b]], 0))
    m['cT'] = np.ascontiguousarray(np.stack([fm(inp['c'][b]), fm(inp['c_ctx'])], -1))
    m['ada_w'] = inp['ada_w']
    m['ada_bT'] = np.ascontiguousarray(np.stack([fm(inp['ada_b'][0]), fm(inp['ada_b'][1])], 1))
    m['normT'] = np.ascontiguousarray(np.stack([fm(inp['norm_mix'][0]), fm(inp['norm_mix'][1]), fm(inp['norm_ffn'][0]),
                                               fm(inp['norm_ffn'][1]), fm(inp['final_norm'])], 1))
    m['ev_w_in'] = inp['ev_w_in'][0]
    m['ev_w_out'] = inp['ev_w_out'][0]
    sh = inp['rwkv_shift'][0]
    m['shiftT'] = np.ascontiguousarray(np.stack([fm(sh[0]), fm(sh[1]), fm(sh[2])], -1))
    rv = [inp['rwkv_w0'][0][0], inp['rwkv_w0'][0][1], inp['rwkv_a0'][0][0], inp['rwkv_a0'][0][1], inp['rwkv_k_k'][0],
          inp['rwkv_k_a'][0], inp['rwkv_r_k'][0], inp['rwkv_ln_g'][0], inp['rwkv_ln_b'][0]]
    m['rvec'] = np.ascontiguousarray(np.stack([fm(v) for v in rv], 1))
    lu = np.zeros((128, 2, 512), np.float32)
    for d in range(2):
        lu[0:64, d] = inp['rwkv_w_up'][0][d]
        lu[64:128, d] = inp['rwkv_a_up'][0][d]
    m['lora_up'] = lu
    m['g_up'] = inp['rwkv_g_up'][0]
    m['pool_w'] = inp['pool_w'][0]
    m['pool_scT'] = fm(inp['pool_scale'][0])
    pi = np.zeros((4, NT), np.float32)
    for gi, win in enumerate((2, 4, 8, 16)):
        for (a, Tn) in ((0, C), (C, S)):
            t = np.arange(Tn)
            lo = np.clip(t - win // 2, 0, Tn)
            hi = np.clip(t - win // 2 + win, 0, Tn)
            pi[gi, a:a + Tn] = 1.0 / (hi - lo)
    m['pool_inv'] = pi
    m['moe_r'] = np.ascontiguousarray(np.concatenate([inp['moe_router_group'], inp['moe_router_expert']], -1))
    m['moe_wg'] = inp['moe_w_gate']
    m['moe_wu'] = inp['moe_w_up']
    m['moe_wd'] = inp['moe_w_down']
    m['od_w_in'] = inp['od_w_in'][0]
    m['od_w_out'] = inp['od_w_out'][0]
    m['dlam'] = np.ascontiguousarray(inp['diff_lambda'][0].reshape(1, 256))
    m['sublnT'] = np.ascontiguousarray(inp['diff_subln'][0].reshape(128, 1))
    m['retgT'] = fm(inp['ret_norm'][0])
    m.update(host_consts())
    m.update(host_consts_l1(S))
    return m


def kernel(**inp):
    inp = {k: np.asarray(v) for k, v in inp.items()}
    S, C = inp['x'].shape[1], inp['ctx'].shape[1]
    B = inp['x'].shape[0]
    P, _ = build(S, C)
    nc = P.build()
    in_maps = [make_inputs(b, S, C, inp) for b in range(B)]
    names = set()
    res = run_bass_kernel_spmd(nc, in_maps, core_ids=list(range(B)))
    return np.stack([np.asarray(r["out"], np.float32) for r in res.results], 0)
```

```python
import math
import numpy as np
from contextlib import ExitStack
import concourse.bass as bass
import concourse.mybir as mybir
from concourse.bass_utils import run_bass_kernel_spmd

F32 = mybir.dt.float32
BF16 = mybir.dt.bfloat16
ALU = mybir.AluOpType
AF = mybir.ActivationFunctionType
AX = mybir.AxisListType

ENGS = ['pe', 'act', 'dve', 'pool', 'sp']
DMAQ = ['sp', 'pool', 'act']


def _prod(s):
    r = 1
    for v in s:
        r *= v
    return r


class T:
    __slots__ = ('ap', 'lw', 'rd')

    def __init__(self, ap):
        self.ap = ap
        self.lw = None
        self.rd = {}


class Prog:
    def __init__(self, arena_words=52000, n_dma_sems=8):
        self.nc = bass.Bass("TRN2", target_bir_lowering=False)
        self.es = ExitStack()
        self.ops = {e: [] for e in ENGS}
        self.known = {e: {} for e in ENGS}
        self.pending = {e: [] for e in ENGS}
        self.n_dma_sems = n_dma_sems
        self.dma_rr = {q: 0 for q in DMAQ}
        self.dma_cum = {}
        self.out_tokens = []
        self.nuid = 0
        self.aw = arena_words
        self.arena = self.es.enter_context(self.nc.sbuf_tensor("arena", [128, arena_words], F32))
        self.top = 0
        self.PS = [T(self.es.enter_context(self.nc.psum_tensor(f"psb{i}", [128, 512], F32))[:, :])
                   for i in range(8)]
        self.ps_rr = {'a': 0, 'b': 0, 'c': 0, 'r': 0}
        self.ps_groups = {'a': [0, 1, 2, 3], 'b': [4, 5], 'c': [4, 5, 6, 7], 'r': [0, 1, 2, 3, 7]}

    def psum(self, g='a'):
        lst = self.ps_groups[g]
        i = self.ps_rr[g]
        self.ps_rr[g] = (i + 1) % len(lst)
        return self.PS[lst[i]]

    def alloc(self, shape, dt=F32):
        shape = list(shape)
        esz = 4 if dt == F32 else 2
        nb = _prod(shape[1:]) * esz
        nw = (nb + 3) // 4
        assert self.top + nw <= self.aw, f"arena overflow {self.top}+{nw}>{self.aw}"
        ap = self.arena[0:shape[0], self.top:self.top + nw]
        self.top += nw
        if dt != F32:
            ap = ap.bitcast(dt)
            ap = ap[:, 0:_prod(shape[1:])]
        if len(shape) > 2:
            names = "abcdefg"[:len(shape) - 1]
            kw = {names[i]: shape[i + 1] for i in range(len(shape) - 2)}
            ap = ap.rearrange("p (" + " ".join(names) + ") -> p " + " ".join(names), **kw)
        return T(ap)

    def mark(self):
        return self.top

    def release(self, m):
        self.barrier()
        self.top = m

    def dram(self, name, shape, dt, kind="Internal"):
        return self.nc.dram_tensor(name, list(shape), dt, kind=kind).ap()

    def barrier(self):
        toks = []
        for f in ENGS:
            if len(self.ops[f]) > 0:
                toks.append(('c', f, len(self.ops[f])))
        for skey, cum in self.dma_cum.items():
            toks.append(('d', skey, cum))
        for e in ENGS:
            self.pending[e] = list(toks)

    def _add_wait(self, e, waits, tok):
        if tok is None:
            return
        kind, key, val = tok
        if kind == 'c' and key == e:
            if e == 'pe':
                return
            if val > len(self.ops[e]):
                return
        kk = (kind, key)
        if self.known[e].get(kk, 0) >= val:
            return
        self.known[e][kk] = val
        waits[kk] = max(waits.get(kk, 0), val)

    def _deps(self, e, reads, writes):
        waits = {}
        if self.pending[e]:
            for tok in self.pending[e]:
                self._add_wait(e, waits, tok)
            self.pending[e] = []
        for t in reads:
            self._add_wait(e, waits, t.lw)
        for t in writes:
            self._add_wait(e, waits, t.lw)
            for tok in t.rd.values():
                self._add_wait(e, waits, tok)
        return waits

    def op(self, e, fn, reads=(), writes=()):
        waits = self._deps(e, reads, writes)
        idx = len(self.ops[e]) + 1
        tok = ('c', e, idx)
        self.ops[e].append(dict(fn=fn, waits=waits, inc=None, flag=False))
        for t in reads:
            t.rd[e] = tok
        for t in writes:
            t.lw = tok
            t.rd = {}
        return tok

    def dma(self, q, out_ap, in_ap, reads=(), writes=(), is_output=False, **kw):
        waits = self._deps(q, reads, writes)
        si = self.dma_rr[q]
        self.dma_rr[q] = (si + 1) % self.n_dma_sems
        skey = (q, si)
        prev = self.dma_cum.get(skey, 0)
        if prev > 0:
            self._add_wait(q, waits, ('d', skey, prev))
        val = prev + 16
        self.dma_cum[skey] = val
        tok = ('d', skey, val)

        def fn(eng, out_ap=out_ap, in_ap=in_ap, kw=kw):
            return eng.dma_start(out=out_ap, in_=in_ap, **kw)
        self.ops[q].append(dict(fn=fn, waits=waits, inc=(skey, 16), flag=True))
        for t in reads:
            t.rd[('dma', skey)] = tok
        for t in writes:
            t.lw = tok
            t.rd = {}
        if is_output:
            self.out_tokens.append(tok)
        return tok

    def dma_group(self, q, pairs, reads=(), writes=(), is_output=False):
        waits = self._deps(q, reads, writes)
        si = self.dma_rr[q]
        self.dma_rr[q] = (si + 1) % self.n_dma_sems
        skey = (q, si)
        prev = self.dma_cum.get(skey, 0)
        if prev > 0:
            self._add_wait(q, waits, ('d', skey, prev))
        val = prev
        for i, (out_ap, in_ap) in enumerate(pairs):
            val += 16

            def fn(eng, out_ap=out_ap, in_ap=in_ap):
                return eng.dma_start(out=out_ap, in_=in_ap)
            self.ops[q].append(dict(fn=fn, waits=waits if i == 0 else {}, inc=(skey, 16), flag=True))
        self.dma_cum[skey] = val
        tok = ('d', skey, val)
        for t in reads:
            t.rd[('dma', skey)] = tok
        for t in writes:
            t.lw = tok
            t.rd = {}
        if is_output:
            self.out_tokens.append(tok)
        return tok

    def build(self):
        nc = self.nc
        waits = {}
        for tok in self.out_tokens:
            self._add_wait('sp', waits, tok)
        self.ops['sp'].append(dict(fn=None, waits=waits, inc=None, flag=False))
        for e in ENGS:
            for o in self.ops[e]:
                for (kind, key), val in o['waits'].items():
                    if kind == 'c':
                        self.ops[key][val - 1]['flag'] = True
        rank = {}
        for e in ENGS:
            r = 0
            rk = []
            for o in self.ops[e]:
                if o['inc'] is None and o['flag']:
                    r += 1
                rk.append(r)
            rank[e] = rk
        csem = {e: self.es.enter_context(nc.semaphore(f"c_{e}")) for e in ENGS}
        dsem = {}
        for q in DMAQ:
            for i in range(self.n_dma_sems):
                if (q, i) in self.dma_cum:
                    dsem[(q, i)] = self.es.enter_context(nc.semaphore(f"d_{q}{i}"))
        block = self.es.enter_context(nc.Block())
        engobj = {'pe': block.tensor, 'act': block.scalar, 'dve': block.vector,
                  'pool': block.gpsimd, 'sp': block.sync}

        def mk(e):
            def body(eng):
                for o in self.ops[e]:
                    for (kind, key), val in o['waits'].items():
                        if kind == 'c':
                            eng.wait_ge(csem[key], rank[key][val - 1])
                        else:
                            eng.wait_ge(dsem[key], val)
                    if o['fn'] is None:
                        continue
                    ins = o['fn'](eng)
                    if o['inc'] is not None:
                        ins.then_inc(dsem[o['inc'][0]], 16)
                    elif o['flag']:
                        ins.then_inc(csem[e], 1)
            return body
        for e in ENGS:
            engobj[e](mk(e))
        self.es.close()
        return nc

    def mm(self, out, lhsT, rhs, start=True, stop=True, reads=(), writes=(), **kw):
        def fn(eng):
            return eng.matmul(out, lhsT, rhs, start=start, stop=stop, **kw)
        return self.op('pe', fn, reads, writes)

    def tr(self, out, in_, ident, reads=(), writes=()):
        def fn(eng):
            return eng.transpose(out, in_, ident)
        return self.op('pe', fn, reads, writes)

    def act(self, out, in_, func, reads=(), writes=(), **kw):
        def fn(e):
            return e.activation(out=out, in_=in_, func=func, **kw)
        return self.op('act', fn, reads, writes)

    def tt(self, e, out, in0, in1, op, reads=(), writes=()):
        def fn(eng):
            return eng.tensor_tensor(out=out, in0=in0, in1=in1, op=op)
        return self.op(e, fn, reads, writes)

    def ts(self, e, out, in0, s1, s2, op0, op1=None, reads=(), writes=()):
        def fn(eng):
            if op1 is None:
                return eng.tensor_scalar(out=out, in0=in0, scalar1=s1, scalar2=None, op0=op0)
            return eng.tensor_scalar(out=out, in0=in0, scalar1=s1, scalar2=s2, op0=op0, op1=op1)
        return self.op(e, fn, reads, writes)

    def stt(self, e, out, in0, scalar, in1, op0, op1, reads=(), writes=()):
        def fn(eng):
            return eng.scalar_tensor_tensor(out=out, in0=in0, scalar=scalar, in1=in1, op0=op0, op1=op1)
        return self.op(e, fn, reads, writes)

    def cp(self, e, out, in_, reads=(), writes=()):
        if e == 'act':
            def fn(eng):
                return eng.copy(out=out, in_=in_)
        else:
            def fn(eng):
                return eng.tensor_copy(out=out, in_=in_)
        return self.op(e, fn, reads, writes)

    def memset(self, e, ap, val, writes=()):
        def fn(eng):
            return eng.memset(ap, val)
        return self.op(e, fn, (), writes)


D = 1024
KD = 8
W_DECAY_SCALE = 0.606531
EV_COLS = 2304
OD_COLS = 3072


def host_consts():
    r = np.arange(128)
    Us = (r[:, None] < r[None, :]).astype(np.float32)
    Ui = (r[:, None] <= r[None, :]).astype(np.float32)
    Ls = (r[:, None] > r[None, :]).astype(np.float32)
    Li = (r[:, None] >= r[None, :]).astype(np.float32)
    blk = np.zeros((128, 128), np.float32)
    blk[:64, :64] = 1
    blk[64:, 64:] = 1
    c = {}
    c['ident'] = np.eye(128, dtype=np.float32)
    c['blk64'] = blk
    rm = np.zeros((2, 128, 896), np.float32)
    for d, (ss, si, tsm) in enumerate([(Us, Ui, Ls), (Ls, Li, Us)]):
        rm[d, :, 0:128] = -ss
        rm[d, :, 128:256] = -si
        rm[d, :, 256:384] = ss
        rm[d, :, 384:512] = si
        rm[d, :, 512:640] = -tsm
        rm[d, :, 640:768] = si
        rm[d, :, 768:896] = ss
    c['rmask'] = rm
    return c


def host_consts_l1(S):
    c = {}
    p = np.arange(128)
    blk32 = p % 32
    partner = np.where(blk32 < 16, p + 16, p - 16)
    perm = np.zeros((128, 128), np.float32)
    perm[partner, p] = 1.0
    c['rope_perm'] = perm
    t = np.arange(S)
    row = (t // 64).astype(np.float32)
    col = (t % 64).astype(np.float32)
    b64 = p % 64
    sub = b64 // 32
    j = (b64 % 16).astype(np.float32)
    inv = (10000.0 ** (-j / 16.0)).astype(np.float32)
    pos = np.where(sub[:, None] == 0, row[None, :], col[None, :]).astype(np.float32)
    ang = (pos * inv[:, None]).astype(np.float32)
    sgn = np.where(blk32 < 16, -1.0, 1.0).astype(np.float32)
    c['ropeC'] = np.cos(ang).astype(np.float32)
    c['ropeS'] = (np.sin(ang) * sgn[:, None]).astype(np.float32)
    lgf = np.log(1.0 - 2.0 ** (-5.0 - np.arange(4, dtype=np.float64)))
    r = np.arange(128, dtype=np.float64)
    retD = np.zeros((8, 128, 128), np.float64)
    retq = np.zeros((8, 128), np.float64)
    retk = np.zeros((128, 8), np.float64)
    for h in range(4):
        for d in range(2):
            lg = lgf[h] if d == 0 else lgf[3 - h]
            hd = h * 2 + d
            s_, i_ = r[:, None], r[None, :]
            if d == 0:
                retD[hd] = np.where(i_ >= s_, np.exp(lg * np.maximum(i_ - s_, 0)), 0.0)
                retq[hd] = np.exp(lg * (r + 1))
                retk[:, hd] = np.exp(lg * (127 - r))
            else:
                retD[hd] = np.where(s_ >= i_, np.exp(lg * np.maximum(s_ - i_, 0)), 0.0)
                retq[hd] = np.exp(lg * (128 - r))
                retk[:, hd] = np.exp(lg * r)
    c['retD'] = retD.astype(np.float32)
    c['retq'] = retq.astype(np.float32)
    c['retk'] = retk.astype(np.float32)
    return c


def build(S, C, dbg=False, RW_DT=(BF16, F32, BF16), STOP_AFTER='all', RW_STOP=99):
    P = Prog()
    NT = C + S
    NCH = NT // 128
    CCH = C // 128
    tiles = []
    for base, ln, isc in ((0, C, 1), (C, S, 0)):
        o = 0
        while o < ln:
            l = min(512, ln - o)
            tiles.append((base + o, l, isc))
            o += l
    IN = lambda n, s: P.dram(n, s, F32, "ExternalInput")
    xin = IN("xin", [NT, D])
    cT = IN("cT", [128, KD, 2])
    ada_w = IN("ada_w", [2, D, 6 * D])
    ada_bT = IN("ada_bT", [128, 2, 48])
    normT = IN("normT", [128, 5, KD])
    ev_w_in = IN("ev_w_in", [D, EV_COLS])
    ev_w_out = IN("ev_w_out", [D, D])
    shiftT = IN("shiftT", [128, 12, 3])
    rvec = IN("rvec", [128, 9, 4])
    lora_up = IN("lora_up", [128, 2, 512])
    g_up = IN("g_up", [128, 512])
    pool_w = IN("pool_w", [4, 128, 128])
    pool_scT = IN("pool_scT", [128, 4])
    pool_inv = IN("pool_inv", [4, NT])
    moe_r = IN("moe_r", [2, D, 36])
    moe_wg = IN("moe_wg", [2, 32, D, 512])
    moe_wu = IN("moe_wu", [2, 32, D, 512])
    moe_wd = IN("moe_wd", [2, 32, 512, D])
    od_w_in = IN("od_w_in", [D, OD_COLS])
    od_w_out = IN("od_w_out", [D, D])
    dlam = IN("dlam", [1, 256])
    sublnT = IN("sublnT", [128, 1])
    retgT = IN("retgT", [128, 4])
    rope_perm = IN("rope_perm", [128, 128])
    ropeC = IN("ropeC", [128, S])
    ropeS = IN("ropeS", [128, S])
    retD = IN("retD", [8, 128, 128])
    retq = IN("retq", [8, 128])
    retk = IN("retk", [128, 8])
    cident = IN("ident", [128, 128])
    cblk = IN("blk64", [128, 128])
    crmask = IN("rmask", [2, 128, 896])
    out = P.dram("out", [S, D], F32, "ExternalOutput")
    xTs = P.dram("xTs", [KD, 128, NT], F32)
    pTs = P.dram("pTs", [24, 128, NT], F32, "ExternalOutput" if dbg else "Internal")
    ymTs = P.dram("ymTs", [KD, 128, NT], BF16, "ExternalOutput" if dbg else "Internal")
    dbgs = {}

    def tap(name, t, shape):
        if dbg:
            d = P.dram("dbg_" + name, shape, F32, "ExternalOutput")
            P.dma('sp', d, t.ap, reads=[t], is_output=True)

    ident = P.alloc([128, 128]); P.dma('sp', ident.ap, cident, writes=[ident])
    identb = P.alloc([128, 128], BF16); P.dma('pool', identb.ap, cident, writes=[identb])
    blk = P.alloc([128, 128]); P.dma('sp', blk.ap, cblk, writes=[blk])
    onesD = P.alloc([128, 128]); P.memset('pool', onesD.ap, 1.0 / D, writes=[onesD])
    normv = P.alloc([128, 5, KD]); P.dma('sp', normv.ap, normT, writes=[normv])
    mod = P.alloc([128, 2, 48, 2])
    m0 = P.mark()
    sc = P.alloc([128, KD, 2]); P.dma('sp', sc.ap, cT, writes=[sc])
    P.act(sc.ap, sc.ap, AF.Silu, reads=[sc], writes=[sc])
    adab = P.alloc([128, 2, 48]); P.dma('sp', adab.ap, ada_bT, writes=[adab])
    wbuf = [P.alloc([128, KD, 1024]) for _ in range(2)]
    for li in range(2):
        for blkc in range(6):
            wb = wbuf[(li * 6 + blkc) % 2]
            P.dma_group('sp' if blkc % 2 == 0 else 'act',
                        [(wb.ap[:, k, :], ada_w[li, k * 128:(k + 1) * 128, blkc * 1024:(blkc + 1) * 1024]) for k in range(KD)],
                        writes=[wb])
            for cc in range(8):
                ps = P.psum('a')
                for k in range(KD):
                    P.mm(ps.ap[:, 0:2], wb.ap[:, k, cc * 128:(cc + 1) * 128], sc.ap[:, k, :],
                         start=(k == 0), stop=(k == KD - 1), reads=[wb, sc], writes=[ps])
                j = blkc * 8 + cc
                P.stt('dve', mod.ap[:, li, j, :], ps.ap[:, 0:2], 1.0,
                      adab.ap[:, li, j:j + 1].to_broadcast([128, 2]), ALU.mult, ALU.add,
                      reads=[ps, adab], writes=[mod])
    P.release(m0)
    AB = P.alloc([128, 2, 2, 2, KD, 2])
    for li in range(2):
        for sub in range(2):
            shc = 24 * sub
            scc = 24 * sub + 8
            nidx = li if sub == 0 else 2 + li
            for w in range(2):
                P.ts('dve', AB.ap[:, li, sub, 0, :, w], mod.ap[:, li, scc:scc + 8, w], 1.0, None, ALU.add,
                     reads=[mod], writes=[AB])
                P.tt('dve', AB.ap[:, li, sub, 0, :, w], AB.ap[:, li, sub, 0, :, w], normv.ap[:, nidx, :], ALU.mult,
                     reads=[AB, normv], writes=[AB])
                P.cp('dve', AB.ap[:, li, sub, 1, :, w], mod.ap[:, li, shc:shc + 8, w], reads=[mod], writes=[AB])
    if dbg:
        tap("mod", mod, [128, 2, 48, 2])

    def norm_mod(xT, TT, li, sub, w, hb, sq, rs, final=False):
        P.act(sq.ap[:, :, 0:TT], xT.ap[:, :, 0:TT], AF.Square, reads=[xT], writes=[sq])
        ps = P.psum('a')
        for k in range(KD):
            P.mm(ps.ap[:, 0:TT], onesD.ap, sq.ap[:, k, 0:TT], start=(k == 0), stop=(k == KD - 1),
                 reads=[onesD, sq], writes=[ps])
        P.ts('dve', rs.ap[:, 0:TT], ps.ap[:, 0:TT], 1e-6, None, ALU.add, reads=[ps], writes=[rs])
        P.act(rs.ap[:, 0:TT], rs.ap[:, 0:TT], AF.Ln, reads=[rs], writes=[rs])
        P.act(rs.ap[:, 0:TT], rs.ap[:, 0:TT], AF.Exp, reads=[rs], writes=[rs], scale=-0.5)
        P.tt('dve', sq.ap[:, :, 0:TT], xT.ap[:, :, 0:TT], rs.ap[:, None, 0:TT].to_broadcast([128, KD, TT]), ALU.mult,
             reads=[xT, rs], writes=[sq])
        for k in range(KD):
            if final:
                P.ts('dve' if k % 2 else 'pool', hb.ap[:, k, 0:TT], sq.ap[:, k, 0:TT], normv.ap[:, 4, k:k + 1], None, ALU.mult,
                     reads=[sq, normv], writes=[hb])
            else:
                P.ts('dve' if k % 2 else 'pool', hb.ap[:, k, 0:TT], sq.ap[:, k, 0:TT], AB.ap[:, li, sub, 0, k, w:w + 1],
                     AB.ap[:, li, sub, 1, k, w:w + 1], ALU.mult, ALU.add, reads=[sq, AB], writes=[hb])

    def load_w_bf(dst, src, K):
        P.dma_group('pool', [(dst.ap[:, k, :], src[k * 128:(k + 1) * 128, :]) for k in range(K)], writes=[dst])

    def phase_inproj(li, w_in_dram, ncols, first):
        m = P.mark()
        NCC = ncols // 128
        Win = P.alloc([128, KD, ncols], BF16)
        load_w_bf(Win, w_in_dram, KD)
        xtok = [P.alloc([128, D]) for _ in range(2)]
        xT = [P.alloc([128, KD, 512]) for _ in range(2)]
        hb = [P.alloc([128, KD, 512], BF16) for _ in range(2)]
        sq = P.alloc([128, KD, 512]); rs = P.alloc([128, 512])
        pst = [P.alloc([128, 6, 512]) for _ in range(2)]
        for ti, (t0, TT, isc) in enumerate(tiles):
            xt = xT[ti % 2]
            if first:
                for b in range(TT // 128):
                    xk = xtok[b % 2]
                    P.dma('sp', xk.ap, xin[t0 + b * 128:t0 + (b + 1) * 128, :], writes=[xk])
                    for half in range(2):
                        ps = P.psum('a')
                        for j in range(4):
                            k = half * 4 + j
                            P.tr(ps.ap[:, j * 128:(j + 1) * 128], xk.ap[:, k * 128:(k + 1) * 128], ident.ap,
                                 reads=[xk, ident], writes=[ps])
                        P.cp('dve' if half else 'act', xt.ap[:, half * 4:half * 4 + 4, b * 128:(b + 1) * 128],
                             ps.ap.rearrange("p (a b) -> p a b", a=4), reads=[ps], writes=[xt])
                P.dma_group('act', [(xTs[k, :, t0:t0 + TT], xt.ap[:, k, 0:TT]) for k in range(KD)], reads=[xt])
            else:
                P.dma_group('sp', [(xt.ap[:, k, 0:TT], xTs[k, :, t0:t0 + TT]) for k in range(KD)], writes=[xt])
            h = hb[ti % 2]
            norm_mod(xt, TT, li, 0, isc, h, sq, rs)
            for g in range(NCC // 6):
                st = pst[g % 2]
                for c6 in range(6):
                    cc = g * 6 + c6
                    ps = P.psum('a')
                    for k in range(KD):
                        P.mm(ps.ap[:, 0:TT], Win.ap[:, k, cc * 128:(cc + 1) * 128], h.ap[:, k, 0:TT],
                             start=(k == 0), stop=(k == KD - 1), reads=[Win, h], writes=[ps])
                    P.cp('act' if c6 % 2 else 'dve', st.ap[:, c6, 0:TT], ps.ap[:, 0:TT], reads=[ps], writes=[st])
                P.dma('sp', pTs[g * 6:(g + 1) * 6, :, t0:t0 + TT].rearrange("c p t -> p c t"), st.ap[:, :, 0:TT],
                      reads=[st])
        P.release(m)


    def phase_moe(li, lat_only):
        m = P.mark()
        tl = [t for t in tiles if not (lat_only and t[2])]
        xres = P.alloc([128, KD, NT])
        hfT = P.alloc([128, KD, NT], BF16)
        gT = P.alloc([32, NT])
        wr = P.alloc([128, KD, 36])
        P.dma('sp', wr.ap, moe_r[li].rearrange("(k p) n -> p k n", p=128), writes=[wr])
        m2 = P.mark()
        sq = P.alloc([128, KD, 512]); rs = P.alloc([128, 512])
        lg = P.alloc([128, 36]); oh = P.alloc([128, 4]); st_ = P.alloc([128, 16]); les = P.alloc([128, 8])
        mk1 = P.alloc([128, 8]); mk2 = P.alloc([128, 8]); g8 = P.alloc([128, 8]); g32 = P.alloc([128, 4, 8])
        ex4 = P.alloc([128, 4])
        for (t0, TT, isc) in tl:
            xt = T(xres.ap[:, :, t0:t0 + TT]); hb = T(hfT.ap[:, :, t0:t0 + TT])
            P.dma_group('sp', [(xt.ap[:, k, :], xTs[k, :, t0:t0 + TT]) for k in range(KD)], writes=[xt, xres])
            norm_mod(xt, TT, li, 1, isc, hb, sq, rs)
            for k in range(KD):
                P.ts('dve', sq.ap[:, k, 0:TT], sq.ap[:, k, 0:TT], AB.ap[:, li, 1, 0, k, isc:isc + 1],
                     AB.ap[:, li, 1, 1, k, isc:isc + 1], ALU.mult, ALU.add, reads=[sq, AB], writes=[sq])
            for b in range(TT // 128):
                bs = slice(b * 128, (b + 1) * 128)
                ps = P.psum('a')
                for k in range(KD):
                    P.mm(ps.ap[:, 0:36], sq.ap[:, k, bs], wr.ap[:, k, :], start=(k == 0), stop=(k == KD - 1),
                         reads=[sq, wr], writes=[ps])
                P.cp('dve', lg.ap, ps.ap[:, 0:36], reads=[ps], writes=[lg])
                def red(out, in_, op):
                    return P.op('dve', lambda e: e.tensor_reduce(out=out, in_=in_, axis=AX.X, op=op), [lg, les, ex4, st_], [st_])
                P.op('dve', lambda e: e.tensor_reduce(out=st_.ap[:, 0:1], in_=lg.ap[:, 0:4], axis=AX.X, op=ALU.max), [lg], [st_])
                P.ts('dve', oh.ap, lg.ap[:, 0:4], st_.ap[:, 0:1], None, ALU.is_equal, reads=[lg, st_], writes=[oh])
                P.ts('dve', st_.ap[:, 1:2], st_.ap[:, 0:1], -1.0, None, ALU.mult, reads=[st_], writes=[st_])
                P.act(ex4.ap, lg.ap[:, 0:4], AF.Exp, reads=[lg, st_], writes=[ex4], bias=st_.ap[:, 1:2])
                P.op('dve', lambda e: e.tensor_reduce(out=st_.ap[:, 2:3], in_=ex4.ap, axis=AX.X, op=ALU.add), [ex4], [st_])
                P.op('dve', lambda e: e.reciprocal(out=st_.ap[:, 3:4], in_=st_.ap[:, 2:3]), [st_], [st_])
                P.ts('dve', les.ap, lg.ap[:, 4:12], oh.ap[:, 0:1], None, ALU.mult, reads=[lg, oh], writes=[les])
                for g in range(1, 4):
                    P.stt('dve', les.ap, lg.ap[:, 4 + 8 * g:12 + 8 * g], oh.ap[:, g:g + 1], les.ap, ALU.mult, ALU.add,
                          reads=[lg, oh, les], writes=[les])
                P.op('dve', lambda e: e.tensor_reduce(out=st_.ap[:, 4:5], in_=les.ap, axis=AX.X, op=ALU.max), [les], [st_])
                P.ts('dve', mk1.ap, les.ap, st_.ap[:, 4:5], None, ALU.is_equal, reads=[les, st_], writes=[mk1])
                P.stt('dve', g8.ap, mk1.ap, -1e30, les.ap, ALU.mult, ALU.add, reads=[mk1, les], writes=[g8])
                P.op('dve', lambda e: e.tensor_reduce(out=st_.ap[:, 5:6], in_=g8.ap, axis=AX.X, op=ALU.max), [g8], [st_])
                P.ts('dve', mk2.ap, g8.ap, st_.ap[:, 5:6], None, ALU.is_equal, reads=[g8, st_], writes=[mk2])
                P.tt('dve', st_.ap[:, 6:7], st_.ap[:, 5:6], st_.ap[:, 4:5], ALU.subtract, reads=[st_], writes=[st_])
                P.act(st_.ap[:, 7:8], st_.ap[:, 6:7], AF.Exp, reads=[st_], writes=[st_])
                P.ts('dve', st_.ap[:, 8:9], st_.ap[:, 7:8], 1.0, None, ALU.add, reads=[st_], writes=[st_])
                P.op('dve', lambda e: e.reciprocal(out=st_.ap[:, 9:10], in_=st_.ap[:, 8:9]), [st_], [st_])
                P.tt('dve', st_.ap[:, 10:11], st_.ap[:, 9:10], st_.ap[:, 3:4], ALU.mult, reads=[st_], writes=[st_])
                P.tt('dve', st_.ap[:, 11:12], st_.ap[:, 10:11], st_.ap[:, 7:8], ALU.mult, reads=[st_], writes=[st_])
                P.ts('dve', g8.ap, mk1.ap, st_.ap[:, 10:11], None, ALU.mult, reads=[mk1, st_], writes=[g8])
                P.stt('dve', g8.ap, mk2.ap, st_.ap[:, 11:12], g8.ap, ALU.mult, ALU.add, reads=[mk2, st_, g8], writes=[g8])
                for g in range(4):
                    P.ts('dve', g32.ap[:, g, :], g8.ap, oh.ap[:, g:g + 1], None, ALU.mult, reads=[g8, oh], writes=[g32])
                pt = P.psum('a')
                P.tr(pt.ap[0:32, 0:128], g32.ap.rearrange("p a b -> p (a b)"), ident.ap, reads=[g32, ident], writes=[pt])
                P.cp('act', gT.ap[:, t0 + b * 128:t0 + (b + 1) * 128], pt.ap[0:32, 0:128], reads=[pt], writes=[gT])
        P.release(m2)
        if dbg:
            tap(f"gT{li}", gT, [32, NT])
        Wg = [P.alloc([128, KD, 512], BF16) for _ in range(2)]
        Wu = [P.alloc([128, KD, 512], BF16) for _ in range(2)]
        Wd = [P.alloc([128, 4, D], BF16) for _ in range(2)]
        selt = [P.alloc([32, 128]) for _ in range(2)]
        gbc = [P.alloc([128, 512]) for _ in range(2)]
        sgl = [P.alloc([128, 512]) for _ in range(2)]
        a1 = [P.alloc([128, 512]) for _ in range(2)]
        actT = [P.alloc([128, 4, 512], BF16) for _ in range(2)]
        it = 0
        pend = [None]
        def load_gu(e):
            wg = Wg[e % 2]; wu = Wu[e % 2]
            P.dma_group('pool', [(wg.ap[:, k, :], moe_wg[li, e, k * 128:(k + 1) * 128, :]) for k in range(KD)], writes=[wg])
            P.dma_group('pool', [(wu.ap[:, k, :], moe_wu[li, e, k * 128:(k + 1) * 128, :]) for k in range(KD)], writes=[wu])

        def load_d(e):
            wd = Wd[e % 2]
            P.dma_group('pool', [(wd.ap[:, k, :], moe_wd[li, e, k * 128:(k + 1) * 128, :]) for k in range(4)], writes=[wd])

        load_gu(0)
        load_d(0)
        for e in range(32):
            wg = Wg[e % 2]; wu = Wu[e % 2]; wd = Wd[e % 2]; se = selt[e % 2]
            P.cp('pool', se.ap, ident.ap[0:32, e:e + 1].to_broadcast([32, 128]), reads=[ident], writes=[se])
            if e + 1 < 32:
                load_gu(e + 1)
            for tix, (t0, TT, isc) in enumerate(tl):
                it += 1
                gb = gbc[it % 2]; at = actT[it % 2]
                pg_ = P.psum('b')
                P.mm(pg_.ap[:, 0:TT], se.ap, gT.ap[:, t0:t0 + TT], reads=[se, gT], writes=[pg_])
                P.cp('act', gb.ap[:, 0:TT], pg_.ap[:, 0:TT], reads=[pg_], writes=[gb])
                for fc in range(4):
                    fs = slice(fc * 128, (fc + 1) * 128)
                    pg = P.psum('a'); pu = P.psum('a')
                    for k in range(KD):
                        P.mm(pg.ap[:, 0:TT], wg.ap[:, k, fs], hfT.ap[:, k, t0:t0 + TT], start=(k == 0), stop=(k == KD - 1),
                             reads=[wg, hfT], writes=[pg])
                    for k in range(KD):
                        P.mm(pu.ap[:, 0:TT], wu.ap[:, k, fs], hfT.ap[:, k, t0:t0 + TT], start=(k == 0), stop=(k == KD - 1),
                             reads=[wu, hfT], writes=[pu])
                    sg_ = sgl[fc % 2]; a_ = a1[fc % 2]
                    P.act(sg_.ap[:, 0:TT], pg.ap[:, 0:TT], AF.Silu, reads=[pg], writes=[sg_])
                    P.tt('dve', a_.ap[:, 0:TT], pu.ap[:, 0:TT], sg_.ap[:, 0:TT], ALU.mult, reads=[pu, sg_], writes=[a_])
                    P.tt('pool', at.ap[:, fc, 0:TT], a_.ap[:, 0:TT], gb.ap[:, 0:TT], ALU.mult, reads=[a_, gb], writes=[at])
                def down(wd=wd, at=at, t0=t0, TT=TT, isc=isc):
                    for dc in range(KD):
                        po = P.psum('c')
                        for fc in range(4):
                            P.mm(po.ap[:, 0:TT], wd.ap[:, fc, dc * 128:(dc + 1) * 128], at.ap[:, fc, 0:TT],
                                 start=(fc == 0), stop=(fc == 3), reads=[wd, at], writes=[po])
                        P.stt('dve', xres.ap[:, dc, t0:t0 + TT], po.ap[:, 0:TT], mod.ap[:, li, 40 + dc, isc:isc + 1],
                              xres.ap[:, dc, t0:t0 + TT], ALU.mult, ALU.add, reads=[po, mod, xres], writes=[xres])
                if pend[0] is not None:
                    pend[0]()
                pend[0] = down
                if tix == 0 and e + 1 < 32:
                    load_d(e + 1)
        pend[0]()
        for (t0, TT, isc) in tl:
            P.dma_group('sp', [(xTs[k, :, t0:t0 + TT], xres.ap[:, k, t0:t0 + TT]) for k in range(KD)], reads=[xres])
        P.release(m)


    LAM_INIT = 0.8 - 0.6 * math.exp(-0.3 * 1)
    RET_G128 = []
    for h_ in range(4):
        lgf = [math.log(1.0 - 2.0 ** (-5.0 - j)) for j in range(4)]
        RET_G128.append((math.exp(lgf[h_] * 128), math.exp(lgf[3 - h_] * 128)))

    def phase_l1mix():
        m = P.mark()
        LT = [(t0 - C, TT) for (t0, TT, isc) in tiles if not isc]
        perm = P.alloc([128, 128]); P.dma('sp', perm.ap, rope_perm, writes=[perm])
        rc_ = P.alloc([128, S]); P.dma('sp', rc_.ap, ropeC, writes=[rc_])
        rs_ = P.alloc([128, S]); P.dma('sp', rs_.ap, ropeS, writes=[rs_])
        ones128 = P.alloc([128, 128]); P.memset('pool', ones128.ap, 1.0 / 128, writes=[ones128])
        onesb = P.alloc([128, 128], BF16); P.memset('pool', onesb.ap, 1.0, writes=[onesb])
        subg = P.alloc([128, 1]); P.dma('sp', subg.ap, sublnT, writes=[subg])
        P.ts('dve', subg.ap, subg.ap, 1.0 - LAM_INIT, None, ALU.mult, reads=[subg], writes=[subg])
        rgv = P.alloc([128, 4]); P.dma('sp', rgv.ap, retgT, writes=[rgv])
        rkv = P.alloc([128, 8]); P.dma('sp', rkv.ap, retk, writes=[rkv])
        dl = P.alloc([1, 4, 64]); P.dma('sp', dl.ap, dlam.rearrange("o (a b) -> o a b", a=4), writes=[dl])
        pr2 = P.alloc([1, 2, 64]); s2 = P.alloc([1, 4]); nlam = P.alloc([128, 1]); onesr = P.alloc([1, 128])
        P.memset('dve', onesr.ap, 1.0, writes=[onesr])
        P.tt('dve', pr2.ap[:, 0, :], dl.ap[:, 0, :], dl.ap[:, 1, :], ALU.mult, reads=[dl], writes=[pr2])
        P.tt('dve', pr2.ap[:, 1, :], dl.ap[:, 2, :], dl.ap[:, 3, :], ALU.mult, reads=[dl], writes=[pr2])
        P.op('dve', lambda e: e.tensor_reduce(out=s2.ap[:, 0:2], in_=pr2.ap, axis=AX.X, op=ALU.add), [pr2], [s2])
        P.act(s2.ap[:, 0:2], s2.ap[:, 0:2], AF.Exp, reads=[s2], writes=[s2])
        P.tt('dve', s2.ap[:, 2:3], s2.ap[:, 1:2], s2.ap[:, 0:1], ALU.subtract, reads=[s2], writes=[s2])
        P.ts('dve', s2.ap[:, 3:4], s2.ap[:, 2:3], -LAM_INIT, None, ALU.add, reads=[s2], writes=[s2])
        psl = P.psum('a')
        P.mm(psl.ap[:, 0:1], onesr.ap, s2.ap[:, 3:4], reads=[onesr, s2], writes=[psl])
        P.cp('dve', nlam.ap, psl.ap[:, 0:1], reads=[psl], writes=[nlam])
        raw = [P.alloc([128, NT]) for _ in range(2)]
        vT = P.alloc([128, NT])
        kT = P.alloc([128, NT], BF16); qT = P.alloc([128, S], BF16)
        Vtok = P.alloc([128, NCH, 128], BF16)
        t512 = [P.alloc([128, 512]) for _ in range(6)]
        pTb = [P.alloc([128, 512], BF16) for _ in range(5)]
        ymt = [P.alloc([128, 512], BF16) for _ in range(2)]
        cnt = [0]

        def rr(lst):
            cnt[0] += 1
            return lst[cnt[0] % len(lst)]

        def rope(dst, dcol0, src, scol0, scale=None):
            for (l0, TT) in LT:
                ps = P.psum('a')
                P.mm(ps.ap[:, 0:TT], perm.ap, src.ap[:, scol0 + l0:scol0 + l0 + TT], reads=[perm, src], writes=[ps])
                a = rr(t512); b = rr(t512)
                P.tt('dve', a.ap[:, 0:TT], ps.ap[:, 0:TT], rs_.ap[:, l0:l0 + TT], ALU.mult, reads=[ps, rs_], writes=[a])
                P.tt('pool', b.ap[:, 0:TT], src.ap[:, scol0 + l0:scol0 + l0 + TT], rc_.ap[:, l0:l0 + TT], ALU.mult, reads=[src, rc_], writes=[b])
                if scale is None:
                    P.tt('dve', dst.ap[:, dcol0 + l0:dcol0 + l0 + TT], a.ap[:, 0:TT], b.ap[:, 0:TT], ALU.add, reads=[a, b], writes=[dst])
                else:
                    P.tt('dve', a.ap[:, 0:TT], a.ap[:, 0:TT], b.ap[:, 0:TT], ALU.add, reads=[a, b], writes=[a])
                    P.ts('dve', dst.ap[:, dcol0 + l0:dcol0 + l0 + TT], a.ap[:, 0:TT], scale, None, ALU.mult, reads=[a], writes=[dst])

        def make_vtok(vsrc):
            for c4 in range(0, NCH, 4):
                n = min(4, NCH - c4)
                ps = P.psum('a')
                for j in range(n):
                    P.tr(ps.ap[:, j * 128:(j + 1) * 128], vsrc.ap[:, (c4 + j) * 128:(c4 + j + 1) * 128], ident.ap,
                         reads=[vsrc, ident], writes=[ps])
                P.cp('act', Vtok.ap[:, c4:c4 + n, :], ps.ap[:, 0:n * 128].rearrange("p (a b) -> p a b", a=n), reads=[ps], writes=[Vtok])

        def post_norm(o, TT, eps, gain_ap, extra, dst_chunk, l0):
            sq_ = rr(t512)
            P.act(sq_.ap[:, 0:TT], o.ap[:, 0:TT], AF.Square, reads=[o], writes=[sq_])
            ps = P.psum('a')
            P.mm(ps.ap[:, 0:TT], ones128.ap, sq_.ap[:, 0:TT], reads=[ones128, sq_], writes=[ps])
            P.ts('dve', sq_.ap[:, 0:TT], ps.ap[:, 0:TT], eps, None, ALU.add, reads=[ps], writes=[sq_])
            P.act(sq_.ap[:, 0:TT], sq_.ap[:, 0:TT], AF.Ln, reads=[sq_], writes=[sq_])
            P.act(sq_.ap[:, 0:TT], sq_.ap[:, 0:TT], AF.Exp, reads=[sq_], writes=[sq_], scale=-0.5)
            P.stt('dve', sq_.ap[:, 0:TT], o.ap[:, 0:TT], gain_ap, sq_.ap[:, 0:TT], ALU.mult, ALU.mult, reads=[o, sq_, subg, rgv], writes=[sq_])
            ym = rr(ymt)
            if extra is None:
                P.cp('dve', ym.ap[:, 0:TT], sq_.ap[:, 0:TT], reads=[sq_], writes=[ym])
            else:
                P.tt('dve', ym.ap[:, 0:TT], sq_.ap[:, 0:TT], extra, ALU.mult, reads=[sq_, raw[0], raw[1]], writes=[ym])
            P.dma('sp', ymTs[dst_chunk, :, C + l0:C + l0 + TT], ym.ap[:, 0:TT], reads=[ym])

        A1, S1, A2, S2 = P.PS[0], P.PS[1], P.PS[2], P.PS[3]
        for h in range(4):
            P.dma('sp', raw[0].ap, pTs[h], writes=[raw[0]])
            P.dma('act', raw[1].ap[:, 0:S], pTs[14 + h][:, C:NT], writes=[raw[1]])
            P.dma('sp', vT.ap, pTs[4 + h], writes=[vT])
            P.cp('pool', kT.ap[:, 0:C], raw[0].ap[:, 0:C], reads=[raw[0]], writes=[kT])
            rope(kT, C, raw[0], C)
            rope(qT, 0, raw[1], 0)
            make_vtok(vT)
            for (l0, TT) in LT:
                pendq = []

                def emit_av(kc, br, pt, TT=TT):
                    Ab, Sb = ((A1, S1), (A2, S2))[br]
                    P.mm(Ab.ap[:, 0:TT], Vtok.ap[:, kc, :], pt.ap[:, 0:TT], start=(kc == 0), stop=(kc == NCH - 1), reads=[Vtok, pt], writes=[Ab])
                    P.mm(Sb.ap[:, 0:TT], onesb.ap, pt.ap[:, 0:TT], start=(kc == 0), stop=(kc == NCH - 1), reads=[onesb, pt], writes=[Sb])
                for kc in range(NCH):
                    for br in range(2):
                        hsb = slice(br * 64, br * 64 + 64)
                        sc = P.psum('c')
                        P.mm(sc.ap[:, 0:TT], kT.ap[hsb, kc * 128:(kc + 1) * 128], qT.ap[hsb, l0:l0 + TT], reads=[kT, qT], writes=[sc])
                        pt = rr(pTb)
                        P.act(pt.ap[:, 0:TT], sc.ap[:, 0:TT], AF.Exp, reads=[sc], writes=[pt], scale=0.125)
                        pendq.append((kc, br, pt))
                        if len(pendq) > 2:
                            emit_av(*pendq.pop(0))
                while pendq:
                    emit_av(*pendq.pop(0))
                r1 = rr(t512); o1 = rr(t512); r2 = rr(t512); o2 = rr(t512)
                P.op('dve', lambda e, r1=r1, TT=TT: e.reciprocal(out=r1.ap[:, 0:TT], in_=S1.ap[:, 0:TT]), [S1], [r1])
                P.tt('dve', o1.ap[:, 0:TT], A1.ap[:, 0:TT], r1.ap[:, 0:TT], ALU.mult, reads=[A1, r1], writes=[o1])
                P.op('dve', lambda e, r2=r2, TT=TT: e.reciprocal(out=r2.ap[:, 0:TT], in_=S2.ap[:, 0:TT]), [S2], [r2])
                P.tt('dve', o2.ap[:, 0:TT], A2.ap[:, 0:TT], r2.ap[:, 0:TT], ALU.mult, reads=[A2, r2], writes=[o2])
                P.stt('dve', o1.ap[:, 0:TT], o2.ap[:, 0:TT], nlam.ap[:, 0:1], o1.ap[:, 0:TT], ALU.mult, ALU.add, reads=[o2, nlam, o1], writes=[o1])
                post_norm(o1, TT, 1e-5, subg.ap[:, 0:1], None, h, l0)
        kTp = kT
        qTp = qT
        ktok = P.alloc([128, NCH, 128], BF16)
        oT = P.alloc([128, S])
        Rf = P.alloc([128, 128]); Rb = P.alloc([128, 128], BF16)
        dm = P.alloc([128, 128]); qrow = P.alloc([128, 128])
        innm = [P.alloc([128, 128], BF16) for _ in range(2)]
        qd = [P.alloc([128, 128], BF16) for _ in range(2)]
        kd = [P.alloc([128, 64], BF16) for _ in range(2)]
        for h in range(4):
            hq = h % 2
            hsq = slice(hq * 64, hq * 64 + 64)
            if hq == 0:
                P.dma('sp', raw[0].ap, pTs[8 + h // 2], writes=[raw[0]])
                P.dma('act', raw[1].ap[:, 0:S], pTs[18 + h // 2][:, C:NT], writes=[raw[1]])
                P.ts('pool', kTp.ap[:, 0:C], raw[0].ap[:, 0:C], 0.125, None, ALU.mult, reads=[raw[0]], writes=[kTp])
                rope(kTp, C, raw[0], C, scale=0.125)
                rope(qTp, 0, raw[1], 0)
                for c4 in range(0, NCH, 4):
                    n = min(4, NCH - c4)
                    ps = P.psum('a'); psb_ = ps.ap.bitcast(BF16)
                    for j in range(n):
                        P.tr(psb_[:, j * 128:(j + 1) * 128], kTp.ap[:, (c4 + j) * 128:(c4 + j + 1) * 128], identb.ap,
                             reads=[kTp, identb], writes=[ps])
                    P.cp('act', ktok.ap[:, c4:c4 + n, :], psb_[:, 0:n * 128].rearrange("p (a b) -> p a b", a=n), reads=[ps], writes=[ktok])
            P.dma('sp', vT.ap, pTs[10 + h], writes=[vT])
            make_vtok(vT)
            for d in range(2):
                hd = h * 2 + d
                g128 = RET_G128[h][d]
                P.dma('sp', dm.ap, retD[hd], writes=[dm])
                P.dma('sp', qrow.ap, retq[hd:hd + 1, :].partition_broadcast(128), writes=[qrow])
                P.memset('dve', Rf.ap, 0.0, writes=[Rf]); P.memset('pool', Rb.ap, 0.0, writes=[Rb])
                order = list(range(0, NCH)) if d == 0 else list(range(CCH - 1, -1, -1)) + list(range(NCH - 1, CCH - 1, -1))
                for oi, c in enumerate(order):
                    ksl = slice(c * 128, (c + 1) * 128)
                    if c >= CCH:
                        i0 = c * 128 - C
                        im = rr(innm); q_ = rr(qd)
                        ps1 = P.psum('c')
                        P.mm(ps1.ap[:, 0:128], kTp.ap[hsq, ksl], qTp.ap[hsq, i0:i0 + 128], reads=[kTp, qTp], writes=[ps1])
                        P.tt('dve', im.ap, ps1.ap[:, 0:128], dm.ap, ALU.mult, reads=[ps1, dm], writes=[im])
                        P.tt('pool', q_.ap[hsq, :], qTp.ap[hsq, i0:i0 + 128], qrow.ap[hsq, :], ALU.mult, reads=[qTp, qrow], writes=[q_])
                        ps2 = P.psum('c')
                        P.mm(ps2.ap[:, 0:128], Vtok.ap[:, c, :], im.ap, start=True, stop=False, reads=[Vtok, im], writes=[ps2])
                        P.mm(ps2.ap[:, 0:128], Rb.ap[hsq, :], q_.ap[hsq, :], start=False, stop=True, reads=[Rb, q_], writes=[ps2])
                        if d == 0:
                            P.cp('act', oT.ap[:, i0:i0 + 128], ps2.ap[:, 0:128], reads=[ps2], writes=[oT])
                        else:
                            P.tt('dve', oT.ap[:, i0:i0 + 128], ps2.ap[:, 0:128], oT.ap[:, i0:i0 + 128], ALU.add, reads=[ps2, oT], writes=[oT])
                    if oi < len(order) - 1:
                        k_ = rr(kd)
                        P.ts('pool', k_.ap, ktok.ap[:, c, hsq], rkv.ap[:, hd:hd + 1], None, ALU.mult, reads=[ktok, rkv], writes=[k_])
                        ps3 = P.psum('c')
                        P.mm(ps3.ap[hsq, 0:128], k_.ap, Vtok.ap[:, c, :], reads=[k_, Vtok], writes=[ps3])
                        P.stt('dve', Rf.ap[hsq, :], Rf.ap[hsq, :], g128, ps3.ap[hsq, 0:128], ALU.mult, ALU.add, reads=[Rf, ps3], writes=[Rf])
                        P.cp('act', Rb.ap[hsq, :], Rf.ap[hsq, :], reads=[Rf], writes=[Rb])
            P.dma('act', raw[1].ap[:, 0:S], pTs[20 + h][:, C:NT], writes=[raw[1]]) if hq == 1 else \
                P.dma('act', raw[0].ap[:, 0:S], pTs[20 + h][:, C:NT], writes=[raw[0]])
            gsrc = raw[1] if hq == 1 else raw[0]
            P.act(gsrc.ap[:, 0:S], gsrc.ap[:, 0:S], AF.Silu, reads=[gsrc], writes=[gsrc])
            for (l0, TT) in LT:
                ot = T(oT.ap[:, l0:l0 + TT])
                ot.lw = oT.lw
                post_norm(ot, TT, 1e-6, rgv.ap[:, h:h + 1], gsrc.ap[:, l0:l0 + TT], 4 + h, l0)
                oT.rd.update(ot.rd)
        P.release(m)

    if STOP_AFTER == 'mod':
        return P, locals()
    phase_inproj(0, ev_w_in, EV_COLS, True)


    def phase_rwkv():
        m = P.mark()
        DIN, DCH, DST = RW_DT[:3]
        DCN = RW_DT[3] if len(RW_DT) > 3 else DCH
        idcn = ident if DCN == F32 else identb
        idch = ident if DCH == F32 else identb
        idin = ident if DIN == F32 else identb
        seqs = [(0, C), (C, NT)]
        lup = P.alloc([128, 2, 512], BF16); P.dma('pool', lup.ap, lora_up, writes=[lup])
        gup = P.alloc([128, 512], BF16); P.dma('pool', gup.ap, g_up, writes=[gup])
        rv = P.alloc([128, 9, 4]); P.dma('sp', rv.ap, rvec, writes=[rv])
        shv = P.alloc([128, 12, 3]); P.dma('sp', shv.ap, shiftT, writes=[shv])
        rmk = P.alloc([128, 2, 896])
        for d in range(2):
            P.dma('sp', rmk.ap[:, d, :], crmask[d], writes=[rmk])
        omka = P.alloc([128, 4])
        P.ts('dve', omka.ap, rv.ap[:, 5, :], -1.0, 1.0, ALU.mult, ALU.add, reads=[rv], writes=[omka])
        tmpA = P.alloc([128, NT]); tmpB = P.alloc([128, NT])
        wdad = P.alloc([128, NT], BF16); sg = P.alloc([128, NT], BF16)
        P.dma('sp', tmpA.ap, pTs[8], writes=[tmpA])
        P.act(wdad.ap[0:64, :], tmpA.ap[0:64, :], AF.Tanh, reads=[tmpA], writes=[wdad])
        P.cp('dve', wdad.ap[64:128, :], tmpA.ap[64:128, :], reads=[tmpA], writes=[wdad])
        P.dma('sp', tmpB.ap, pTs[13], writes=[tmpB])
        P.act(sg.ap, tmpB.ap, AF.Sigmoid, reads=[tmpB], writes=[sg])
        kc = P.alloc([128, NT]); lw = [P.alloc([128, NT]) for _ in range(2)]
        vc = P.alloc([128, NT], DIN); rc = P.alloc([128, NT], DIN); kk = P.alloc([128, NT], DIN)
        kt = [P.alloc([128, NT], DIN) for _ in range(2)]; bb = [P.alloc([128, NT], DIN) for _ in range(2)]
        MTb = P.alloc([128, 2, NCH, 128], DST); P.memset('pool', MTb.ap, 0.0, writes=[MTb])
        Sbk = P.alloc([128, 2, NCH, 128], DST); P.memset('pool', Sbk.ap, 0.0, writes=[Sbk])
        Gst = P.alloc([128, 2, NCH, 64]); Qs = P.alloc([128, 2, NCH, 128], DST); Y0 = P.alloc([128, NCH, 128])
        Vpad = [P.alloc([128, 2, 128], DCH) for _ in range(2)]
        P2p = [[P.alloc([128, 128], DCH) for _ in range(2)] for _ in range(2)]
        for t_ in Vpad + P2p[0] + P2p[1]:
            P.memset('pool', t_.ap, 0.0, writes=[t_])
        Vtk = [P.alloc([128, 128], DCH) for _ in range(2)]
        lwtok = [P.alloc([128, 128]) for _ in range(2)]
        E1 = [P.alloc([128, 128]) for _ in range(2)]; E0 = [P.alloc([128, 128]) for _ in range(2)]
        Ei = [P.alloc([128, 128]) for _ in range(2)]; nWC = [P.alloc([128, 1]) for _ in range(2)]
        QR = [P.alloc([128, 2, 128], DCH) for _ in range(2)]
        Bt = [P.alloc([128, 128], DCH) for _ in range(2)]; Kt = [P.alloc([128, 128], DCH) for _ in range(2)]
        nBh = [P.alloc([128, 128], DCH) for _ in range(2)]; K2 = [P.alloc([128, 128], DCH) for _ in range(2)]
        TK = [P.alloc([128, 3, 128], DCH) for _ in range(2)]
        evA = [[P.alloc([128, 256], DCH) for _ in range(2)] for _ in range(2)]; evB = [[P.alloc([128, 256], DCH) for _ in range(2)] for _ in range(2)]
        Xr = [[[P.alloc([128, 128], DCN) for _ in range(3)] for _ in range(2)] for _ in range(2)]; XTr = [[[P.alloc([128, 128], DCN) for _ in range(3)] for _ in range(2)] for _ in range(2)]
        TTr = [[[P.alloc([128, 128], DCN) for _ in range(3)] for _ in range(2)] for _ in range(2)]
        TTf = [P.alloc([128, 128], DCH) for _ in range(2)]
        n2v = [[P.alloc([128, 64], DCH) for _ in range(2)] for _ in range(2)]; Pcat = [[P.alloc([128, 128], DCH) for _ in range(2)] for _ in range(2)]
        Sst = [P.alloc([128, 64], DST) for _ in range(2)]
        t512 = [P.alloc([128, 512]) for _ in range(4)]
        ymt = [P.alloc([128, 512], BF16) for _ in range(2)]
        cnt = [0]

        def rr(lst):
            cnt[0] += 1
            return lst[cnt[0] % len(lst)]

        def conv(dst, src, idx):
            for (a, b) in seqs:
                P.ts('dve', dst.ap[:, a:b], src.ap[:, a:b], shv.ap[:, idx, 1:2], None, ALU.mult, reads=[src, shv], writes=[dst])
                P.stt('dve', dst.ap[:, a + 1:b], src.ap[:, a:b - 1], shv.ap[:, idx, 0:1], dst.ap[:, a + 1:b], ALU.mult, ALU.add,
                      reads=[src, shv, dst], writes=[dst])
                P.stt('dve', dst.ap[:, a:b - 1], src.ap[:, a + 1:b], shv.ap[:, idx, 2:3], dst.ap[:, a:b - 1], ALU.mult, ALU.add,
                      reads=[src, shv, dst], writes=[dst])

        for pr in range(4):
            if RW_STOP == 0:
                break
            cs_ = slice(pr * 128, (pr + 1) * 128)
            P.dma('sp', tmpA.ap, pTs[pr], writes=[tmpA]); conv(kc, tmpA, pr)
            P.dma('sp', tmpB.ap, pTs[4 + pr], writes=[tmpB]); conv(vc, tmpB, 4 + pr)
            P.dma('sp', tmpA.ap, pTs[9 + pr], writes=[tmpA]); conv(rc, tmpA, 8 + pr)
            P.ts('dve', tmpA.ap, kc.ap, rv.ap[:, 4, pr:pr + 1], None, ALU.mult, reads=[kc, rv], writes=[tmpA])
            P.act(tmpB.ap, tmpA.ap, AF.Square, reads=[tmpA], writes=[tmpB])
            for (t0, TT, isc) in tiles:
                ps = P.psum('a')
                P.mm(ps.ap[:, 0:TT], blk.ap, tmpB.ap[:, t0:t0 + TT], reads=[blk, tmpB], writes=[ps])
                tq = rr(t512)
                P.ts('dve', tq.ap[:, 0:TT], ps.ap[:, 0:TT], 1e-12, None, ALU.max, reads=[ps], writes=[tq])
                P.act(tq.ap[:, 0:TT], tq.ap[:, 0:TT], AF.Ln, reads=[tq], writes=[tq])
                P.act(tq.ap[:, 0:TT], tq.ap[:, 0:TT], AF.Exp, reads=[tq], writes=[tq], scale=-0.5)
                P.tt('dve', kk.ap[:, t0:t0 + TT], tmpA.ap[:, t0:t0 + TT], tq.ap[:, 0:TT], ALU.mult, reads=[tmpA, tq], writes=[kk])
            for d in range(2):
                for (t0, TT, isc) in tiles:
                    ps = P.psum('a')
                    P.mm(ps.ap[:, 0:TT], lup.ap[0:64, d, cs_], wdad.ap[0:64, t0:t0 + TT], reads=[lup, wdad], writes=[ps])
                    P.act(lw[d].ap[:, t0:t0 + TT], ps.ap[:, 0:TT], AF.Sigmoid, reads=[ps, rv], writes=[lw[d]], bias=rv.ap[:, d, pr:pr + 1])
                    ps2 = P.psum('a')
                    P.mm(ps2.ap[:, 0:TT], lup.ap[64:128, d, cs_], wdad.ap[64:128, t0:t0 + TT], reads=[lup, wdad], writes=[ps2])
                    ta = rr(t512)
                    P.act(ta.ap[:, 0:TT], ps2.ap[:, 0:TT], AF.Sigmoid, reads=[ps2, rv], writes=[ta], bias=rv.ap[:, 2 + d, pr:pr + 1])
                    P.tt('dve', bb[d].ap[:, t0:t0 + TT], ta.ap[:, 0:TT], kk.ap[:, t0:t0 + TT], ALU.mult, reads=[ta, kk], writes=[bb[d]])
                    P.ts('dve', ta.ap[:, 0:TT], ta.ap[:, 0:TT], rv.ap[:, 5, pr:pr + 1], omka.ap[:, pr:pr + 1], ALU.mult, ALU.add,
                         reads=[ta, rv, omka], writes=[ta])
                    P.tt('dve', kt[d].ap[:, t0:t0 + TT], ta.ap[:, 0:TT], kc.ap[:, t0:t0 + TT], ALU.mult, reads=[ta, kc], writes=[kt[d]])
                P.ts('pool', lw[d].ap, lw[d].ap, -W_DECAY_SCALE, None, ALU.mult, reads=[lw[d]], writes=[lw[d]])
            if RW_STOP == 1:
                break
            PS6 = P.PS[6]
            for c in range(NCH):
                cs = slice(c * 128, (c + 1) * 128)
                vp = Vpad[c % 2]; vt = Vtk[c % 2]
                psb = P.psum('b')
                pv_ = psb.ap if DIN == F32 else psb.ap.bitcast(BF16)
                P.tr(pv_[:, 0:128], vc.ap[:, cs], idin.ap, reads=[vc, idin], writes=[psb])
                P.cp('act', vt.ap, pv_[:, 0:128], reads=[psb], writes=[vt])
                for hp in range(2):
                    P.cp('pool', vp.ap[:, hp, hp * 64:hp * 64 + 64], vt.ap[:, hp * 64:hp * 64 + 64], reads=[vt], writes=[vp])
                nmm = 0
                DD = [None, None]
                for d in range(2):
                    i2 = (c * 2 + d) % 2
                    lt = lwtok[i2]; e1 = E1[i2]; e0 = E0[i2]; ei = Ei[i2]; nw = nWC[i2]
                    qr = QR[i2]; bt = Bt[i2]; ktt = Kt[i2]; nb = nBh[i2]; k2 = K2[i2]; tk = TK[i2]
                    ps = P.psum('b')
                    P.tr(ps.ap[:, 0:128], lw[d].ap[:, cs], ident.ap, reads=[lw[d], ident], writes=[ps])
                    P.cp('dve', lt.ap, ps.ap[:, 0:128], reads=[ps], writes=[lt])
                    psc = P.psum('b')
                    P.mm(psc.ap[:, 0:256], lt.ap, rmk.ap[:, d, 640:896], reads=[lt, rmk], writes=[psc])
                    P.act(e1.ap, psc.ap[:, 0:128], AF.Exp, reads=[psc], writes=[e1])
                    P.act(e0.ap, psc.ap[:, 128:256], AF.Exp, reads=[psc], writes=[e0])
                    P.act(ei.ap, psc.ap[:, 0:128], AF.Exp, reads=[psc], writes=[ei], scale=-1.0)
                    wc = e1.ap[:, 127:128] if d == 0 else e1.ap[:, 0:1]
                    P.ts('dve', nw.ap, wc, -1.0, None, ALU.mult, reads=[e1], writes=[nw])
                    P.tt('dve', qr.ap[:, 0, :], kk.ap[:, cs], e0.ap, ALU.mult, reads=[kk, e0], writes=[qr])
                    P.tt('dve', qr.ap[:, 1, :], rc.ap[:, cs], e1.ap, ALU.mult, reads=[rc, e1], writes=[qr])
                    P.tt('pool', bt.ap, bb[d].ap[:, cs], ei.ap, ALU.mult, reads=[bb[d], ei], writes=[bt])
                    P.tt('pool', ktt.ap, kt[d].ap[:, cs], ei.ap, ALU.mult, reads=[kt[d], ei], writes=[ktt])
                    P.ts('dve', nb.ap, bt.ap, nw.ap[:, 0:1], None, ALU.mult, reads=[bt, nw], writes=[nb])
                    P.ts('dve', k2.ap, ktt.ap, wc, None, ALU.mult, reads=[ktt, e1], writes=[k2])
                    pst = P.psum('b'); pstb = pst.ap if DCH == F32 else pst.ap.bitcast(BF16)
                    P.tr(pstb[:, 0:128], qr.ap[:, 0, :], idch.ap, reads=[qr, idch], writes=[pst])
                    P.tr(pstb[:, 128:256], nb.ap, idch.ap, reads=[nb, idch], writes=[pst])
                    P.tr(pstb[:, 256:384], k2.ap, idch.ap, reads=[k2, idch], writes=[pst])
                    P.cp('act', tk.ap, pstb[:, 0:384].rearrange("p (a b) -> p a b", a=3), reads=[pst], writes=[tk])
                    if RW_STOP == 2:
                        pass
                    DD[d] = dict(qr=qr, bt=bt, ktt=ktt, tk=tk, wc=wc, e1=e1)
                H = [[dict(), dict()], [dict(), dict()]]
                for d in range(2):
                    qr = DD[d]['qr']; bt = DD[d]['bt']; ktt = DD[d]['ktt']; tk = DD[d]['tk']; wc = DD[d]['wc']; e1 = DD[d]['e1']
                    qr2s = [qr.ap[slice(hp * 64, hp * 64 + 64), :, :].rearrange("p a b -> p (a b)") for hp in range(2)]
                    for hp in range(2):
                        hs = slice(hp * 64, hp * 64 + 64)
                        ea = evA[d][hp]; eb = evB[d][hp]
                        qr2 = qr2s[hp]
                        p1 = P.psum('r')
                        P.mm(p1.ap[:, 0:256], bt.ap[hs, :], qr2, reads=[bt, qr], writes=[p1])
                        P.tt('dve', ea.ap, p1.ap[:, 0:256], rmk.ap[:, d, 0:256], ALU.mult, reads=[p1, rmk], writes=[ea])
                        p2 = P.psum('r')
                        P.mm(p2.ap[:, 0:256], ktt.ap[hs, :], qr2, reads=[ktt, qr], writes=[p2])
                        P.tt('dve', eb.ap, p2.ap[:, 0:256], rmk.ap[:, d, 256:512], ALU.mult, reads=[p2, rmk], writes=[eb])
                        p3 = P.psum('r')
                        P.mm(p3.ap[:, 0:128], qr.ap[hs, 0, :], bt.ap[hs, :], reads=[qr, bt], writes=[p3])
                        X = Xr[d][hp][0]
                        P.tt('dve', X.ap, p3.ap[:, 0:128], rmk.ap[:, d, 512:640], ALU.mult, reads=[p3, rmk], writes=[X])
                        XT = XTr[d][hp][0]
                        P.cp('pool', XT.ap, ea.ap[:, 0:128], reads=[ea], writes=[XT])
                        TTc = TTr[d][hp][0]
                        P.tt('pool', TTc.ap, ea.ap[:, 0:128], ident.ap, ALU.add, reads=[ea, ident], writes=[TTc])
                        H[d][hp] = dict(X=X, XT=XT, TT=TTc, xi=0, xti=0, ti=0)
                for j in range(1, 7):
                    for d, hp in ((0, 0), (1, 0), (0, 1), (1, 1)):
                        st = H[d][hp]
                        X = st['X']; XT = st['XT']; TTc = st['TT']
                        pX = P.psum('r')
                        P.mm(pX.ap[:, 0:128], XT.ap, X.ap, reads=[XT, X], writes=[pX])
                        st['xi'] = (st['xi'] + 1) % 3
                        Xn = Xr[d][hp][st['xi']]
                        P.cp('act', Xn.ap, pX.ap[:, 0:128], reads=[pX], writes=[Xn])
                        if j < 6:
                            pXT = P.psum('r')
                            P.mm(pXT.ap[:, 0:128], X.ap, XT.ap, reads=[XT, X], writes=[pXT])
                            st['xti'] = (st['xti'] + 1) % 3
                            XTn = XTr[d][hp][st['xti']]
                            P.cp('dve', XTn.ap, pXT.ap[:, 0:128], reads=[pXT], writes=[XTn])
                            st['XT'] = XTn
                        pT = P.psum('r')
                        P.mm(pT.ap[:, 0:128], Xn.ap, TTc.ap, reads=[Xn, TTc], writes=[pT])
                        st['ti'] = (st['ti'] + 1) % 3
                        TTn = TTr[d][hp][st['ti']]
                        P.tt('dve', TTn.ap, pT.ap[:, 0:128], TTc.ap, ALU.add, reads=[pT, TTc], writes=[TTn])
                        st['X'] = Xn
                        st['TT'] = TTn
                for d in range(2):
                    qr = DD[d]['qr']; bt = DD[d]['bt']; ktt = DD[d]['ktt']; tk = DD[d]['tk']; wc = DD[d]['wc']; e1 = DD[d]['e1']
                    for hp in range(2):
                        hs = slice(hp * 64, hp * 64 + 64)
                        ea = evA[d][hp]; eb = evB[d][hp]; TTc = H[d][hp]['TT']
                        nv = n2v[d][hp]; pc = Pcat[d][hp]; p2p = P2p[hp][d]
                        p4 = P.psum('r')
                        P.mm(p4.ap[:, 0:64], eb.ap[:, 0:128], vt.ap[:, hs], reads=[eb, vt], writes=[p4])
                        P.cp('act', nv.ap, p4.ap[:, 0:64], reads=[p4], writes=[nv])
                        p5 = P.psum('r')
                        P.mm(p5.ap[:, 0:64], TTc.ap, tk.ap[:, 0, hs], reads=[TTc, tk], writes=[p5])
                        P.mm(p5.ap[:, 64:128], TTc.ap, nv.ap, reads=[TTc, nv], writes=[p5])
                        P.cp('dve', pc.ap, p5.ap[:, 0:128], reads=[p5], writes=[pc])
                        P.cp('pool', p2p.ap[:, hs], pc.ap[:, 64:128], reads=[pc], writes=[p2p])
                        p6 = P.psum('r')
                        P.mm(p6.ap[hs, 0:64], pc.ap[:, 0:64], tk.ap[:, 1, hs], reads=[pc, tk], writes=[p6])
                        P.stt('dve', MTb.ap[hs, d, c, hs], ident.ap[hs, hs], wc[hs, :], p6.ap[hs, 0:64], ALU.mult, ALU.add,
                              reads=[ident, e1, p6], writes=[MTb])
                        p7 = P.psum('r')
                        P.mm(p7.ap[hs, 0:64], tk.ap[:, 2, hs], vt.ap[:, hs], start=True, stop=False, reads=[tk, vt], writes=[p7])
                        P.mm(p7.ap[hs, 0:64], tk.ap[:, 1, hs], pc.ap[:, 64:128], start=False, stop=True, reads=[tk, pc], writes=[p7])
                        P.cp('act', Gst.ap[hs, d, c, :], p7.ap[hs, 0:64], reads=[p7], writes=[Gst])
                        p8 = P.psum('r')
                        P.mm(p8.ap[hs, 0:128], pc.ap[:, 0:64], ea.ap[:, 128:256], reads=[pc, ea], writes=[p8])
                        P.tt('dve', Qs.ap[hs, d, c, :], p8.ap[hs, 0:128], qr.ap[hs, 1, :], ALU.add, reads=[p8, qr], writes=[Qs])
                        P.mm(PS6.ap[:, 0:128], vp.ap[:, hp, :], eb.ap[:, 128:256], start=(nmm == 0), stop=False,
                             reads=[vp, eb], writes=[PS6])
                        P.mm(PS6.ap[:, 0:128], p2p.ap, ea.ap[:, 128:256], start=False, stop=(nmm == 3),
                             reads=[p2p, ea], writes=[PS6])
                        nmm += 1
                if RW_STOP > 2 and RW_STOP not in (25, 26, 27, 28, 261, 262):
                    P.cp('dve', Y0.ap[:, c, :], PS6.ap[:, 0:128], reads=[PS6], writes=[Y0])
            if RW_STOP <= 3 or RW_STOP in (25, 26, 27, 28, 261, 262):
                break
            for d in range(2):
                order = list(range(0, CCH)) + list(range(CCH, NCH)) if d == 0 else \
                    list(range(CCH - 1, -1, -1)) + list(range(NCH - 1, CCH - 1, -1))
                s_cur = Sst[0]
                P.memset('dve', s_cur.ap, 0.0, writes=[s_cur])
                P.memset('dve', Sbk.ap[:, d, order[0], :], 0.0, writes=[Sbk])
                for i, c in enumerate(order[:-1]):
                    ps = P.psum('b')
                    P.mm(ps.ap[:, 0:64], MTb.ap[:, d, c, :], s_cur.ap, reads=[MTb, s_cur], writes=[ps])
                    s_nx = Sst[(i + 1) % 2]
                    P.tt('dve', s_nx.ap, ps.ap[:, 0:64], Gst.ap[:, d, c, :], ALU.add, reads=[ps, Gst], writes=[s_nx])
                    c2 = order[i + 1]
                    for hp in range(2):
                        hs = slice(hp * 64, hp * 64 + 64)
                        P.cp('pool', Sbk.ap[hs, d, c2, hs], s_nx.ap[hs, :], reads=[s_nx], writes=[Sbk])
                    s_cur = s_nx
            if RW_STOP == 4:
                break
            for c in range(NCH):
                ps = P.psum('b')
                P.mm(ps.ap[:, 0:128], Sbk.ap[:, 0, c, :], Qs.ap[:, 0, c, :], start=True, stop=False, reads=[Sbk, Qs], writes=[ps])
                P.mm(ps.ap[:, 0:128], Sbk.ap[:, 1, c, :], Qs.ap[:, 1, c, :], start=False, stop=True, reads=[Sbk, Qs], writes=[ps])
                P.tt('dve', tmpA.ap[:, c * 128:(c + 1) * 128], ps.ap[:, 0:128], Y0.ap[:, c, :], ALU.add, reads=[ps, Y0], writes=[tmpA])
            if dbg and pr == 0:
                tap("yr0", tmpA, [128, NT])
            for ti, (t0, TT, isc) in enumerate(tiles):
                tsl = slice(t0, t0 + TT)
                ps = P.psum('a')
                P.mm(ps.ap[:, 0:TT], blk.ap, tmpA.ap[:, tsl], reads=[blk, tmpA], writes=[ps])
                dc = rr(t512)
                P.stt('dve', dc.ap[:, 0:TT], ps.ap[:, 0:TT], -1.0 / 64, tmpA.ap[:, tsl], ALU.mult, ALU.add, reads=[ps, tmpA], writes=[dc])
                sq_ = rr(t512)
                P.act(sq_.ap[:, 0:TT], dc.ap[:, 0:TT], AF.Square, reads=[dc], writes=[sq_])
                ps2 = P.psum('a')
                P.mm(ps2.ap[:, 0:TT], blk.ap, sq_.ap[:, 0:TT], reads=[blk, sq_], writes=[ps2])
                P.ts('dve', sq_.ap[:, 0:TT], ps2.ap[:, 0:TT], 1.0 / 64, 64e-5, ALU.mult, ALU.add, reads=[ps2], writes=[sq_])
                P.act(sq_.ap[:, 0:TT], sq_.ap[:, 0:TT], AF.Ln, reads=[sq_], writes=[sq_])
                P.act(sq_.ap[:, 0:TT], sq_.ap[:, 0:TT], AF.Exp, reads=[sq_], writes=[sq_], scale=-0.5)
                P.tt('dve', dc.ap[:, 0:TT], dc.ap[:, 0:TT], sq_.ap[:, 0:TT], ALU.mult, reads=[dc, sq_], writes=[dc])
                P.ts('dve', dc.ap[:, 0:TT], dc.ap[:, 0:TT], rv.ap[:, 7, pr:pr + 1], rv.ap[:, 8, pr:pr + 1], ALU.mult, ALU.add,
                     reads=[dc, rv], writes=[dc])
                bo = rr(t512)
                P.tt('pool', bo.ap[:, 0:TT], kt[0].ap[:, tsl], kt[1].ap[:, tsl], ALU.add, reads=[kt[0], kt[1]], writes=[bo])
                P.tt('pool', bo.ap[:, 0:TT], bo.ap[:, 0:TT], rc.ap[:, tsl], ALU.mult, reads=[bo, rc], writes=[bo])
                P.ts('pool', bo.ap[:, 0:TT], bo.ap[:, 0:TT], rv.ap[:, 6, pr:pr + 1], None, ALU.mult, reads=[bo, rv], writes=[bo])
                ps3 = P.psum('a')
                P.mm(ps3.ap[:, 0:TT], blk.ap, bo.ap[:, 0:TT], reads=[blk, bo], writes=[ps3])
                P.tt('dve', bo.ap[:, 0:TT], ps3.ap[:, 0:TT], vc.ap[:, tsl], ALU.mult, reads=[ps3, vc], writes=[bo])
                P.tt('dve', dc.ap[:, 0:TT], dc.ap[:, 0:TT], bo.ap[:, 0:TT], ALU.add, reads=[dc, bo], writes=[dc])
                ps4 = P.psum('a')
                P.mm(ps4.ap[:, 0:TT], gup.ap[:, cs_], sg.ap[:, tsl], reads=[gup, sg], writes=[ps4])
                ym = ymt[ti % 2]
                P.tt('dve', ym.ap[:, 0:TT], ps4.ap[:, 0:TT], dc.ap[:, 0:TT], ALU.mult, reads=[ps4, dc], writes=[ym])
                P.dma('sp', ymTs[pr, :, tsl], ym.ap[:, 0:TT], reads=[ym])
        P.release(m)

    def phase_pool():
        m = P.mark()
        seqs = [(0, C), (C, NT)]
        pw = P.alloc([128, 4, 128], BF16)
        for gi in range(4):
            P.dma('pool', pw.ap[:, gi, :], pool_w[gi], writes=[pw])
        psc = P.alloc([128, 4]); P.dma('sp', psc.ap, pool_scT, writes=[psc])
        u = P.alloc([128, NT]); acc = P.alloc([128, NT]); inv = P.alloc([128, NT]); df = P.alloc([128, NT], BF16)
        ymt = [P.alloc([128, 512], BF16) for _ in range(2)]
        for gi, win in enumerate((2, 4, 8, 16)):
            P.dma('sp', u.ap, pTs[14 + gi], writes=[u])
            P.dma('sp', inv.ap, pool_inv[gi:gi + 1, :].partition_broadcast(128), writes=[inv])
            P.cp('pool', acc.ap, u.ap, reads=[u], writes=[acc])
            for o in range(-(win // 2), win // 2):
                if o == 0:
                    continue
                for (a, b) in seqs:
                    if o < 0:
                        P.tt('dve', acc.ap[:, a - o:b], acc.ap[:, a - o:b], u.ap[:, a:b + o], ALU.add, reads=[acc, u], writes=[acc])
                    else:
                        P.tt('dve', acc.ap[:, a:b - o], acc.ap[:, a:b - o], u.ap[:, a + o:b], ALU.add, reads=[acc, u], writes=[acc])
            P.tt('dve', acc.ap, acc.ap, inv.ap, ALU.mult, reads=[acc, inv], writes=[acc])
            P.tt('dve', df.ap, acc.ap, u.ap, ALU.subtract, reads=[acc, u], writes=[df])
            for ti, (t0, TT, isc) in enumerate(tiles):
                ps = P.psum('a')
                P.mm(ps.ap[:, 0:TT], pw.ap[:, gi, :], df.ap[:, t0:t0 + TT], reads=[pw, df], writes=[ps])
                ym = ymt[ti % 2]
                P.ts('dve', ym.ap[:, 0:TT], ps.ap[:, 0:TT], psc.ap[:, gi:gi + 1], None, ALU.mult, reads=[ps, psc], writes=[ym])
                P.dma('sp', ymTs[4 + gi, :, t0:t0 + TT], ym.ap[:, 0:TT], reads=[ym])
        P.release(m)

    RUN_RWKV = STOP_AFTER not in ('inproj',)
    RUN_POOL = STOP_AFTER not in ('inproj', 'rwkv')

    def phase_outproj(li, w_out_dram, lat_only):
        m = P.mark()
        Wout = P.alloc([128, KD, D], BF16)
        load_w_bf(Wout, w_out_dram, KD)
        ymb = [P.alloc([128, KD, 512], BF16) for _ in range(2)]
        xT = [P.alloc([128, KD, 512]) for _ in range(2)]
        for ti, (t0, TT, isc) in enumerate(tiles):
            if lat_only and isc:
                continue
            ym = ymb[ti % 2]; xt = xT[ti % 2]
            P.dma_group('sp', [(ym.ap[:, k, 0:TT], ymTs[k, :, t0:t0 + TT]) for k in range(KD)], writes=[ym])
            P.dma_group('act', [(xt.ap[:, k, 0:TT], xTs[k, :, t0:t0 + TT]) for k in range(KD)], writes=[xt])
            for dc in range(KD):
                ps = P.psum('a')
                for k in range(KD):
                    P.mm(ps.ap[:, 0:TT], Wout.ap[:, k, dc * 128:(dc + 1) * 128], ym.ap[:, k, 0:TT],
                         start=(k == 0), stop=(k == KD - 1), reads=[Wout, ym], writes=[ps])
                P.stt('dve', xt.ap[:, dc, 0:TT], ps.ap[:, 0:TT], mod.ap[:, li, 16 + dc, isc:isc + 1], xt.ap[:, dc, 0:TT],
                      ALU.mult, ALU.add, reads=[ps, mod, xt], writes=[xt])
            P.dma_group('sp', [(xTs[k, :, t0:t0 + TT], xt.ap[:, k, 0:TT]) for k in range(KD)], reads=[xt])
        P.release(m)

    def phase_final():
        m = P.mark()
        xT = [P.alloc([128, KD, 512]) for _ in range(2)]
        sq = P.alloc([128, KD, 512]); rs = P.alloc([128, 512])
        ob = [P.alloc([128, KD, 512]) for _ in range(2)]
        otok = [P.alloc([128, D]) for _ in range(2)]
        for ti, (t0, TT, isc) in enumerate(tiles):
            if isc:
                continue
            xt = xT[ti % 2]; o = ob[ti % 2]
            P.dma_group('sp', [(xt.ap[:, k, 0:TT], xTs[k, :, t0:t0 + TT]) for k in range(KD)], writes=[xt])
            norm_mod(xt, TT, 0, 0, 0, o, sq, rs, final=True)
            for b in range(TT // 128):
                ot = otok[b % 2]
                for half in range(2):
                    ps = P.psum('a')
                    for j in range(4):
                        k = half * 4 + j
                        P.tr(ps.ap[:, j * 128:(j + 1) * 128], o.ap[:, k, b * 128:(b + 1) * 128], ident.ap,
                             reads=[o, ident], writes=[ps])
                    P.cp('dve' if half else 'act', ot.ap[:, half * 512:(half + 1) * 512], ps.ap, reads=[ps], writes=[ot])
                r0 = t0 - C + b * 128
                P.dma('sp', out[r0:r0 + 128, :], ot.ap, reads=[ot], is_output=True)
        P.release(m)

    if RUN_RWKV:
        phase_rwkv()
    if RUN_POOL:
        phase_pool()
    if STOP_AFTER in ('inproj', 'rwkv', 'pool'):
        phase_final()
        return P, locals()
    phase_outproj(0, ev_w_out, False)
    if STOP_AFTER == 'l0mix':
        phase_final()
        return P, locals()
    phase_moe(0, False)
    if STOP_AFTER == 'l0':
        phase_final()
        return P, locals()
    phase_inproj(1, od_w_in, OD_COLS, False)
    phase_l1mix()
    phase_outproj(1, od_w_out, True)
    if STOP_AFTER == 'l1mix':
        phase_final()
        return P, locals()
    phase_moe(1, True)
    phase_final()
    return P, locals()


def fm(v, nch=None):
    v = np.asarray(v, np.float32)
    return np.ascontiguousarray(v.reshape(-1, 128).T)


def make_inputs(b, S, C, inp):
    NT = C + S
    m = {}
    m['xin'] = np.ascontiguousarray(np.concatenate([inp['ctx'][b], inp['x'][b]], 0))
    m['cT'] = np.ascontiguousarray(np.stack([fm(inp['c'][b]), fm(inp['c_ctx'])], -1))
    m['ada_w'] = inp['ada_w']
    m['ada_bT'] = np.ascontiguousarray(np.stack([fm(inp['ada_b'][0]), fm(inp['ada_b'][1])], 1))
    m['normT'] = np.ascontiguousarray(np.stack([fm(inp['norm_mix'][0]), fm(inp['norm_mix'][1]), fm(inp['norm_ffn'][0]),
                                               fm(inp['norm_ffn'][1]), fm(inp['final_norm'])], 1))
    m['ev_w_in'] = inp['ev_w_in'][0]
    m['ev_w_out'] = inp['ev_w_out'][0]
    sh = inp['rwkv_shift'][0]
    m['shiftT'] = np.ascontiguousarray(np.stack([fm(sh[0]), fm(sh[1]), fm(sh[2])], -1))
    rv = [inp['rwkv_w0'][0][0], inp['rwkv_w0'][0][1], inp['rwkv_a0'][0][0], inp['rwkv_a0'][0][1], inp['rwkv_k_k'][0],
          inp['rwkv_k_a'][0], inp['rwkv_r_k'][0], inp['rwkv_ln_g'][0], inp['rwkv_ln_b'][0]]
    m['rvec'] = np.ascontiguousarray(np.stack([fm(v) for v in rv], 1))
    lu = np.zeros((128, 2, 512), np.float32)
    for d in range(2):
        lu[0:64, d] = inp['rwkv_w_up'][0][d]
        lu[64:128, d] = inp['rwkv_a_up'][0][d]
    m['lora_up'] = lu
    m['g_up'] = inp['rwkv_g_up'][0]
    m['pool_w'] = inp['pool_w'][0]
    m['pool_scT'] = fm(inp['pool_scale'][0])
    pi = np.zeros((4, NT), np.float32)
    for gi, win in enumerate((2, 4, 8, 16)):
        for (a, Tn) in ((0, C), (C, S)):
            t = np.arange(Tn)
            lo = np.clip(t - win // 2, 0, Tn)
            hi = np.clip(t - win // 2 + win, 0, Tn)
            pi[gi, a:a + Tn] = 1.0 / (hi - lo)
    m['pool_inv'] = pi
    m['moe_r'] = np.ascontiguousarray(np.concatenate([inp['moe_router_group'], inp['moe_router_expert']], -1))
    m['moe_wg'] = inp['moe_w_gate']
    m['moe_wu'] = inp['moe_w_up']
    m['moe_wd'] = inp['moe_w_down']
    m['od_w_in'] = inp['od_w_in'][0]
    m['od_w_out'] = inp['od_w_out'][0]
    m['dlam'] = np.ascontiguousarray(inp['diff_lambda'][0].reshape(1, 256))
    m['sublnT'] = np.ascontiguousarray(inp['diff_subln'][0].reshape(128, 1))
    m['retgT'] = fm(inp['ret_norm'][0])
    m.update(host_consts())
    m.update(host_consts_l1(S))
    return m


def kernel(**inp):
    inp = {k: np.asarray(v) for k, v in inp.items()}
    S, C = inp['x'].shape[1], inp['ctx'].shape[1]
    B = inp['x'].shape[0]
    P, _ = build(S, C)
    nc = P.build()
    in_maps = [make_inputs(b, S, C, inp) for b in range(B)]
    names = set()
    res = run_bass_kernel_spmd(nc, in_maps, core_ids=list(range(B)))
    return np.stack([np.asarray(r["out"], np.float32) for r in res.results], 0)
```

```python
import math
import numpy as np
from contextlib import ExitStack
import concourse.bass as bass
import concourse.mybir as mybir
from concourse.bass_utils import run_bass_kernel_spmd

F32 = mybir.dt.float32
BF16 = mybir.dt.bfloat16
ALU = mybir.AluOpType
AF = mybir.ActivationFunctionType
AX = mybir.AxisListType

ENGS = ['pe', 'act', 'dve', 'pool', 'sp']
DMAQ = ['sp', 'pool', 'act']


def _prod(s):
    r = 1
    for v in s:
        r *= v
    return r


class T:
    __slots__ = ('ap', 'lw', 'rd')

    def __init__(self, ap):
        self.ap = ap
        self.lw = None
        self.rd = {}


class Prog:
    def __init__(self, arena_words=52000, n_dma_sems=8):
        self.nc = bass.Bass("TRN2", target_bir_lowering=False)
        self.es = ExitStack()
        self.ops = {e: [] for e in ENGS}
        self.known = {e: {} for e in ENGS}
        self.pending = {e: [] for e in ENGS}
        self.n_dma_sems = n_dma_sems
        self.dma_rr = {q: 0 for q in DMAQ}
        self.dma_cum = {}
        self.out_tokens = []
        self.nuid = 0
        self.aw = arena_words
        self.arena = self.es.enter_context(self.nc.sbuf_tensor("arena", [128, arena_words], F32))
        self.top = 0
        self.PS = [T(self.es.enter_context(self.nc.psum_tensor(f"psb{i}", [128, 512], F32))[:, :])
                   for i in range(8)]
        self.ps_rr = {'a': 0, 'b': 0, 'c': 0, 'r': 0}
        self.ps_groups = {'a': [0, 1, 2, 3], 'b': [4, 5], 'c': [4, 5, 6, 7], 'r': [0, 1, 2, 3, 7]}

    def psum(self, g='a'):
        lst = self.ps_groups[g]
        i = self.ps_rr[g]
        self.ps_rr[g] = (i + 1) % len(lst)
        return self.PS[lst[i]]

    def alloc(self, shape, dt=F32):
        shape = list(shape)
        esz = 4 if dt == F32 else 2
        nb = _prod(shape[1:]) * esz
        nw = (nb + 3) // 4
        assert self.top + nw <= self.aw, f"arena overflow {self.top}+{nw}>{self.aw}"
        ap = self.arena[0:shape[0], self.top:self.top + nw]
        self.top += nw
        if dt != F32:
            ap = ap.bitcast(dt)
            ap = ap[:, 0:_prod(shape[1:])]
        if len(shape) > 2:
            names = "abcdefg"[:len(shape) - 1]
            kw = {names[i]: shape[i + 1] for i in range(len(shape) - 2)}
            ap = ap.rearrange("p (" + " ".join(names) + ") -> p " + " ".join(names), **kw)
        return T(ap)

    def mark(self):
        return self.top

    def release(self, m):
        self.barrier()
        self.top = m

    def dram(self, name, shape, dt, kind="Internal"):
        return self.nc.dram_tensor(name, list(shape), dt, kind=kind).ap()

    def barrier(self):
        toks = []
        for f in ENGS:
            if len(self.ops[f]) > 0:
                toks.append(('c', f, len(self.ops[f])))
        for skey, cum in self.dma_cum.items():
            toks.append(('d', skey, cum))
        for e in ENGS:
            self.pending[e] = list(toks)

    def _add_wait(self, e, waits, tok):
        if tok is None:
            return
        kind, key, val = tok
        if kind == 'c' and key == e:
            if e == 'pe':
                return
            if val > len(self.ops[e]):
                return
        kk = (kind, key)
        if self.known[e].get(kk, 0) >= val:
            return
        self.known[e][kk] = val
        waits[kk] = max(waits.get(kk, 0), val)

    def _deps(self, e, reads, writes):
        waits = {}
        if self.pending[e]:
            for tok in self.pending[e]:
                self._add_wait(e, waits, tok)
            self.pending[e] = []
        for t in reads:
            self._add_wait(e, waits, t.lw)
        for t in writes:
            self._add_wait(e, waits, t.lw)
            for tok in t.rd.values():
                self._add_wait(e, waits, tok)
        return waits

    def op(self, e, fn, reads=(), writes=()):
        waits = self._deps(e, reads, writes)
        idx = len(self.ops[e]) + 1
        tok = ('c', e, idx)
        self.ops[e].append(dict(fn=fn, waits=waits, inc=None, flag=False))
        for t in reads:
            t.rd[e] = tok
        for t in writes:
            t.lw = tok
            t.rd = {}
        return tok

    def dma(self, q, out_ap, in_ap, reads=(), writes=(), is_output=False, **kw):
        waits = self._deps(q, reads, writes)
        si = self.dma_rr[q]
        self.dma_rr[q] = (si + 1) % self.n_dma_sems
        skey = (q, si)
        prev = self.dma_cum.get(skey, 0)
        if prev > 0:
            self._add_wait(q, waits, ('d', skey, prev))
        val = prev + 16
        self.dma_cum[skey] = val
        tok = ('d', skey, val)

        def fn(eng, out_ap=out_ap, in_ap=in_ap, kw=kw):
            return eng.dma_start(out=out_ap, in_=in_ap, **kw)
        self.ops[q].append(dict(fn=fn, waits=waits, inc=(skey, 16), flag=True))
        for t in reads:
            t.rd[('dma', skey)] = tok
        for t in writes:
            t.lw = tok
            t.rd = {}
        if is_output:
            self.out_tokens.append(tok)
        return tok

    def dma_group(self, q, pairs, reads=(), writes=(), is_output=False):
        waits = self._deps(q, reads, writes)
        si = self.dma_rr[q]
        self.dma_rr[q] = (si + 1) % self.n_dma_sems
        skey = (q, si)
        prev = self.dma_cum.get(skey, 0)
        if prev > 0:
            self._add_wait(q, waits, ('d', skey, prev))
        val = prev
        for i, (out_ap, in_ap) in enumerate(pairs):
            val += 16

            def fn(eng, out_ap=out_ap, in_ap=in_ap):
                return eng.dma_start(out=out_ap, in_=in_ap)
            self.ops[q].append(dict(fn=fn, waits=waits if i == 0 else {}, inc=(skey, 16), flag=True))
        self.dma_cum[skey] = val
        tok = ('d', skey, val)
        for t in reads:
            t.rd[('dma', skey)] = tok
        for t in writes:
            t.lw = tok
            t.rd = {}
        if is_output:
            self.out_tokens.append(tok)
        return tok

    def build(self):
        nc = self.nc
        waits = {}
        for tok in self.out_tokens:
            self._add_wait('sp', waits, tok)
        self.ops['sp'].append(dict(fn=None, waits=waits, inc=None, flag=False))
        for e in ENGS:
            for o in self.ops[e]:
                for (kind, key), val in o['waits'].items():
                    if kind == 'c':
                        self.ops[key][val - 1]['flag'] = True
        rank = {}
        for e in ENGS:
            r = 0
            rk = []
            for o in self.ops[e]:
                if o['inc'] is None and o['flag']:
                    r += 1
                rk.append(r)
            rank[e] = rk
        csem = {e: self.es.enter_context(nc.semaphore(f"c_{e}")) for e in ENGS}
        dsem = {}
        for q in DMAQ:
            for i in range(self.n_dma_sems):
                if (q, i) in self.dma_cum:
                    dsem[(q, i)] = self.es.enter_context(nc.semaphore(f"d_{q}{i}"))
        block = self.es.enter_context(nc.Block())
        engobj = {'pe': block.tensor, 'act': block.scalar, 'dve': block.vector,
                  'pool': block.gpsimd, 'sp': block.sync}

        def mk(e):
            def body(eng):
                for o in self.ops[e]:
                    for (kind, key), val in o['waits'].items():
                        if kind == 'c':
                            eng.wait_ge(csem[key], rank[key][val - 1])
                        else:
                            eng.wait_ge(dsem[key], val)
                    if o['fn'] is None:
                        continue
                    ins = o['fn'](eng)
                    if o['inc'] is not None:
                        ins.then_inc(dsem[o['inc'][0]], 16)
                    elif o['flag']:
                        ins.then_inc(csem[e], 1)
            return body
        for e in ENGS:
            engobj[e](mk(e))
        self.es.close()
        return nc

    def mm(self, out, lhsT, rhs, start=True, stop=True, reads=(), writes=(), **kw):
        def fn(eng):
            return eng.matmul(out, lhsT, rhs, start=start, stop=stop, **kw)
        return self.op('pe', fn, reads, writes)

    def tr(self, out, in_, ident, reads=(), writes=()):
        def fn(eng):
            return eng.transpose(out, in_, ident)
        return self.op('pe', fn, reads, writes)

    def act(self, out, in_, func, reads=(), writes=(), **kw):
        def fn(e):
            return e.activation(out=out, in_=in_, func=func, **kw)
        return self.op('act', fn, reads, writes)

    def tt(self, e, out, in0, in1, op, reads=(), writes=()):
        def fn(eng):
            return eng.tensor_tensor(out=out, in0=in0, in1=in1, op=op)
        return self.op(e, fn, reads, writes)

    def ts(self, e, out, in0, s1, s2, op0, op1=None, reads=(), writes=()):
        def fn(eng):
            if op1 is None:
                return eng.tensor_scalar(out=out, in0=in0, scalar1=s1, scalar2=None, op0=op0)
            return eng.tensor_scalar(out=out, in0=in0, scalar1=s1, scalar2=s2, op0=op0, op1=op1)
        return self.op(e, fn, reads, writes)

    def stt(self, e, out, in0, scalar, in1, op0, op1, reads=(), writes=()):
        def fn(eng):
            return eng.scalar_tensor_tensor(out=out, in0=in0, scalar=scalar, in1=in1, op0=op0, op1=op1)
        return self.op(e, fn, reads, writes)

    def cp(self, e, out, in_, reads=(), writes=()):
        if e == 'act':
            def fn(eng):
                return eng.copy(out=out, in_=in_)
        else:
            def fn(eng):
                return eng.tensor_copy(out=out, in_=in_)
        return self.op(e, fn, reads, writes)

    def memset(self, e, ap, val, writes=()):
        def fn(eng):
            return eng.memset(ap, val)
        return self.op(e, fn, (), writes)


D = 1024
KD = 8
W_DECAY_SCALE = 0.606531
EV_COLS = 2304
OD_COLS = 3072


def host_consts():
    r = np.arange(128)
    Us = (r[:, None] < r[None, :]).astype(np.float32)
    Ui = (r[:, None] <= r[None, :]).astype(np.float32)
    Ls = (r[:, None] > r[None, :]).astype(np.float32)
    Li = (r[:, None] >= r[None, :]).astype(np.float32)
    blk = np.zeros((128, 128), np.float32)
    blk[:64, :64] = 1
    blk[64:, 64:] = 1
    c = {}
    c['ident'] = np.eye(128, dtype=np.float32)
    c['blk64'] = blk
    rm = np.zeros((2, 128, 896), np.float32)
    for d, (ss, si, tsm) in enumerate([(Us, Ui, Ls), (Ls, Li, Us)]):
        rm[d, :, 0:128] = -ss
        rm[d, :, 128:256] = -si
        rm[d, :, 256:384] = ss
        rm[d, :, 384:512] = si
        rm[d, :, 512:640] = -tsm
        rm[d, :, 640:768] = si
        rm[d, :, 768:896] = ss
    c['rmask'] = rm
    return c


def host_consts_l1(S):
    c = {}
    p = np.arange(128)
    blk32 = p % 32
    partner = np.where(blk32 < 16, p + 16, p - 16)
    perm = np.zeros((128, 128), np.float32)
    perm[partner, p] = 1.0
    c['rope_perm'] = perm
    t = np.arange(S)
    row = (t // 64).astype(np.float32)
    col = (t % 64).astype(np.float32)
    b64 = p % 64
    sub = b64 // 32
    j = (b64 % 16).astype(np.float32)
    inv = (10000.0 ** (-j / 16.0)).astype(np.float32)
    pos = np.where(sub[:, None] == 0, row[None, :], col[None, :]).astype(np.float32)
    ang = (pos * inv[:, None]).astype(np.float32)
    sgn = np.where(blk32 < 16, -1.0, 1.0).astype(np.float32)
    c['ropeC'] = np.cos(ang).astype(np.float32)
    c['ropeS'] = (np.sin(ang) * sgn[:, None]).astype(np.float32)
    lgf = np.log(1.0 - 2.0 ** (-5.0 - np.arange(4, dtype=np.float64)))
    r = np.arange(128, dtype=np.float64)
    retD = np.zeros((8, 128, 128), np.float64)
    retq = np.zeros((8, 128), np.float64)
    retk = np.zeros((128, 8), np.float64)
    for h in range(4):
        for d in range(2):
            lg = lgf[h] if d == 0 else lgf[3 - h]
            hd = h * 2 + d
            s_, i_ = r[:, None], r[None, :]
            if d == 0:
                retD[hd] = np.where(i_ >= s_, np.exp(lg * np.maximum(i_ - s_, 0)), 0.0)
                retq[hd] = np.exp(lg * (r + 1))
                retk[:, hd] = np.exp(lg * (127 - r))
            else:
                retD[hd] = np.where(s_ >= i_, np.exp(lg * np.maximum(s_ - i_, 0)), 0.0)
                retq[hd] = np.exp(lg * (128 - r))
                retk[:, hd] = np.exp(lg * r)
    c['retD'] = retD.astype(np.float32)
    c['retq'] = retq.astype(np.float32)
    c['retk'] = retk.astype(np.float32)
    return c


def build(S, C, dbg=False, RW_DT=(BF16, F32, BF16), STOP_AFTER='all', RW_STOP=99):
    P = Prog()
    NT = C + S
    NCH = NT // 128
    CCH = C // 128
    tiles = []
    for base, ln, isc in ((0, C, 1), (C, S, 0)):
        o = 0
        while o < ln:
            l = min(512, ln - o)
            tiles.append((base + o, l, isc))
            o += l
    IN = lambda n, s: P.dram(n, s, F32, "ExternalInput")
    xin = IN("xin", [NT, D])
    cT = IN("cT", [128, KD, 2])
    ada_w = IN("ada_w", [2, D, 6 * D])
    ada_bT = IN("ada_bT", [128, 2, 48])
    normT = IN("normT", [128, 5, KD])
    ev_w_in = IN("ev_w_in", [D, EV_COLS])
    ev_w_out = IN("ev_w_out", [D, D])
    shiftT = IN("shiftT", [128, 12, 3])
    rvec = IN("rvec", [128, 9, 4])
    lora_up = IN("lora_up", [128, 2, 512])
    g_up = IN("g_up", [128, 512])
    pool_w = IN("pool_w", [4, 128, 128])
    pool_scT = IN("pool_scT", [128, 4])
    pool_inv = IN("pool_inv", [4, NT])
    moe_r = IN("moe_r", [2, D, 36])
    moe_wg = IN("moe_wg", [2, 32, D, 512])
    moe_wu = IN("moe_wu", [2, 32, D, 512])
    moe_wd = IN("moe_wd", [2, 32, 512, D])
    od_w_in = IN("od_w_in", [D, OD_COLS])
    od_w_out = IN("od_w_out", [D, D])
    dlam = IN("dlam", [1, 256])
    sublnT = IN("sublnT", [128, 1])
    retgT = IN("retgT", [128, 4])
    rope_perm = IN("rope_perm", [128, 128])
    ropeC = IN("ropeC", [128, S])
    ropeS = IN("ropeS", [128, S])
    retD = IN("retD", [8, 128, 128])
    retq = IN("retq", [8, 128])
    retk = IN("retk", [128, 8])
    cident = IN("ident", [128, 128])
    cblk = IN("blk64", [128, 128])
    crmask = IN("rmask", [2, 128, 896])
    out = P.dram("out", [S, D], F32, "ExternalOutput")
    xTs = P.dram("xTs", [KD, 128, NT], F32)
    pTs = P.dram("pTs", [24, 128, NT], F32, "ExternalOutput" if dbg else "Internal")
    ymTs = P.dram("ymTs", [KD, 128, NT], BF16, "ExternalOutput" if dbg else "Internal")
    dbgs = {}

    def tap(name, t, shape):
        if dbg:
            d = P.dram("dbg_" + name, shape, F32, "ExternalOutput")
            P.dma('sp', d, t.ap, reads=[t], is_output=True)

    ident = P.alloc([128, 128]); P.dma('sp', ident.ap, cident, writes=[ident])
    identb = P.alloc([128, 128], BF16); P.dma('pool', identb.ap, cident, writes=[identb])
    blk = P.alloc([128, 128]); P.dma('sp', blk.ap, cblk, writes=[blk])
    onesD = P.alloc([128, 128]); P.memset('pool', onesD.ap, 1.0 / D, writes=[onesD])
    normv = P.alloc([128, 5, KD]); P.dma('sp', normv.ap, normT, writes=[normv])
    mod = P.alloc([128, 2, 48, 2])
    m0 = P.mark()
    sc = P.alloc([128, KD, 2]); P.dma('sp', sc.ap, cT, writes=[sc])
    P.act(sc.ap, sc.ap, AF.Silu, reads=[sc], writes=[sc])
    adab = P.alloc([128, 2, 48]); P.dma('sp', adab.ap, ada_bT, writes=[adab])
    wbuf = [P.alloc([128, KD, 1024]) for _ in range(2)]
    for li in range(2):
        for blkc in range(6):
            wb = wbuf[(li * 6 + blkc) % 2]
            P.dma_group('sp' if blkc % 2 == 0 else 'act',
                        [(wb.ap[:, k, :], ada_w[li, k * 128:(k + 1) * 128, blkc * 1024:(blkc + 1) * 1024]) for k in range(KD)],
                        writes=[wb])
            for cc in range(8):
                ps = P.psum('a')
                for k in range(KD):
                    P.mm(ps.ap[:, 0:2], wb.ap[:, k, cc * 128:(cc + 1) * 128], sc.ap[:, k, :],
                         start=(k == 0), stop=(k == KD - 1), reads=[wb, sc], writes=[ps])
                j = blkc * 8 + cc
                P.stt('dve', mod.ap[:, li, j, :], ps.ap[:, 0:2], 1.0,
                      adab.ap[:, li, j:j + 1].to_broadcast([128, 2]), ALU.mult, ALU.add,
                      reads=[ps, adab], writes=[mod])
    P.release(m0)
    AB = P.alloc([128, 2, 2, 2, KD, 2])
    for li in range(2):
        for sub in range(2):
            shc = 24 * sub
            scc = 24 * sub + 8
            nidx = li if sub == 0 else 2 + li
            for w in range(2):
                P.ts('dve', AB.ap[:, li, sub, 0, :, w], mod.ap[:, li, scc:scc + 8, w], 1.0, None, ALU.add,
                     reads=[mod], writes=[AB])
                P.tt('dve', AB.ap[:, li, sub, 0, :, w], AB.ap[:, li, sub, 0, :, w], normv.ap[:, nidx, :], ALU.mult,
                     reads=[AB, normv], writes=[AB])
                P.cp('dve', AB.ap[:, li, sub, 1, :, w], mod.ap[:, li, shc:shc + 8, w], reads=[mod], writes=[AB])
    if dbg:
        tap("mod", mod, [128, 2, 48, 2])

    def norm_mod(xT, TT, li, sub, w, hb, sq, rs, final=False):
        P.act(sq.ap[:, :, 0:TT], xT.ap[:, :, 0:TT], AF.Square, reads=[xT], writes=[sq])
        ps = P.psum('a')
        for k in range(KD):
            P.mm(ps.ap[:, 0:TT], onesD.ap, sq.ap[:, k, 0:TT], start=(k == 0), stop=(k == KD - 1),
                 reads=[onesD, sq], writes=[ps])
        P.ts('dve', rs.ap[:, 0:TT], ps.ap[:, 0:TT], 1e-6, None, ALU.add, reads=[ps], writes=[rs])
        P.act(rs.ap[:, 0:TT], rs.ap[:, 0:TT], AF.Ln, reads=[rs], writes=[rs])
        P.act(rs.ap[:, 0:TT], rs.ap[:, 0:TT], AF.Exp, reads=[rs], writes=[rs], scale=-0.5)
        P.tt('dve', sq.ap[:, :, 0:TT], xT.ap[:, :, 0:TT], rs.ap[:, None, 0:TT].to_broadcast([128, KD, TT]), ALU.mult,
             reads=[xT, rs], writes=[sq])
        for k in range(KD):
            if final:
                P.ts('dve' if k % 2 else 'pool', hb.ap[:, k, 0:TT], sq.ap[:, k, 0:TT], normv.ap[:, 4, k:k + 1], None, ALU.mult,
                     reads=[sq, normv], writes=[hb])
            else:
                P.ts('dve' if k % 2 else 'pool', hb.ap[:, k, 0:TT], sq.ap[:, k, 0:TT], AB.ap[:, li, sub, 0, k, w:w + 1],
                     AB.ap[:, li, sub, 1, k, w:w + 1], ALU.mult, ALU.add, reads=[sq, AB], writes=[hb])

    def load_w_bf(dst, src, K):
        P.dma_group('pool', [(dst.ap[:, k, :], src[k * 128:(k + 1) * 128, :]) for k in range(K)], writes=[dst])

    def phase_inproj(li, w_in_dram, ncols, first):
        m = P.mark()
        NCC = ncols // 128
        Win = P.alloc([128, KD, ncols], BF16)
        load_w_bf(Win, w_in_dram, KD)
        xtok = [P.alloc([128, D]) for _ in range(2)]
        xT = [P.alloc([128, KD, 512]) for _ in range(2)]
        hb = [P.alloc([128, KD, 512], BF16) for _ in range(2)]
        sq = P.alloc([128, KD, 512]); rs = P.alloc([128, 512])
        pst = [P.alloc([128, 6, 512]) for _ in range(2)]
        for ti, (t0, TT, isc) in enumerate(tiles):
            xt = xT[ti % 2]
            if first:
                for b in range(TT // 128):
                    xk = xtok[b % 2]
                    P.dma('sp', xk.ap, xin[t0 + b * 128:t0 + (b + 1) * 128, :], writes=[xk])
                    for half in range(2):
                        ps = P.psum('a')
                        for j in range(4):
                            k = half * 4 + j
                            P.tr(ps.ap[:, j * 128:(j + 1) * 128], xk.ap[:, k * 128:(k + 1) * 128], ident.ap,
                                 reads=[xk, ident], writes=[ps])
                        P.cp('dve' if half else 'act', xt.ap[:, half * 4:half * 4 + 4, b * 128:(b + 1) * 128],
                             ps.ap.rearrange("p (a b) -> p a b", a=4), reads=[ps], writes=[xt])
                P.dma_group('act', [(xTs[k, :, t0:t0 + TT], xt.ap[:, k, 0:TT]) for k in range(KD)], reads=[xt])
            else:
                P.dma_group('sp', [(xt.ap[:, k, 0:TT], xTs[k, :, t0:t0 + TT]) for k in range(KD)], writes=[xt])
            h = hb[ti % 2]
            norm_mod(xt, TT, li, 0, isc, h, sq, rs)
            for g in range(NCC // 6):
                st = pst[g % 2]
                for c6 in range(6):
                    cc = g * 6 + c6
                    ps = P.psum('a')
                    for k in range(KD):
                        P.mm(ps.ap[:, 0:TT], Win.ap[:, k, cc * 128:(cc + 1) * 128], h.ap[:, k, 0:TT],
                             start=(k == 0), stop=(k == KD - 1), reads=[Win, h], writes=[ps])
                    P.cp('act' if c6 % 2 else 'dve', st.ap[:, c6, 0:TT], ps.ap[:, 0:TT], reads=[ps], writes=[st])
                P.dma('sp', pTs[g * 6:(g + 1) * 6, :, t0:t0 + TT].rearrange("c p t -> p c t"), st.ap[:, :, 0:TT],
                      reads=[st])
        P.release(m)


    def phase_moe(li, lat_only):
        m = P.mark()
        tl = [t for t in tiles if not (lat_only and t[2])]
        xres = P.alloc([128, KD, NT])
        hfT = P.alloc([128, KD, NT], BF16)
        gT = P.alloc([32, NT])
        wr = P.alloc([128, KD, 36])
        P.dma('sp', wr.ap, moe_r[li].rearrange("(k p) n -> p k n", p=128), writes=[wr])
        m2 = P.mark()
        sq = P.alloc([128, KD, 512]); rs = P.alloc([128, 512])
        lg = P.alloc([128, 36]); oh = P.alloc([128, 4]); st_ = P.alloc([128, 16]); les = P.alloc([128, 8])
        mk1 = P.alloc([128, 8]); mk2 = P.alloc([128, 8]); g8 = P.alloc([128, 8]); g32 = P.alloc([128, 4, 8])
        ex4 = P.alloc([128, 4])
        for (t0, TT, isc) in tl:
            xt = T(xres.ap[:, :, t0:t0 + TT]); hb = T(hfT.ap[:, :, t0:t0 + TT])
            P.dma_group('sp', [(xt.ap[:, k, :], xTs[k, :, t0:t0 + TT]) for k in range(KD)], writes=[xt, xres])
            norm_mod(xt, TT, li, 1, isc, hb, sq, rs)
            for k in range(KD):
                P.ts('dve', sq.ap[:, k, 0:TT], sq.ap[:, k, 0:TT], AB.ap[:, li, 1, 0, k, isc:isc + 1],
                     AB.ap[:, li, 1, 1, k, isc:isc + 1], ALU.mult, ALU.add, reads=[sq, AB], writes=[sq])
            for b in range(TT // 128):
                bs = slice(b * 128, (b + 1) * 128)
                ps = P.psum('a')
                for k in range(KD):
                    P.mm(ps.ap[:, 0:36], sq.ap[:, k, bs], wr.ap[:, k, :], start=(k == 0), stop=(k == KD - 1),
                         reads=[sq, wr], writes=[ps])
                P.cp('dve', lg.ap, ps.ap[:, 0:36], reads=[ps], writes=[lg])
                def red(out, in_, op):
                    return P.op('dve', lambda e: e.tensor_reduce(out=out, in_=in_, axis=AX.X, op=op), [lg, les, ex4, st_], [st_])
                P.op('dve', lambda e: e.tensor_reduce(out=st_.ap[:, 0:1], in_=lg.ap[:, 0:4], axis=AX.X, op=ALU.max), [lg], [st_])
                P.ts('dve', oh.ap, lg.ap[:, 0:4], st_.ap[:, 0:1], None, ALU.is_equal, reads=[lg, st_], writes=[oh])
                P.ts('dve', st_.ap[:, 1:2], st_.ap[:, 0:1], -1.0, None, ALU.mult, reads=[st_], writes=[st_])
                P.act(ex4.ap, lg.ap[:, 0:4], AF.Exp, reads=[lg, st_], writes=[ex4], bias=st_.ap[:, 1:2])
                P.op('dve', lambda e: e.tensor_reduce(out=st_.ap[:, 2:3], in_=ex4.ap, axis=AX.X, op=ALU.add), [ex4], [st_])
                P.op('dve', lambda e: e.reciprocal(out=st_.ap[:, 3:4], in_=st_.ap[:, 2:3]), [st_], [st_])
                P.ts('dve', les.ap, lg.ap[:, 4:12], oh.ap[:, 0:1], None, ALU.mult, reads=[lg, oh], writes=[les])
                for g in range(1, 4):
                    P.stt('dve', les.ap, lg.ap[:, 4 + 8 * g:12 + 8 * g], oh.ap[:, g:g + 1], les.ap, ALU.mult, ALU.add,
                          reads=[lg, oh, les], writes=[les])
                P.op('dve', lambda e: e.tensor_reduce(out=st_.ap[:, 4:5], in_=les.ap, axis=AX.X, op=ALU.max), [les], [st_])
                P.ts('dve', mk1.ap, les.ap, st_.ap[:, 4:5], None, ALU.is_equal, reads=[les, st_], writes=[mk1])
                P.stt('dve', g8.ap, mk1.ap, -1e30, les.ap, ALU.mult, ALU.add, reads=[mk1, les], writes=[g8])
                P.op('dve', lambda e: e.tensor_reduce(out=st_.ap[:, 5:6], in_=g8.ap, axis=AX.X, op=ALU.max), [g8], [st_])
                P.ts('dve', mk2.ap, g8.ap, st_.ap[:, 5:6], None, ALU.is_equal, reads=[g8, st_], writes=[mk2])
                P.tt('dve', st_.ap[:, 6:7], st_.ap[:, 5:6], st_.ap[:, 4:5], ALU.subtract, reads=[st_], writes=[st_])
                P.act(st_.ap[:, 7:8], st_.ap[:, 6:7], AF.Exp, reads=[st_], writes=[st_])
                P.ts('dve', st_.ap[:, 8:9], st_.ap[:, 7:8], 1.0, None, ALU.add, reads=[st_], writes=[st_])
                P.op('dve', lambda e: e.reciprocal(out=st_.ap[:, 9:10], in_=st_.ap[:, 8:9]), [st_], [st_])
                P.tt('dve', st_.ap[:, 10:11], st_.ap[:, 9:10], st_.ap[:, 3:4], ALU.mult, reads=[st_], writes=[st_])
                P.tt('dve', st_.ap[:, 11:12], st_.ap[:, 10:11], st_.ap[:, 7:8], ALU.mult, reads=[st_], writes=[st_])
                P.ts('dve', g8.ap, mk1.ap, st_.ap[:, 10:11], None, ALU.mult, reads=[mk1, st_], writes=[g8])
                P.stt('dve', g8.ap, mk2.ap, st_.ap[:, 11:12], g8.ap, ALU.mult, ALU.add, reads=[mk2, st_, g8], writes=[g8])
                for g in range(4):
                    P.ts('dve', g32.ap[:, g, :], g8.ap, oh.ap[:, g:g + 1], None, ALU.mult, reads=[g8, oh], writes=[g32])
                pt = P.psum('a')
                P.tr(pt.ap[0:32, 0:128], g32.ap.rearrange("p a b -> p (a b)"), ident.ap, reads=[g32, ident], writes=[pt])
                P.cp('act', gT.ap[:, t0 + b * 128:t0 + (b + 1) * 128], pt.ap[0:32, 0:128], reads=[pt], writes=[gT])
        P.release(m2)
        if dbg:
            tap(f"gT{li}", gT, [32, NT])
        Wg = [P.alloc([128, KD, 512], BF16) for _ in range(2)]
        Wu = [P.alloc([128, KD, 512], BF16) for _ in range(2)]
        Wd = [P.alloc([128, 4, D], BF16) for _ in range(2)]
        selt = [P.alloc([32, 128]) for _ in range(2)]
        gbc = [P.alloc([128, 512]) for _ in range(2)]
        sgl = [P.alloc([128, 512]) for _ in range(2)]
        a1 = [P.alloc([128, 512]) for _ in range(2)]
        actT = [P.alloc([128, 4, 512], BF16) for _ in range(2)]
        it = 0
        pend = [None]
        def load_gu(e):
            wg = Wg[e % 2]; wu = Wu[e % 2]
            P.dma_group('pool', [(wg.ap[:, k, :], moe_wg[li, e, k * 128:(k + 1) * 128, :]) for k in range(KD)], writes=[wg])
            P.dma_group('pool', [(wu.ap[:, k, :], moe_wu[li, e, k * 128:(k + 1) * 128, :]) for k in range(KD)], writes=[wu])

        def load_d(e):
            wd = Wd[e % 2]
            P.dma_group('pool', [(wd.ap[:, k, :], moe_wd[li, e, k * 128:(k + 1) * 128, :]) for k in range(4)], writes=[wd])

        load_gu(0)
        load_d(0)
        for e in range(32):
            wg = Wg[e % 2]; wu = Wu[e % 2]; wd = Wd[e % 2]; se = selt[e % 2]
            P.cp('pool', se.ap, ident.ap[0:32, e:e + 1].to_broadcast([32, 128]), reads=[ident], writes=[se])
            if e + 1 < 32:
                load_gu(e + 1)
            for tix, (t0, TT, isc) in enumerate(tl):
                it += 1
                gb = gbc[it % 2]; at = actT[it % 2]
                pg_ = P.psum('b')
                P.mm(pg_.ap[:, 0:TT], se.ap, gT.ap[:, t0:t0 + TT], reads=[se, gT], writes=[pg_])
                P.cp('act', gb.ap[:, 0:TT], pg_.ap[:, 0:TT], reads=[pg_], writes=[gb])
                for fc in range(4):
                    fs = slice(fc * 128, (fc + 1) * 128)
                    pg = P.psum('a'); pu = P.psum('a')
                    for k in range(KD):
                        P.mm(pg.ap[:, 0:TT], wg.ap[:, k, fs], hfT.ap[:, k, t0:t0 + TT], start=(k == 0), stop=(k == KD - 1),
                             reads=[wg, hfT], writes=[pg])
                    for k in range(KD):
                        P.mm(pu.ap[:, 0:TT], wu.ap[:, k, fs], hfT.ap[:, k, t0:t0 + TT], start=(k == 0), stop=(k == KD - 1),
                             reads=[wu, hfT], writes=[pu])
                    sg_ = sgl[fc % 2]; a_ = a1[fc % 2]
                    P.act(sg_.ap[:, 0:TT], pg.ap[:, 0:TT], AF.Silu, reads=[pg], writes=[sg_])
                    P.tt('dve', a_.ap[:, 0:TT], pu.ap[:, 0:TT], sg_.ap[:, 0:TT], ALU.mult, reads=[pu, sg_], writes=[a_])
                    P.tt('pool', at.ap[:, fc, 0:TT], a_.ap[:, 0:TT], gb.ap[:, 0:TT], ALU.mult, reads=[a_, gb], writes=[at])
                def down(wd=wd, at=at, t0=t0, TT=TT, isc=isc):
                    for dc in range(KD):
                        po = P.psum('c')
                        for fc in range(4):
                            P.mm(po.ap[:, 0:TT], wd.ap[:, fc, dc * 128:(dc + 1) * 128], at.ap[:, fc, 0:TT],
                                 start=(fc == 0), stop=(fc == 3), reads=[wd, at], writes=[po])
                        P.stt('dve', xres.ap[:, dc, t0:t0 + TT], po.ap[:, 0:TT], mod.ap[:, li, 40 + dc, isc:isc + 1],
                              xres.ap[:, dc, t0:t0 + TT], ALU.mult, ALU.add, reads=[po, mod, xres], writes=[xres])
                if pend[0] is not None:
                    pend[0]()
                pend[0] = down
                if tix == 0 and e + 1 < 32:
                    load_d(e + 1)
        pend[0]()
        for (t0, TT, isc) in tl:
            P.dma_group('sp', [(xTs[k, :, t0:t0 + TT], xres.ap[:, k, t0:t0 + TT]) for k in range(KD)], reads=[xres])
        P.release(m)


    LAM_INIT = 0.8 - 0.6 * math.exp(-0.3 * 1)
    RET_G128 = []
    for h_ in range(4):
        lgf = [math.log(1.0 - 2.0 ** (-5.0 - j)) for j in range(4)]
        RET_G128.append((math.exp(lgf[h_] * 128), math.exp(lgf[3 - h_] * 128)))

    def phase_l1mix():
        m = P.mark()
        LT = [(t0 - C, TT) for (t0, TT, isc) in tiles if not isc]
        perm = P.alloc([128, 128]); P.dma('sp', perm.ap, rope_perm, writes=[perm])
        rc_ = P.alloc([128, S]); P.dma('sp', rc_.ap, ropeC, writes=[rc_])
        rs_ = P.alloc([128, S]); P.dma('sp', rs_.ap, ropeS, writes=[rs_])
        ones128 = P.alloc([128, 128]); P.memset('pool', ones128.ap, 1.0 / 128, writes=[ones128])
        onesb = P.alloc([128, 128], BF16); P.memset('pool', onesb.ap, 1.0, writes=[onesb])
        subg = P.alloc([128, 1]); P.dma('sp', subg.ap, sublnT, writes=[subg])
        P.ts('dve', subg.ap, subg.ap, 1.0 - LAM_INIT, None, ALU.mult, reads=[subg], writes=[subg])
        rgv = P.alloc([128, 4]); P.dma('sp', rgv.ap, retgT, writes=[rgv])
        rkv = P.alloc([128, 8]); P.dma('sp', rkv.ap, retk, writes=[rkv])
        dl = P.alloc([1, 4, 64]); P.dma('sp', dl.ap, dlam.rearrange("o (a b) -> o a b", a=4), writes=[dl])
        pr2 = P.alloc([1, 2, 64]); s2 = P.alloc([1, 4]); nlam = P.alloc([128, 1]); onesr = P.alloc([1, 128])
        P.memset('dve', onesr.ap, 1.0, writes=[onesr])
        P.tt('dve', pr2.ap[:, 0, :], dl.ap[:, 0, :], dl.ap[:, 1, :], ALU.mult, reads=[dl], writes=[pr2])
        P.tt('dve', pr2.ap[:, 1, :], dl.ap[:, 2, :], dl.ap[:, 3, :], ALU.mult, reads=[dl], writes=[pr2])
        P.op('dve', lambda e: e.tensor_reduce(out=s2.ap[:, 0:2], in_=pr2.ap, axis=AX.X, op=ALU.add), [pr2], [s2])
        P.act(s2.ap[:, 0:2], s2.ap[:, 0:2], AF.Exp, reads=[s2], writes=[s2])
        P.tt('dve', s2.ap[:, 2:3], s2.ap[:, 1:2], s2.ap[:, 0:1], ALU.subtract, reads=[s2], writes=[s2])
        P.ts('dve', s2.ap[:, 3:4], s2.ap[:, 2:3], -LAM_INIT, None, ALU.add, reads=[s2], writes=[s2])
        psl = P.psum('a')
        P.mm(psl.ap[:, 0:1], onesr.ap, s2.ap[:, 3:4], reads=[onesr, s2], writes=[psl])
        P.cp('dve', nlam.ap, psl.ap[:, 0:1], reads=[psl], writes=[nlam])
        raw = [P.alloc([128, NT]) for _ in range(2)]
        vT = P.alloc([128, NT])
        kT = P.alloc([128, NT], BF16); qT = P.alloc([128, S], BF16)
        Vtok = P.alloc([128, NCH, 128], BF16)
        t512 = [P.alloc([128, 512]) for _ in range(6)]
        pTb = [P.alloc([128, 512], BF16) for _ in range(5)]
        ymt = [P.alloc([128, 512], BF16) for _ in range(2)]
        cnt = [0]

        def rr(lst):
            cnt[0] += 1
            return lst[cnt[0] % len(lst)]

        def rope(dst, dcol0, src, scol0, scale=None):
            for (l0, TT) in LT:
                ps = P.psum('a')
                P.mm(ps.ap[:, 0:TT], perm.ap, src.ap[:, scol0 + l0:scol0 + l0 + TT], reads=[perm, src], writes=[ps])
                a = rr(t512); b = rr(t512)
                P.tt('dve', a.ap[:, 0:TT], ps.ap[:, 0:TT], rs_.ap[:, l0:l0 + TT], ALU.mult, reads=[ps, rs_], writes=[a])
                P.tt('pool', b.ap[:, 0:TT], src.ap[:, scol0 + l0:scol0 + l0 + TT], rc_.ap[:, l0:l0 + TT], ALU.mult, reads=[src, rc_], writes=[b])
                if scale is None:
                    P.tt('dve', dst.ap[:, dcol0 + l0:dcol0 + l0 + TT], a.ap[:, 0:TT], b.ap[:, 0:TT], ALU.add, reads=[a, b], writes=[dst])
                else:
                    P.tt('dve', a.ap[:, 0:TT], a.ap[:, 0:TT], b.ap[:, 0:TT], ALU.add, reads=[a, b], writes=[a])
                    P.ts('dve', dst.ap[:, dcol0 + l0:dcol0 + l0 + TT], a.ap[:, 0:TT], scale, None, ALU.mult, reads=[a], writes=[dst])

        def make_vtok(vsrc):
            for c4 in range(0, NCH, 4):
                n = min(4, NCH - c4)
                ps = P.psum('a')
                for j in range(n):
                    P.tr(ps.ap[:, j * 128:(j + 1) * 128], vsrc.ap[:, (c4 + j) * 128:(c4 + j + 1) * 128], ident.ap,
                         reads=[vsrc, ident], writes=[ps])
                P.cp('act', Vtok.ap[:, c4:c4 + n, :], ps.ap[:, 0:n * 128].rearrange("p (a b) -> p a b", a=n), reads=[ps], writes=[Vtok])

        def post_norm(o, TT, eps, gain_ap, extra, dst_chunk, l0):
            sq_ = rr(t512)
            P.act(sq_.ap[:, 0:TT], o.ap[:, 0:TT], AF.Square, reads=[o], writes=[sq_])
            ps = P.psum('a')
            P.mm(ps.ap[:, 0:TT], ones128.ap, sq_.ap[:, 0:TT], reads=[ones128, sq_], writes=[ps])
            P.ts('dve', sq_.ap[:, 0:TT], ps.ap[:, 0:TT], eps, None, ALU.add, reads=[ps], writes=[sq_])
            P.act(sq_.ap[:, 0:TT], sq_.ap[:, 0:TT], AF.Ln, reads=[sq_], writes=[sq_])
            P.act(sq_.ap[:, 0:TT], sq_.ap[:, 0:TT], AF.Exp, reads=[sq_], writes=[sq_], scale=-0.5)
            P.stt('dve', sq_.ap[:, 0:TT], o.ap[:, 0:TT], gain_ap, sq_.ap[:, 0:TT], ALU.mult, ALU.mult, reads=[o, sq_, subg, rgv], writes=[sq_])
            ym = rr(ymt)
            if extra is None:
                P.cp('dve', ym.ap[:, 0:TT], sq_.ap[:, 0:TT], reads=[sq_], writes=[ym])
            else:
                P.tt('dve', ym.ap[:, 0:TT], sq_.ap[:, 0:TT], extra, ALU.mult, reads=[sq_, raw[0], raw[1]], writes=[ym])
            P.dma('sp', ymTs[dst_chunk, :, C + l0:C + l0 + TT], ym.ap[:, 0:TT], reads=[ym])

        A1, S1, A2, S2 = P.PS[0], P.PS[1], P.PS[2], P.PS[3]
        for h in range(4):
            P.dma('sp', raw[0].ap, pTs[h], writes=[raw[0]])
            P.dma('act', raw[1].ap[:, 0:S], pTs[14 + h][:, C:NT], writes=[raw[1]])
            P.dma('sp', vT.ap, pTs[4 + h], writes=[vT])
            P.cp('pool', kT.ap[:, 0:C], raw[0].ap[:, 0:C], reads=[raw[0]], writes=[kT])
            rope(kT, C, raw[0], C)
            rope(qT, 0, raw[1], 0)
            make_vtok(vT)
            for (l0, TT) in LT:
                pendq = []

                def emit_av(kc, br, pt, TT=TT):
                    Ab, Sb = ((A1, S1), (A2, S2))[br]
                    P.mm(Ab.ap[:, 0:TT], Vtok.ap[:, kc, :], pt.ap[:, 0:TT], start=(kc == 0), stop=(kc == NCH - 1), reads=[Vtok, pt], writes=[Ab])
                    P.mm(Sb.ap[:, 0:TT], onesb.ap, pt.ap[:, 0:TT], start=(kc == 0), stop=(kc == NCH - 1), reads=[onesb, pt], writes=[Sb])
                for kc in range(NCH):
                    for br in range(2):
                        hsb = slice(br * 64, br * 64 + 64)
                        sc = P.psum('c')
                        P.mm(sc.ap[:, 0:TT], kT.ap[hsb, kc * 128:(kc + 1) * 128], qT.ap[hsb, l0:l0 + TT], reads=[kT, qT], writes=[sc])
                        pt = rr(pTb)
                        P.act(pt.ap[:, 0:TT], sc.ap[:, 0:TT], AF.Exp, reads=[sc], writes=[pt], scale=0.125)
                        pendq.append((kc, br, pt))
                        if len(pendq) > 2:
                            emit_av(*pendq.pop(0))
                while pendq:
                    emit_av(*pendq.pop(0))
                r1 = rr(t512); o1 = rr(t512); r2 = rr(t512); o2 = rr(t512)
                P.op('dve', lambda e, r1=r1, TT=TT: e.reciprocal(out=r1.ap[:, 0:TT], in_=S1.ap[:, 0:TT]), [S1], [r1])
                P.tt('dve', o1.ap[:, 0:TT], A1.ap[:, 0:TT], r1.ap[:, 0:TT], ALU.mult, reads=[A1, r1], writes=[o1])
                P.op('dve', lambda e, r2=r2, TT=TT: e.reciprocal(out=r2.ap[:, 0:TT], in_=S2.ap[:, 0:TT]), [S2], [r2])
                P.tt('dve', o2.ap[:, 0:TT], A2.ap[:, 0:TT], r2.ap[:, 0:TT], ALU.mult, reads=[A2, r2], writes=[o2])
                P.stt('dve', o1.ap[:, 0:TT], o2.ap[:, 0:TT], nlam.ap[:, 0:1], o1.ap[:, 0:TT], ALU.mult, ALU.add, reads=[o2, nlam, o1], writes=[o1])
                post_norm(o1, TT, 1e-5, subg.ap[:, 0:1], None, h, l0)
        kTp = kT
        qTp = qT
        ktok = P.alloc([128, NCH, 128], BF16)
        oT = P.alloc([128, S])
        Rf = P.alloc([128, 128]); Rb = P.alloc([128, 128], BF16)
        dm = P.alloc([128, 128]); qrow = P.alloc([128, 128])
        innm = [P.alloc([128, 128], BF16) for _ in range(2)]
        qd = [P.alloc([128, 128], BF16) for _ in range(2)]
        kd = [P.alloc([128, 64], BF16) for _ in range(2)]
        for h in range(4):
            hq = h % 2
            hsq = slice(hq * 64, hq * 64 + 64)
            if hq == 0:
                P.dma('sp', raw[0].ap, pTs[8 + h // 2], writes=[raw[0]])
                P.dma('act', raw[1].ap[:, 0:S], pTs[18 + h // 2][:, C:NT], writes=[raw[1]])
                P.ts('pool', kTp.ap[:, 0:C], raw[0].ap[:, 0:C], 0.125, None, ALU.mult, reads=[raw[0]], writes=[kTp])
                rope(kTp, C, raw[0], C, scale=0.125)
                rope(qTp, 0, raw[1], 0)
                for c4 in range(0, NCH, 4):
                    n = min(4, NCH - c4)
                    ps = P.psum('a'); psb_ = ps.ap.bitcast(BF16)
                    for j in range(n):
                        P.tr(psb_[:, j * 128:(j + 1) * 128], kTp.ap[:, (c4 + j) * 128:(c4 + j + 1) * 128], identb.ap,
                             reads=[kTp, identb], writes=[ps])
                    P.cp('act', ktok.ap[:, c4:c4 + n, :], psb_[:, 0:n * 128].rearrange("p (a b) -> p a b", a=n), reads=[ps], writes=[ktok])
            P.dma('sp', vT.ap, pTs[10 + h], writes=[vT])
            make_vtok(vT)
            for d in range(2):
                hd = h * 2 + d
                g128 = RET_G128[h][d]
                P.dma('sp', dm.ap, retD[hd], writes=[dm])
                P.dma('sp', qrow.ap, retq[hd:hd + 1, :].partition_broadcast(128), writes=[qrow])
                P.memset('dve', Rf.ap, 0.0, writes=[Rf]); P.memset('pool', Rb.ap, 0.0, writes=[Rb])
                order = list(range(0, NCH)) if d == 0 else list(range(CCH - 1, -1, -1)) + list(range(NCH - 1, CCH - 1, -1))
                for oi, c in enumerate(order):
                    ksl = slice(c * 128, (c + 1) * 128)
                    if c >= CCH:
                        i0 = c * 128 - C
                        im = rr(innm); q_ = rr(qd)
                        ps1 = P.psum('c')
                        P.mm(ps1.ap[:, 0:128], kTp.ap[hsq, ksl], qTp.ap[hsq, i0:i0 + 128], reads=[kTp, qTp], writes=[ps1])
                        P.tt('dve', im.ap, ps1.ap[:, 0:128], dm.ap, ALU.mult, reads=[ps1, dm], writes=[im])
                        P.tt('pool', q_.ap[hsq, :], qTp.ap[hsq, i0:i0 + 128], qrow.ap[hsq, :], ALU.mult, reads=[qTp, qrow], writes=[q_])
                        ps2 = P.psum('c')
                        P.mm(ps2.ap[:, 0:128], Vtok.ap[:, c, :], im.ap, start=True, stop=False, reads=[Vtok, im], writes=[ps2])
                        P.mm(ps2.ap[:, 0:128], Rb.ap[hsq, :], q_.ap[hsq, :], start=False, stop=True, reads=[Rb, q_], writes=[ps2])
                        if d == 0:
                            P.cp('act', oT.ap[:, i0:i0 + 128], ps2.ap[:, 0:128], reads=[ps2], writes=[oT])
                        else:
                            P.tt('dve', oT.ap[:, i0:i0 + 128], ps2.ap[:, 0:128], oT.ap[:, i0:i0 + 128], ALU.add, reads=[ps2, oT], writes=[oT])
                    if oi < len(order) - 1:
                        k_ = rr(kd)
                        P.ts('pool', k_.ap, ktok.ap[:, c, hsq], rkv.ap[:, hd:hd + 1], None, ALU.mult, reads=[ktok, rkv], writes=[k_])
                        ps3 = P.psum('c')
                        P.mm(ps3.ap[hsq, 0:128], k_.ap, Vtok.ap[:, c, :], reads=[k_, Vtok], writes=[ps3])
                        P.stt('dve', Rf.ap[hsq, :], Rf.ap[hsq, :], g128, ps3.ap[hsq, 0:128], ALU.mult, ALU.add, reads=[Rf, ps3], writes=[Rf])
                        P.cp('act', Rb.ap[hsq, :], Rf.ap[hsq, :], reads=[Rf], writes=[Rb])
            P.dma('act', raw[1].ap[:, 0:S], pTs[20 + h][:, C:NT], writes=[raw[1]]) if hq == 1 else \
                P.dma('act', raw[0].ap[:, 0:S], pTs[20 + h][:, C:NT], writes=[raw[0]])
            gsrc = raw[1] if hq == 1 else raw[0]
            P.act(gsrc.ap[:, 0:S], gsrc.ap[:, 0:S], AF.Silu, reads=[gsrc], writes=[gsrc])
            for (l0, TT) in LT:
                ot = T(oT.ap[:, l0:l0 + TT])
                ot.lw = oT.lw
                post_norm(ot, TT, 1e-6, rgv.ap[:, h:h + 1], gsrc.ap[:, l0:l0 + TT], 4 + h, l0)
                oT.rd.update(ot.rd)
        P.release(m)

    if STOP_AFTER == 'mod':
        return P, locals()
    phase_inproj(0, ev_w_in, EV_COLS, True)


    def phase_rwkv():
        m = P.mark()
        DIN, DCH, DST = RW_DT[:3]
        DCN = RW_DT[3] if len(RW_DT) > 3 else DCH
        idcn = ident if DCN == F32 else identb
        idch = ident if DCH == F32 else identb
        idin = ident if DIN == F32 else identb
        seqs = [(0, C), (C, NT)]
        lup = P.alloc([128, 2, 512], BF16); P.dma('pool', lup.ap, lora_up, writes=[lup])
        gup = P.alloc([128, 512], BF16); P.dma('pool', gup.ap, g_up, writes=[gup])
        rv = P.alloc([128, 9, 4]); P.dma('sp', rv.ap, rvec, writes=[rv])
        shv = P.alloc([128, 12, 3]); P.dma('sp', shv.ap, shiftT, writes=[shv])
        rmk = P.alloc([128, 2, 896])
        for d in range(2):
            P.dma('sp', rmk.ap[:, d, :], crmask[d], writes=[rmk])
        omka = P.alloc([128, 4])
        P.ts('dve', omka.ap, rv.ap[:, 5, :], -1.0, 1.0, ALU.mult, ALU.add, reads=[rv], writes=[omka])
        tmpA = P.alloc([128, NT]); tmpB = P.alloc([128, NT])
        wdad = P.alloc([128, NT], BF16); sg = P.alloc([128, NT], BF16)
        P.dma('sp', tmpA.ap, pTs[8], writes=[tmpA])
        P.act(wdad.ap[0:64, :], tmpA.ap[0:64, :], AF.Tanh, reads=[tmpA], writes=[wdad])
        P.cp('dve', wdad.ap[64:128, :], tmpA.ap[64:128, :], reads=[tmpA], writes=[wdad])
        P.dma('sp', tmpB.ap, pTs[13], writes=[tmpB])
        P.act(sg.ap, tmpB.ap, AF.Sigmoid, reads=[tmpB], writes=[sg])
        kc = P.alloc([128, NT]); lw = [P.alloc([128, NT]) for _ in range(2)]
        vc = P.alloc([128, NT], DIN); rc = P.alloc([128, NT], DIN); kk = P.alloc([128, NT], DIN)
        kt = [P.alloc([128, NT], DIN) for _ in range(2)]; bb = [P.alloc([128, NT], DIN) for _ in range(2)]
        MTb = P.alloc([128, 2, NCH, 128], DST); P.memset('pool', MTb.ap, 0.0, writes=[MTb])
        Sbk = P.alloc([128, 2, NCH, 128], DST); P.memset('pool', Sbk.ap, 0.0, writes=[Sbk])
        Gst = P.alloc([128, 2, NCH, 64]); Qs = P.alloc([128, 2, NCH, 128], DST); Y0 = P.alloc([128, NCH, 128])
        Vpad = [P.alloc([128, 2, 128], DCH) for _ in range(2)]
        P2p = [[P.alloc([128, 128], DCH) for _ in range(2)] for _ in range(2)]
        for t_ in Vpad + P2p[0] + P2p[1]:
            P.memset('pool', t_.ap, 0.0, writes=[t_])
        Vtk = [P.alloc([128, 128], DCH) for _ in range(2)]
        lwtok = [P.alloc([128, 128]) for _ in range(2)]
        E1 = [P.alloc([128, 128]) for _ in range(2)]; E0 = [P.alloc([128, 128]) for _ in range(2)]
        Ei = [P.alloc([128, 128]) for _ in range(2)]; nWC = [P.alloc([128, 1]) for _ in range(2)]
        QR = [P.alloc([128, 2, 128], DCH) for _ in range(2)]
        Bt = [P.alloc([128, 128], DCH) for _ in range(2)]; Kt = [P.alloc([128, 128], DCH) for _ in range(2)]
        nBh = [P.alloc([128, 128], DCH) for _ in range(2)]; K2 = [P.alloc([128, 128], DCH) for _ in range(2)]
        TK = [P.alloc([128, 3, 128], DCH) for _ in range(2)]
        evA = [[P.alloc([128, 256], DCH) for _ in range(2)] for _ in range(2)]; evB = [[P.alloc([128, 256], DCH) for _ in range(2)] for _ in range(2)]
        Xr = [[[P.alloc([128, 128], DCN) for _ in range(3)] for _ in range(2)] for _ in range(2)]; XTr = [[[P.alloc([128, 128], DCN) for _ in range(3)] for _ in range(2)] for _ in range(2)]
        TTr = [[[P.alloc([128, 128], DCN) for _ in range(3)] for _ in range(2)] for _ in range(2)]
        TTf = [P.alloc([128, 128], DCH) for _ in range(2)]
        n2v = [[P.alloc([128, 64], DCH) for _ in range(2)] for _ in range(2)]; Pcat = [[P.alloc([128, 128], DCH) for _ in range(2)] for _ in range(2)]
        Sst = [P.alloc([128, 64], DST) for _ in range(2)]
        t512 = [P.alloc([128, 512]) for _ in range(4)]
        ymt = [P.alloc([128, 512], BF16) for _ in range(2)]
        cnt = [0]

        def rr(lst):
            cnt[0] += 1
            return lst[cnt[0] % len(lst)]

        def conv(dst, src, idx):
            for (a, b) in seqs:
                P.ts('dve', dst.ap[:, a:b], src.ap[:, a:b], shv.ap[:, idx, 1:2], None, ALU.mult, reads=[src, shv], writes=[dst])
                P.stt('dve', dst.ap[:, a + 1:b], src.ap[:, a:b - 1], shv.ap[:, idx, 0:1], dst.ap[:, a + 1:b], ALU.mult, ALU.add,
                      reads=[src, shv, dst], writes=[dst])
                P.stt('dve', dst.ap[:, a:b - 1], src.ap[:, a + 1:b], shv.ap[:, idx, 2:3], dst.ap[:, a:b - 1], ALU.mult, ALU.add,
                      reads=[src, shv, dst], writes=[dst])

        for pr in range(4):
            if RW_STOP == 0:
                break
            cs_ = slice(pr * 128, (pr + 1) * 128)
            P.dma('sp', tmpA.ap, pTs[pr], writes=[tmpA]); conv(kc, tmpA, pr)
            P.dma('sp', tmpB.ap, pTs[4 + pr], writes=[tmpB]); conv(vc, tmpB, 4 + pr)
            P.dma('sp', tmpA.ap, pTs[9 + pr], writes=[tmpA]); conv(rc, tmpA, 8 + pr)
            P.ts('dve', tmpA.ap, kc.ap, rv.ap[:, 4, pr:pr + 1], None, ALU.mult, reads=[kc, rv], writes=[tmpA])
            P.act(tmpB.ap, tmpA.ap, AF.Square, reads=[tmpA], writes=[tmpB])
            for (t0, TT, isc) in tiles:
                ps = P.psum('a')
                P.mm(ps.ap[:, 0:TT], blk.ap, tmpB.ap[:, t0:t0 + TT], reads=[blk, tmpB], writes=[ps])
                tq = rr(t512)
                P.ts('dve', tq.ap[:, 0:TT], ps.ap[:, 0:TT], 1e-12, None, ALU.max, reads=[ps], writes=[tq])
                P.act(tq.ap[:, 0:TT], tq.ap[:, 0:TT], AF.Ln, reads=[tq], writes=[tq])
                P.act(tq.ap[:, 0:TT], tq.ap[:, 0:TT], AF.Exp, reads=[tq], writes=[tq], scale=-0.5)
                P.tt('dve', kk.ap[:, t0:t0 + TT], tmpA.ap[:, t0:t0 + TT], tq.ap[:, 0:TT], ALU.mult, reads=[tmpA, tq], writes=[kk])
            for d in range(2):
                for (t0, TT, isc) in tiles:
                    ps = P.psum('a')
                    P.mm(ps.ap[:, 0:TT], lup.ap[0:64, d, cs_], wdad.ap[0:64, t0:t0 + TT], reads=[lup, wdad], writes=[ps])
                    P.act(lw[d].ap[:, t0:t0 + TT], ps.ap[:, 0:TT], AF.Sigmoid, reads=[ps, rv], writes=[lw[d]], bias=rv.ap[:, d, pr:pr + 1])
                    ps2 = P.psum('a')
                    P.mm(ps2.ap[:, 0:TT], lup.ap[64:128, d, cs_], wdad.ap[64:128, t0:t0 + TT], reads=[lup, wdad], writes=[ps2])
                    ta = rr(t512)
                    P.act(ta.ap[:, 0:TT], ps2.ap[:, 0:TT], AF.Sigmoid, reads=[ps2, rv], writes=[ta], bias=rv.ap[:, 2 + d, pr:pr + 1])
                    P.tt('dve', bb[d].ap[:, t0:t0 + TT], ta.ap[:, 0:TT], kk.ap[:, t0:t0 + TT], ALU.mult, reads=[ta, kk], writes=[bb[d]])
                    P.ts('dve', ta.ap[:, 0:TT], ta.ap[:, 0:TT], rv.ap[:, 5, pr:pr + 1], omka.ap[:, pr:pr + 1], ALU.mult, ALU.add,
                         reads=[ta, rv, omka], writes=[ta])
                    P.tt('dve', kt[d].ap[:, t0:t0 + TT], ta.ap[:, 0:TT], kc.ap[:, t0:t0 + TT], ALU.mult, reads=[ta, kc], writes=[kt[d]])
                P.ts('pool', lw[d].ap, lw[d].ap, -W_DECAY_SCALE, None, ALU.mult, reads=[lw[d]], writes=[lw[d]])
            if RW_STOP == 1:
                break
            PS6 = P.PS[6]
            for c in range(NCH):
                cs = slice(c * 128, (c + 1) * 128)
                vp = Vpad[c % 2]; vt = Vtk[c % 2]
                psb = P.psum('b')
                pv_ = psb.ap if DIN == F32 else psb.ap.bitcast(BF16)
                P.tr(pv_[:, 0:128], vc.ap[:, cs], idin.ap, reads=[vc, idin], writes=[psb])
                P.cp('act', vt.ap, pv_[:, 0:128], reads=[psb], writes=[vt])
                for hp in range(2):
                    P.cp('pool', vp.ap[:, hp, hp * 64:hp * 64 + 64], vt.ap[:, hp * 64:hp * 64 + 64], reads=[vt], writes=[vp])
                nmm = 0
                DD = [None, None]
                for d in range(2):
                    i2 = (c * 2 + d) % 2
                    lt = lwtok[i2]; e1 = E1[i2]; e0 = E0[i2]; ei = Ei[i2]; nw = nWC[i2]
                    qr = QR[i2]; bt = Bt[i2]; ktt = Kt[i2]; nb = nBh[i2]; k2 = K2[i2]; tk = TK[i2]
                    ps = P.psum('b')
                    P.tr(ps.ap[:, 0:128], lw[d].ap[:, cs], ident.ap, reads=[lw[d], ident], writes=[ps])
                    P.cp('dve', lt.ap, ps.ap[:, 0:128], reads=[ps], writes=[lt])
                    psc = P.psum('b')
                    P.mm(psc.ap[:, 0:256], lt.ap, rmk.ap[:, d, 640:896], reads=[lt, rmk], writes=[psc])
                    P.act(e1.ap, psc.ap[:, 0:128], AF.Exp, reads=[psc], writes=[e1])
                    P.act(e0.ap, psc.ap[:, 128:256], AF.Exp, reads=[psc], writes=[e0])
                    P.act(ei.ap, psc.ap[:, 0:128], AF.Exp, reads=[psc], writes=[ei], scale=-1.0)
                    wc = e1.ap[:, 127:128] if d == 0 else e1.ap[:, 0:1]
                    P.ts('dve', nw.ap, wc, -1.0, None, ALU.mult, reads=[e1], writes=[nw])
                    P.tt('dve', qr.ap[:, 0, :], kk.ap[:, cs], e0.ap, ALU.mult, reads=[kk, e0], writes=[qr])
                    P.tt('dve', qr.ap[:, 1, :], rc.ap[:, cs], e1.ap, ALU.mult, reads=[rc, e1], writes=[qr])
                    P.tt('pool', bt.ap, bb[d].ap[:, cs], ei.ap, ALU.mult, reads=[bb[d], ei], writes=[bt])
                    P.tt('pool', ktt.ap, kt[d].ap[:, cs], ei.ap, ALU.mult, reads=[kt[d], ei], writes=[ktt])
                    P.ts('dve', nb.ap, bt.ap, nw.ap[:, 0:1], None, ALU.mult, reads=[bt, nw], writes=[nb])
                    P.ts('dve', k2.ap, ktt.ap, wc, None, ALU.mult, reads=[ktt, e1], writes=[k2])
                    pst = P.psum('b'); pstb = pst.ap if DCH == F32 else pst.ap.bitcast(BF16)
                    P.tr(pstb[:, 0:128], qr.ap[:, 0, :], idch.ap, reads=[qr, idch], writes=[pst])
                    P.tr(pstb[:, 128:256], nb.ap, idch.ap, reads=[nb, idch], writes=[pst])
                    P.tr(pstb[:, 256:384], k2.ap, idch.ap, reads=[k2, idch], writes=[pst])
                    P.cp('act', tk.ap, pstb[:, 0:384].rearrange("p (a b) -> p a b", a=3), reads=[pst], writes=[tk])
                    if RW_STOP == 2:
                        pass
                    DD[d] = dict(qr=qr, bt=bt, ktt=ktt, tk=tk, wc=wc, e1=e1)
                H = [[dict(), dict()], [dict(), dict()]]
                for d in range(2):
                    qr = DD[d]['qr']; bt = DD[d]['bt']; ktt = DD[d]['ktt']; tk = DD[d]['tk']; wc = DD[d]['wc']; e1 = DD[d]['e1']
                    qr2s = [qr.ap[slice(hp * 64, hp * 64 + 64), :, :].rearrange("p a b -> p (a b)") for hp in range(2)]
                    for hp in range(2):
                        hs = slice(hp * 64, hp * 64 + 64)
                        ea = evA[d][hp]; eb = evB[d][hp]
                        qr2 = qr2s[hp]
                        p1 = P.psum('r')
                        P.mm(p1.ap[:, 0:256], bt.ap[hs, :], qr2, reads=[bt, qr], writes=[p1])
                        P.tt('dve', ea.ap, p1.ap[:, 0:256], rmk.ap[:, d, 0:256], ALU.mult, reads=[p1, rmk], writes=[ea])
                        p2 = P.psum('r')
                        P.mm(p2.ap[:, 0:256], ktt.ap[hs, :], qr2, reads=[ktt, qr], writes=[p2])
                        P.tt('dve', eb.ap, p2.ap[:, 0:256], rmk.ap[:, d, 256:512], ALU.mult, reads=[p2, rmk], writes=[eb])
                        p3 = P.psum('r')
                        P.mm(p3.ap[:, 0:128], qr.ap[hs, 0, :], bt.ap[hs, :], reads=[qr, bt], writes=[p3])
                        X = Xr[d][hp][0]
                        P.tt('dve', X.ap, p3.ap[:, 0:128], rmk.ap[:, d, 512:640], ALU.mult, reads=[p3, rmk], writes=[X])
                        XT = XTr[d][hp][0]
                        P.cp('act', XT.ap, ea.ap[:, 0:128], reads=[ea], writes=[XT])
                        TTc = TTr[d][hp][0]
                        P.tt('pool', TTc.ap, ea.ap[:, 0:128], ident.ap, ALU.add, reads=[ea, ident], writes=[TTc])
                        H[d][hp] = dict(X=X, XT=XT, TT=TTc, xi=0, xti=0, ti=0)
                for j in range(1, 7):
                    for d, hp in ((0, 0), (1, 0), (0, 1), (1, 1)):
                        st = H[d][hp]
                        X = st['X']; XT = st['XT']; TTc = st['TT']
                        pX = P.psum('r')
                        P.mm(pX.ap[:, 0:128], XT.ap, X.ap, reads=[XT, X], writes=[pX])
                        st['xi'] = (st['xi'] + 1) % 3
                        Xn = Xr[d][hp][st['xi']]
                        P.cp('act', Xn.ap, pX.ap[:, 0:128], reads=[pX], writes=[Xn])
                        if j < 6:
                            pXT = P.psum('r')
                            P.mm(pXT.ap[:, 0:128], X.ap, XT.ap, reads=[XT, X], writes=[pXT])
                            st['xti'] = (st['xti'] + 1) % 3
                            XTn = XTr[d][hp][st['xti']]
                            P.cp('dve', XTn.ap, pXT.ap[:, 0:128], reads=[pXT], writes=[XTn])
                            st['XT'] = XTn
                        pT = P.psum('r')
                        P.mm(pT.ap[:, 0:128], Xn.ap, TTc.ap, reads=[Xn, TTc], writes=[pT])
                        st['ti'] = (st['ti'] + 1) % 3
                        TTn = TTr[d][hp][st['ti']]
                        P.tt('dve', TTn.ap, pT.ap[:, 0:128], TTc.ap, ALU.add, reads=[pT, TTc], writes=[TTn])
                        st['X'] = Xn
                        st['TT'] = TTn
                for d in range(2):
                    qr = DD[d]['qr']; bt = DD[d]['bt']; ktt = DD[d]['ktt']; tk = DD[d]['tk']; wc = DD[d]['wc']; e1 = DD[d]['e1']
                    for hp in range(2):
                        hs = slice(hp * 64, hp * 64 + 64)
                        ea = evA[d][hp]; eb = evB[d][hp]; TTc = H[d][hp]['TT']
                        nv = n2v[d][hp]; pc = Pcat[d][hp]; p2p = P2p[hp][d]
                        p4 = P.psum('r')
                        P.mm(p4.ap[:, 0:64], eb.ap[:, 0:128], vt.ap[:, hs], reads=[eb, vt], writes=[p4])
                        P.cp('act', nv.ap, p4.ap[:, 0:64], reads=[p4], writes=[nv])
                        p5 = P.psum('r')
                        P.mm(p5.ap[:, 0:64], TTc.ap, tk.ap[:, 0, hs], reads=[TTc, tk], writes=[p5])
                        P.mm(p5.ap[:, 64:128], TTc.ap, nv.ap, reads=[TTc, nv], writes=[p5])
                        P.cp('dve', pc.ap, p5.ap[:, 0:128], reads=[p5], writes=[pc])
                        P.cp('pool', p2p.ap[:, hs], pc.ap[:, 64:128], reads=[pc], writes=[p2p])
                        p6 = P.psum('r')
                        P.mm(p6.ap[hs, 0:64], pc.ap[:, 0:64], tk.ap[:, 1, hs], reads=[pc, tk], writes=[p6])
                        P.stt('dve', MTb.ap[hs, d, c, hs], ident.ap[hs, hs], wc[hs, :], p6.ap[hs, 0:64], ALU.mult, ALU.add,
                              reads=[ident, e1, p6], writes=[MTb])
                        p7 = P.psum('r')
                        P.mm(p7.ap[hs, 0:64], tk.ap[:, 2, hs], vt.ap[:, hs], start=True, stop=False, reads=[tk, vt], writes=[p7])
                        P.mm(p7.ap[hs, 0:64], tk.ap[:, 1, hs], pc.ap[:, 64:128], start=False, stop=True, reads=[tk, pc], writes=[p7])
                        P.cp('act', Gst.ap[hs, d, c, :], p7.ap[hs, 0:64], reads=[p7], writes=[Gst])
                        p8 = P.psum('r')
                        P.mm(p8.ap[hs, 0:128], pc.ap[:, 0:64], ea.ap[:, 128:256], reads=[pc, ea], writes=[p8])
                        P.tt('dve', Qs.ap[hs, d, c, :], p8.ap[hs, 0:128], qr.ap[hs, 1, :], ALU.add, reads=[p8, qr], writes=[Qs])
                        P.mm(PS6.ap[:, 0:128], vp.ap[:, hp, :], eb.ap[:, 128:256], start=(nmm == 0), stop=False,
                             reads=[vp, eb], writes=[PS6])
                        P.mm(PS6.ap[:, 0:128], p2p.ap, ea.ap[:, 128:256], start=False, stop=(nmm == 3),
                             reads=[p2p, ea], writes=[PS6])
                        nmm += 1
                if RW_STOP > 2 and RW_STOP not in (25, 26, 27, 28, 261, 262):
                    P.cp('dve', Y0.ap[:, c, :], PS6.ap[:, 0:128], reads=[PS6], writes=[Y0])
            if RW_STOP <= 3 or RW_STOP in (25, 26, 27, 28, 261, 262):
                break
            for d in range(2):
                order = list(range(0, CCH)) + list(range(CCH, NCH)) if d == 0 else \
                    list(range(CCH - 1, -1, -1)) + list(range(NCH - 1, CCH - 1, -1))
                s_cur = Sst[0]
                P.memset('dve', s_cur.ap, 0.0, writes=[s_cur])
                P.memset('dve', Sbk.ap[:, d, order[0], :], 0.0, writes=[Sbk])
                for i, c in enumerate(order[:-1]):
                    ps = P.psum('b')
                    P.mm(ps.ap[:, 0:64], MTb.ap[:, d, c, :], s_cur.ap, reads=[MTb, s_cur], writes=[ps])
                    s_nx = Sst[(i + 1) % 2]
                    P.tt('dve', s_nx.ap, ps.ap[:, 0:64], Gst.ap[:, d, c, :], ALU.add, reads=[ps, Gst], writes=[s_nx])
                    c2 = order[i + 1]
                    for hp in range(2):
                        hs = slice(hp * 64, hp * 64 + 64)
                        P.cp('pool', Sbk.ap[hs, d, c2, hs], s_nx.ap[hs, :], reads=[s_nx], writes=[Sbk])
                    s_cur = s_nx
            if RW_STOP == 4:
                break
            for c in range(NCH):
                ps = P.psum('b')
                P.mm(ps.ap[:, 0:128], Sbk.ap[:, 0, c, :], Qs.ap[:, 0, c, :], start=True, stop=False, reads=[Sbk, Qs], writes=[ps])
                P.mm(ps.ap[:, 0:128], Sbk.ap[:, 1, c, :], Qs.ap[:, 1, c, :], start=False, stop=True, reads=[Sbk, Qs], writes=[ps])
                P.tt('dve', tmpA.ap[:, c * 128:(c + 1) * 128], ps.ap[:, 0:128], Y0.ap[:, c, :], ALU.add, reads=[ps, Y0], writes=[tmpA])
            if dbg and pr == 0:
                tap("yr0", tmpA, [128, NT])
            for ti, (t0, TT, isc) in enumerate(tiles):
                tsl = slice(t0, t0 + TT)
                ps = P.psum('a')
                P.mm(ps.ap[:, 0:TT], blk.ap, tmpA.ap[:, tsl], reads=[blk, tmpA], writes=[ps])
                dc = rr(t512)
                P.stt('dve', dc.ap[:, 0:TT], ps.ap[:, 0:TT], -1.0 / 64, tmpA.ap[:, tsl], ALU.mult, ALU.add, reads=[ps, tmpA], writes=[dc])
                sq_ = rr(t512)
                P.act(sq_.ap[:, 0:TT], dc.ap[:, 0:TT], AF.Square, reads=[dc], writes=[sq_])
                ps2 = P.psum('a')
                P.mm(ps2.ap[:, 0:TT], blk.ap, sq_.ap[:, 0:TT], reads=[blk, sq_], writes=[ps2])
                P.ts('dve', sq_.ap[:, 0:TT], ps2.ap[:, 0:TT], 1.0 / 64, 64e-5, ALU.mult, ALU.add, reads=[ps2], writes=[sq_])
                P.act(sq_.ap[:, 0:TT], sq_.ap[:, 0:TT], AF.Ln, reads=[sq_], writes=[sq_])
                P.act(sq_.ap[:, 0:TT], sq_.ap[:, 0:TT], AF.Exp, reads=[sq_], writes=[sq_], scale=-0.5)
                P.tt('dve', dc.ap[:, 0:TT], dc.ap[:, 0:TT], sq_.ap[:, 0:TT], ALU.mult, reads=[dc, sq_], writes=[dc])
                P.ts('dve', dc.ap[:, 0:TT], dc.ap[:, 0:TT], rv.ap[:, 7, pr:pr + 1], rv.ap[:, 8, pr:pr + 1], ALU.mult, ALU.add,
                     reads=[dc, rv], writes=[dc])
                bo = rr(t512)
                P.tt('pool', bo.ap[:, 0:TT], kt[0].ap[:, tsl], kt[1].ap[:, tsl], ALU.add, reads=[kt[0], kt[1]], writes=[bo])
                P.tt('pool', bo.ap[:, 0:TT], bo.ap[:, 0:TT], rc.ap[:, tsl], ALU.mult, reads=[bo, rc], writes=[bo])
                P.ts('pool', bo.ap[:, 0:TT], bo.ap[:, 0:TT], rv.ap[:, 6, pr:pr + 1], None, ALU.mult, reads=[bo, rv], writes=[bo])
                ps3 = P.psum('a')
                P.mm(ps3.ap[:, 0:TT], blk.ap, bo.ap[:, 0:TT], reads=[blk, bo], writes=[ps3])
                P.tt('dve', bo.ap[:, 0:TT], ps3.ap[:, 0:TT], vc.ap[:, tsl], ALU.mult, reads=[ps3, vc], writes=[bo])
                P.tt('dve', dc.ap[:, 0:TT], dc.ap[:, 0:TT], bo.ap[:, 0:TT], ALU.add, reads=[dc, bo], writes=[dc])
                ps4 = P.psum('a')
                P.mm(ps4.ap[:, 0:TT], gup.ap[:, cs_], sg.ap[:, tsl], reads=[gup, sg], writes=[ps4])
                ym = ymt[ti % 2]
                P.tt('dve', ym.ap[:, 0:TT], ps4.ap[:, 0:TT], dc.ap[:, 0:TT], ALU.mult, reads=[ps4, dc], writes=[ym])
                P.dma('sp', ymTs[pr, :, tsl], ym.ap[:, 0:TT], reads=[ym])
        P.release(m)

    def phase_pool():
        m = P.mark()
        seqs = [(0, C), (C, NT)]
        pw = P.alloc([128, 4, 128], BF16)
        for gi in range(4):
            P.dma('pool', pw.ap[:, gi, :], pool_w[gi], writes=[pw])
        psc = P.alloc([128, 4]); P.dma('sp', psc.ap, pool_scT, writes=[psc])
        u = P.alloc([128, NT]); acc = P.alloc([128, NT]); inv = P.alloc([128, NT]); df = P.alloc([128, NT], BF16)
        ymt = [P.alloc([128, 512], BF16) for _ in range(2)]
        for gi, win in enumerate((2, 4, 8, 16)):
            P.dma('sp', u.ap, pTs[14 + gi], writes=[u])
            P.dma('sp', inv.ap, pool_inv[gi:gi + 1, :].partition_broadcast(128), writes=[inv])
            P.cp('pool', acc.ap, u.ap, reads=[u], writes=[acc])
            for o in range(-(win // 2), win // 2):
                if o == 0:
                    continue
                for (a, b) in seqs:
                    if o < 0:
                        P.tt('dve', acc.ap[:, a - o:b], acc.ap[:, a - o:b], u.ap[:, a:b + o], ALU.add, reads=[acc, u], writes=[acc])
                    else:
                        P.tt('dve', acc.ap[:, a:b - o], acc.ap[:, a:b - o], u.ap[:, a + o:b], ALU.add, reads=[acc, u], writes=[acc])
            P.tt('dve', acc.ap, acc.ap, inv.ap, ALU.mult, reads=[acc, inv], writes=[acc])
            P.tt('dve', df.ap, acc.ap, u.ap, ALU.subtract, reads=[acc, u], writes=[df])
            for ti, (t0, TT, isc) in enumerate(tiles):
                ps = P.psum('a')
                P.mm(ps.ap[:, 0:TT], pw.ap[:, gi, :], df.ap[:, t0:t0 + TT], reads=[pw, df], writes=[ps])
                ym = ymt[ti % 2]
                P.ts('dve', ym.ap[:, 0:TT], ps.ap[:, 0:TT], psc.ap[:, gi:gi + 1], None, ALU.mult, reads=[ps, psc], writes=[ym])
                P.dma('sp', ymTs[4 + gi, :, t0:t0 + TT], ym.ap[:, 0:TT], reads=[ym])
        P.release(m)

    RUN_RWKV = STOP_AFTER not in ('inproj',)
    RUN_POOL = STOP_AFTER not in ('inproj', 'rwkv')

    def phase_outproj(li, w_out_dram, lat_only):
        m = P.mark()
        Wout = P.alloc([128, KD, D], BF16)
        load_w_bf(Wout, w_out_dram, KD)
        ymb = [P.alloc([128, KD, 512], BF16) for _ in range(2)]
        xT = [P.alloc([128, KD, 512]) for _ in range(2)]
        for ti, (t0, TT, isc) in enumerate(tiles):
            if lat_only and isc:
                continue
            ym = ymb[ti % 2]; xt = xT[ti % 2]
            P.dma_group('sp', [(ym.ap[:, k, 0:TT], ymTs[k, :, t0:t0 + TT]) for k in range(KD)], writes=[ym])
            P.dma_group('act', [(xt.ap[:, k, 0:TT], xTs[k, :, t0:t0 + TT]) for k in range(KD)], writes=[xt])
            for dc in range(KD):
                ps = P.psum('a')
                for k in range(KD):
                    P.mm(ps.ap[:, 0:TT], Wout.ap[:, k, dc * 128:(dc + 1) * 128], ym.ap[:, k, 0:TT],
                         start=(k == 0), stop=(k == KD - 1), reads=[Wout, ym], writes=[ps])
                P.stt('dve', xt.ap[:, dc, 0:TT], ps.ap[:, 0:TT], mod.ap[:, li, 16 + dc, isc:isc + 1], xt.ap[:, dc, 0:TT],
                      ALU.mult, ALU.add, reads=[ps, mod, xt], writes=[xt])
            P.dma_group('sp', [(xTs[k, :, t0:t0 + TT], xt.ap[:, k, 0:TT]) for k in range(KD)], reads=[xt])
        P.release(m)

    def phase_final():
        m = P.mark()
        xT = [P.alloc([128, KD, 512]) for _ in range(2)]
        sq = P.alloc([128, KD, 512]); rs = P.alloc([128, 512])
        ob = [P.alloc([128, KD, 512]) for _ in range(2)]
        otok = [P.alloc([128, D]) for _ in range(2)]
        for ti, (t0, TT, isc) in enumerate(tiles):
            if isc:
                continue
            xt = xT[ti % 2]; o = ob[ti % 2]
            P.dma_group('sp', [(xt.ap[:, k, 0:TT], xTs[k, :, t0:t0 + TT]) for k in range(KD)], writes=[xt])
            norm_mod(xt, TT, 0, 0, 0, o, sq, rs, final=True)
            for b in range(TT // 128):
                ot = otok[b % 2]
                for half in range(2):
                    ps = P.psum('a')
                    for j in range(4):
                        k = half * 4 + j
                        P.tr(ps.ap[:, j * 128:(j + 1) * 128], o.ap[:, k, b * 128:(b + 1) * 128], ident.ap,
                             reads=[o, ident], writes=[ps])
                    P.cp('dve' if half else 'act', ot.ap[:, half * 512:(half + 1) * 512], ps.ap, reads=[ps], writes=[ot])
                r0 = t0 - C + b * 128
                P.dma('sp', out[r0:r0 + 128, :], ot.ap, reads=[ot], is_output=True)
        P.release(m)

    if RUN_RWKV:
        phase_rwkv()
    if RUN_POOL:
        phase_pool()
    if STOP_AFTER in ('inproj', 'rwkv', 'pool'):
        phase_final()
        return P, locals()
    phase_outproj(0, ev_w_out, False)
    if STOP_AFTER == 'l0mix':
        phase_final()
        return P, locals()
    phase_moe(0, False)
    if STOP_AFTER == 'l0':
        phase_final()
        return P, locals()
    phase_inproj(1, od_w_in, OD_COLS, False)
    phase_l1mix()
    phase_outproj(1, od_w_out, True)
    if STOP_AFTER == 'l1mix':
        phase_final()
        return P, locals()
    phase_moe(1, True)
    phase_final()
    return P, locals()


def fm(v, nch=None):
    v = np.asarray(v, np.float32)
    return np.ascontiguousarray(v.reshape(-1, 128).T)


def make_inputs(b, S, C, inp):
    NT = C + S
    m = {}
    m['xin'] = np.ascontiguousarray(np.concatenate([inp['ctx'][b], inp['x'][b]], 0))
    m['cT'] = np.ascontiguousarray(np.stack([fm(inp['c'][b]), fm(inp['c_ctx'])], -1))
    m['ada_w'] = inp['ada_w']
    m['ada_bT'] = np.ascontiguousarray(np.stack([fm(inp['ada_b'][0]), fm(inp['ada_b'][1])], 1))
    m['normT'] = np.ascontiguousarray(np.stack([fm(inp['norm_mix'][0]), fm(inp['norm_mix'][1]), fm(inp['norm_ffn'][0]),
                                               fm(inp['norm_ffn'][1]), fm(inp['final_norm'])], 1))
    m['ev_w_in'] = inp['ev_w_in'][0]
    m['ev_w_out'] = inp['ev_w_out'][0]
    sh = inp['rwkv_shift'][0]
    m['shiftT'] = np.ascontiguousarray(np.stack([fm(sh[0]), fm(sh[1]), fm(sh[2])], -1))
    rv = [inp['rwkv_w0'][0][0], inp['rwkv_w0'][0][1], inp['rwkv_a0'][0][0], inp['rwkv_a0'][0][1], inp['rwkv_k_k'][0],
          inp['rwkv_k_a'][0], inp['rwkv_r_k'][0], inp['rwkv_ln_g'][0], inp['rwkv_ln_b'][0]]
    m['rvec'] = np.ascontiguousarray(np.stack([fm(v) for v in rv], 1))
    lu = np.zeros((128, 2, 512), np.float32)
    for d in range(2):
        lu[0:64, d] = inp['rwkv_w_up'][0][d]
        lu[64:128, d] = inp['rwkv_a_up'][0][d]
    m['lora_up'] = lu
    m['g_up'] = inp['rwkv_g_up'][0]
    m['pool_w'] = inp['pool_w'][0]
    m['pool_scT'] = fm(inp['pool_scale'][0])
    pi = np.zeros((4, NT), np.float32)
    for gi, win in enumerate((2, 4, 8, 16)):
        for (a, Tn) in ((0, C), (C, S)):
            t = np.arange(Tn)
            lo = np.clip(t - win // 2, 0, Tn)
            hi = np.clip(t - win // 2 + win, 0, Tn)
            pi[gi, a:a + Tn] = 1.0 / (hi - lo)
    m['pool_inv'] = pi
    m['moe_r'] = np.ascontiguousarray(np.concatenate([inp['moe_router_group'], inp['moe_router_expert']], -1))
    m['moe_wg'] = inp['moe_w_gate']
    m['moe_wu'] = inp['moe_w_up']
    m['moe_wd'] = inp['moe_w_down']
    m['od_w_in'] = inp['od_w_in'][0]
    m['od_w_out'] = inp['od_w_out'][0]
    m['dlam'] = np.ascontiguousarray(inp['diff_lambda'][0].reshape(1, 256))
    m['sublnT'] = np.ascontiguousarray(inp['diff_subln'][0].reshape(128, 1))
    m['retgT'] = fm(inp['ret_norm'][0])
    m.update(host_consts())
    m.update(host_consts_l1(S))
    return m


def kernel(**inp):
    inp = {k: np.asarray(v) for k, v in inp.items()}
    S, C = inp['x'].shape[1], inp['ctx'].shape[1]
    B = inp['x'].shape[0]
    P, _ = build(S, C)
    nc = P.build()
    in_maps = [make_inputs(b, S, C, inp) for b in range(B)]
    names = set()
    res = run_bass_kernel_spmd(nc, in_maps, core_ids=list(range(B)))
    return np.stack([np.asarray(r["out"], np.float32) for r in res.results], 0)
```
